# Optimizing a Trainium2 kernel written in Bass

```python
import math
import jax
import jax.numpy as jnp
from jax import lax
import numpy as np


D_MODEL = 1024
BATCH = 8
SEQ = 2048
DEPTH = 4

GRID_W = 64
CTX_LEN = 256
N_MIXERS = 4
EPS = 1e-6
NEG_INF = -1e30
F32 = jnp.float32

HY_ORDER = 2
HY_SHORT = 3
HY_EMB = 33
HY_FFN = 64
HY_FAST_DECAY = 0.3
HY_SLOW_DECAY = 1.5
HY_TARGET = 1e-2
HY_SHIFT = 0.05

SW_HQ = 16
SW_HKV = 4
SW_REP = SW_HQ // SW_HKV
SW_HD = 64
SW_WINDOW = 128
SW_BLOCK = SW_WINDOW
ROPE_BASE = 10000.0

GD_H = 8
GD_DK = 128
GD_DV = 128
GD_QK = GD_H * GD_DK
GD_V = GD_H * GD_DV
GD_CONV = 3
GD_CHUNK = 64

HG_DK = 128
HG_H = D_MODEL // HG_DK
HG_DV = D_MODEL // HG_H
HG_CHUNK = 64

N_EXPERTS = 16
EXPERT_FF = 1024
EC_CAPACITY = 2

kernel_name = "hybrid_hyena_swa_gdn_hgrn2_ecmoe_diffusion"


def rms_norm(x, w):
    xf = x.astype(F32)
    y = xf * lax.rsqrt(jnp.mean(xf * xf, axis=-1, keepdims=True) + EPS)
    return (y * w.astype(F32)).astype(x.dtype)


def l2_normalize(x):
    xf = x.astype(F32)
    return xf * lax.rsqrt(jnp.sum(xf * xf, axis=-1, keepdims=True) + EPS)


def orient(t, direction):
    return jnp.flip(t, axis=1) if direction == 1 else t


def centred_depthwise_conv(x, w, b=None):
    K, C = w.shape
    y = lax.conv_general_dilated(x, w[:, None, :].astype(x.dtype), window_strides=(1,),
                                 padding=[(K // 2, K // 2)],
                                 dimension_numbers=('NWC', 'WIO', 'NWC'), feature_group_count=C)
    return y if b is None else y + b


def hyena_positional_features(L):
    t = jnp.linspace(0.0, 1.0, L, dtype=F32)[:, None]
    bands = (HY_EMB - 1) // 2
    w = 2.0 * math.pi * jnp.arange(L, dtype=F32)[:, None] / L
    f = jnp.linspace(1e-4, bands - 1, bands, dtype=F32)[None, :]
    z = jnp.concatenate([t, jnp.cos(f * w), -jnp.sin(f * w)], axis=-1)
    return t, z


def hyena_filters(L, w1, b1, w2, b2, w3, freq):
    t, z = hyena_positional_features(L)
    h = jnp.sin(freq[0].astype(F32) * (z @ w1.astype(F32) + b1.astype(F32)))
    h = jnp.sin(freq[1].astype(F32) * (h @ w2.astype(F32) + b2.astype(F32)))
    h = (h @ w3.astype(F32)).reshape(L, HY_ORDER, 2, D_MODEL)
    max_decay = math.log(HY_TARGET) / HY_FAST_DECAY
    min_decay = math.log(HY_TARGET) / HY_SLOW_DECAY
    deltas = jnp.abs(jnp.linspace(min_decay, max_decay, D_MODEL, dtype=F32))
    window = jnp.exp(-t * deltas) + HY_SHIFT
    return h * window[:, None, None, :]


def two_sided_fft_conv(u, h_fwd, h_bwd, bias):
    L = u.shape[1]
    kern = jnp.concatenate([h_fwd, jnp.zeros((1, h_fwd.shape[1]), F32), h_bwd[:0:-1]], axis=0)
    uf = u.astype(F32)
    spec = jnp.fft.rfft(uf, n=2 * L, axis=1) * jnp.fft.rfft(kern, n=2 * L, axis=0)[None]
    y = jnp.fft.irfft(spec, n=2 * L, axis=1)[:, :L]
    return (y + uf * bias.astype(F32)).astype(u.dtype)


def hyena_mixer(h, w_in, b_in, short_w, short_b, w1, b1, w2, b2, w3, freq, filt_bias, w_out, b_out):
    L = h.shape[1]
    u = centred_depthwise_conv(h @ w_in + b_in, short_w, short_b)
    v, x1, x2 = jnp.split(u, 3, axis=-1)
    filt = hyena_filters(L, w1, b1, w2, b2, w3, freq)
    z = x1 * two_sided_fft_conv(v, filt[:, 0, 0], filt[:, 0, 1], filt_bias[0])
    z = x2 * two_sided_fft_conv(z, filt[:, 1, 0], filt[:, 1, 1], filt_bias[1])
    return z @ w_out + b_out


def axial_rope_tables(L):
    rows = L // GRID_W
    row = jnp.repeat(jnp.arange(rows, dtype=F32), GRID_W)
    col = jnp.tile(jnp.arange(GRID_W, dtype=F32), rows)
    nf = SW_HD // 4
    inv = ROPE_BASE ** (-jnp.arange(nf, dtype=F32) / nf)
    ang = jnp.concatenate([row[:, None] * inv, col[:, None] * inv], axis=-1)
    return jnp.cos(ang), jnp.sin(ang)


def apply_rope(x, cos, sin):
    shape = (1, cos.shape[0]) + (1,) * (x.ndim - 3) + (cos.shape[1],)
    cos = cos.reshape(shape).astype(x.dtype)
    sin = sin.reshape(shape).astype(x.dtype)
    x1, x2 = jnp.split(x, 2, axis=-1)
    return jnp.concatenate([x1 * cos - x2 * sin, x2 * cos + x1 * sin], axis=-1)


def sink_softmax(s, sink):
    sk = jnp.broadcast_to(sink.astype(F32).reshape(SW_HKV, SW_REP, 1, 1), s.shape[:-1] + (1,))
    return jax.nn.softmax(jnp.concatenate([s, sk], axis=-1), axis=-1)[..., :-1]


def swa_mixer(h_ctx, h_lat, w_in, sink, w_out, with_ctx_out):
    B, L, _ = h_lat.shape
    scale = SW_HD ** -0.5

    def project(h):
        n = h.shape[1]
        u = h @ w_in
        q = u[..., :SW_HQ * SW_HD].reshape(B, n, SW_HKV, SW_REP, SW_HD)
        k = u[..., SW_HQ * SW_HD:(SW_HQ + SW_HKV) * SW_HD].reshape(B, n, SW_HKV, SW_HD)
        v = u[..., (SW_HQ + SW_HKV) * SW_HD:].reshape(B, n, SW_HKV, SW_HD)
        return q, k, v

    q_c, k_c, v_c = project(h_ctx)
    q_l, k_l, v_l = project(h_lat)
    cos, sin = axial_rope_tables(L)
    q_l = apply_rope(q_l, cos, sin)
    k_l = apply_rope(k_l, cos, sin)
    n_ctx = k_c.shape[1]
    nb = L // SW_BLOCK

    def band(t):
        tp = jnp.pad(t, ((0, 0), (SW_BLOCK, SW_BLOCK), (0, 0), (0, 0)))
        tp = tp.reshape(B, nb + 2, SW_BLOCK, SW_HKV, SW_HD)
        return jnp.concatenate([tp[:, :-2], tp[:, 1:-1], tp[:, 2:]], axis=2).swapaxes(0, 1)

    blk = jnp.arange(nb)[:, None, None]
    qpos = blk * SW_BLOCK + jnp.arange(SW_BLOCK)[None, :, None]
    kpos = (blk - 1) * SW_BLOCK + jnp.arange(3 * SW_BLOCK)[None, None, :]
    band_mask = (jnp.abs(qpos - kpos) <= SW_WINDOW) & (kpos >= 0) & (kpos < L)
    q_blocks = q_l.reshape(B, nb, SW_BLOCK, SW_HKV, SW_REP, SW_HD).swapaxes(0, 1)

    def block_attention(xs):
        q_b, k_b, v_b, m_b = xs
        s_ctx = jnp.einsum('bqgrd,bkgd->bgrqk', q_b, k_c).astype(F32) * scale
        s_loc = jnp.einsum('bqgrd,bkgd->bgrqk', q_b, k_b).astype(F32) * scale
        s_loc = jnp.where(m_b, s_loc, NEG_INF)
        p = sink_softmax(jnp.concatenate([s_ctx, s_loc], axis=-1), sink).astype(v_b.dtype)
        return (jnp.einsum('bgrqk,bkgd->bqgrd', p[..., :n_ctx], v_c)
                + jnp.einsum('bgrqk,bkgd->bqgrd', p[..., n_ctx:], v_b))

    o_l = lax.map(block_attention, (q_blocks, band(k_l), band(v_l), band_mask))
    y_lat = o_l.swapaxes(0, 1).reshape(B, L, SW_HQ * SW_HD) @ w_out
    y_ctx = None
    if with_ctx_out:
        s = jnp.einsum('bqgrd,bkgd->bgrqk', q_c, k_c).astype(F32) * scale
        p = sink_softmax(s, sink).astype(v_c.dtype)
        y_ctx = jnp.einsum('bgrqk,bkgd->bqgrd', p, v_c).reshape(B, n_ctx, SW_HQ * SW_HD) @ w_out
    return y_ctx, y_lat


def to_chunks(t, size):
    B, L, H = t.shape[:3]
    t = t.reshape((B, L // size, size, H) + t.shape[3:])
    return jnp.moveaxis(t, (1, 3), (0, 2))


def from_chunks(t):
    t = jnp.moveaxis(t, (0, 2), (1, 3))
    return t.reshape((t.shape[0], t.shape[1] * t.shape[2]) + t.shape[3:])


def chunk_gated_delta(q, k, v, g, beta, S0):
    dv = v.shape[-1]
    C = GD_CHUNK
    qc, kc, vc = to_chunks(q, C), to_chunks(k, C), to_chunks(v, C)
    gc, bc = to_chunks(g, C), to_chunks(beta, C)
    G = jnp.cumsum(gc, axis=-1)
    incl = jnp.tril(jnp.ones((C, C), bool))
    strict = jnp.tril(jnp.ones((C, C), bool), -1)
    decay = jnp.exp(jnp.where(incl, G[..., :, None] - G[..., None, :], -jnp.inf))
    kb = kc * bc[..., None]
    A = jnp.where(strict, jnp.einsum('nbhid,nbhjd->nbhij', kb, kc) * decay, 0.0)
    rhs = jnp.concatenate([vc * bc[..., None], kb * jnp.exp(G)[..., None]], axis=-1)
    uw = lax.linalg.triangular_solve(jnp.eye(C, dtype=F32) + A, rhs, left_side=True, lower=True,
                                     unit_diagonal=True)
    u, w = uw[..., :dv], uw[..., dv:]

    def step(S, xs):
        q_i, k_i, u_i, w_i, G_i, dec_i = xs
        v_new = u_i - jnp.einsum('bhck,bhkv->bhcv', w_i, S)
        attn = jnp.einsum('bhik,bhjk->bhij', q_i, k_i) * dec_i
        o = (jnp.einsum('bhck,bhkv->bhcv', q_i * jnp.exp(G_i)[..., None], S)
             + jnp.einsum('bhij,bhjv->bhiv', attn, v_new))
        g_last = G_i[..., -1:]
        S = (S * jnp.exp(g_last)[..., None]
             + jnp.einsum('bhck,bhcv->bhkv', k_i * jnp.exp(g_last - G_i)[..., None], v_new))
        return S, o

    S, o = lax.scan(step, S0, (qc, kc, u, w, G, decay))
    return from_chunks(o), S


def chunk_gla(q, k, v, logf, S0):
    C = HG_CHUNK
    qc, kc, vc, gc = to_chunks(q, C), to_chunks(k, C), to_chunks(v, C), to_chunks(logf, C)
    G = jnp.cumsum(gc, axis=-2)
    incl = jnp.tril(jnp.ones((C, C), bool))[:, :, None]

    def step(S, xs):
        q_i, k_i, v_i, G_i = xs
        dec = jnp.exp(jnp.where(incl, G_i[:, :, :, None, :] - G_i[:, :, None, :, :], -jnp.inf))
        attn = jnp.einsum('bhtk,bhtsk->bhts', q_i, dec * k_i[:, :, None, :, :])
        o = (jnp.einsum('bhtk,bhkv->bhtv', q_i * jnp.exp(G_i), S)
             + jnp.einsum('bhts,bhsv->bhtv', attn, v_i))
        g_last = G_i[:, :, -1:, :]
        S = (S * jnp.exp(g_last[:, :, 0, :])[..., None]
             + jnp.einsum('bhsk,bhsv->bhkv', k_i * jnp.exp(g_last - G_i), v_i))
        return S, o

    S, o = lax.scan(step, S0, (qc, kc, vc, G))
    return from_chunks(o), S


def gdn_mixer(h_ctx, h_lat, w_in, conv_w, a_log, dt_bias, norm_w, w_out, with_ctx_out):
    n_qkv = 2 * GD_QK + GD_V

    def project(h):
        B, n, _ = h.shape
        u = h @ w_in
        qkv = jax.nn.silu(centred_depthwise_conv(u[..., :n_qkv], conv_w))
        q = l2_normalize(qkv[..., :GD_QK].reshape(B, n, GD_H, GD_DK)) * GD_DK ** -0.5
        k = l2_normalize(qkv[..., GD_QK:2 * GD_QK].reshape(B, n, GD_H, GD_DK))
        v = qkv[..., 2 * GD_QK:].astype(F32).reshape(B, n, GD_H, GD_DV)
        z = u[..., n_qkv:n_qkv + GD_V]
        ba = u[..., n_qkv + GD_V:].astype(F32).reshape(B, n, 2, 2, GD_H)
        beta = jax.nn.sigmoid(ba[:, :, 0])
        g = -jnp.exp(a_log.astype(F32)) * jax.nn.softplus(ba[:, :, 1] + dt_bias.astype(F32))
        return q, k, v, z, beta, g

    qc, kc, vc, zc, bc, gc = project(h_ctx)
    ql, kl, vl, zl, bl, gl = project(h_lat)
    S0 = jnp.zeros((h_lat.shape[0], GD_H, GD_DK, GD_DV), F32)
    o_c = 0.0
    o_l = 0.0
    for d in range(2):
        oc_d, S_c = chunk_gated_delta(orient(qc, d), orient(kc, d), orient(vc, d),
                                      orient(gc[:, :, d], d), orient(bc[:, :, d], d), S0)
        ol_d, _ = chunk_gated_delta(orient(ql, d), orient(kl, d), orient(vl, d),
                                    orient(gl[:, :, d], d), orient(bl[:, :, d], d), S_c)
        o_c = o_c + orient(oc_d, d)
        o_l = o_l + orient(ol_d, d)

    def readout(o, z):
        B, n = o.shape[:2]
        y = rms_norm(o, norm_w) * jax.nn.silu(z.astype(F32)).reshape(B, n, GD_H, GD_DV)
        return y.reshape(B, n, GD_V).astype(z.dtype) @ w_out

    y_ctx = readout(o_c, zc) if with_ctx_out else None
    return y_ctx, readout(o_l, zl)


def hgrn2_mixer(h_ctx, h_lat, w_in, lb, norm_w, w_out, with_ctx_out):
    lbh = lb.reshape(HG_H, HG_DK)

    def project(h):
        B, n, _ = h.shape
        q, f_fw, f_bw, i, g = jnp.split(h @ w_in, 5, axis=-1)
        q = jax.nn.silu(q).astype(F32).reshape(B, n, HG_H, HG_DK)
        f = jnp.stack([f_fw, f_bw], axis=2).astype(F32).reshape(B, n, 2, HG_H, HG_DK)
        logf = jnp.log(lbh + (1.0 - lbh) * jax.nn.sigmoid(f))
        k = (1.0 - lbh) * jax.nn.sigmoid(-f)
        v = i.astype(F32).reshape(B, n, HG_H, HG_DV)
        return q, k, v, logf, g

    qc, kc, vc, fc, gc = project(h_ctx)
    ql, kl, vl, fl, gl = project(h_lat)
    S0 = jnp.zeros((h_lat.shape[0], HG_H, HG_DK, HG_DV), F32)
    o_c = 0.0
    o_l = 0.0
    for d in range(2):
        oc_d, S_c = chunk_gla(orient(qc, d), orient(kc[:, :, d], d), orient(vc, d),
                              orient(fc[:, :, d], d), S0)
        ol_d, _ = chunk_gla(orient(ql, d), orient(kl[:, :, d], d), orient(vl, d),
                            orient(fl[:, :, d], d), S_c)
        o_c = o_c + orient(oc_d, d)
        o_l = o_l + orient(ol_d, d)

    def readout(o, g):
        B, n = o.shape[:2]
        y = rms_norm(o, norm_w) * jax.nn.silu(g.astype(F32)).reshape(B, n, HG_H, HG_DV)
        return y.reshape(B, n, D_MODEL).astype(g.dtype) @ w_out

    y_ctx = readout(o_c, gc) if with_ctx_out else None
    return y_ctx, readout(o_l, gl)


def expert_choice_ffn(h, router_w, w_gate, w_up, w_down):
    B, n, D = h.shape
    cap = EC_CAPACITY * n // N_EXPERTS
    aff = jax.nn.softmax((h @ router_w).astype(F32), axis=-1)
    gate, idx = lax.top_k(jnp.swapaxes(aff, 1, 2), cap)
    xe = jax.vmap(lambda hb, ib: hb[ib])(h, idx)
    a = jnp.einsum('becd,edf->becf', xe, w_gate)
    u = jnp.einsum('becd,edf->becf', xe, w_up)
    ye = jnp.einsum('becf,efd->becd', jax.nn.silu(a) * u, w_down) * gate[..., None].astype(h.dtype)
    return jax.vmap(lambda ib, yb: jnp.zeros((n, D), h.dtype).at[ib.reshape(-1)].add(yb.reshape(-1, D)))(idx, ye)


def setup_inputs(seed: int = 0) -> dict:
    key = jax.random.key(seed)
    keys = iter(jax.random.split(key, 48))

    def nrm(shape, std):
        return std * jax.random.normal(next(keys), shape, F32)

    def gain(shape):
        return 1.0 + nrm(shape, 0.02)

    D = D_MODEL
    gd_cols = 2 * GD_QK + 2 * GD_V + 4 * GD_H
    gd_a_log = jnp.log(jax.random.uniform(next(keys), (2, GD_H), F32, 1.0, 16.0))
    dt = jnp.exp(jax.random.uniform(next(keys), (2, GD_H), F32, math.log(1e-3), math.log(1e-1)))
    gd_dt_bias = jnp.log(jnp.expm1(dt))
    return {
        'x': nrm((BATCH, SEQ, D), 1.0),
        'c': nrm((BATCH, D), 1.0),
        'ctx': nrm((BATCH, CTX_LEN, D), 1.0),
        'c_ctx': nrm((D,), 1.0),
        'ada_w': nrm((DEPTH, D, 6 * D), 0.5 * D ** -0.5),
        'ada_b': nrm((DEPTH, 6 * D), 0.02),
        'norm1_w': gain((DEPTH, D)),
        'norm2_w': gain((DEPTH, D)),
        'final_norm_w': gain((D,)),
        'hy_w_in': nrm((D, 3 * D), D ** -0.5),
        'hy_b_in': nrm((3 * D,), 0.02),
        'hy_short_w': nrm((HY_SHORT, 3 * D), HY_SHORT ** -0.5),
        'hy_short_b': nrm((3 * D,), 0.02),
        'hy_ffn_w1': nrm((HY_EMB, HY_FFN), HY_EMB ** -0.5),
        'hy_ffn_b1': nrm((HY_FFN,), 0.02),
        'hy_ffn_w2': nrm((HY_FFN, HY_FFN), HY_FFN ** -0.5),
        'hy_ffn_b2': nrm((HY_FFN,), 0.02),
        'hy_ffn_w3': nrm((HY_FFN, HY_ORDER * 2 * D), 0.01),
        'hy_sin_freq': gain((2, HY_FFN)),
        'hy_filter_bias': nrm((HY_ORDER, D), 0.5),
        'hy_w_out': nrm((D, D), D ** -0.5),
        'hy_b_out': nrm((D,), 0.02),
        'sw_w_in': nrm((D, (SW_HQ + 2 * SW_HKV) * SW_HD), D ** -0.5),
        'sw_sink': nrm((SW_HQ,), 0.5),
        'sw_w_out': nrm((SW_HQ * SW_HD, D), (SW_HQ * SW_HD) ** -0.5),
        'gd_w_in': nrm((D, gd_cols), D ** -0.5),
        'gd_conv_w': nrm((GD_CONV, 2 * GD_QK + GD_V), GD_CONV ** -0.5),
        'gd_a_log': gd_a_log,
        'gd_dt_bias': gd_dt_bias,
        'gd_norm_w': gain((GD_DV,)),
        'gd_w_out': nrm((GD_V, D), GD_V ** -0.5),
        'hg_w_in': nrm((D, 5 * D), D ** -0.5),
        'hg_lb': nrm((DEPTH, D), 1.0),
        'hg_norm_w': gain((HG_DV,)),
        'hg_w_out': nrm((D, D), D ** -0.5),
        'moe_router': nrm((DEPTH, D, N_EXPERTS), D ** -0.5),
        'moe_w_gate': nrm((DEPTH, N_EXPERTS, D, EXPERT_FF), D ** -0.5),
        'moe_w_up': nrm((DEPTH, N_EXPERTS, D, EXPERT_FF), D ** -0.5),
        'moe_w_down': nrm((DEPTH, N_EXPERTS, EXPERT_FF, D), EXPERT_FF ** -0.5),
    }


def reference(x, c, ctx, c_ctx, ada_w, ada_b, norm1_w, norm2_w, final_norm_w,
              hy_w_in, hy_b_in, hy_short_w, hy_short_b, hy_ffn_w1, hy_ffn_b1, hy_ffn_w2, hy_ffn_b2,
              hy_ffn_w3, hy_sin_freq, hy_filter_bias, hy_w_out, hy_b_out,
              sw_w_in, sw_sink, sw_w_out,
              gd_w_in, gd_conv_w, gd_a_log, gd_dt_bias, gd_norm_w, gd_w_out,
              hg_w_in, hg_lb, hg_norm_w, hg_w_out,
              moe_router, moe_w_gate, moe_w_up, moe_w_down):
    lat, cx = x, ctx
    lb_all = jnp.cumsum(jax.nn.softmax(hg_lb.astype(F32), axis=0), axis=0)
    for layer in range(DEPTH):
        kind = layer % N_MIXERS
        keep_ctx = layer < DEPTH - 1
        mod_l = [m[:, None, :] for m in jnp.split(jax.nn.silu(c) @ ada_w[layer] + ada_b[layer], 6, axis=-1)]
        mod_c = jnp.split(jax.nn.silu(c_ctx) @ ada_w[layer] + ada_b[layer], 6, axis=-1)
        h_l = rms_norm(lat, norm1_w[layer]) * (1.0 + mod_l[1]) + mod_l[0]
        h_c = rms_norm(cx, norm1_w[layer]) * (1.0 + mod_c[1]) + mod_c[0]
        if kind == 0:
            hy_args = (hy_w_in, hy_b_in, hy_short_w, hy_short_b, hy_ffn_w1, hy_ffn_b1, hy_ffn_w2,
                       hy_ffn_b2, hy_ffn_w3, hy_sin_freq, hy_filter_bias, hy_w_out, hy_b_out)
            y_l = hyena_mixer(h_l, *hy_args)
            y_c = hyena_mixer(h_c, *hy_args) if keep_ctx else None
        elif kind == 1:
            y_c, y_l = swa_mixer(h_c, h_l, sw_w_in, sw_sink, sw_w_out, keep_ctx)
        elif kind == 2:
            y_c, y_l = gdn_mixer(h_c, h_l, gd_w_in, gd_conv_w, gd_a_log, gd_dt_bias, gd_norm_w,
                                 gd_w_out, keep_ctx)
        else:
            lb = lb_all[layer] - lb_all[0]
            y_c, y_l = hgrn2_mixer(h_c, h_l, hg_w_in, lb, hg_norm_w, hg_w_out, keep_ctx)
        lat = lat + mod_l[2] * y_l
        h_l = rms_norm(lat, norm2_w[layer]) * (1.0 + mod_l[4]) + mod_l[3]
        lat = lat + mod_l[5] * expert_choice_ffn(h_l, moe_router[layer], moe_w_gate[layer],
                                                 moe_w_up[layer], moe_w_down[layer])
        if keep_ctx:
            cx = cx + mod_c[2] * y_c
            h_c = rms_norm(cx, norm2_w[layer]) * (1.0 + mod_c[4]) + mod_c[3]
            cx = cx + mod_c[5] * expert_choice_ffn(h_c, moe_router[layer], moe_w_gate[layer],
                                                   moe_w_up[layer], moe_w_down[layer])
    return rms_norm(lat, final_norm_w)
```

```python
import numpy as np
from contextlib import ExitStack
import concourse.bass as bass
import concourse.mybir as mybir
from concourse.bass_utils import run_bass_kernel_spmd

F32 = mybir.dt.float32
BF16 = mybir.dt.bfloat16
I32 = mybir.dt.int32
AF = mybir.ActivationFunctionType
ALU = mybir.AluOpType
AX = mybir.AxisListType

D = 1024
NCH = 8
TL = 2048
TC = 256
DEPTH = 4
NE = 16
FF = 1024
EPS = 1e-6
NDS = 12


class Buf:
    __slots__ = ("t", "w", "r", "name", "psum")

    def __init__(self, t, name=""):
        self.t = t
        self.w = None
        self.r = {}
        self.name = name
        self.psum = False

    def __getitem__(self, idx):
        return self.t[idx]


class Sched:
    def __init__(self, nc):
        self.nc = nc
        self.eng = {"pe": nc.tensor, "act": nc.scalar, "dve": nc.vector, "pool": nc.gpsimd, "sp": nc.sync}
        self.semstack = ExitStack()
        self.csem = {e: self.semstack.enter_context(nc.semaphore("s_" + e)) for e in ("pe", "act", "dve", "pool")}
        self.prog = {e: [] for e in self.eng}
        self.ccnt = {e: 0 for e in self.csem}
        self.seen = {e: {} for e in self.eng}
        self.dq = {}
        for q in ("sp", "pool"):
            self.dq[q] = {"sems": [self.semstack.enter_context(nc.semaphore("d_%s%d" % (q, i))) for i in range(NDS)],
                          "vals": [0] * NDS, "rr": 0}
        self.nbuf = 0
        self.stacks = []
        self.arena = None
        self.ps = [self.psum_raw("psb%d" % i) for i in range(8)]
        self.ps_rr = 0
        self.n_inst = 0

    def sb(self, shape, dt=F32, name=None):
        self.nbuf += 1
        nm = "%s_%d" % (name or "b", self.nbuf)
        if self.arena is None:
            self.arena_words = 212800 // 4
            self.arena = self.nc.alloc_sbuf_tensor("arena", [128, self.arena_words], F32)
            self.top = 0
        esz = 2 if dt == BF16 else 4
        n = 1
        for d_ in shape[1:]:
            n *= d_
        words = (n * esz + 3) // 4
        words = (words + 7) // 8 * 8
        assert self.top + words <= self.arena_words, "SBUF arena overflow allocating %s %s (top=%d)" % (nm, shape, self.top * 4)
        ap = self.arena[0:shape[0], self.top:self.top + words]
        self.top += words
        if dt != F32:
            ap = ap.bitcast(dt)
        ap = ap[:, 0:n]
        if len(shape) == 3:
            ap = ap.rearrange("p (a b) -> p a b", b=shape[2])
        elif len(shape) == 4:
            ap = ap.rearrange("p (a b c) -> p a b c", b=shape[2], c=shape[3])
        return Buf(ap, nm)

    def push(self):
        self.stacks.append(self.top)

    def pop(self):
        self.barrier()
        self.top = self.stacks.pop()

    def barrier(self):
        evs = [(self.csem[e], self.ccnt[e], e) for e in self.csem if self.ccnt[e] > 0]
        for q in self.dq.values():
            for sem, v in zip(q["sems"], q["vals"]):
                if v > 0:
                    evs.append((sem, v, "dma"))
        for e in self.eng:
            for ev in evs:
                if e == "pe" and ev[2] == "pe":
                    continue
                self._wait(e, ev)

    def psum_raw(self, name):
        b = Buf(self.nc.alloc_psum_tensor(name, [128, 512], F32), name)
        b.psum = True
        return b

    def psum(self):
        b = self.ps[self.ps_rr]
        self.ps_rr = (self.ps_rr + 1) % 8
        return b

    def _wait(self, e, ev):
        sem, val, src = ev
        k = id(sem)
        if self.seen[e].get(k, 0) < val:
            self.prog[e].append(("w", sem, val))
            self.seen[e][k] = val

    def _deps(self, e, reads, writes):
        for b in reads:
            if b.w is not None and not (e == "pe" and b.w[2] == "pe"):
                self._wait(e, b.w)
            if b.psum:
                for ev in b.r.values():
                    if ev[2] != e:
                        self._wait(e, ev)
        for b in writes:
            if b.w is not None and not (e == "pe" and b.w[2] == "pe"):
                self._wait(e, b.w)
            for ev in b.r.values():
                if not (e == "pe" and ev[2] == "pe"):
                    self._wait(e, ev)

    def _mark(self, ev, reads, writes):
        k = id(ev[0])
        for b in reads:
            b.r[k] = ev
        for b in writes:
            b.w = ev
            b.r = {}

    def op(self, e, fn, reads=(), writes=()):
        self._deps(e, reads, writes)
        self.ccnt[e] += 1
        self.prog[e].append(("i", fn, self.csem[e], 1))
        self._mark((self.csem[e], self.ccnt[e], e), reads, writes)
        self.n_inst += 1

    def dma(self, q, out, in_, reads=(), writes=(), **kw):
        d = self.dq[q]
        i = d["rr"]
        d["rr"] = (i + 1) % NDS
        sem = d["sems"][i]
        if d["vals"][i] > 0:
            self._wait(q, (sem, d["vals"][i], "dma"))
        self._deps(q, reads, writes)
        self.prog[q].append(("i", lambda g: g.dma_start(out=out, in_=in_, allow_slow_non_contiguous=True, **kw), sem, 16))
        d["vals"][i] += 16
        ev = (sem, d["vals"][i], "dma")
        self._mark(ev, reads, writes)
        self.n_inst += 1
        return ev

    def emit(self):
        prog = self.prog

        def replay(name, eng):
            for it in prog[name]:
                if it[0] == "w":
                    eng.wait_ge(it[1], it[2])
                else:
                    it[1](eng).then_inc(it[2], it[3])

        with self.nc.Block() as block:
            @block.sync
            def _(eng):
                replay("sp", eng)

            @block.tensor
            def _(eng):
                replay("pe", eng)

            @block.scalar
            def _(eng):
                replay("act", eng)

            @block.vector
            def _(eng):
                replay("dve", eng)

            @block.gpsimd
            def _(eng):
                replay("pool", eng)

    def mm(self, out, lhsT, rhs, start, stop, reads, writes, **kw):
        self.op("pe", lambda e: e.matmul(out, lhsT, rhs, start=start, stop=stop, **kw), reads, writes)

    def tr(self, out, in_, ident, reads, writes):
        self.op("pe", lambda e: e.transpose(out, in_, ident), reads, writes)

    def trf(self, out, in_, ident, reads, writes):
        self.op("pe", lambda e: e.matmul(out, in_, ident, start=True, stop=True), reads, writes)

    def act(self, out, in_, func, reads, writes, bias=None, scale=None, accum_out=None):
        kw = {}
        if bias is not None:
            kw["bias"] = bias
        if scale is not None:
            kw["scale"] = scale
        if accum_out is not None:
            kw["accum_out"] = accum_out
        self.op("act", lambda e: e.activation(out, in_, func, **kw), reads, writes)

    def ts(self, out, in0, s1, s2, op0, op1, reads, writes, e="dve", accum_out=None):
        if op1 is None:
            self.op(e, lambda g: g.tensor_scalar(out, in0, s1, None, op0, accum_out=accum_out) if accum_out is not None
                    else g.tensor_scalar(out, in0, s1, None, op0), reads, writes)
        else:
            self.op(e, lambda g: g.tensor_scalar(out, in0, s1, s2, op0, op1, accum_out=accum_out) if accum_out is not None
                    else g.tensor_scalar(out, in0, s1, s2, op0, op1), reads, writes)

    def tt(self, out, in0, in1, op, reads, writes, e="dve"):
        self.op(e, lambda g: g.tensor_tensor(out, in0, in1, op), reads, writes)

    def stt(self, out, in0, scalar, in1, op0, op1, reads, writes):
        self.op("dve", lambda g: g.scalar_tensor_tensor(out, in0, scalar, in1, op0, op1), reads, writes)

    def recip(self, out, in_, reads, writes):
        self.op("dve", lambda g: g.reciprocal(out, in_), reads, writes)

    def red(self, out, in_, op, reads, writes, negate=False):
        self.op("dve", lambda g: g.tensor_reduce(out, in_, AX.X, op, negate=negate), reads, writes)

    def max8(self, out, in_, reads, writes):
        self.op("dve", lambda g: g.max(out, in_), reads, writes)

    def mrep(self, out, in_to_replace, in_values, imm, reads, writes):
        self.op("dve", lambda g: g.match_replace(out, in_to_replace, in_values, imm), reads, writes)

    def scan(self, out, d0, d1, init, op0, op1, reads, writes):
        self.op("dve", lambda g: g.tensor_tensor_scan(out, d0, d1, init, op0, op1), reads, writes)

    def memset(self, out, val, writes, e="dve"):
        self.op(e, lambda g: g.memset(out, val), [], writes)

    def cp(self, out, in_, reads, writes, e="dve"):
        if e == "act":
            self.op("act", lambda g: g.activation(out, in_, AF.Identity), reads, writes)
        else:
            self.op(e, lambda g: g.tensor_copy(out, in_), reads, writes)


def host_consts():
    c = {}
    c["ident_f"] = np.eye(128, dtype=np.float32)
    c["ones_f"] = np.ones((128, 128), np.float32)
    io = np.zeros((128, 288), np.float32)
    io[:] = np.arange(288, dtype=np.float32)[None, :]
    c["iota_row"] = io
    ip = np.zeros((128, 4), np.float32)
    for k in range(4):
        ip[:, k] = np.arange(128) + 128 * k
    c["iota_part"] = ip
    sel = np.zeros((16, 16, 128), np.float32)
    for e in range(16):
        sel[e, e, :] = 1.0
    c["sel"] = sel.reshape(16, 16 * 128)
    t = np.arange(TL)
    row = (t // 64).astype(np.float32)
    col = (t % 64).astype(np.float32)
    inv = (10000.0 ** (-np.arange(16, dtype=np.float32) / 16)).astype(np.float32)
    ang = np.concatenate([row[:, None] * inv, col[:, None] * inv], -1).astype(np.float32)
    cosf = np.zeros((128, TL), np.float32)
    sinf = np.zeros((128, TL), np.float32)
    for p in range(128):
        i = p % 64
        cosf[p] = np.cos(ang[:, i % 32])
        sinf[p] = np.sin(ang[:, i % 32]) * (-1.0 if i < 32 else 1.0)
    c["rope_cos"] = cosf
    c["rope_sin"] = sinf
    jj = np.arange(128)[:, None]
    ii = np.arange(128)[None, :]
    mp = (jj >= ii).astype(np.float32)
    mn = (jj <= ii).astype(np.float32)
    cm = np.ones((128, 128), np.float32)
    cm[:, 0] = 0.0
    cm[:, 64] = 0.0
    c["chunk_reset"] = cm
    blk = (jj // 64 == ii // 64)
    c["tri_fwd"] = (blk & (jj <= ii)).astype(np.float32)
    c["tri_bwd"] = (blk & (jj >= ii)).astype(np.float32)
    import ml_dtypes
    for nm, L in (("l", TL), ("c", TC)):
        N = 2 * L
        T_ = L // 128
        tt_ = np.linspace(0.0, 1.0, L, dtype=np.float32)
        w_ = (2.0 * np.pi * np.arange(L, dtype=np.float32) / L).astype(np.float32)
        f_ = np.linspace(1e-4, 15.0, 16, dtype=np.float32)[None, :]
        z_ = np.concatenate([tt_[:, None], np.cos(f_ * w_[:, None]), -np.sin(f_ * w_[:, None])], -1).astype(np.float32)
        c["hy_zT_" + nm] = np.ascontiguousarray(z_.T)
        c["hy_tcol_" + nm] = np.ascontiguousarray(tt_.reshape(T_, 128).T)
        tpos = np.arange(L, dtype=np.float64)
        fr = np.arange(L, dtype=np.float64) + 0.5
        th = 2.0 * np.pi * np.outer(tpos, fr) / N
        for tn, tab in (("C", np.cos(th)), ("S", np.sin(th))):
            fw = tab.reshape(T_, 128, T_, 128).transpose(2, 1, 0, 3)
            c["hy_%sf_%s" % (tn, nm)] = np.ascontiguousarray(fw).reshape(T_ * 128, T_ * 128).astype(ml_dtypes.bfloat16)
            c["hy_%st_%s" % (tn, nm)] = np.ascontiguousarray(tab.T).astype(ml_dtypes.bfloat16)
    maxd = np.log(1e-2) / 0.3
    mind = np.log(1e-2) / 1.5
    c["hy_delta"] = np.abs(np.linspace(mind, maxd, D, dtype=np.float32)).reshape(1, D).astype(np.float32)
    eye = np.eye(128, dtype=np.float32)
    c["tri_fwd_s"] = c["tri_fwd"] - eye
    c["tri_bwd_s"] = c["tri_bwd"] - eye
    c["mask_prev"] = np.concatenate([mp, mp], 1)
    c["mask_next"] = np.concatenate([mn, mn], 1)
    return c


class Prog:
    def __init__(self, cfg):
        self.cfg = cfg
        self.nc = bass.Bass("TRN2", target_bir_lowering=False)
        self.S = Sched(self.nc)
        self.din = {}
        self.consts = host_consts()

    def dram_in(self, name, shape, dt=F32):
        t = self.nc.dram_tensor(name, list(shape), dt, kind="ExternalInput")
        self.din[name] = t
        return t.ap()

    def declare(self):
        nc = self.nc
        A = {}
        A["x"] = self.dram_in("x", [TL, D])
        A["ctx"] = self.dram_in("ctx", [TC, D])
        A["cc"] = self.dram_in("cc", [2, D])
        A["ada_w"] = self.dram_in("ada_w", [DEPTH * D, 6 * D])
        A["ada_b"] = self.dram_in("ada_b", [DEPTH, 6 * D])
        A["norm1_w"] = self.dram_in("norm1_w", [DEPTH, D])
        A["norm2_w"] = self.dram_in("norm2_w", [DEPTH, D])
        A["final_norm_w"] = self.dram_in("final_norm_w", [1, D])
        A["moe_router"] = self.dram_in("moe_router", [DEPTH * D, NE])
        A["moe_w_gate"] = self.dram_in("moe_w_gate", [DEPTH * NE * D, FF])
        A["moe_w_up"] = self.dram_in("moe_w_up", [DEPTH * NE * D, FF])
        A["moe_w_down"] = self.dram_in("moe_w_down", [DEPTH * NE * FF, D])
        A["gd_w_in"] = self.dram_in("gd_w_in", [D, 4128])
        A["gd_conv_w"] = self.dram_in("gd_conv_w", [3, 3072])
        A["gd_a_log"] = self.dram_in("gd_a_log", [1, 16])
        A["gd_dt_bias"] = self.dram_in("gd_dt_bias", [1, 16])
        A["gd_norm_w"] = self.dram_in("gd_norm_w", [1, 128])
        A["gd_w_out"] = self.dram_in("gd_w_out", [D, D])
        A["hg_w_in"] = self.dram_in("hg_w_in", [D, 5 * D])
        A["hg_lb"] = self.dram_in("hg_lb", [DEPTH, D])
        A["hg_norm_w"] = self.dram_in("hg_norm_w", [1, 128])
        A["hg_w_out"] = self.dram_in("hg_w_out", [D, D])
        A["sw_w_in"] = self.dram_in("sw_w_in", [D, 1536])
        A["sw_sink"] = self.dram_in("sw_sink", [1, 16])
        A["sw_w_out"] = self.dram_in("sw_w_out", [D, D])
        for nm, shp in (("hy_w_in", [D, 3 * D]), ("hy_b_in", [1, 3 * D]), ("hy_short_w", [3, 3 * D]),
                        ("hy_short_b", [1, 3 * D]), ("hy_ffn_w1", [33, 64]), ("hy_ffn_b1", [1, 64]),
                        ("hy_ffn_w2", [64, 64]), ("hy_ffn_b2", [1, 64]), ("hy_ffn_w3", [64, 4 * D]),
                        ("hy_sin_freq", [2, 64]), ("hy_filter_bias", [2, D]), ("hy_w_out", [D, D]),
                        ("hy_b_out", [1, D])):
            A[nm] = self.dram_in(nm, shp)
        for k, v in self.consts.items():
            A[k] = self.dram_in("k_" + k, list(v.shape), BF16 if v.dtype != np.float32 else F32)
        self.out = nc.dram_tensor("out", [TL, D], F32, kind="ExternalOutput").ap()
        if self.cfg.get("dump_ctx"):
            self.out_c = nc.dram_tensor("out_c", [TC, D], F32, kind="ExternalOutput").ap()
        self.A = A

    def setup(self):
        S = self.S
        A = self.A
        self.ident_f = S.sb([128, 128], F32, "identf")
        self.ident_b = S.sb([128, 128], BF16, "identb")
        self.ones_b = S.sb([128, 128], BF16, "onesb")
        self.iota_row = S.sb([128, 256], F32, "iotar")
        self.iota_part = S.sb([128, 4], F32, "iotap")
        self.sel = S.sb([16, 16 * 128], BF16, "sel")
        self.epsb = S.sb([128, 1], F32, "eps")
        self.hs_tok = S.sb([1, 8], F32, "hstok")
        self.RL = [[S.sb([128, 512], F32, "RL") for g in range(4)] for c in range(NCH)]
        self.RC = [S.sb([128, 256], F32, "RC") for c in range(NCH)]
        self.sc = S.sb([128, NCH, 2], F32, "sc")
        self.modT = S.sb([128, 48, 2], F32, "modT")
        self.a1 = [S.sb([128, NCH], F32, "a1") for _ in range(2)]
        self.a2 = [S.sb([128, NCH], F32, "a2") for _ in range(2)]
        S.dma("sp", self.ident_f[:], A["ident_f"], writes=[self.ident_f])
        S.dma("sp", self.iota_row[:], A["iota_row"][:, 0:256], writes=[self.iota_row])
        S.dma("sp", self.iota_part[:], A["iota_part"], writes=[self.iota_part])
        S.push()
        tmp = S.sb([128, 128], F32, "onesf")
        tsel = S.sb([16, 16 * 128], F32, "tsel")
        craw = S.sb([128, NCH, 2], F32, "craw")
        S.dma("sp", tsel[:], A["sel"], writes=[tsel])
        S.dma("sp", tmp[:], A["ones_f"], writes=[tmp])
        S.cp(self.sel[:], tsel[:], [tsel], [self.sel])
        S.cp(self.ones_b[:], tmp[:], [tmp], [self.ones_b])
        S.cp(self.ident_b[:], self.ident_f[:], [self.ident_f], [self.ident_b])
        S.memset(self.epsb[:], EPS, [self.epsb])
        with self.nc.allow_non_contiguous_dma("tiny"):
            for j in range(2):
                S.dma("sp", craw[:, :, j], A["cc"][j].rearrange("(c p) -> p c", p=128), writes=[craw])
        S.act(self.sc[:], craw[:], AF.Silu, [craw], [self.sc])
        S.pop()
        self.groups = [("l", g, g * 512, 512) for g in range(4)] + [("c", 0, 0, 256)]

    def dbg(self, name, ap, buf, shape):
        if not self.cfg.get("debug"):
            return
        d = self.nc.dram_tensor("dbg_" + name, list(shape), ap.dtype, kind="ExternalOutput").ap()
        ev = self.S.dma("sp", d, ap, reads=[buf])
        self.S._wait("sp", ev)

    def rbuf(self, grp, c):
        return self.RL[c][grp[1]] if grp[0] == "l" else self.RC[c]

    def load_stream(self):
        S = self.S
        A = self.A
        S.push()
        tin = [S.sb([128, D], F32, "tin") for _ in range(2)]
        k = 0
        for grp in self.groups:
            src = A["x"] if grp[0] == "l" else A["ctx"]
            for tt in range(grp[3] // 128):
                t0 = grp[2] + tt * 128
                tb = tin[k % 2]
                if k >= self.cfg.get("ls_tiles", 99):
                    continue
                k += 1
                S.dma("sp", tb[:], src[t0:t0 + 128, :], writes=[tb])
                for half in range(2):
                    ps = S.psum()
                    for j in range(4):
                        c = half * 4 + j
                        S.trf(ps[:, j * 128:(j + 1) * 128], tb[:, c * 128:(c + 1) * 128], self.ident_f[:],
                             [tb, self.ident_f], [ps])
                    for j in range(4):
                        c = half * 4 + j
                        rb = self.rbuf(grp, c)
                        S.cp(rb[:, tt * 128:(tt + 1) * 128], ps[:, j * 128:(j + 1) * 128], [ps], [rb],
                             e=("act" if j % 2 else "dve"))
        S.pop()

    def adaln(self, layer):
        S = self.S
        A = self.A
        S.push()
        modT = self.modT
        adab = S.sb([128, 48], F32, "adab")
        with self.nc.allow_non_contiguous_dma("tiny"):
            S.dma("sp", adab[:], A["ada_b"][layer].rearrange("(j p) -> p j", p=128), writes=[adab])
        wv = A["ada_w"][layer * D:(layer + 1) * D, :].rearrange("(c p) n -> p c n", p=128)
        wb = [S.sb([128, NCH, 512], F32, "adaw") for _ in range(2)]
        ps = S.psum()
        for piece in range(12):
            w = wb[piece % 2]
            S.dma("sp", w[:], wv[:, :, piece * 512:(piece + 1) * 512], writes=[w])
            for jj in range(4):
                j = piece * 4 + jj
                for c in range(NCH):
                    S.mm(ps[:, 2 * j:2 * j + 2], w[:, c, jj * 128:(jj + 1) * 128], self.sc[:, c, :],
                         c == 0, c == NCH - 1, [w, self.sc], [ps])
        for s in range(2):
            S.tt(modT[:, :, s], ps[:, 0:96].rearrange("p (j s) -> p j s", s=2)[:, :, s], adab[:], ALU.add,
                 [ps, adab], [modT])
        n1 = S.sb([128, NCH], F32, "n1w")
        n2 = S.sb([128, NCH], F32, "n2w")
        with self.nc.allow_non_contiguous_dma("tiny"):
            S.dma("sp", n1[:], A["norm1_w"][layer].rearrange("(c p) -> p c", p=128), writes=[n1])
            S.dma("sp", n2[:], A["norm2_w"][layer].rearrange("(c p) -> p c", p=128), writes=[n2])
        M = {}
        for s, sn in enumerate(("l", "c")):
            S.stt(self.a1[s][:], modT[:, 8:16, s], 1.0, n1[:], ALU.add, ALU.mult, [modT, n1], [self.a1[s]])
            S.stt(self.a2[s][:], modT[:, 32:40, s], 1.0, n2[:], ALU.add, ALU.mult, [modT, n2], [self.a2[s]])
            M[sn] = {"a1": self.a1[s], "a2": self.a2[s], "modT": modT, "s": s}
        self.M = M
        S.pop()
        return M

    def mvec(self, sn, which, c):
        m = self.M[sn]
        return m["modT"][:, which * 8 + c, m["s"]:m["s"] + 1]

    def norm_scratch(self):
        S = self.S
        self._sq = S.sb([128, NCH, 512], BF16, "sq")
        self._rstd = S.sb([128, 512], F32, "rstd")
        self._ntmp = S.sb([128, 512], F32, "ntmp")

    def rstd_group(self, grp):
        S = self.S
        n = grp[3]
        sq, rstd = self._sq, self._rstd
        rbs = [self.rbuf(grp, c) for c in range(NCH)]
        for c in range(NCH):
            S.act(sq[:, c, 0:n], rbs[c][:, 0:n], AF.Square, [rbs[c]], [sq])
        ps = S.psum()
        for c in range(NCH):
            S.mm(ps[:, 0:n], self.ones_b[:], sq[:, c, 0:n], c == 0, c == NCH - 1, [self.ones_b, sq], [ps])
        S.act(rstd[:, 0:n], ps[:, 0:n], AF.Sqrt, [ps, self.epsb], [rstd], bias=self.epsb[:, 0:1], scale=1.0 / D)
        S.recip(rstd[:, 0:n], rstd[:, 0:n], [rstd], [rstd])
        return rbs, rstd

    def norm_group(self, grp, a_buf, a_which_shift, sn, out_bf=None, out_f=None):
        S = self.S
        n = grp[3]
        tmp = self._ntmp
        rbs, rstd = self.rstd_group(grp)
        for c in range(NCH):
            S.stt(tmp[:, 0:n], rbs[c][:, 0:n], a_buf[:, c:c + 1], rstd[:, 0:n], ALU.mult, ALU.mult,
                  [rbs[c], a_buf, rstd], [tmp])
            sh = self.mvec(sn, a_which_shift, c)
            if out_f is not None:
                S.act(out_f[c][0], tmp[:, 0:n], AF.Identity, [tmp, self.modT], [out_f[c][1]], bias=sh)
            if out_bf is not None:
                S.act(out_bf[c][0], tmp[:, 0:n], AF.Identity, [tmp, self.modT], [out_bf[c][1]], bias=sh)

    def moe_layer(self, layer, keep_ctx):
        S = self.S
        A = self.A
        S.push()
        m = {}
        m["h2tm"] = [S.sb([128, D], BF16, "h2tm") for _ in range(18)]
        m["slot_tm"] = S.sb([128, 18, NE], F32, "slottm")
        m["affTb"] = S.sb([16, 2304], BF16, "affTb")
        m["slotTb"] = S.sb([16, 2304], BF16, "slotTb")
        m["rw"] = S.sb([128, NCH, NE], F32, "rw")
        m["sm"] = S.sb([128, 4], F32, "sm")
        m["m8"] = S.sb([16, 8], F32, "m8")
        groups = self.groups if keep_ctx else self.groups[:4]
        ntiles = 18 if keep_ctx else 16
        ncap = 288 if keep_ctx else 256
        with self.nc.allow_non_contiguous_dma("tiny"):
            S.dma("sp", m["rw"][:], A["moe_router"][layer * D:(layer + 1) * D, :].rearrange("(c p) e -> p c e", p=128),
                  writes=[m["rw"]])
        self._wk = 0

        def wsrc(kind, e):
            nm = {"g": "moe_w_gate", "u": "moe_w_up", "d": "moe_w_down"}[kind]
            base = (layer * NE + e) * D
            return A[nm][base:base + D, :].rearrange("(c p) n -> p c n", p=128)

        def wload(kind, e):
            b = m["w"][self._wk % 2]
            self._wk += 1
            S.dma("pool", b[:], wsrc(kind, e), writes=[b])
            return b

        S.push()
        m["affT"] = S.sb([16, 2304], F32, "affT")
        m["aff_tm"] = S.sb([128, 18, NE], F32, "afftm")
        S.push()
        self.norm_scratch()
        m["hf"] = [S.sb([128, 512], F32, "hf") for _ in range(NCH)]
        m["hb"] = [S.sb([128, 512], BF16, "hb") for _ in range(NCH)]
        sm = m["sm"]
        tile = 0
        for grp in groups:
            sn = grp[0]
            n = grp[3]
            self.norm_group(grp, self.M[sn]["a2"], 3, sn,
                            out_bf=[(m["hb"][c][:, 0:n], m["hb"][c]) for c in range(NCH)],
                            out_f=[(m["hf"][c][:, 0:n], m["hf"][c]) for c in range(NCH)])
            for tt in range(n // 128):
                tsl = slice(tt * 128, (tt + 1) * 128)
                ps = S.psum()
                for c in range(NCH):
                    S.mm(ps[:, 0:NE], m["hf"][c][:, tsl], m["rw"][:, c, :], c == 0, c == NCH - 1,
                         [m["hf"][c], m["rw"]], [ps])
                S.red(sm[:, 0:1], ps[:, 0:NE], ALU.max, [ps], [sm], negate=True)
                S.act(m["aff_tm"][:, tile, :], ps[:, 0:NE], AF.Exp, [ps, sm], [m["aff_tm"], sm], bias=sm[:, 0:1],
                      accum_out=sm[:, 1:2])
                S.recip(sm[:, 2:3], sm[:, 1:2], [sm], [sm])
                S.ts(m["aff_tm"][:, tile, :], m["aff_tm"][:, tile, :], sm[:, 2:3], None, ALU.mult, None,
                     [m["aff_tm"], sm], [m["aff_tm"]])
                ps2 = S.psum()
                S.trf(ps2[0:NE, 0:128], m["aff_tm"][:, tile, :], self.ident_f[:], [m["aff_tm"], self.ident_f], [ps2])
                S.cp(m["affT"][:, tile * 128:(tile + 1) * 128], ps2[0:NE, 0:128], [ps2], [m["affT"]], e="act")
                ps3 = S.psum()
                psb = ps3[:].bitcast(BF16)
                for c in range(NCH):
                    S.tr(psb[:, c * 128:(c + 1) * 128], m["hb"][c][:, tsl], self.ident_b[:],
                         [m["hb"][c], self.ident_b], [ps3])
                S.cp(m["h2tm"][tile][:], psb[:, 0:D], [ps3], [m["h2tm"][tile]], e=("act" if tile % 2 else "dve"))
                tile += 1
        for c_ in range(NCH):
            self.dbg("hf%d" % c_, m["hf"][c_][:], m["hf"][c_], [128, 512])
        self.dbg("sm", m["sm"][:, 0:3], m["sm"], [128, 3])
        self.dbg("rc0", self.RC[0][:], self.RC[0], [128, 256])
        self.dbg("modT", self.modT[:].rearrange("p a b -> p (a b)"), self.modT, [128, 96])
        self.dbg("aff", m["aff_tm"][:].rearrange("p a b -> p (a b)"), m["aff_tm"], [128, 18 * NE])
        self.dbg("h2tm0", m["h2tm"][0][:], m["h2tm"][0], [128, D])
        S.pop()

        self.dbg("affT", m["affT"][:], m["affT"], [16, 2304])
        S.push()
        m["wk"] = S.sb([16, 2304], F32, "wk")
        m["rankT"] = S.sb([16, 2304], F32, "rankT")
        regions = [(0, 2048, 256)] + ([(2048, 256, 32)] if keep_ctx else [])
        for (r0, rn, k) in regions:
            rs = slice(r0, r0 + rn)
            S.cp(m["wk"][:, rs], m["affT"][:, rs], [m["affT"]], [m["wk"]])
            for rnd in range(k // 8):
                S.max8(m["m8"][:], m["wk"][:, rs], [m["wk"]], [m["m8"]])
                if rnd < k // 8 - 1:
                    S.mrep(m["wk"][:, rs], m["m8"][:], m["wk"][:, rs], -1.0, [m["wk"], m["m8"]], [m["wk"]])
            S.ts(m["wk"][:, rs], m["affT"][:, rs], m["m8"][:, 7:8], None, ALU.is_ge, None,
                 [m["affT"], m["m8"]], [m["wk"]])
            S.scan(m["rankT"][:, rs], m["wk"][:, rs], m["wk"][:, rs], 0.0, ALU.add, ALU.max, [m["wk"]], [m["rankT"]])
            S.tt(m["rankT"][:, rs], m["rankT"][:, rs], m["wk"][:, rs], ALU.mult, [m["rankT"], m["wk"]], [m["rankT"]])
            S.ts(m["rankT"][:, rs], m["rankT"][:, rs], -1.0, None, ALU.add, None, [m["rankT"]], [m["rankT"]])
        nT = ntiles * 128
        S.cp(m["slotTb"][:, 0:nT], m["rankT"][:, 0:nT], [m["rankT"]], [m["slotTb"]])
        S.cp(m["affTb"][:, 0:nT], m["affT"][:, 0:nT], [m["affT"]], [m["affTb"]], e="act")
        for t in range(ntiles):
            ps = S.psum()
            S.trf(ps[:, 0:NE], m["rankT"][:, t * 128:(t + 1) * 128], self.ident_f[0:16, 0:16],
                 [m["rankT"], self.ident_f], [ps])
            S.cp(m["slot_tm"][:, t, :], ps[:, 0:NE], [ps], [m["slot_tm"]], e=("act" if t % 2 else "dve"))
        self.dbg("slot", m["slot_tm"][:].rearrange("p a b -> p (a b)"), m["slot_tm"], [128, 18 * NE])
        self.dbg("m8", m["m8"][:], m["m8"], [16, 8])
        S.pop()
        S.pop()

        S.push()
        m["w"] = [S.sb([128, NCH, 1024], BF16, "wexp") for _ in range(2)]
        wq = [wload("g", 0), wload("u", 0)]
        m["P"] = [S.sb([128, 16 * 256 + 2 * 32], BF16, "P") for _ in range(1)]
        m["PT"] = [S.sb([128, 2048], BF16, "PT") for _ in range(2)] + [S.sb([32, 256], BF16, "PTc")]
        m["affbc"] = S.sb([128, 512], BF16, "affbc")
        m["xeT"] = S.sb([128, NCH, 288], BF16, "xeT")
        m["actT"] = S.sb([128, NCH, 288], BF16, "actT")
        m["sil"] = S.sb([128, NCH, 288], BF16, "sil")
        m["ye"] = [S.sb([128, D], BF16, "ye") for _ in range(3)]
        g2 = {sn: [self.mvec(sn, 5, c) for c in range(NCH)] for sn in ("l", "c")}
        modT = self.modT
        PT = m["PT"]
        for e in range(NE):
            P = m["P"][0]
            wg, wu = wq
            for t in range(ntiles):
                if t < 16:
                    S.ts(P[:, t * 256:(t + 1) * 256], self.iota_row[:, 0:256], m["slot_tm"][:, t, e:e + 1], None,
                         ALU.is_equal, None, [self.iota_row, m["slot_tm"]], [P])
                else:
                    o = 4096 + (t - 16) * 32
                    S.ts(P[:, o:o + 32], self.iota_row[:, 0:32], m["slot_tm"][:, t, e:e + 1], None,
                         ALU.is_equal, None, [self.iota_row, m["slot_tm"]], [P])
            for c in range(NCH):
                ps = S.psum()
                for t in range(16):
                    S.mm(ps[:, 0:256], m["h2tm"][t][:, c * 128:(c + 1) * 128], P[:, t * 256:(t + 1) * 256],
                         t == 0, t == 15, [m["h2tm"][t], P], [ps])
                if keep_ctx:
                    for t in range(16, 18):
                        o = 4096 + (t - 16) * 32
                        S.mm(ps[:, 256:288], m["h2tm"][t][:, c * 128:(c + 1) * 128], P[:, o:o + 32],
                             t == 16, t == 17, [m["h2tm"][t], P], [ps])
                S.cp(m["xeT"][:, c, 0:ncap], ps[:, 0:ncap], [ps], [m["xeT"]], e=("act" if c % 2 else "dve"))
            selE = self.sel[:, e * 128:(e + 1) * 128]
            ab = m["affbc"]
            for gi, grp in enumerate(groups):
                n = grp[3]
                r0 = grp[2] if grp[0] == "l" else 2048
                psB = S.psum()
                S.mm(psB[:, 0:n], selE, m["affTb"][:, r0:r0 + n], True, True, [self.sel, m["affTb"]], [psB])
                S.cp(ab[:, 0:n], psB[:, 0:n], [psB], [ab], e="act")
                psA = S.psum()
                if grp[0] == "l":
                    S.mm(psA[:, 0:n], selE, m["slotTb"][:, r0:r0 + n], True, True, [self.sel, m["slotTb"]], [psA])
                    for cc in range(2):
                        S.stt(PT[cc][:, r0:r0 + n], psA[:, 0:n], self.iota_part[:, cc:cc + 1], ab[:, 0:n],
                              ALU.is_equal, ALU.mult, [psA, self.iota_part, ab], [PT[cc]])
                else:
                    S.mm(psA[0:32, 0:n], selE[:, 0:32], m["slotTb"][:, r0:r0 + n], True, True,
                         [self.sel, m["slotTb"]], [psA])
                    S.stt(PT[2][:, 0:n], psA[0:32, 0:n], self.iota_part[0:32, 0:1], ab[0:32, 0:n],
                          ALU.is_equal, ALU.mult, [psA, self.iota_part, ab], [PT[2]])
            for f in range(NCH):
                psA = S.psum()
                for c in range(NCH):
                    S.mm(psA[:, 0:ncap], wg[:, c, f * 128:(f + 1) * 128], m["xeT"][:, c, 0:ncap], c == 0, c == NCH - 1,
                         [wg, m["xeT"]], [psA])
                S.act(m["sil"][:, f, 0:ncap], psA[:, 0:ncap], AF.Silu, [psA], [m["sil"]])
            wd = wload("d", e)
            for f in range(NCH):
                psU = S.psum()
                for c in range(NCH):
                    S.mm(psU[:, 0:ncap], wu[:, c, f * 128:(f + 1) * 128], m["xeT"][:, c, 0:ncap], c == 0, c == NCH - 1,
                         [wu, m["xeT"]], [psU])
                S.tt(m["actT"][:, f, 0:ncap], m["sil"][:, f, 0:ncap], psU[:, 0:ncap], ALU.mult, [m["sil"], psU], [m["actT"]])
            if e + 1 < NE:
                wg_n = wload("g", e + 1)
            for cc, rows in enumerate((128, 128, 32)[:3 if keep_ctx else 2]):
                for dh in range(2):
                    ps = S.psum()
                    for f in range(NCH):
                        S.mm(ps[0:rows, :], m["actT"][:, f, cc * 128:cc * 128 + rows], wd[:, f, dh * 512:(dh + 1) * 512],
                             f == 0, f == NCH - 1, [m["actT"], wd], [ps])
                    S.cp(m["ye"][cc][0:rows, dh * 512:(dh + 1) * 512], ps[0:rows, :], [ps], [m["ye"][cc]],
                         e=("act" if dh else "dve"))
            if e + 1 < NE:
                wu_n = wload("u", e + 1)
                wq = [wg_n, wu_n]
            for grp in groups:
                n = grp[3]
                r0 = grp[2]
                for c in range(NCH):
                    ps = S.psum()
                    rb = self.rbuf(grp, c)
                    if grp[0] == "l":
                        for cc in range(2):
                            S.mm(ps[:, 0:n], m["ye"][cc][:, c * 128:(c + 1) * 128], PT[cc][:, r0:r0 + n], cc == 0, cc == 1,
                                 [m["ye"][cc], PT[cc]], [ps])
                    else:
                        S.mm(ps[:, 0:n], m["ye"][2][0:32, c * 128:(c + 1) * 128], PT[2][:, 0:n], True, True,
                             [m["ye"][2], PT[2]], [ps])
                    S.stt(rb[:, 0:n], ps[:, 0:n], g2[grp[0]][c], rb[:, 0:n], ALU.mult, ALU.add, [ps, modT, rb], [rb])
        S.pop()
        S.pop()

    def final_store(self, do_norm=True):
        S = self.S
        A = self.A
        S.push()
        self.norm_scratch()
        fw = S.sb([128, NCH], F32, "fnw")
        with self.nc.allow_non_contiguous_dma("tiny"):
            S.dma("sp", fw[:], A["final_norm_w"][0].rearrange("(c p) -> p c", p=128), writes=[fw])
        ob = [S.sb([128, D], F32, "ob") for _ in range(2)]
        hf = [S.sb([128, 512], F32, "hf") for _ in range(NCH)]
        k = 0
        out_evs = []
        glist = [(g, self.out) for g in self.groups[:4]]
        if self.cfg.get("dump_ctx"):
            glist.append((self.groups[4], self.out_c))
        for grp, dst in glist:
            n = grp[3]
            norm = do_norm and grp[0] == "l"
            if norm:
                rbs, rstd = self.rstd_group(grp)
                for c in range(NCH):
                    S.stt(hf[c][:, 0:n], rbs[c][:, 0:n], fw[:, c:c + 1], rstd[:, 0:n], ALU.mult, ALU.mult,
                          [rbs[c], fw, rstd], [hf[c]])
            for tt in range(n // 128):
                o = ob[k % 2]
                k += 1
                for half in range(2):
                    ps = S.psum()
                    for j in range(4):
                        c = half * 4 + j
                        src = hf[c] if norm else self.rbuf(grp, c)
                        S.trf(ps[:, j * 128:(j + 1) * 128], src[:, tt * 128:(tt + 1) * 128], self.ident_f[:],
                             [src, self.ident_f], [ps])
                    S.cp(o[:, half * 512:(half + 1) * 512], ps[:, :], [ps], [o], e=("act" if half else "dve"))
                t0 = grp[2] + tt * 128
                out_evs.append(S.dma("sp", dst[t0:t0 + 128, :], o[:], reads=[o]))
        for ev in out_evs:
            S._wait("sp", ev)
        S.pop()

    def build(self):
        cfg = self.cfg
        self.declare()
        self.setup()
        self.load_stream()
        for layer in cfg.get("layers", range(DEPTH)):
            keep_ctx = layer < DEPTH - 1
            self.adaln(layer)
            if cfg.get("mixer", True):
                self.mixer(layer, keep_ctx)
            if cfg.get("moe", True):
                self.moe_layer(layer, keep_ctx)
        self.final_store(cfg.get("final_norm", True))
        self.S.emit()
        return self.nc

    def hnorm(self, keep_ctx=True):
        S = self.S
        H = [S.sb([128, 2304], BF16, "H") for _ in range(NCH)]
        S.push()
        self.norm_scratch()
        for grp in self.groups:
            sn = grp[0]
            n = grp[3]
            o = grp[2] if sn == "l" else 2048
            self.norm_group(grp, self.M[sn]["a1"], 0, sn, out_bf=[(H[c][:, o:o + n], H[c]) for c in range(NCH)])
        S.pop()
        return H

    def tok_groups(self, keep_ctx=True):
        gs = [(g, g[2], 512) for g in self.groups[:4]]
        if keep_ctx:
            gs.append((self.groups[4], 2048, 256))
        return gs

    def out_proj(self, Z, w_ap, keep_ctx):
        S = self.S
        wo = S.sb([128, NCH, D], BF16, "wo")
        S.dma("pool", wo[:], w_ap.rearrange("(c p) n -> p c n", p=128), writes=[wo])
        for (grp, o, n) in self.tok_groups(keep_ctx):
            for c in range(NCH):
                ps = S.psum()
                for k in range(NCH):
                    S.mm(ps[:, 0:n], wo[:, k, c * 128:(c + 1) * 128], Z[k][:, o:o + n], k == 0, k == NCH - 1,
                         [wo, Z[k]], [ps])
                rb = self.rbuf(grp, c)
                S.stt(rb[:, 0:n], ps[:, 0:n], self.mvec(grp[0], 2, c), rb[:, 0:n], ALU.mult, ALU.add,
                      [ps, self.modT, rb], [rb])

    def mixer(self, layer, keep_ctx):
        kind = layer % 4
        if kind == 0:
            self.mixer_hyena(keep_ctx)
        if kind == 1:
            self.mixer_swa(keep_ctx)
        if kind == 2:
            self.mixer_gdn(keep_ctx)
        if kind == 3:
            self.mixer_hgrn2(layer, keep_ctx)

    def mixer_hyena(self, keep_ctx):
        S = self.S
        A = self.A
        nc = self.nc
        seqs = [("l", TL, 0)] + ([("c", TC, 2048)] if keep_ctx else [])
        TWO_PI = 2.0 * np.pi
        HS = {}
        for nm, L, _ in seqs:
            HS[nm] = nc.dram_tensor("hy_spec_" + nm, [2, 8, 2, 128, L], BF16, kind="Internal").ap()

        S.push()
        w1 = S.sb([33, 64], F32, "w1")
        w2 = S.sb([64, 64], F32, "w2")
        S.dma("sp", w1[:], A["hy_ffn_w1"], writes=[w1])
        S.dma("sp", w2[:], A["hy_ffn_w2"], writes=[w2])
        pcol = S.sb([64, 4], F32, "pcol")
        S.dma("sp", pcol[:, 0:1], A["hy_ffn_b1"].rearrange("o p -> p o"), writes=[pcol])
        S.dma("sp", pcol[:, 1:2], A["hy_sin_freq"][0:1, :].rearrange("o p -> p o"), writes=[pcol])
        S.dma("sp", pcol[:, 2:3], A["hy_ffn_b2"].rearrange("o p -> p o"), writes=[pcol])
        S.dma("sp", pcol[:, 3:4], A["hy_sin_freq"][1:2, :].rearrange("o p -> p o"), writes=[pcol])
        w3p = S.sb([64, 2, D], F32, "w3p")
        w3m = S.sb([64, 2, D], F32, "w3m")
        dbc = S.sb([128, D], F32, "dbc")
        sa = {nm: S.sb([64, 256], F32, "sa_" + nm) for nm in ("arg", "t", "kf", "m")}
        ki = S.sb([64, 256], I32, "ki")
        S.push()
        w3 = S.sb([64, 4 * D], F32, "w3")
        S.dma("sp", w3[:], A["hy_ffn_w3"], writes=[w3])
        for o in range(2):
            fw_ = w3[:, o * 2048:o * 2048 + D]
            bw_ = w3[:, o * 2048 + D:o * 2048 + 2 * D]
            S.tt(w3p[:, o, :], fw_, bw_, ALU.add, [w3], [w3p])
            S.tt(w3m[:, o, :], bw_, fw_, ALU.subtract, [w3], [w3m], e="pool")
        S.pop()
        S.dma("sp", dbc[:], A["hy_delta"][0].partition_broadcast(128), writes=[dbc])

        def sinfn(dst, ps, n, bcol, fcol):
            S.ts(sa["arg"][:, 0:n], ps, pcol[:, bcol:bcol + 1], pcol[:, fcol:fcol + 1], ALU.add, ALU.mult,
                 [pcol] + ([] if isinstance(ps, int) else []), [sa["arg"]])
            S.ts(sa["t"][:, 0:n], sa["arg"][:, 0:n], 1.0 / TWO_PI, 8.5, ALU.mult, ALU.add, [sa["arg"]], [sa["t"]])
            S.cp(ki[:, 0:n], sa["t"][:, 0:n], [sa["t"]], [ki])
            S.cp(sa["kf"][:, 0:n], ki[:, 0:n], [ki], [sa["kf"]])
            S.tt(sa["t"][:, 0:n], sa["t"][:, 0:n], sa["kf"][:, 0:n], ALU.subtract, [sa["t"], sa["kf"]], [sa["t"]])
            S.ts(sa["m"][:, 0:n], sa["t"][:, 0:n], 0.5, None, ALU.is_gt, None, [sa["t"]], [sa["m"]])
            S.tt(sa["t"][:, 0:n], sa["t"][:, 0:n], sa["m"][:, 0:n], ALU.subtract, [sa["t"], sa["m"]], [sa["t"]])
            S.act(dst, sa["t"][:, 0:n], AF.Sin, [sa["t"]], [], scale=-TWO_PI)

        for nm, L, _ in seqs:
            T_ = L // 128
            N = 2 * L
            S.push()
            tcol = S.sb([128, T_], F32, "tcol")
            S.dma("sp", tcol[:], A["hy_tcol_" + nm], writes=[tcol])
            S.ts(tcol[:], tcol[:], -1.0, None, ALU.mult, None, [tcol], [tcol])
            h2T = S.sb([64, L], F32, "h2T")
            S.push()
            zT = S.sb([33, L], F32, "zT")
            S.dma("sp", zT[:], A["hy_zT_" + nm], writes=[zT])
            h1T = S.sb([64, L], F32, "h1T")
            for b0 in range(0, L, 256):
                n = min(256, L - b0)
                ps = S.psum()
                S.mm(ps[0:64, 0:n], w1[:], zT[:, b0:b0 + n], True, True, [w1, zT], [ps])
                S._deps("dve", [ps], [])
                sinfn(h1T[:, b0:b0 + n], ps[0:64, 0:n], n, 0, 1)
                S._mark((S.csem["act"], S.ccnt["act"], "act"), [ps], [h1T])
            for b0 in range(0, L, 256):
                n = min(256, L - b0)
                ps = S.psum()
                S.mm(ps[0:64, 0:n], w2[:], h1T[:, b0:b0 + n], True, True, [w2, h1T], [ps])
                S._deps("dve", [ps], [])
                sinfn(h2T[:, b0:b0 + n], ps[0:64, 0:n], n, 2, 3)
                S._mark((S.csem["act"], S.ccnt["act"], "act"), [ps], [h2T])
            S.pop()
            win = S.sb([128, T_, 128], F32, "win")
            hp = [[S.sb([128, T_, 128], BF16, "hp") for o in range(2)] for _ in range(2)]
            hm = [[S.sb([128, T_, 128], BF16, "hm") for o in range(2)] for _ in range(2)]
            Hs = [[[S.sb([128, T_, 128], BF16, "Hs") for ri in range(2)] for o in range(2)] for _ in range(2)]
            Cf = [S.sb([128, T_, 128], BF16, "Cf") for _ in range(2)]
            Sf_ = [S.sb([128, T_, 128], BF16, "Sf") for _ in range(2)]
            for pair in range(4):
                for bi in range(2):
                    cb = pair * 2 + bi
                    ccs = slice(cb * 128, (cb + 1) * 128)
                    for tt in range(T_):
                        S.act(win[:, tt, :], dbc[:, ccs], AF.Exp, [dbc, tcol], [win], scale=tcol[:, tt:tt + 1])
                    S.ts(win[:], win[:], 0.05, None, ALU.add, None, [win], [win])
                    for o in range(2):
                        for tt in range(T_):
                            dsl = slice(tt * 128, (tt + 1) * 128)
                            psP = S.psum()
                            psM = S.psum()
                            S.mm(psP[:, 0:128], h2T[:, dsl], w3p[:, o, ccs], True, True, [h2T, w3p], [psP])
                            S.mm(psM[:, 0:128], h2T[:, dsl], w3m[:, o, ccs], True, True, [h2T, w3m], [psM])
                            S.tt(hp[bi][o][:, tt, :], psP[:, 0:128], win[:, tt, :], ALU.mult, [psP, win], [hp[bi][o]])
                            S.tt(hm[bi][o][:, tt, :], psM[:, 0:128], win[:, tt, :], ALU.mult, [psM, win], [hm[bi][o]])
                        S.tt(hp[bi][o][0:1, 0, :], hp[bi][o][0:1, 0, :], hm[bi][o][0:1, 0, :], ALU.subtract,
                             [hp[bi][o], hm[bi][o]], [hp[bi][o]])
                        S.ts(hp[bi][o][0:1, 0, :], hp[bi][o][0:1, 0, :], 0.5, None, ALU.mult, None, [hp[bi][o]], [hp[bi][o]])
                for ft in range(T_):
                    cf, sf = Cf[ft % 2], Sf_[ft % 2]
                    S.dma("sp", cf[:].rearrange("p a b -> p (a b)"), A["hy_Cf_" + nm][ft * 128:(ft + 1) * 128, :], writes=[cf])
                    S.dma("sp", sf[:].rearrange("p a b -> p (a b)"), A["hy_Sf_" + nm][ft * 128:(ft + 1) * 128, :], writes=[sf])
                    for bi in range(2):
                        for o in range(2):
                            for ri, (tab, src) in enumerate(((cf, hp[bi][o]), (sf, hm[bi][o]))):
                                ps = S.psum()
                                for tt in range(T_):
                                    S.mm(ps[:, 0:128], tab[:, tt, :], src[:, tt, :], tt == 0, tt == T_ - 1, [tab, src], [ps])
                                S.act(Hs[bi][o][ri][:, ft, :], ps[:, 0:128], AF.Identity, [ps], [Hs[bi][o][ri]], scale=2.0 / N)
                for bi in range(2):
                    cb = pair * 2 + bi
                    for o in range(2):
                        for ri in range(2):
                            S.dma("sp", HS[nm][o, cb, ri], Hs[bi][o][ri][:].rearrange("p a b -> p (a b)"),
                                  reads=[Hs[bi][o][ri]], writes=[self.hs_tok])
            S.pop()
        S.pop()

        S.push()
        H = self.hnorm()
        Win = A["hy_w_in"].rearrange("(c p) n -> p c n", p=128)
        pv = S.sb([128, 24, 6], F32, "hyp")
        S.dma("sp", pv[:, :, 0], A["hy_b_in"][0].rearrange("(c p) -> p c", p=128), writes=[pv])
        for k in range(3):
            S.dma("sp", pv[:, :, 1 + k], A["hy_short_w"][k].rearrange("(c p) -> p c", p=128), writes=[pv])
        S.dma("sp", pv[:, :, 4], A["hy_short_b"][0].rearrange("(c p) -> p c", p=128), writes=[pv])
        fb = S.sb([128, 2, NCH], F32, "hyfb")
        for o in range(2):
            S.dma("sp", fb[:, o, :], A["hy_filter_bias"][o].rearrange("(c p) -> p c", p=128), writes=[fb])
        bo = S.sb([128, NCH], F32, "hybo")
        S.dma("sp", bo[:], A["hy_b_out"][0].rearrange("(c p) -> p c", p=128), writes=[bo])
        W3b = S.sb([128, NCH, 3, 128], BF16, "W3b")
        wo = S.sb([128, D], BF16, "wo_h")
        upad = S.sb([128, 2312], BF16, "upad")
        S.memset(upad[:], 0.0, [upad])
        cv = S.sb([128, 2304], F32, "cv")
        ufm = [S.sb([128, 2304], BF16, "ufm") for _ in range(3)]
        z1 = S.sb([128, 2304], BF16, "z1")
        zT_ = S.sb([128, 2304], BF16, "zTh")
        utm = S.sb([128, 16, 128], BF16, "utm")
        Hr = S.sb([128, 16, 128], BF16, "Hr")
        Hi = S.sb([128, 16, 128], BF16, "Hi")
        Yr = S.sb([128, 16, 128], BF16, "Yr")
        nYi = S.sb([128, 16, 128], BF16, "nYi")
        Cf = [S.sb([128, 16, 128], BF16, "Cf") for _ in range(2)]
        Sf_ = [S.sb([128, 16, 128], BF16, "Sf") for _ in range(2)]
        ring = [S.sb([128, 512], BF16, "ring") for _ in range(8)]
        ur = S.sb([128, 128], F32, "ur")
        ui = S.sb([128, 128], F32, "ui")
        t1 = S.sb([128, 128], F32, "t1")
        t2 = S.sb([128, 128], F32, "t2")
        ytmp = S.sb([128, 512], F32, "ytmp")
        LO, CO = 1, 2052
        rk = 0

        for cb in range(8):
            for k in range(3):
                S.dma("pool", W3b[:, :, k, :], Win[:, :, k * D + cb * 128:k * D + (cb + 1) * 128], writes=[W3b])
            for k in range(3):
                ch = k * 8 + cb
                for (grp, o, n) in self.tok_groups(keep_ctx):
                    ps = S.psum()
                    for c in range(NCH):
                        S.mm(ps[:, 0:n], W3b[:, c, k, :], H[c][:, o:o + n], c == 0, c == NCH - 1, [W3b, H[c]], [ps])
                    po_ = (LO + o) if o < 2048 else (CO + o - 2048)
                    S.act(upad[:, po_:po_ + n], ps[:, 0:n], AF.Identity, [ps, pv], [upad], bias=pv[:, ch, 0:1])
                for (nm, L, o_) in seqs:
                    base = LO if nm == "l" else CO
                    S.ts(cv[:, o_:o_ + L], upad[:, base - 1:base - 1 + L], pv[:, ch, 1:2], pv[:, ch, 4:5], ALU.mult, ALU.add,
                         [upad, pv], [cv])
                    S.stt(cv[:, o_:o_ + L], upad[:, base:base + L], pv[:, ch, 2:3], cv[:, o_:o_ + L], ALU.mult, ALU.add,
                          [upad, pv, cv], [cv])
                    S.stt(ufm[k][:, o_:o_ + L], upad[:, base + 1:base + 1 + L], pv[:, ch, 3:4], cv[:, o_:o_ + L],
                          ALU.mult, ALU.add, [upad, pv, cv], [ufm[k]])
            for (nm, L, o_) in seqs:
                T_ = L // 128
                for o in range(2):
                    src = ufm[0] if o == 0 else z1
                    xg = ufm[1 + o]
                    dst = z1 if o == 0 else zT_
                    for tt in range(T_):
                        pT = S.psum()
                        pTb = pT[:].bitcast(BF16)
                        S.tr(pTb[:, 0:128], src[:, o_ + tt * 128:o_ + (tt + 1) * 128], self.ident_b[:], [src, self.ident_b], [pT])
                        S.cp(utm[:, tt, :], pTb[:, 0:128], [pT], [utm], e="act")
                    S.dma("sp", Hr[:, 0:T_, :].rearrange("p a b -> p (a b)"), HS[nm][o, cb, 0], reads=[self.hs_tok], writes=[Hr])
                    S.dma("sp", Hi[:, 0:T_, :].rearrange("p a b -> p (a b)"), HS[nm][o, cb, 1], reads=[self.hs_tok], writes=[Hi])
                    for ft in range(T_):
                        cf, sf = Cf[ft % 2], Sf_[ft % 2]
                        S.dma("sp", cf[:, 0:T_, :].rearrange("p a b -> p (a b)"), A["hy_Cf_" + nm][ft * 128:(ft + 1) * 128, :], writes=[cf])
                        S.dma("sp", sf[:, 0:T_, :].rearrange("p a b -> p (a b)"), A["hy_Sf_" + nm][ft * 128:(ft + 1) * 128, :], writes=[sf])
                        pr = S.psum()
                        for tt in range(T_):
                            S.mm(pr[:, 0:128], cf[:, tt, :], utm[:, tt, :], tt == 0, tt == T_ - 1, [cf, utm], [pr])
                        pi_ = S.psum()
                        for tt in range(T_):
                            S.mm(pi_[:, 0:128], sf[:, tt, :], utm[:, tt, :], tt == 0, tt == T_ - 1, [sf, utm], [pi_])
                        S.cp(ur[:], pr[:, 0:128], [pr], [ur], e="act")
                        S.cp(ui[:], pi_[:, 0:128], [pi_], [ui], e="act")
                        S.tt(t1[:], ur[:], Hr[:, ft, :], ALU.mult, [ur, Hr], [t1])
                        S.tt(t2[:], ui[:], Hi[:, ft, :], ALU.mult, [ui, Hi], [t2], e="pool")
                        S.tt(Yr[:, ft, :], t1[:], t2[:], ALU.add, [t1, t2], [Yr])
                        S.tt(t1[:], ui[:], Hr[:, ft, :], ALU.mult, [ui, Hr], [t1])
                        S.tt(t2[:], ur[:], Hi[:, ft, :], ALU.mult, [ur, Hi], [t2], e="pool")
                        S.tt(nYi[:, ft, :], t1[:], t2[:], ALU.subtract, [t1, t2], [nYi])
                    for tg in range(0, L, 512):
                        n = min(512, L - tg)
                        py = S.psum()
                        for ft in range(T_):
                            rc = ring[rk % 8]
                            rs = ring[(rk + 1) % 8]
                            rk += 2
                            S.dma("sp", rc[:, 0:n], A["hy_Ct_" + nm][ft * 128:(ft + 1) * 128, tg:tg + n], writes=[rc])
                            S.dma("sp", rs[:, 0:n], A["hy_St_" + nm][ft * 128:(ft + 1) * 128, tg:tg + n], writes=[rs])
                            S.mm(py[:, 0:n], Yr[:, ft, :], rc[:, 0:n], ft == 0, False, [Yr, rc], [py])
                            S.mm(py[:, 0:n], nYi[:, ft, :], rs[:, 0:n], False, ft == T_ - 1, [nYi, rs], [py])
                        osl = slice(o_ + tg, o_ + tg + n)
                        S.stt(ytmp[:, 0:n], src[:, osl], fb[:, o, cb:cb + 1], py[:, 0:n], ALU.mult, ALU.add, [src, fb, py], [ytmp])
                        S.tt(dst[:, osl], ytmp[:, 0:n], xg[:, osl], ALU.mult, [ytmp, xg], [dst], e="pool")
            self.out_proj_head(zT_, A["hy_w_out"], cb, wo, keep_ctx)
        gb = S.sb([128, 2, NCH], F32, "hygb")
        for si, sn in enumerate(("l", "c")):
            S.tt(gb[:, si, :], self.modT[:, 16:24, si], bo[:], ALU.mult, [self.modT, bo], [gb])
        for (grp, o, n) in self.tok_groups(keep_ctx):
            si = 0 if grp[0] == "l" else 1
            for c in range(NCH):
                rb = self.rbuf(grp, c)
                S.ts(rb[:, 0:n], rb[:, 0:n], gb[:, si, c:c + 1], None, ALU.add, None, [rb, gb], [rb])
        S.pop()

    def out_proj_head(self, zT, w_ap, h, wo, keep_ctx):
        S = self.S
        S.dma("pool", wo[:], w_ap[h * 128:(h + 1) * 128, :], writes=[wo])
        for (grp, o, n) in self.tok_groups(keep_ctx):
            for c in range(NCH):
                ps = S.psum()
                S.mm(ps[:, 0:n], wo[:, c * 128:(c + 1) * 128], zT[:, o:o + n], True, True, [wo, zT], [ps])
                rb = self.rbuf(grp, c)
                S.stt(rb[:, 0:n], ps[:, 0:n], self.mvec(grp[0], 2, c), rb[:, 0:n], ALU.mult, ALU.add,
                      [ps, self.modT, rb], [rb])

    def mixer_gdn(self, keep_ctx):
        S = self.S
        A = self.A
        W = A["gd_w_in"].rearrange("(c p) n -> p c n", p=128)
        ntile = 18
        S.push()
        H = self.hnorm()
        I_f = self.ident_f
        cw = S.sb([128, 3, 24], F32, "cw")
        for k in range(3):
            S.dma("sp", cw[:, k, :], A["gd_conv_w"][k].rearrange("(c p) -> p c", p=128), writes=[cw])
        nea = S.sb([1, 16], F32, "nea")
        dtb = S.sb([1, 16], F32, "dtb")
        S.dma("sp", nea[:], A["gd_a_log"], writes=[nea])
        S.dma("sp", dtb[:], A["gd_dt_bias"], writes=[dtb])
        S.act(nea[:], nea[:], AF.Exp, [nea], [nea])
        S.ts(nea[:], nea[:], -1.0, None, ALU.mult, None, [nea], [nea])
        onesr = S.sb([1, 128], F32, "onesr")
        S.memset(onesr[:], 1.0, [onesr])
        nwbc = S.sb([128, 128], F32, "nwbc")
        S.dma("sp", nwbc[:], A["gd_norm_w"][0].partition_broadcast(128), writes=[nwbc])
        cresr = S.sb([1, 128], F32, "cresr")
        S.dma("sp", cresr[:], A["chunk_reset"][0:1, :], writes=[cresr])
        tri = [S.sb([128, 128], F32, "tri") for _ in range(2)]
        tris = [S.sb([128, 128], F32, "tris") for _ in range(2)]
        S.dma("sp", tri[0][:], A["tri_fwd"], writes=[tri[0]])
        S.dma("sp", tri[1][:], A["tri_bwd"], writes=[tri[1]])
        S.dma("sp", tris[0][:], A["tri_fwd_s"], writes=[tris[0]])
        S.dma("sp", tris[1][:], A["tri_bwd_s"], writes=[tris[1]])
        eps1 = self.epsb
        wbab = S.sb([128, NCH, 32], BF16, "wbab")
        S.dma("pool", wbab[:], W[:, :, 4096:4128], writes=[wbab])
        W4 = S.sb([128, NCH, 4, 128], BF16, "W4")
        wo = S.sb([128, D], BF16, "wo_h")
        upad = S.sb([128, 2312], BF16, "upad")
        cv = S.sb([128, 2304], F32, "cv")
        sqb = S.sb([128, 512], BF16, "sqb")
        rin = S.sb([128, 512], F32, "rin")
        qT = S.sb([128, 2304], BF16, "qT")
        kT = S.sb([128, 2304], BF16, "kT")
        ktm = S.sb([128, ntile, 128], BF16, "ktm")
        vtm = S.sb([128, ntile, 128], BF16, "vtm")
        ztm = S.sb([128, ntile, 128], BF16, "ztm")
        of = S.sb([128, ntile, 128], F32, "of")
        zT = S.sb([128, 2304], BF16, "zT")
        vT = zT
        brow = S.sb([1, 128], F32, "brow")
        grow = S.sb([1, 128], F32, "grow")
        R = {nm: S.sb([1, 128], F32, nm) for nm in ("Gi", "G", "nG", "dl", "EGr")}
        Sf = S.sb([128, 128], F32, "Sf")
        Sb = S.sb([128, 128], BF16, "Sb")
        W2 = S.sb([128, 2, 128], BF16, "W2")
        Q2 = S.sb([128, 2, 128], BF16, "Q2")
        S.memset(W2[:], 0.0, [W2])
        S.memset(Q2[:], 0.0, [Q2])
        S.memset(upad[:], 0.0, [upad])
        T = {nm: S.sb([128, 128], F32, nm) for nm in ("Em", "Ee", "Ei", "Es", "EB", "A", "B", "IA", "IB", "P", "Pt",
                                                     "u", "av", "os", "nwg", "ysq", "o")}
        rhs = S.sb([128, 256], F32, "rhs")
        cols = S.sb([128, 8], F32, "cols")
        wsb = S.sb([128, 128], BF16, "wsb")
        kdtm = S.sb([128, 128], BF16, "kdtm")
        attnT = S.sb([128, 128], BF16, "attnT")
        vnew = S.sb([128, 128], BF16, "vnew")
        yb = S.sb([128, 128], BF16, "yb")
        st = S.sb([128, 4], F32, "gdst")
        LO, CO = 1, 2052

        def pad_view(o, n):
            return (LO + o) if o < 2048 else (CO + o - 2048)

        for h in range(8):
            for k in range(4):
                S.dma("pool", W4[:, :, k, :], W[:, :, k * D + h * 128:k * D + (h + 1) * 128], writes=[W4])
            for k, dst in ((0, qT), (1, kT), (2, vT)):
                for (grp, o, n) in self.tok_groups(True):
                    ps = S.psum()
                    for c in range(NCH):
                        S.mm(ps[:, 0:n], W4[:, c, k, :], H[c][:, o:o + n], c == 0, c == NCH - 1, [W4, H[c]], [ps])
                    po_ = pad_view(o, n)
                    S.cp(upad[:, po_:po_ + n], ps[:, 0:n], [ps], [upad], e="act")
                ch = k * 8 + h
                for (base, n_, o_) in ((LO, 2048, 0), (CO, 256, 2048)):
                    S.ts(cv[:, o_:o_ + n_], upad[:, base - 1:base - 1 + n_], cw[:, 0, ch:ch + 1], None, ALU.mult, None,
                         [upad, cw], [cv])
                    S.stt(cv[:, o_:o_ + n_], upad[:, base:base + n_], cw[:, 1, ch:ch + 1], cv[:, o_:o_ + n_],
                          ALU.mult, ALU.add, [upad, cw, cv], [cv])
                    S.stt(cv[:, o_:o_ + n_], upad[:, base + 1:base + 1 + n_], cw[:, 2, ch:ch + 1], cv[:, o_:o_ + n_],
                          ALU.mult, ALU.add, [upad, cw, cv], [cv])
                S.act(cv[:], cv[:], AF.Silu, [cv], [cv])
                if k == 2:
                    S.cp(dst[:], cv[:], [cv], [dst])
                    continue
                for (grp, o, n) in self.tok_groups(True):
                    S.act(sqb[:, 0:n], cv[:, o:o + n], AF.Square, [cv], [sqb])
                    ps = S.psum()
                    S.mm(ps[:, 0:n], self.ones_b[:], sqb[:, 0:n], True, True, [self.ones_b, sqb], [ps])
                    S.act(rin[:, 0:n], ps[:, 0:n], AF.Sqrt, [ps, eps1], [rin], bias=eps1[:, 0:1],
                          scale=(128.0 if k == 0 else 1.0))
                    S.recip(rin[:, 0:n], rin[:, 0:n], [rin], [rin])
                    S.tt(dst[:, o:o + n], cv[:, o:o + n], rin[:, 0:n], ALU.mult, [cv, rin], [dst])
            for t in range(ntile):
                tsl = slice(t * 128, (t + 1) * 128)
                for src, dstm in ((kT, ktm), (vT, vtm)):
                    pT = S.psum()
                    pTb = pT[:].bitcast(BF16)
                    S.tr(pTb[:, 0:128], src[:, tsl], self.ident_b[:], [src, self.ident_b], [pT])
                    S.cp(dstm[:, t, :], pTb[:, 0:128], [pT], [dstm], e="act")
                ps = S.psum()
                for c in range(NCH):
                    S.mm(ps[:, 0:128], H[c][:, tsl], W4[:, c, 3, :], c == 0, c == NCH - 1, [H[c], W4], [ps])
                S.act(ztm[:, t, :], ps[:, 0:128], AF.Silu, [ps], [ztm])
            for d in range(2):
                idx = d * 8 + h
                S.memset(Sf[:], 0.0, [Sf])
                S.memset(Sb[:], 0.0, [Sb])
                order = [16, 17] + list(range(16)) if d == 0 else [17, 16] + list(range(15, -1, -1))
                idx = d * 8 + h
                for tile in order:
                    o0 = tile * 128
                    tsl = slice(o0, o0 + 128)
                    for kind, dstr in ((0, brow), (1, grow)):
                        col = kind * 16 + idx
                        ps = S.psum()
                        for c in range(NCH):
                            S.mm(ps[0:1, 0:128], wbab[:, c, col:col + 1], H[c][:, tsl], c == 0, c == NCH - 1,
                                 [wbab, H[c]], [ps])
                        if kind == 0:
                            S.act(dstr[:], ps[0:1, 0:128], AF.Sigmoid, [ps], [dstr])
                        else:
                            S.act(dstr[:], ps[0:1, 0:128], AF.Exp, [ps, dtb], [dstr], bias=dtb[0:1, idx:idx + 1])
                            S.act(dstr[:], dstr[:], AF.Ln, [dstr, onesr], [dstr], bias=onesr[0:1, 0:1])
                            S.ts(dstr[:], dstr[:], nea[0:1, idx:idx + 1], None, ALU.mult, None, [dstr, nea], [dstr])
                    gr = grow[:]
                    S.scan(R["Gi"][:], cresr[:], gr, 0.0, ALU.mult, ALU.add, [cresr, grow], [R["Gi"]])
                    if d == 0:
                        G = R["Gi"]
                    else:
                        G = R["G"]
                        S.tt(G[:], gr, R["Gi"][:], ALU.subtract, [grow, R["Gi"]], [G])
                        for ci in range(2):
                            cs = slice(64 * ci, 64 * ci + 64)
                            S.ts(G[:, cs], G[:, cs], R["Gi"][:, 64 * ci + 63:64 * ci + 64], None, ALU.add, None,
                                 [G, R["Gi"]], [G])
                    S.ts(R["nG"][:], G[:], -1.0, None, ALU.mult, None, [G], [R["nG"]])
                    for ci in range(2):
                        cs = slice(64 * ci, 64 * ci + 64)
                        last = 64 * ci + (63 if d == 0 else 0)
                        S.ts(R["dl"][:, cs], G[:, cs], G[:, last:last + 1], None, ALU.subtract, None, [G], [R["dl"]])
                    S.act(R["EGr"][:], G[:], AF.Exp, [G], [R["EGr"]])
                    pD = S.psum()
                    S.mm(pD[:, 0:128], R["nG"][:], onesr[:], True, False, [R["nG"], onesr], [pD])
                    S.mm(pD[:, 0:128], onesr[:], G[:], False, True, [onesr, G], [pD])
                    pC = S.psum()
                    S.mm(pC[:, 0:1], G[:], onesr[:, 0:1], True, True, [G, onesr], [pC])
                    S.mm(pC[:, 1:2], R["dl"][:], onesr[:, 0:1], True, True, [R["dl"], onesr], [pC])
                    S.mm(pC[:, 2:3], brow[:], onesr[:, 0:1], True, True, [brow, onesr], [pC])
                    for ci in range(2):
                        last = 64 * ci + (63 if d == 0 else 0)
                        S.mm(pC[:, 4 + ci:5 + ci], onesr[:], R["EGr"][:, last:last + 1], True, True, [onesr, R["EGr"]], [pC])
                    pB = S.psum()
                    S.mm(pB[:, 0:128], onesr[:], brow[:], True, True, [onesr, brow], [pB])
                    S.ts(T["Em"][:], pD[:, 0:128], 0.0, None, ALU.min, None, [pD], [T["Em"]])
                    S.act(T["Ee"][:], T["Em"][:], AF.Exp, [T["Em"]], [T["Ee"]])
                    S.tt(T["Ei"][:], T["Ee"][:], tri[d][:], ALU.mult, [T["Ee"], tri[d]], [T["Ei"]], e="pool")
                    S.tt(T["Es"][:], T["Ee"][:], tris[d][:], ALU.mult, [T["Ee"], tris[d]], [T["Es"]], e="pool")
                    S.act(cols[:, 0:1], pC[:, 0:1], AF.Exp, [pC], [cols])
                    S.act(cols[:, 1:2], pC[:, 1:2], AF.Exp, [pC], [cols], scale=-1.0)
                    S.act(cols[:, 2:3], pC[:, 2:3], AF.Identity, [pC], [cols])
                    S.act(cols[:, 4:6], pC[:, 4:6], AF.Identity, [pC], [cols])
                    S.tt(cols[:, 3:4], cols[:, 2:3], cols[:, 0:1], ALU.mult, [cols], [cols])
                    S.tt(T["EB"][:], pB[:, 0:128], T["Es"][:], ALU.mult, [pB, T["Es"]], [T["EB"]])
                    pK = S.psum()
                    S.mm(pK[:, 0:128], kT[:, tsl], kT[:, tsl], True, True, [kT], [pK])
                    S.tt(T["B"][:], pK[:, 0:128], T["EB"][:], ALU.mult, [pK, T["EB"]], [T["B"]])
                    pA = S.psum()
                    S.trf(pA[:, 0:128], T["B"][:], I_f[:], [T["B"], I_f], [pA])
                    S.cp(T["A"][:], pA[:, 0:128], [pA], [T["A"]], e="act")
                    S.tt(T["P"][:], I_f[:], T["B"][:], ALU.subtract, [I_f, T["B"]], [T["P"]])
                    S.tt(T["Pt"][:], I_f[:], T["A"][:], ALU.subtract, [I_f, T["A"]], [T["Pt"]], e="pool")
                    for lvl in range(5):
                        pA2 = S.psum()
                        pB2 = S.psum()
                        S.mm(pA2[:, 0:128], T["B"][:], T["A"][:], True, True, [T["B"], T["A"]], [pA2])
                        S.mm(pB2[:, 0:128], T["A"][:], T["B"][:], True, True, [T["A"], T["B"]], [pB2])
                        S.tt(T["IA"][:], pA2[:, 0:128], I_f[:], ALU.add, [pA2, I_f], [T["IA"]])
                        S.tt(T["IB"][:], pB2[:, 0:128], I_f[:], ALU.add, [pB2, I_f], [T["IB"]])
                        if lvl < 4:
                            S.cp(T["A"][:], pA2[:, 0:128], [pA2], [T["A"]], e="act")
                            S.cp(T["B"][:], pB2[:, 0:128], [pB2], [T["B"]], e="act")
                        pP = S.psum()
                        pPt = S.psum()
                        S.mm(pP[:, 0:128], T["Pt"][:], T["IB"][:], True, True, [T["Pt"], T["IB"]], [pP])
                        S.mm(pPt[:, 0:128], T["P"][:], T["IA"][:], True, True, [T["P"], T["IA"]], [pPt])
                        S.cp(T["P"][:], pP[:, 0:128], [pP], [T["P"]])
                        S.cp(T["Pt"][:], pPt[:, 0:128], [pPt], [T["Pt"]], e="act")
                    S.ts(rhs[:, 0:128], vtm[:, tile, :], cols[:, 2:3], None, ALU.mult, None, [vtm, cols], [rhs])
                    S.ts(rhs[:, 128:256], ktm[:, tile, :], cols[:, 3:4], None, ALU.mult, None, [ktm, cols], [rhs])
                    pU = S.psum()
                    S.mm(pU[:, 0:256], T["P"][:], rhs[:], True, True, [T["P"], rhs], [pU])
                    S.cp(T["u"][:], pU[:, 0:128], [pU], [T["u"]], e="act")
                    S.cp(wsb[:], pU[:, 128:256], [pU], [wsb], e="act")
                    pW = S.psum()
                    pWb = pW[:].bitcast(BF16)
                    S.tr(pWb[:, 0:128], wsb[:], self.ident_b[:], [wsb, self.ident_b], [pW])
                    S.cp(W2[:, 0, 0:64], pWb[:, 0:64], [pW], [W2])
                    S.cp(W2[:, 1, 64:128], pWb[:, 64:128], [pW], [W2])
                    S.cp(Q2[:, 0, 0:64], qT[:, o0:o0 + 64], [qT], [Q2], e="pool")
                    S.cp(Q2[:, 1, 64:128], qT[:, o0 + 64:o0 + 128], [qT], [Q2], e="pool")
                    S.ts(kdtm[:], ktm[:, tile, :], cols[:, 1:2], None, ALU.mult, None, [ktm, cols], [kdtm])
                    pQK = S.psum()
                    S.mm(pQK[:, 0:128], kT[:, tsl], qT[:, tsl], True, True, [kT, qT], [pQK])
                    S.tt(attnT[:], pQK[:, 0:128], T["Ei"][:], ALU.mult, [pQK, T["Ei"]], [attnT])
                    pq = S.psum()
                    corder = (0, 1) if d == 0 else (1, 0)
                    for n_, ci in enumerate(corder):
                        pr = slice(64 * ci, 64 * ci + 64)
                        pv = S.psum()
                        S.mm(pv[:, 0:128], W2[:, ci, :], Sb[:], True, True, [W2, Sb], [pv])
                        S.tt(vnew[pr, :], T["u"][pr, :], pv[pr, 0:128], ALU.subtract, [T["u"], pv], [vnew])
                        S.mm(pq[:, 0:128], Q2[:, ci, :], Sb[:], n_ == 0, n_ == 1, [Q2, Sb], [pq])
                        psS = S.psum()
                        S.mm(psS[:, 0:128], kdtm[pr, :], vnew[pr, :], True, True, [kdtm, vnew], [psS])
                        S.stt(Sf[:], Sf[:], cols[:, 4 + ci:5 + ci], psS[:, 0:128], ALU.mult, ALU.add, [Sf, cols, psS], [Sf])
                        S.cp(Sb[:], Sf[:], [Sf], [Sb], e="act")
                    pav = S.psum()
                    S.mm(pav[:, 0:128], attnT[:], vnew[:], True, True, [attnT, vnew], [pav])
                    S.cp(T["av"][:], pav[:, 0:128], [pav], [T["av"]], e="act")
                    S.stt(T["o"][:], pq[:, 0:128], cols[:, 0:1], T["av"][:], ALU.mult, ALU.add, [pq, cols, T["av"]], [T["o"]])
                    if d == 0:
                        S.cp(of[:, tile, :], T["o"][:], [T["o"]], [of], e="pool")
                        continue
                    if tile >= 16 and not keep_ctx:
                        continue
                    S.tt(T["os"][:], T["o"][:], of[:, tile, :], ALU.add, [T["o"], of], [T["os"]])
                    S.act(T["ysq"][:], T["os"][:], AF.Square, [T["os"]], [T["ysq"], st], accum_out=st[:, 0:1])
                    S.act(st[:, 1:2], st[:, 0:1], AF.Sqrt, [st, eps1], [st], bias=eps1[:, 0:1], scale=1.0 / 128)
                    S.recip(st[:, 2:3], st[:, 1:2], [st], [st])
                    S.tt(T["nwg"][:], nwbc[:], ztm[:, tile, :], ALU.mult, [nwbc, ztm], [T["nwg"]], e="pool")
                    S.stt(yb[:], T["os"][:], st[:, 2:3], T["nwg"][:], ALU.mult, ALU.mult, [T["os"], st, T["nwg"]], [yb])
                    pY = S.psum()
                    pYb = pY[:].bitcast(BF16)
                    S.tr(pYb[:, 0:128], yb[:], self.ident_b[:], [yb, self.ident_b], [pY])
                    S.cp(zT[:, tsl], pYb[:, 0:128], [pY], [zT], e="act")
            self.out_proj_head(zT, A["gd_w_out"], h, wo, keep_ctx)
        S.pop()

    def mixer_hgrn2(self, layer, keep_ctx):
        assert not keep_ctx, "only the last layer uses HGRN2 here (no context outputs needed)"
        S = self.S
        A = self.A
        W = A["hg_w_in"].rearrange("(c p) n -> p c n", p=128)
        S.push()
        ZT = [S.sb([128, 2304], BF16, "ZT") for _ in range(8)]
        S.push()
        H = self.hnorm()
        lbr = S.sb([128, DEPTH, NCH], F32, "lbr")
        for i in range(DEPTH):
            S.dma("sp", lbr[:, i, :], A["hg_lb"][i].rearrange("(c p) -> p c", p=128), writes=[lbr])
        mx = S.sb([128, NCH], F32, "lbmx")
        S.tt(mx[:], lbr[:, 0, :], lbr[:, 1, :], ALU.max, [lbr], [mx])
        for i in range(2, DEPTH):
            S.tt(mx[:], mx[:], lbr[:, i, :], ALU.max, [mx, lbr], [mx])
        for i in range(DEPTH):
            S.tt(lbr[:, i, :], lbr[:, i, :], mx[:], ALU.subtract, [lbr, mx], [lbr])
        S.act(lbr[:], lbr[:], AF.Exp, [lbr], [lbr])
        den = S.sb([128, NCH], F32, "lbden")
        num = S.sb([128, NCH], F32, "lbnum")
        S.tt(den[:], lbr[:, 0, :], lbr[:, 1, :], ALU.add, [lbr], [den])
        for i in range(2, DEPTH):
            S.tt(den[:], den[:], lbr[:, i, :], ALU.add, [den, lbr], [den])
        S.cp(num[:], lbr[:, 1, :], [lbr], [num])
        for i in range(2, layer + 1):
            S.tt(num[:], num[:], lbr[:, i, :], ALU.add, [num, lbr], [num])
        S.recip(den[:], den[:], [den], [den])
        lb = S.sb([128, NCH], F32, "lb")
        oml = S.sb([128, NCH], F32, "oml")
        S.tt(lb[:], num[:], den[:], ALU.mult, [num, den], [lb])
        S.ts(oml[:], lb[:], -1.0, 1.0, ALU.mult, ALU.add, [lb], [oml])
        nwbc = S.sb([128, 128], F32, "nwbc")
        S.dma("sp", nwbc[:], A["hg_norm_w"][0].partition_broadcast(128), writes=[nwbc])
        creset = S.sb([128, 128], F32, "creset")
        S.dma("sp", creset[:], A["chunk_reset"], writes=[creset])
        tri = [S.sb([128, 128], BF16, "tri") for _ in range(2)]
        S.dma("pool", tri[0][:], A["tri_fwd"], writes=[tri[0]])
        S.dma("pool", tri[1][:], A["tri_bwd"], writes=[tri[1]])
        eps1 = self.epsb
        W5 = S.sb([128, NCH, 5, 128], BF16, "W5")
        qT = S.sb([128, TL], BF16, "qT")
        logf = S.sb([128, 2304], F32, "logf")
        kf = S.sb([128, 2304], BF16, "kf")
        vtm = S.sb([128, 18, 128], BF16, "vtm")
        gtm = S.sb([128, 16, 128], BF16, "gtm")
        of = S.sb([128, 16, 128], F32, "of")
        sgt = S.sb([128, 512], F32, "sgt")
        Sf = S.sb([128, 128], F32, "Sf")
        Sb = S.sb([128, 128], BF16, "Sb")
        K2 = S.sb([128, 2, 128], BF16, "K2")
        Q2 = S.sb([128, 2, 128], BF16, "Q2")
        S.memset(K2[:], 0.0, [K2])
        S.memset(Q2[:], 0.0, [Q2])
        T = {nm: S.sb([128, 128], F32, nm) for nm in ("Gi", "G", "df", "dl", "Eq", "Ek", "Ed", "EG", "at", "os", "nwg", "ysq")}
        qg = S.sb([128, 128], BF16, "qg")
        kdT = S.sb([128, 128], BF16, "kdT")
        kdtm = S.sb([128, 128], BF16, "kdtm")
        attnT = S.sb([128, 128], BF16, "attnT")
        yb = S.sb([128, 128], BF16, "yb")
        st = S.sb([128, 4], F32, "hgst")

        def strided_halves(buf):
            return [buf[:, 0, 0:64], buf[:, 1, 64:128]]

        for h in range(8):
            for k in range(5):
                S.dma("pool", W5[:, :, k, :], W[:, :, k * D + h * 128:k * D + (h + 1) * 128], writes=[W5])
            for (grp, o, n) in self.tok_groups(False):
                ps = S.psum()
                for c in range(NCH):
                    S.mm(ps[:, 0:n], W5[:, c, 0, :], H[c][:, o:o + n], c == 0, c == NCH - 1, [W5, H[c]], [ps])
                S.act(qT[:, o:o + n], ps[:, 0:n], AF.Silu, [ps], [qT])
            for t in range(18):
                ps = S.psum()
                for c in range(NCH):
                    S.mm(ps[:, 0:128], H[c][:, t * 128:(t + 1) * 128], W5[:, c, 3, :], c == 0, c == NCH - 1, [H[c], W5], [ps])
                S.cp(vtm[:, t, :], ps[:, 0:128], [ps], [vtm], e=("act" if t % 2 else "dve"))
                if t < 16:
                    ps2 = S.psum()
                    for c in range(NCH):
                        S.mm(ps2[:, 0:128], H[c][:, t * 128:(t + 1) * 128], W5[:, c, 4, :], c == 0, c == NCH - 1,
                             [H[c], W5], [ps2])
                    S.act(gtm[:, t, :], ps2[:, 0:128], AF.Silu, [ps2], [gtm])
            for d in range(2):
                for (grp, o, n) in self.tok_groups(True):
                    ps = S.psum()
                    for c in range(NCH):
                        S.mm(ps[:, 0:n], W5[:, c, 1 + d, :], H[c][:, o:o + n], c == 0, c == NCH - 1, [W5, H[c]], [ps])
                    S.act(sgt[:, 0:n], ps[:, 0:n], AF.Sigmoid, [ps], [sgt])
                    S.ts(sgt[:, 0:n], sgt[:, 0:n], oml[:, h:h + 1], lb[:, h:h + 1], ALU.mult, ALU.add, [sgt, oml, lb], [sgt])
                    S.act(logf[:, o:o + n], sgt[:, 0:n], AF.Ln, [sgt], [logf])
                    S.ts(kf[:, o:o + n], sgt[:, 0:n], -1.0, 1.0, ALU.mult, ALU.add, [sgt], [kf])
                S.memset(Sf[:], 0.0, [Sf])
                S.memset(Sb[:], 0.0, [Sb])
                order = [16, 17] + list(range(16)) if d == 0 else [17, 16] + list(range(15, -1, -1))
                for tile in order:
                    o0 = tile * 128
                    lf = logf[:, o0:o0 + 128]
                    kk = kf[:, o0:o0 + 128]
                    S.scan(T["Gi"][:], creset[:], lf, 0.0, ALU.mult, ALU.add, [creset, logf], [T["Gi"]])
                    if d == 0:
                        G = T["Gi"]
                    else:
                        G = T["G"]
                        S.tt(G[:], lf, T["Gi"][:], ALU.subtract, [logf, T["Gi"]], [G])
                        for ci in range(2):
                            cs = slice(64 * ci, 64 * ci + 64)
                            S.ts(G[:, cs], G[:, cs], T["Gi"][:, 64 * ci + 63:64 * ci + 64], None, ALU.add, None,
                                 [G, T["Gi"]], [G])
                    for ci in range(2):
                        cs = slice(64 * ci, 64 * ci + 64)
                        mid = 64 * ci + 32
                        last = 64 * ci + (63 if d == 0 else 0)
                        S.ts(T["df"][:, cs], G[:, cs], G[:, mid:mid + 1], None, ALU.subtract, None, [G], [T["df"]])
                        S.ts(T["dl"][:, cs], G[:, cs], G[:, last:last + 1], None, ALU.subtract, None, [G], [T["dl"]])
                    S.act(T["Eq"][:], T["df"][:], AF.Exp, [T["df"]], [T["Eq"]])
                    S.act(T["Ek"][:], T["df"][:], AF.Exp, [T["df"]], [T["Ek"]], scale=-1.0)
                    S.act(T["Ed"][:], T["dl"][:], AF.Exp, [T["dl"]], [T["Ed"]], scale=-1.0)
                    S.act(T["EG"][:], G[:], AF.Exp, [G], [T["EG"]])
                    islat = tile < 16
                    if islat:
                        qq = qT[:, o0:o0 + 128]
                        S.stt(qg[:], T["Eq"][:], 1e30, qq, ALU.min, ALU.mult, [T["Eq"], qT], [qg])
                        for ci, dst in enumerate(strided_halves(Q2)):
                            cs = slice(64 * ci, 64 * ci + 64)
                            S.tt(dst, T["EG"][:, cs], qT[:, o0 + 64 * ci:o0 + 64 * ci + 64], ALU.mult, [T["EG"], qT], [Q2])
                        for ci, dst in enumerate(strided_halves(K2)):
                            cs = slice(64 * ci, 64 * ci + 64)
                            S.stt(dst, T["Ek"][:, cs], 1e30, kk[:, cs], ALU.min, ALU.mult, [T["Ek"], kf], [K2])
                    S.tt(kdT[:], T["Ed"][:], kk, ALU.mult, [T["Ed"], kf], [kdT])
                    pT = S.psum()
                    pTb = pT[:].bitcast(BF16)
                    S.tr(pTb[:, 0:128], kdT[:], self.ident_b[:], [kdT, self.ident_b], [pT])
                    S.cp(kdtm[:], pTb[:, 0:128], [pT], [kdtm], e="act")
                    if islat:
                        psA = S.psum()
                        for ci in range(2):
                            cs = slice(64 * ci, 64 * ci + 64)
                            S.mm(psA[:, cs], K2[:, ci, :], qg[:, cs], True, True, [K2, qg], [psA])
                        S.ts(T["at"][:], psA[:, 0:128], 1e30, -1e30, ALU.min, ALU.max, [psA], [T["at"]])
                        S.tt(attnT[:], T["at"][:], tri[d][:], ALU.mult, [T["at"], tri[d]], [attnT])
                        po = S.psum()
                        S.mm(po[:, 0:128], attnT[:], vtm[:, tile, :], True, False, [attnT, vtm], [po])
                    corder = (0, 1) if d == 0 else (1, 0)
                    for n_, ci in enumerate(corder):
                        pr = slice(64 * ci, 64 * ci + 64)
                        last = 64 * ci + (63 if d == 0 else 0)
                        if islat:
                            S.mm(po[:, 0:128], Q2[:, ci, :], Sb[:], False, n_ == 1, [Q2, Sb], [po])
                        psS = S.psum()
                        S.mm(psS[:, 0:128], kdtm[pr, :], vtm[pr, tile, :], True, True, [kdtm, vtm], [psS])
                        S.stt(Sf[:], Sf[:], T["EG"][:, last:last + 1], psS[:, 0:128], ALU.mult, ALU.add,
                              [Sf, T["EG"], psS], [Sf])
                        S.cp(Sb[:], Sf[:], [Sf], [Sb], e="act")
                    if not islat:
                        continue
                    if d == 0:
                        S.cp(of[:, tile, :], po[:, 0:128], [po], [of])
                        continue
                    S.tt(T["os"][:], po[:, 0:128], of[:, tile, :], ALU.add, [po, of], [T["os"]])
                    S.act(T["ysq"][:], T["os"][:], AF.Square, [T["os"]], [T["ysq"], st], accum_out=st[:, 0:1])
                    S.act(st[:, 1:2], st[:, 0:1], AF.Sqrt, [st, eps1], [st], bias=eps1[:, 0:1], scale=1.0 / 128)
                    S.recip(st[:, 2:3], st[:, 1:2], [st], [st])
                    S.tt(T["nwg"][:], nwbc[:], gtm[:, tile, :], ALU.mult, [nwbc, gtm], [T["nwg"]], e="pool")
                    S.stt(yb[:], T["os"][:], st[:, 2:3], T["nwg"][:], ALU.mult, ALU.mult, [T["os"], st, T["nwg"]], [yb])
                    pY = S.psum()
                    pYb = pY[:].bitcast(BF16)
                    S.tr(pYb[:, 0:128], yb[:], self.ident_b[:], [yb, self.ident_b], [pY])
                    S.cp(ZT[h][:, o0:o0 + 128], pYb[:, 0:128], [pY], [ZT[h]], e="act")
        S.pop()
        self.out_proj(ZT, A["hg_w_out"], False)
        S.pop()

    def mixer_swa(self, keep_ctx):
        S = self.S
        A = self.A
        W = A["sw_w_in"].rearrange("(c p) n -> p c n", p=128)
        S.push()
        Q = [S.sb([128, 2304], BF16, "Q") for _ in range(8)]
        KD = [S.sb([128, 2304], BF16, "KD") for _ in range(4)]
        V1 = S.sb([128, 18, 4, 65], BF16, "V1")
        S.push()
        H = self.hnorm()
        cosb = S.sb([128, TL], BF16, "cosb")
        sinb = S.sb([128, TL], BF16, "sinb")
        S.dma("pool", cosb[:], A["rope_cos"], writes=[cosb])
        S.dma("pool", sinb[:], A["rope_sin"], writes=[sinb])
        wj = [S.sb([128, NCH, 128], BF16, "wj") for _ in range(2)]
        wjs = [S.sb([128, NCH, 128], BF16, "wjs") for _ in range(2)]
        t1 = S.sb([128, 512], F32, "t1")
        t2 = S.sb([128, 512], F32, "t2")
        S.memset(V1[:, :, :, 64:65], 1.0, [V1])

        def swapped(dst, src):
            d5 = dst[:].rearrange("p c (h two i) -> p c h two i", two=2, i=32)
            s5 = src[:].rearrange("p c (h two i) -> p c h two i", two=2, i=32)
            for c in range(NCH):
                S.cp(d5[:, c, :, 0, :], s5[:, c, :, 1, :], [src], [dst], e="pool")
                S.cp(d5[:, c, :, 1, :], s5[:, c, :, 0, :], [src], [dst], e="pool")

        def project_roped(dst, w, ws):
            for (grp, o, n) in self.tok_groups(True):
                psq = S.psum()
                for c in range(NCH):
                    S.mm(psq[:, 0:n], w[:, c, :], H[c][:, o:o + n], c == 0, c == NCH - 1, [w, H[c]], [psq])
                if grp[0] == "c":
                    S.cp(dst[:, o:o + n], psq[:, 0:n], [psq], [dst], e="act")
                    continue
                pss = S.psum()
                for c in range(NCH):
                    S.mm(pss[:, 0:n], ws[:, c, :], H[c][:, o:o + n], c == 0, c == NCH - 1, [ws, H[c]], [pss])
                S.tt(t1[:, 0:n], psq[:, 0:n], cosb[:, o:o + n], ALU.mult, [psq, cosb], [t1])
                S.tt(t2[:, 0:n], pss[:, 0:n], sinb[:, o:o + n], ALU.mult, [pss, sinb], [t2])
                S.tt(dst[:, o:o + n], t1[:, 0:n], t2[:, 0:n], ALU.add, [t1, t2], [dst], e="pool")

        k = 0
        stage = self.cfg.get("swa_stage", 9)
        for j in range(8 if stage >= 1 else 0):
            w, ws = wj[k % 2], wjs[k % 2]
            k += 1
            S.dma("pool", w[:], W[:, :, j * 128:(j + 1) * 128], writes=[w])
            swapped(ws, w)
            project_roped(Q[j], w, ws)
        for g in range(4 if stage >= 2 else 0):
            w, ws = wj[k % 2], wjs[k % 2]
            k += 1
            for half in range(2):
                S.dma("pool", w[:, :, half * 64:(half + 1) * 64], W[:, :, 1024 + g * 64:1024 + (g + 1) * 64], writes=[w])
            swapped(ws, w)
            project_roped(KD[g], w, ws)
        wv = S.sb([128, NCH, 256], BF16, "wv")
        S.dma("pool", wv[:], W[:, :, 1280:1536], writes=[wv])
        for t in range(18 if stage >= 3 else 0):
            ps = S.psum()
            for c in range(NCH):
                S.mm(ps[:, 0:256], H[c][:, t * 128:(t + 1) * 128], wv[:, c, :], c == 0, c == NCH - 1, [H[c], wv], [ps])
            S.cp(V1[:, t, :, 0:64], ps[:, 0:256].rearrange("p (g d) -> p g d", d=64), [ps], [V1],
                 e=("act" if t % 2 else "dve"))
        S.pop()

        S.push()
        OT = [S.sb([128, 2304], BF16, "OT") for _ in range(8)]
        sinkE = S.sb([128, 16], F32, "sinkE")
        S.dma("sp", sinkE[:], A["sw_sink"][0].partition_broadcast(128), writes=[sinkE])
        S.act(sinkE[:], sinkE[:], AF.Exp, [sinkE], [sinkE])
        mP = S.sb([128, 256], BF16, "mP")
        mN = S.sb([128, 256], BF16, "mN")
        S.dma("pool", mP[:], A["mask_prev"], writes=[mP])
        S.dma("pool", mN[:], A["mask_next"], writes=[mN])
        PT = [S.sb([128, 2, 8, 128], BF16, "PT") for _ in range(2)]
        ob = [S.sb([128, 128], BF16, "ob") for _ in range(2)]
        den = S.sb([128, 4], F32, "den")
        nblk = 18 if keep_ctx else 16
        it = 0
        for j in range(8 if stage >= 4 else 0):
            g = j // 2
            for blk in range(nblk):
                qo = blk * 128
                if blk < 16:
                    tiles = [(16, None), (17, None)]
                    if blk > 0:
                        tiles.append((blk - 1, mP))
                    tiles.append((blk, None))
                    if blk < 15:
                        tiles.append((blk + 1, mN))
                else:
                    tiles = [(16, None), (17, None)]
                nt = len(tiles)
                pt = PT[it % 2]
                o2 = ob[it % 2]
                it += 1
                nb = (nt + 3) // 4
                pss = [[S.psum() for _ in range(nb)] for hh in range(2)]
                for hh in range(2):
                    pr = slice(hh * 64, (hh + 1) * 64)
                    for k2, (kt, msk) in enumerate(tiles):
                        ps = pss[hh][k2 // 4]
                        co = (k2 % 4) * 128
                        S.mm(ps[:, co:co + 128], KD[g][pr, kt * 128:(kt + 1) * 128], Q[j][pr, qo:qo + 128], True, True,
                             [KD[g], Q[j]], [ps])
                for hh in range(2):
                    for b in range(nb):
                        w_ = min(4, nt - 4 * b) * 128
                        S.act(pt[:, hh, 4 * b:4 * b + 4, :].rearrange("p a b -> p (a b)")[:, 0:w_], pss[hh][b][:, 0:w_],
                              AF.Exp, [pss[hh][b]], [pt], scale=0.125)
                for k2, (kt, msk) in enumerate(tiles):
                    if msk is not None:
                        for hh in range(2):
                            S.tt(pt[:, hh, k2, :], pt[:, hh, k2, :], msk[:, 0:128], ALU.mult, [pt, msk], [pt], e="pool")
                po = S.psum()
                for hh in range(2):
                    for k2, (kt, msk) in enumerate(tiles):
                        S.mm(po[:, hh * 65:(hh + 1) * 65], pt[:, hh, k2, :], V1[:, kt, g, :],
                             k2 == 0, k2 == nt - 1, [pt, V1], [po])
                for hh in range(2):
                    h = 2 * j + hh
                    S.tt(den[:, hh:hh + 1], po[:, hh * 65 + 64:hh * 65 + 65], sinkE[:, h:h + 1], ALU.add,
                         [po, sinkE], [den])
                S.recip(den[:, 2:4], den[:, 0:2], [den], [den])
                for hh in range(2):
                    S.ts(o2[:, hh * 64:(hh + 1) * 64], po[:, hh * 65:hh * 65 + 64], den[:, 2 + hh:3 + hh], None,
                         ALU.mult, None, [po, den], [o2])
                pT = S.psum()
                pTb = pT[:].bitcast(BF16)
                S.tr(pTb[:, 0:128], o2[:], self.ident_b[:], [o2, self.ident_b], [pT])
                S.cp(OT[j][:, qo:qo + 128], pTb[:, 0:128], [pT], [OT[j]], e="act")
        if stage >= 5:
            self.out_proj(OT, A["sw_w_out"], keep_ctx)
        S.pop()
        S.pop()


def make_in_maps(inputs, consts, n_cores=8):
    f = lambda a: np.ascontiguousarray(np.asarray(a, dtype=np.float32))
    shared = {
        "ada_w": f(inputs["ada_w"]).reshape(DEPTH * D, 6 * D),
        "ada_b": f(inputs["ada_b"]),
        "norm1_w": f(inputs["norm1_w"]),
        "norm2_w": f(inputs["norm2_w"]),
        "final_norm_w": f(inputs["final_norm_w"]).reshape(1, D),
        "moe_router": f(inputs["moe_router"]).reshape(DEPTH * D, NE),
        "moe_w_gate": f(inputs["moe_w_gate"]).reshape(DEPTH * NE * D, FF),
        "moe_w_up": f(inputs["moe_w_up"]).reshape(DEPTH * NE * D, FF),
        "moe_w_down": f(inputs["moe_w_down"]).reshape(DEPTH * NE * FF, D),
        "hy_w_in": f(inputs["hy_w_in"]), "hy_b_in": f(inputs["hy_b_in"]).reshape(1, 3 * D),
        "hy_short_w": f(inputs["hy_short_w"]), "hy_short_b": f(inputs["hy_short_b"]).reshape(1, 3 * D),
        "hy_ffn_w1": f(inputs["hy_ffn_w1"]), "hy_ffn_b1": f(inputs["hy_ffn_b1"]).reshape(1, 64),
        "hy_ffn_w2": f(inputs["hy_ffn_w2"]), "hy_ffn_b2": f(inputs["hy_ffn_b2"]).reshape(1, 64),
        "hy_ffn_w3": f(inputs["hy_ffn_w3"]), "hy_sin_freq": f(inputs["hy_sin_freq"]),
        "hy_filter_bias": f(inputs["hy_filter_bias"]), "hy_w_out": f(inputs["hy_w_out"]),
        "hy_b_out": f(inputs["hy_b_out"]).reshape(1, D),
        "gd_w_in": f(inputs["gd_w_in"]),
        "gd_conv_w": f(inputs["gd_conv_w"]),
        "gd_a_log": f(inputs["gd_a_log"]).reshape(1, 16),
        "gd_dt_bias": f(inputs["gd_dt_bias"]).reshape(1, 16),
        "gd_norm_w": f(inputs["gd_norm_w"]).reshape(1, 128),
        "gd_w_out": f(inputs["gd_w_out"]),
        "hg_w_in": f(inputs["hg_w_in"]),
        "hg_lb": f(inputs["hg_lb"]),
        "hg_norm_w": f(inputs["hg_norm_w"]).reshape(1, 128),
        "hg_w_out": f(inputs["hg_w_out"]),
        "sw_w_in": f(inputs["sw_w_in"]),
        "sw_sink": f(inputs["sw_sink"]).reshape(1, 16),
        "sw_w_out": f(inputs["sw_w_out"]),
    }
    for k, v in consts.items():
        shared["k_" + k] = v
    maps = []
    for b in range(n_cores):
        mp = dict(shared)
        mp["x"] = f(inputs["x"][b])
        mp["ctx"] = f(inputs["ctx"][b])
        mp["cc"] = np.stack([f(inputs["c"][b]), f(inputs["c_ctx"])], 0)
        maps.append(mp)
    return maps


def kernel(**inputs):
    p = Prog({})
    nc = p.build()
    maps = make_in_maps(inputs, p.consts, 8)
    res = run_bass_kernel_spmd(nc, maps, core_ids=list(range(8)))
    return np.stack([r["out"] for r in res.results], 0).astype(np.float32)
```

```python
import numpy as np
from contextlib import ExitStack
import concourse.bass as bass
import concourse.mybir as mybir
from concourse.bass_utils import run_bass_kernel_spmd

F32 = mybir.dt.float32
BF16 = mybir.dt.bfloat16
I32 = mybir.dt.int32
AF = mybir.ActivationFunctionType
ALU = mybir.AluOpType
AX = mybir.AxisListType

D = 1024
NCH = 8
TL = 2048
TC = 256
DEPTH = 4
NE = 16
FF = 1024
EPS = 1e-6
NDS = 12


class Buf:
    __slots__ = ("t", "w", "r", "name", "psum")

    def __init__(self, t, name=""):
        self.t = t
        self.w = None
        self.r = {}
        self.name = name
        self.psum = False

    def __getitem__(self, idx):
        return self.t[idx]


class Sched:
    def __init__(self, nc):
        self.nc = nc
        self.eng = {"pe": nc.tensor, "act": nc.scalar, "dve": nc.vector, "pool": nc.gpsimd, "sp": nc.sync}
        self.semstack = ExitStack()
        self.csem = {e: self.semstack.enter_context(nc.semaphore("s_" + e)) for e in ("pe", "act", "dve", "pool")}
        self.prog = {e: [] for e in self.eng}
        self.ccnt = {e: 0 for e in self.csem}
        self.seen = {e: {} for e in self.eng}
        self.dq = {}
        for q in ("sp", "pool"):
            self.dq[q] = {"sems": [self.semstack.enter_context(nc.semaphore("d_%s%d" % (q, i))) for i in range(NDS)],
                          "vals": [0] * NDS, "rr": 0}
        self.nbuf = 0
        import threading
        self._coop = None
        self._tls = threading.local()
        self.stacks = []
        self.arena = None
        self.ps = [self.psum_raw("psb%d" % i) for i in range(8)]
        self.ps_rr = 0
        self.n_inst = 0

    def sb(self, shape, dt=F32, name=None):
        self.nbuf += 1
        nm = "%s_%d" % (name or "b", self.nbuf)
        if self.arena is None:
            self.arena_words = 212800 // 4
            self.arena = self.nc.alloc_sbuf_tensor("arena", [128, self.arena_words], F32)
            self.top = 0
        esz = 2 if dt == BF16 else 4
        n = 1
        for d_ in shape[1:]:
            n *= d_
        words = (n * esz + 3) // 4
        words = (words + 7) // 8 * 8
        assert self.top + words <= self.arena_words, "SBUF arena overflow allocating %s %s (top=%d)" % (nm, shape, self.top * 4)
        ap = self.arena[0:shape[0], self.top:self.top + words]
        self.top += words
        if dt != F32:
            ap = ap.bitcast(dt)
        ap = ap[:, 0:n]
        if len(shape) == 3:
            ap = ap.rearrange("p (a b) -> p a b", b=shape[2])
        elif len(shape) == 4:
            ap = ap.rearrange("p (a b c) -> p a b c", b=shape[2], c=shape[3])
        return Buf(ap, nm)

    def push(self):
        self.stacks.append(self.top)

    def pop(self):
        self.barrier()
        self.top = self.stacks.pop()

    def barrier(self):
        evs = [(self.csem[e], self.ccnt[e], e) for e in self.csem if self.ccnt[e] > 0]
        for q in self.dq.values():
            for sem, v in zip(q["sems"], q["vals"]):
                if v > 0:
                    evs.append((sem, v, "dma"))
        for e in self.eng:
            for ev in evs:
                if e == "pe" and ev[2] == "pe":
                    continue
                self._wait(e, ev)

    def psum_raw(self, name):
        b = Buf(self.nc.alloc_psum_tensor(name, [128, 512], F32), name)
        b.psum = True
        return b

    def psum(self):
        co = self._coop
        if co is not None:
            i = self._tls.idx
            b = self.ps[4 * i + co["rr"][i]]
            co["rr"][i] = (co["rr"][i] + 1) % 4
            return b
        b = self.ps[self.ps_rr]
        self.ps_rr = (self.ps_rr + 1) % 8
        return b

    def run_interleaved(self, fns):
        import threading
        assert len(fns) == 2
        cv = threading.Condition()
        co = {"turn": 0, "alive": [True, True], "rr": [0, 0], "cv": cv, "err": []}
        self._coop = co

        def body(i, fn):
            self._tls.idx = i
            with cv:
                while co["turn"] != i:
                    cv.wait()
            try:
                fn()
            except BaseException as ex:
                co["err"].append(ex)
            with cv:
                co["alive"][i] = False
                co["turn"] = 1 - i
                cv.notify_all()

        th = [threading.Thread(target=body, args=(i, f)) for i, f in enumerate(fns)]
        for t in th:
            t.start()
        for t in th:
            t.join()
        self._coop = None
        if co["err"]:
            raise co["err"][0]

    def _yield(self):
        co = self._coop
        if co is None:
            return
        i = self._tls.idx
        if not co["alive"][1 - i]:
            return
        cv = co["cv"]
        with cv:
            co["turn"] = 1 - i
            cv.notify_all()
            while co["turn"] != i and co["alive"][1 - i]:
                cv.wait()

    def _wait(self, e, ev):
        sem, val, src = ev
        k = id(sem)
        if self.seen[e].get(k, 0) < val:
            self.prog[e].append(("w", sem, val))
            self.seen[e][k] = val

    def _deps(self, e, reads, writes):
        for b in reads:
            if b.w is not None and not (e == "pe" and b.w[2] == "pe"):
                self._wait(e, b.w)
            if b.psum:
                for ev in b.r.values():
                    if ev[2] != e:
                        self._wait(e, ev)
        for b in writes:
            if b.w is not None and not (e == "pe" and b.w[2] == "pe"):
                self._wait(e, b.w)
            for ev in b.r.values():
                if not (e == "pe" and ev[2] == "pe"):
                    self._wait(e, ev)

    def _mark(self, ev, reads, writes):
        k = id(ev[0])
        for b in reads:
            b.r[k] = ev
        for b in writes:
            b.w = ev
            b.r = {}

    def op(self, e, fn, reads=(), writes=()):
        self._deps(e, reads, writes)
        self.ccnt[e] += 1
        self.prog[e].append(("i", fn, self.csem[e], 1))
        self._mark((self.csem[e], self.ccnt[e], e), reads, writes)
        self.n_inst += 1
        self._yield()

    def dma(self, q, out, in_, reads=(), writes=(), **kw):
        d = self.dq[q]
        i = d["rr"]
        d["rr"] = (i + 1) % NDS
        sem = d["sems"][i]
        if d["vals"][i] > 0:
            self._wait(q, (sem, d["vals"][i], "dma"))
        self._deps(q, reads, writes)
        self.prog[q].append(("i", lambda g: g.dma_start(out=out, in_=in_, allow_slow_non_contiguous=True, **kw), sem, 16))
        d["vals"][i] += 16
        ev = (sem, d["vals"][i], "dma")
        self._mark(ev, reads, writes)
        self.n_inst += 1
        return ev

    def emit(self):
        prog = self.prog

        def replay(name, eng):
            for it in prog[name]:
                if it[0] == "w":
                    eng.wait_ge(it[1], it[2])
                else:
                    it[1](eng).then_inc(it[2], it[3])

        with self.nc.Block() as block:
            @block.sync
            def _(eng):
                replay("sp", eng)

            @block.tensor
            def _(eng):
                replay("pe", eng)

            @block.scalar
            def _(eng):
                replay("act", eng)

            @block.vector
            def _(eng):
                replay("dve", eng)

            @block.gpsimd
            def _(eng):
                replay("pool", eng)

    def mm(self, out, lhsT, rhs, start, stop, reads, writes, **kw):
        self.op("pe", lambda e: e.matmul(out, lhsT, rhs, start=start, stop=stop, **kw), reads, writes)

    def tr(self, out, in_, ident, reads, writes):
        self.op("pe", lambda e: e.transpose(out, in_, ident), reads, writes)

    def trf(self, out, in_, ident, reads, writes):
        self.op("pe", lambda e: e.matmul(out, in_, ident, start=True, stop=True), reads, writes)

    def act(self, out, in_, func, reads, writes, bias=None, scale=None, accum_out=None):
        kw = {}
        if bias is not None:
            kw["bias"] = bias
        if scale is not None:
            kw["scale"] = scale
        if accum_out is not None:
            kw["accum_out"] = accum_out
        self.op("act", lambda e: e.activation(out, in_, func, **kw), reads, writes)

    def ts(self, out, in0, s1, s2, op0, op1, reads, writes, e="dve", accum_out=None):
        if op1 is None:
            self.op(e, lambda g: g.tensor_scalar(out, in0, s1, None, op0, accum_out=accum_out) if accum_out is not None
                    else g.tensor_scalar(out, in0, s1, None, op0), reads, writes)
        else:
            self.op(e, lambda g: g.tensor_scalar(out, in0, s1, s2, op0, op1, accum_out=accum_out) if accum_out is not None
                    else g.tensor_scalar(out, in0, s1, s2, op0, op1), reads, writes)

    def tt(self, out, in0, in1, op, reads, writes, e="dve"):
        self.op(e, lambda g: g.tensor_tensor(out, in0, in1, op), reads, writes)

    def stt(self, out, in0, scalar, in1, op0, op1, reads, writes):
        self.op("dve", lambda g: g.scalar_tensor_tensor(out, in0, scalar, in1, op0, op1), reads, writes)

    def recip(self, out, in_, reads, writes):
        self.op("dve", lambda g: g.reciprocal(out, in_), reads, writes)

    def red(self, out, in_, op, reads, writes, negate=False):
        self.op("dve", lambda g: g.tensor_reduce(out, in_, AX.X, op, negate=negate), reads, writes)

    def max8(self, out, in_, reads, writes):
        self.op("dve", lambda g: g.max(out, in_), reads, writes)

    def mrep(self, out, in_to_replace, in_values, imm, reads, writes):
        self.op("dve", lambda g: g.match_replace(out, in_to_replace, in_values, imm), reads, writes)

    def scan(self, out, d0, d1, init, op0, op1, reads, writes):
        self.op("dve", lambda g: g.tensor_tensor_scan(out, d0, d1, init, op0, op1), reads, writes)

    def memset(self, out, val, writes, e="dve"):
        self.op(e, lambda g: g.memset(out, val), [], writes)

    def cp(self, out, in_, reads, writes, e="dve"):
        if e == "act":
            self.op("act", lambda g: g.activation(out, in_, AF.Identity), reads, writes)
        else:
            self.op(e, lambda g: g.tensor_copy(out, in_), reads, writes)


def host_consts():
    c = {}
    c["ident_f"] = np.eye(128, dtype=np.float32)
    c["ones_f"] = np.ones((128, 128), np.float32)
    io = np.zeros((128, 288), np.float32)
    io[:] = np.arange(288, dtype=np.float32)[None, :]
    c["iota_row"] = io
    ip = np.zeros((128, 4), np.float32)
    for k in range(4):
        ip[:, k] = np.arange(128) + 128 * k
    c["iota_part"] = ip
    sel = np.zeros((16, 16, 128), np.float32)
    for e in range(16):
        sel[e, e, :] = 1.0
    c["sel"] = sel.reshape(16, 16 * 128)
    t = np.arange(TL)
    row = (t // 64).astype(np.float32)
    col = (t % 64).astype(np.float32)
    inv = (10000.0 ** (-np.arange(16, dtype=np.float32) / 16)).astype(np.float32)
    ang = np.concatenate([row[:, None] * inv, col[:, None] * inv], -1).astype(np.float32)
    cosf = np.zeros((128, TL), np.float32)
    sinf = np.zeros((128, TL), np.float32)
    for p in range(128):
        i = p % 64
        cosf[p] = np.cos(ang[:, i % 32])
        sinf[p] = np.sin(ang[:, i % 32]) * (-1.0 if i < 32 else 1.0)
    c["rope_cos"] = cosf
    c["rope_sin"] = sinf
    jj = np.arange(128)[:, None]
    ii = np.arange(128)[None, :]
    mp = (jj >= ii).astype(np.float32)
    mn = (jj <= ii).astype(np.float32)
    cm = np.ones((128, 128), np.float32)
    cm[:, 0] = 0.0
    cm[:, 64] = 0.0
    c["chunk_reset"] = cm
    blk = (jj // 64 == ii // 64)
    c["tri_fwd"] = (blk & (jj <= ii)).astype(np.float32)
    c["tri_bwd"] = (blk & (jj >= ii)).astype(np.float32)
    import ml_dtypes
    for nm, L in (("l", TL), ("c", TC)):
        N = 2 * L
        T_ = L // 128
        tt_ = np.linspace(0.0, 1.0, L, dtype=np.float32)
        w_ = (2.0 * np.pi * np.arange(L, dtype=np.float32) / L).astype(np.float32)
        f_ = np.linspace(1e-4, 15.0, 16, dtype=np.float32)[None, :]
        z_ = np.concatenate([tt_[:, None], np.cos(f_ * w_[:, None]), -np.sin(f_ * w_[:, None])], -1).astype(np.float32)
        c["hy_zT_" + nm] = np.ascontiguousarray(z_.T)
        c["hy_tcol_" + nm] = np.ascontiguousarray(tt_.reshape(T_, 128).T)
        tpos = np.arange(L, dtype=np.float64)
        fr = np.arange(L, dtype=np.float64) + 0.5
        th = 2.0 * np.pi * np.outer(tpos, fr) / N
        for tn, tab in (("C", np.cos(th)), ("S", np.sin(th))):
            fw = tab.reshape(T_, 128, T_, 128).transpose(2, 1, 0, 3)
            c["hy_%sf_%s" % (tn, nm)] = np.ascontiguousarray(fw).reshape(T_ * 128, T_ * 128).astype(ml_dtypes.bfloat16)
            c["hy_%st_%s" % (tn, nm)] = np.ascontiguousarray(tab.T).astype(ml_dtypes.bfloat16)
    maxd = np.log(1e-2) / 0.3
    mind = np.log(1e-2) / 1.5
    c["hy_delta"] = np.abs(np.linspace(mind, maxd, D, dtype=np.float32)).reshape(1, D).astype(np.float32)
    eye = np.eye(128, dtype=np.float32)
    c["tri_fwd_s"] = c["tri_fwd"] - eye
    c["tri_bwd_s"] = c["tri_bwd"] - eye
    c["mask_prev"] = np.concatenate([mp, mp], 1)
    c["mask_next"] = np.concatenate([mn, mn], 1)
    return c


class Prog:
    def __init__(self, cfg):
        self.cfg = cfg
        self.nc = bass.Bass("TRN2", target_bir_lowering=False)
        self.S = Sched(self.nc)
        self.din = {}
        self.consts = host_consts()

    def dram_in(self, name, shape, dt=F32):
        t = self.nc.dram_tensor(name, list(shape), dt, kind="ExternalInput")
        self.din[name] = t
        return t.ap()

    def declare(self):
        nc = self.nc
        A = {}
        A["x"] = self.dram_in("x", [TL, D])
        A["ctx"] = self.dram_in("ctx", [TC, D])
        A["cc"] = self.dram_in("cc", [2, D])
        A["ada_w"] = self.dram_in("ada_w", [DEPTH * D, 6 * D])
        A["ada_b"] = self.dram_in("ada_b", [DEPTH, 6 * D])
        A["norm1_w"] = self.dram_in("norm1_w", [DEPTH, D])
        A["norm2_w"] = self.dram_in("norm2_w", [DEPTH, D])
        A["final_norm_w"] = self.dram_in("final_norm_w", [1, D])
        A["moe_router"] = self.dram_in("moe_router", [DEPTH * D, NE])
        A["moe_w_gate"] = self.dram_in("moe_w_gate", [DEPTH * NE * D, FF])
        A["moe_w_up"] = self.dram_in("moe_w_up", [DEPTH * NE * D, FF])
        A["moe_w_down"] = self.dram_in("moe_w_down", [DEPTH * NE * FF, D])
        A["gd_w_in"] = self.dram_in("gd_w_in", [D, 4128])
        A["gd_conv_w"] = self.dram_in("gd_conv_w", [3, 3072])
        A["gd_a_log"] = self.dram_in("gd_a_log", [1, 16])
        A["gd_dt_bias"] = self.dram_in("gd_dt_bias", [1, 16])
        A["gd_norm_w"] = self.dram_in("gd_norm_w", [1, 128])
        A["gd_w_out"] = self.dram_in("gd_w_out", [D, D])
        A["hg_w_in"] = self.dram_in("hg_w_in", [D, 5 * D])
        A["hg_lb"] = self.dram_in("hg_lb", [DEPTH, D])
        A["hg_norm_w"] = self.dram_in("hg_norm_w", [1, 128])
        A["hg_w_out"] = self.dram_in("hg_w_out", [D, D])
        A["sw_w_in"] = self.dram_in("sw_w_in", [D, 1536])
        A["sw_sink"] = self.dram_in("sw_sink", [1, 16])
        A["sw_w_out"] = self.dram_in("sw_w_out", [D, D])
        for nm, shp in (("hy_w_in", [D, 3 * D]), ("hy_b_in", [1, 3 * D]), ("hy_short_w", [3, 3 * D]),
                        ("hy_short_b", [1, 3 * D]), ("hy_ffn_w1", [33, 64]), ("hy_ffn_b1", [1, 64]),
                        ("hy_ffn_w2", [64, 64]), ("hy_ffn_b2", [1, 64]), ("hy_ffn_w3", [64, 4 * D]),
                        ("hy_sin_freq", [2, 64]), ("hy_filter_bias", [2, D]), ("hy_w_out", [D, D]),
                        ("hy_b_out", [1, D])):
            A[nm] = self.dram_in(nm, shp)
        for k, v in self.consts.items():
            A[k] = self.dram_in("k_" + k, list(v.shape), BF16 if v.dtype != np.float32 else F32)
        self.out = nc.dram_tensor("out", [TL, D], F32, kind="ExternalOutput").ap()
        if self.cfg.get("dump_ctx"):
            self.out_c = nc.dram_tensor("out_c", [TC, D], F32, kind="ExternalOutput").ap()
        self.A = A

    def setup(self):
        S = self.S
        A = self.A
        self.ident_f = S.sb([128, 128], F32, "identf")
        self.ident_b = S.sb([128, 128], BF16, "identb")
        self.ones_b = S.sb([128, 128], BF16, "onesb")
        self.iota_row = S.sb([128, 256], F32, "iotar")
        self.iota_part = S.sb([128, 4], F32, "iotap")
        self.sel = S.sb([16, 16 * 128], BF16, "sel")
        self.epsb = S.sb([128, 1], F32, "eps")
        self.hs_tok = S.sb([1, 8], F32, "hstok")
        self.RL = [[S.sb([128, 512], F32, "RL") for g in range(4)] for c in range(NCH)]
        self.RC = [S.sb([128, 256], F32, "RC") for c in range(NCH)]
        self.sc = S.sb([128, NCH, 2], F32, "sc")
        self.modT = S.sb([128, 48, 2], F32, "modT")
        self.a1 = [S.sb([128, NCH], F32, "a1") for _ in range(2)]
        self.a2 = [S.sb([128, NCH], F32, "a2") for _ in range(2)]
        S.dma("sp", self.ident_f[:], A["ident_f"], writes=[self.ident_f])
        S.dma("sp", self.iota_row[:], A["iota_row"][:, 0:256], writes=[self.iota_row])
        S.dma("sp", self.iota_part[:], A["iota_part"], writes=[self.iota_part])
        S.push()
        tmp = S.sb([128, 128], F32, "onesf")
        tsel = S.sb([16, 16 * 128], F32, "tsel")
        craw = S.sb([128, NCH, 2], F32, "craw")
        S.dma("sp", tsel[:], A["sel"], writes=[tsel])
        S.dma("sp", tmp[:], A["ones_f"], writes=[tmp])
        S.cp(self.sel[:], tsel[:], [tsel], [self.sel])
        S.cp(self.ones_b[:], tmp[:], [tmp], [self.ones_b])
        S.cp(self.ident_b[:], self.ident_f[:], [self.ident_f], [self.ident_b])
        S.memset(self.epsb[:], EPS, [self.epsb])
        with self.nc.allow_non_contiguous_dma("tiny"):
            for j in range(2):
                S.dma("sp", craw[:, :, j], A["cc"][j].rearrange("(c p) -> p c", p=128), writes=[craw])
        S.act(self.sc[:], craw[:], AF.Silu, [craw], [self.sc])
        S.pop()
        self.groups = [("l", g, g * 512, 512) for g in range(4)] + [("c", 0, 0, 256)]

    def dbg(self, name, ap, buf, shape):
        if not self.cfg.get("debug"):
            return
        d = self.nc.dram_tensor("dbg_" + name, list(shape), ap.dtype, kind="ExternalOutput").ap()
        ev = self.S.dma("sp", d, ap, reads=[buf])
        self.S._wait("sp", ev)

    def rbuf(self, grp, c):
        return self.RL[c][grp[1]] if grp[0] == "l" else self.RC[c]

    def load_stream(self):
        S = self.S
        A = self.A
        S.push()
        tin = [S.sb([128, D], F32, "tin") for _ in range(2)]
        k = 0
        for grp in self.groups:
            src = A["x"] if grp[0] == "l" else A["ctx"]
            for tt in range(grp[3] // 128):
                t0 = grp[2] + tt * 128
                tb = tin[k % 2]
                if k >= self.cfg.get("ls_tiles", 99):
                    continue
                k += 1
                S.dma("sp", tb[:], src[t0:t0 + 128, :], writes=[tb])
                for half in range(2):
                    ps = S.psum()
                    for j in range(4):
                        c = half * 4 + j
                        S.trf(ps[:, j * 128:(j + 1) * 128], tb[:, c * 128:(c + 1) * 128], self.ident_f[:],
                             [tb, self.ident_f], [ps])
                    for j in range(4):
                        c = half * 4 + j
                        rb = self.rbuf(grp, c)
                        S.cp(rb[:, tt * 128:(tt + 1) * 128], ps[:, j * 128:(j + 1) * 128], [ps], [rb],
                             e=("act" if j % 2 else "dve"))
        S.pop()

    def adaln(self, layer):
        S = self.S
        A = self.A
        S.push()
        modT = self.modT
        adab = S.sb([128, 48], F32, "adab")
        with self.nc.allow_non_contiguous_dma("tiny"):
            S.dma("sp", adab[:], A["ada_b"][layer].rearrange("(j p) -> p j", p=128), writes=[adab])
        wv = A["ada_w"][layer * D:(layer + 1) * D, :].rearrange("(c p) n -> p c n", p=128)
        wb = [S.sb([128, NCH, 512], F32, "adaw") for _ in range(2)]
        ps = S.psum()
        for piece in range(12):
            w = wb[piece % 2]
            S.dma("sp", w[:], wv[:, :, piece * 512:(piece + 1) * 512], writes=[w])
            for jj in range(4):
                j = piece * 4 + jj
                for c in range(NCH):
                    S.mm(ps[:, 2 * j:2 * j + 2], w[:, c, jj * 128:(jj + 1) * 128], self.sc[:, c, :],
                         c == 0, c == NCH - 1, [w, self.sc], [ps])
        for s in range(2):
            S.tt(modT[:, :, s], ps[:, 0:96].rearrange("p (j s) -> p j s", s=2)[:, :, s], adab[:], ALU.add,
                 [ps, adab], [modT])
        n1 = S.sb([128, NCH], F32, "n1w")
        n2 = S.sb([128, NCH], F32, "n2w")
        with self.nc.allow_non_contiguous_dma("tiny"):
            S.dma("sp", n1[:], A["norm1_w"][layer].rearrange("(c p) -> p c", p=128), writes=[n1])
            S.dma("sp", n2[:], A["norm2_w"][layer].rearrange("(c p) -> p c", p=128), writes=[n2])
        M = {}
        for s, sn in enumerate(("l", "c")):
            S.stt(self.a1[s][:], modT[:, 8:16, s], 1.0, n1[:], ALU.add, ALU.mult, [modT, n1], [self.a1[s]])
            S.stt(self.a2[s][:], modT[:, 32:40, s], 1.0, n2[:], ALU.add, ALU.mult, [modT, n2], [self.a2[s]])
            M[sn] = {"a1": self.a1[s], "a2": self.a2[s], "modT": modT, "s": s}
        self.M = M
        S.pop()
        return M

    def mvec(self, sn, which, c):
        m = self.M[sn]
        return m["modT"][:, which * 8 + c, m["s"]:m["s"] + 1]

    def norm_scratch(self):
        S = self.S
        self._sq = S.sb([128, NCH, 512], BF16, "sq")
        self._rstd = S.sb([128, 512], F32, "rstd")
        self._ntmp = S.sb([128, 512], F32, "ntmp")

    def rstd_group(self, grp):
        S = self.S
        n = grp[3]
        sq, rstd = self._sq, self._rstd
        rbs = [self.rbuf(grp, c) for c in range(NCH)]
        for c in range(NCH):
            S.act(sq[:, c, 0:n], rbs[c][:, 0:n], AF.Square, [rbs[c]], [sq])
        ps = S.psum()
        for c in range(NCH):
            S.mm(ps[:, 0:n], self.ones_b[:], sq[:, c, 0:n], c == 0, c == NCH - 1, [self.ones_b, sq], [ps])
        S.act(rstd[:, 0:n], ps[:, 0:n], AF.Sqrt, [ps, self.epsb], [rstd], bias=self.epsb[:, 0:1], scale=1.0 / D)
        S.recip(rstd[:, 0:n], rstd[:, 0:n], [rstd], [rstd])
        return rbs, rstd

    def norm_group(self, grp, a_buf, a_which_shift, sn, out_bf=None, out_f=None):
        S = self.S
        n = grp[3]
        tmp = self._ntmp
        rbs, rstd = self.rstd_group(grp)
        for c in range(NCH):
            S.stt(tmp[:, 0:n], rbs[c][:, 0:n], a_buf[:, c:c + 1], rstd[:, 0:n], ALU.mult, ALU.mult,
                  [rbs[c], a_buf, rstd], [tmp])
            sh = self.mvec(sn, a_which_shift, c)
            if out_f is not None:
                S.act(out_f[c][0], tmp[:, 0:n], AF.Identity, [tmp, self.modT], [out_f[c][1]], bias=sh)
            if out_bf is not None:
                S.act(out_bf[c][0], tmp[:, 0:n], AF.Identity, [tmp, self.modT], [out_bf[c][1]], bias=sh)

    def moe_layer(self, layer, keep_ctx):
        S = self.S
        A = self.A
        S.push()
        m = {}
        m["h2tm"] = [S.sb([128, D], BF16, "h2tm") for _ in range(18)]
        m["slot_tm"] = S.sb([128, 18, NE], F32, "slottm")
        m["affTb"] = S.sb([16, 2304], BF16, "affTb")
        m["slotTb"] = S.sb([16, 2304], BF16, "slotTb")
        m["rw"] = S.sb([128, NCH, NE], F32, "rw")
        m["sm"] = S.sb([128, 4], F32, "sm")
        m["m8"] = S.sb([16, 8], F32, "m8")
        groups = self.groups if keep_ctx else self.groups[:4]
        ntiles = 18 if keep_ctx else 16
        ncap = 288 if keep_ctx else 256
        with self.nc.allow_non_contiguous_dma("tiny"):
            S.dma("sp", m["rw"][:], A["moe_router"][layer * D:(layer + 1) * D, :].rearrange("(c p) e -> p c e", p=128),
                  writes=[m["rw"]])
        self._wk = 0

        def wsrc(kind, e):
            nm = {"g": "moe_w_gate", "u": "moe_w_up", "d": "moe_w_down"}[kind]
            base = (layer * NE + e) * D
            return A[nm][base:base + D, :].rearrange("(c p) n -> p c n", p=128)

        def wload(kind, e):
            b = m["w"][self._wk % 2]
            self._wk += 1
            S.dma("pool", b[:], wsrc(kind, e), writes=[b])
            return b

        S.push()
        m["affT"] = S.sb([16, 2304], F32, "affT")
        m["aff_tm"] = S.sb([128, 18, NE], F32, "afftm")
        S.push()
        self.norm_scratch()
        m["hf"] = [S.sb([128, 512], F32, "hf") for _ in range(NCH)]
        m["hb"] = [S.sb([128, 512], BF16, "hb") for _ in range(NCH)]
        sm = m["sm"]
        tile = 0
        for grp in groups:
            sn = grp[0]
            n = grp[3]
            self.norm_group(grp, self.M[sn]["a2"], 3, sn,
                            out_bf=[(m["hb"][c][:, 0:n], m["hb"][c]) for c in range(NCH)],
                            out_f=[(m["hf"][c][:, 0:n], m["hf"][c]) for c in range(NCH)])
            for tt in range(n // 128):
                tsl = slice(tt * 128, (tt + 1) * 128)
                ps = S.psum()
                for c in range(NCH):
                    S.mm(ps[:, 0:NE], m["hf"][c][:, tsl], m["rw"][:, c, :], c == 0, c == NCH - 1,
                         [m["hf"][c], m["rw"]], [ps])
                S.red(sm[:, 0:1], ps[:, 0:NE], ALU.max, [ps], [sm], negate=True)
                S.act(m["aff_tm"][:, tile, :], ps[:, 0:NE], AF.Exp, [ps, sm], [m["aff_tm"], sm], bias=sm[:, 0:1],
                      accum_out=sm[:, 1:2])
                S.recip(sm[:, 2:3], sm[:, 1:2], [sm], [sm])
                S.ts(m["aff_tm"][:, tile, :], m["aff_tm"][:, tile, :], sm[:, 2:3], None, ALU.mult, None,
                     [m["aff_tm"], sm], [m["aff_tm"]])
                ps2 = S.psum()
                S.trf(ps2[0:NE, 0:128], m["aff_tm"][:, tile, :], self.ident_f[:], [m["aff_tm"], self.ident_f], [ps2])
                S.cp(m["affT"][:, tile * 128:(tile + 1) * 128], ps2[0:NE, 0:128], [ps2], [m["affT"]], e="act")
                ps3 = S.psum()
                psb = ps3[:].bitcast(BF16)
                for c in range(NCH):
                    S.tr(psb[:, c * 128:(c + 1) * 128], m["hb"][c][:, tsl], self.ident_b[:],
                         [m["hb"][c], self.ident_b], [ps3])
                S.cp(m["h2tm"][tile][:], psb[:, 0:D], [ps3], [m["h2tm"][tile]], e=("act" if tile % 2 else "dve"))
                tile += 1
        for c_ in range(NCH):
            self.dbg("hf%d" % c_, m["hf"][c_][:], m["hf"][c_], [128, 512])
        self.dbg("sm", m["sm"][:, 0:3], m["sm"], [128, 3])
        self.dbg("rc0", self.RC[0][:], self.RC[0], [128, 256])
        self.dbg("modT", self.modT[:].rearrange("p a b -> p (a b)"), self.modT, [128, 96])
        self.dbg("aff", m["aff_tm"][:].rearrange("p a b -> p (a b)"), m["aff_tm"], [128, 18 * NE])
        self.dbg("h2tm0", m["h2tm"][0][:], m["h2tm"][0], [128, D])
        S.pop()

        self.dbg("affT", m["affT"][:], m["affT"], [16, 2304])
        S.push()
        m["wk"] = S.sb([16, 2304], F32, "wk")
        m["rankT"] = S.sb([16, 2304], F32, "rankT")
        regions = [(0, 2048, 256)] + ([(2048, 256, 32)] if keep_ctx else [])
        for (r0, rn, k) in regions:
            rs = slice(r0, r0 + rn)
            S.cp(m["wk"][:, rs], m["affT"][:, rs], [m["affT"]], [m["wk"]])
            for rnd in range(k // 8):
                S.max8(m["m8"][:], m["wk"][:, rs], [m["wk"]], [m["m8"]])
                if rnd < k // 8 - 1:
                    S.mrep(m["wk"][:, rs], m["m8"][:], m["wk"][:, rs], -1.0, [m["wk"], m["m8"]], [m["wk"]])
            S.ts(m["wk"][:, rs], m["affT"][:, rs], m["m8"][:, 7:8], None, ALU.is_ge, None,
                 [m["affT"], m["m8"]], [m["wk"]])
            S.scan(m["rankT"][:, rs], m["wk"][:, rs], m["wk"][:, rs], 0.0, ALU.add, ALU.max, [m["wk"]], [m["rankT"]])
            S.tt(m["rankT"][:, rs], m["rankT"][:, rs], m["wk"][:, rs], ALU.mult, [m["rankT"], m["wk"]], [m["rankT"]])
            S.ts(m["rankT"][:, rs], m["rankT"][:, rs], -1.0, None, ALU.add, None, [m["rankT"]], [m["rankT"]])
        nT = ntiles * 128
        S.cp(m["slotTb"][:, 0:nT], m["rankT"][:, 0:nT], [m["rankT"]], [m["slotTb"]])
        S.cp(m["affTb"][:, 0:nT], m["affT"][:, 0:nT], [m["affT"]], [m["affTb"]], e="act")
        for t in range(ntiles):
            ps = S.psum()
            S.trf(ps[:, 0:NE], m["rankT"][:, t * 128:(t + 1) * 128], self.ident_f[0:16, 0:16],
                 [m["rankT"], self.ident_f], [ps])
            S.cp(m["slot_tm"][:, t, :], ps[:, 0:NE], [ps], [m["slot_tm"]], e=("act" if t % 2 else "dve"))
        self.dbg("slot", m["slot_tm"][:].rearrange("p a b -> p (a b)"), m["slot_tm"], [128, 18 * NE])
        self.dbg("m8", m["m8"][:], m["m8"], [16, 8])
        S.pop()
        S.pop()

        S.push()
        m["w"] = [S.sb([128, NCH, 1024], BF16, "wexp") for _ in range(2)]
        wq = [wload("g", 0), wload("u", 0)]
        m["P"] = [S.sb([128, 16 * 256 + 2 * 32], BF16, "P") for _ in range(1)]
        m["PT"] = [S.sb([128, 2048], BF16, "PT") for _ in range(2)] + [S.sb([32, 256], BF16, "PTc")]
        m["affbc"] = S.sb([128, 512], BF16, "affbc")
        m["xeT"] = S.sb([128, NCH, 288], BF16, "xeT")
        m["actT"] = S.sb([128, NCH, 288], BF16, "actT")
        m["sil"] = S.sb([128, NCH, 288], BF16, "sil")
        m["ye"] = [S.sb([128, D], BF16, "ye") for _ in range(3)]
        g2 = {sn: [self.mvec(sn, 5, c) for c in range(NCH)] for sn in ("l", "c")}
        modT = self.modT
        PT = m["PT"]
        for e in range(NE):
            P = m["P"][0]
            wg, wu = wq
            for t in range(ntiles):
                if t < 16:
                    S.ts(P[:, t * 256:(t + 1) * 256], self.iota_row[:, 0:256], m["slot_tm"][:, t, e:e + 1], None,
                         ALU.is_equal, None, [self.iota_row, m["slot_tm"]], [P])
                else:
                    o = 4096 + (t - 16) * 32
                    S.ts(P[:, o:o + 32], self.iota_row[:, 0:32], m["slot_tm"][:, t, e:e + 1], None,
                         ALU.is_equal, None, [self.iota_row, m["slot_tm"]], [P])
            for c in range(NCH):
                ps = S.psum()
                for t in range(16):
                    S.mm(ps[:, 0:256], m["h2tm"][t][:, c * 128:(c + 1) * 128], P[:, t * 256:(t + 1) * 256],
                         t == 0, t == 15, [m["h2tm"][t], P], [ps])
                if keep_ctx:
                    for t in range(16, 18):
                        o = 4096 + (t - 16) * 32
                        S.mm(ps[:, 256:288], m["h2tm"][t][:, c * 128:(c + 1) * 128], P[:, o:o + 32],
                             t == 16, t == 17, [m["h2tm"][t], P], [ps])
                S.cp(m["xeT"][:, c, 0:ncap], ps[:, 0:ncap], [ps], [m["xeT"]], e=("act" if c % 2 else "dve"))
            selE = self.sel[:, e * 128:(e + 1) * 128]
            ab = m["affbc"]
            for gi, grp in enumerate(groups):
                n = grp[3]
                r0 = grp[2] if grp[0] == "l" else 2048
                psB = S.psum()
                S.mm(psB[:, 0:n], selE, m["affTb"][:, r0:r0 + n], True, True, [self.sel, m["affTb"]], [psB])
                S.cp(ab[:, 0:n], psB[:, 0:n], [psB], [ab], e="act")
                psA = S.psum()
                if grp[0] == "l":
                    S.mm(psA[:, 0:n], selE, m["slotTb"][:, r0:r0 + n], True, True, [self.sel, m["slotTb"]], [psA])
                    for cc in range(2):
                        S.stt(PT[cc][:, r0:r0 + n], psA[:, 0:n], self.iota_part[:, cc:cc + 1], ab[:, 0:n],
                              ALU.is_equal, ALU.mult, [psA, self.iota_part, ab], [PT[cc]])
                else:
                    S.mm(psA[0:32, 0:n], selE[:, 0:32], m["slotTb"][:, r0:r0 + n], True, True,
                         [self.sel, m["slotTb"]], [psA])
                    S.stt(PT[2][:, 0:n], psA[0:32, 0:n], self.iota_part[0:32, 0:1], ab[0:32, 0:n],
                          ALU.is_equal, ALU.mult, [psA, self.iota_part, ab], [PT[2]])
            for f in range(NCH):
                psA = S.psum()
                for c in range(NCH):
                    S.mm(psA[:, 0:ncap], wg[:, c, f * 128:(f + 1) * 128], m["xeT"][:, c, 0:ncap], c == 0, c == NCH - 1,
                         [wg, m["xeT"]], [psA])
                S.act(m["sil"][:, f, 0:ncap], psA[:, 0:ncap], AF.Silu, [psA], [m["sil"]])
            wd = wload("d", e)
            for f in range(NCH):
                psU = S.psum()
                for c in range(NCH):
                    S.mm(psU[:, 0:ncap], wu[:, c, f * 128:(f + 1) * 128], m["xeT"][:, c, 0:ncap], c == 0, c == NCH - 1,
                         [wu, m["xeT"]], [psU])
                S.tt(m["actT"][:, f, 0:ncap], m["sil"][:, f, 0:ncap], psU[:, 0:ncap], ALU.mult, [m["sil"], psU], [m["actT"]])
            if e + 1 < NE:
                wg_n = wload("g", e + 1)
            for cc, rows in enumerate((128, 128, 32)[:3 if keep_ctx else 2]):
                for dh in range(2):
                    ps = S.psum()
                    for f in range(NCH):
                        S.mm(ps[0:rows, :], m["actT"][:, f, cc * 128:cc * 128 + rows], wd[:, f, dh * 512:(dh + 1) * 512],
                             f == 0, f == NCH - 1, [m["actT"], wd], [ps])
                    S.cp(m["ye"][cc][0:rows, dh * 512:(dh + 1) * 512], ps[0:rows, :], [ps], [m["ye"][cc]],
                         e=("act" if dh else "dve"))
            if e + 1 < NE:
                wu_n = wload("u", e + 1)
                wq = [wg_n, wu_n]
            for grp in groups:
                n = grp[3]
                r0 = grp[2]
                for c in range(NCH):
                    ps = S.psum()
                    rb = self.rbuf(grp, c)
                    if grp[0] == "l":
                        for cc in range(2):
                            S.mm(ps[:, 0:n], m["ye"][cc][:, c * 128:(c + 1) * 128], PT[cc][:, r0:r0 + n], cc == 0, cc == 1,
                                 [m["ye"][cc], PT[cc]], [ps])
                    else:
                        S.mm(ps[:, 0:n], m["ye"][2][0:32, c * 128:(c + 1) * 128], PT[2][:, 0:n], True, True,
                             [m["ye"][2], PT[2]], [ps])
                    S.stt(rb[:, 0:n], ps[:, 0:n], g2[grp[0]][c], rb[:, 0:n], ALU.mult, ALU.add, [ps, modT, rb], [rb])
        S.pop()
        S.pop()

    def final_store(self, do_norm=True):
        S = self.S
        A = self.A
        S.push()
        self.norm_scratch()
        fw = S.sb([128, NCH], F32, "fnw")
        with self.nc.allow_non_contiguous_dma("tiny"):
            S.dma("sp", fw[:], A["final_norm_w"][0].rearrange("(c p) -> p c", p=128), writes=[fw])
        ob = [S.sb([128, D], F32, "ob") for _ in range(2)]
        hf = [S.sb([128, 512], F32, "hf") for _ in range(NCH)]
        k = 0
        out_evs = []
        glist = [(g, self.out) for g in self.groups[:4]]
        if self.cfg.get("dump_ctx"):
            glist.append((self.groups[4], self.out_c))
        for grp, dst in glist:
            n = grp[3]
            norm = do_norm and grp[0] == "l"
            if norm:
                rbs, rstd = self.rstd_group(grp)
                for c in range(NCH):
                    S.stt(hf[c][:, 0:n], rbs[c][:, 0:n], fw[:, c:c + 1], rstd[:, 0:n], ALU.mult, ALU.mult,
                          [rbs[c], fw, rstd], [hf[c]])
            for tt in range(n // 128):
                o = ob[k % 2]
                k += 1
                for half in range(2):
                    ps = S.psum()
                    for j in range(4):
                        c = half * 4 + j
                        src = hf[c] if norm else self.rbuf(grp, c)
                        S.trf(ps[:, j * 128:(j + 1) * 128], src[:, tt * 128:(tt + 1) * 128], self.ident_f[:],
                             [src, self.ident_f], [ps])
                    S.cp(o[:, half * 512:(half + 1) * 512], ps[:, :], [ps], [o], e=("act" if half else "dve"))
                t0 = grp[2] + tt * 128
                out_evs.append(S.dma("sp", dst[t0:t0 + 128, :], o[:], reads=[o]))
        for ev in out_evs:
            S._wait("sp", ev)
        S.pop()

    def build(self):
        cfg = self.cfg
        self.declare()
        self.setup()
        self.load_stream()
        for layer in cfg.get("layers", range(DEPTH)):
            keep_ctx = layer < DEPTH - 1
            self.adaln(layer)
            if cfg.get("mixer", True):
                self.mixer(layer, keep_ctx)
            if cfg.get("moe", True):
                self.moe_layer(layer, keep_ctx)
        self.final_store(cfg.get("final_norm", True))
        self.S.emit()
        return self.nc

    def hnorm(self, keep_ctx=True):
        S = self.S
        H = [S.sb([128, 2304], BF16, "H") for _ in range(NCH)]
        S.push()
        self.norm_scratch()
        for grp in self.groups:
            sn = grp[0]
            n = grp[3]
            o = grp[2] if sn == "l" else 2048
            self.norm_group(grp, self.M[sn]["a1"], 0, sn, out_bf=[(H[c][:, o:o + n], H[c]) for c in range(NCH)])
        S.pop()
        return H

    def tok_groups(self, keep_ctx=True):
        gs = [(g, g[2], 512) for g in self.groups[:4]]
        if keep_ctx:
            gs.append((self.groups[4], 2048, 256))
        return gs

    def out_proj(self, Z, w_ap, keep_ctx):
        S = self.S
        wo = S.sb([128, NCH, D], BF16, "wo")
        S.dma("pool", wo[:], w_ap.rearrange("(c p) n -> p c n", p=128), writes=[wo])
        for (grp, o, n) in self.tok_groups(keep_ctx):
            for c in range(NCH):
                ps = S.psum()
                for k in range(NCH):
                    S.mm(ps[:, 0:n], wo[:, k, c * 128:(c + 1) * 128], Z[k][:, o:o + n], k == 0, k == NCH - 1,
                         [wo, Z[k]], [ps])
                rb = self.rbuf(grp, c)
                S.stt(rb[:, 0:n], ps[:, 0:n], self.mvec(grp[0], 2, c), rb[:, 0:n], ALU.mult, ALU.add,
                      [ps, self.modT, rb], [rb])

    def mixer(self, layer, keep_ctx):
        kind = layer % 4
        if kind == 0:
            self.mixer_hyena(keep_ctx)
        if kind == 1:
            self.mixer_swa(keep_ctx)
        if kind == 2:
            self.mixer_gdn(keep_ctx)
        if kind == 3:
            self.mixer_hgrn2(layer, keep_ctx)

    def mixer_hyena(self, keep_ctx):
        S = self.S
        A = self.A
        nc = self.nc
        seqs = [("l", TL, 0)] + ([("c", TC, 2048)] if keep_ctx else [])
        TWO_PI = 2.0 * np.pi
        HS = {}
        for nm, L, _ in seqs:
            HS[nm] = nc.dram_tensor("hy_spec_" + nm, [2, 8, 2, 128, L], BF16, kind="Internal").ap()

        S.push()
        w1 = S.sb([33, 64], F32, "w1")
        w2 = S.sb([64, 64], F32, "w2")
        S.dma("sp", w1[:], A["hy_ffn_w1"], writes=[w1])
        S.dma("sp", w2[:], A["hy_ffn_w2"], writes=[w2])
        pcol = S.sb([64, 4], F32, "pcol")
        S.dma("sp", pcol[:, 0:1], A["hy_ffn_b1"].rearrange("o p -> p o"), writes=[pcol])
        S.dma("sp", pcol[:, 1:2], A["hy_sin_freq"][0:1, :].rearrange("o p -> p o"), writes=[pcol])
        S.dma("sp", pcol[:, 2:3], A["hy_ffn_b2"].rearrange("o p -> p o"), writes=[pcol])
        S.dma("sp", pcol[:, 3:4], A["hy_sin_freq"][1:2, :].rearrange("o p -> p o"), writes=[pcol])
        w3p = S.sb([64, 2, D], F32, "w3p")
        w3m = S.sb([64, 2, D], F32, "w3m")
        dbc = S.sb([128, D], F32, "dbc")
        sa = {nm: S.sb([64, 256], F32, "sa_" + nm) for nm in ("arg", "t", "kf", "m")}
        ki = S.sb([64, 256], I32, "ki")
        S.push()
        w3 = S.sb([64, 4 * D], F32, "w3")
        S.dma("sp", w3[:], A["hy_ffn_w3"], writes=[w3])
        for o in range(2):
            fw_ = w3[:, o * 2048:o * 2048 + D]
            bw_ = w3[:, o * 2048 + D:o * 2048 + 2 * D]
            S.tt(w3p[:, o, :], fw_, bw_, ALU.add, [w3], [w3p])
            S.tt(w3m[:, o, :], bw_, fw_, ALU.subtract, [w3], [w3m], e="pool")
        S.pop()
        S.dma("sp", dbc[:], A["hy_delta"][0].partition_broadcast(128), writes=[dbc])

        def sinfn(dst, ps, n, bcol, fcol):
            S.ts(sa["arg"][:, 0:n], ps, pcol[:, bcol:bcol + 1], pcol[:, fcol:fcol + 1], ALU.add, ALU.mult,
                 [pcol] + ([] if isinstance(ps, int) else []), [sa["arg"]])
            S.ts(sa["t"][:, 0:n], sa["arg"][:, 0:n], 1.0 / TWO_PI, 8.5, ALU.mult, ALU.add, [sa["arg"]], [sa["t"]])
            S.cp(ki[:, 0:n], sa["t"][:, 0:n], [sa["t"]], [ki])
            S.cp(sa["kf"][:, 0:n], ki[:, 0:n], [ki], [sa["kf"]])
            S.tt(sa["t"][:, 0:n], sa["t"][:, 0:n], sa["kf"][:, 0:n], ALU.subtract, [sa["t"], sa["kf"]], [sa["t"]])
            S.ts(sa["m"][:, 0:n], sa["t"][:, 0:n], 0.5, None, ALU.is_gt, None, [sa["t"]], [sa["m"]])
            S.tt(sa["t"][:, 0:n], sa["t"][:, 0:n], sa["m"][:, 0:n], ALU.subtract, [sa["t"], sa["m"]], [sa["t"]])
            S.act(dst, sa["t"][:, 0:n], AF.Sin, [sa["t"]], [], scale=-TWO_PI)

        for nm, L, _ in seqs:
            T_ = L // 128
            N = 2 * L
            S.push()
            tcol = S.sb([128, T_], F32, "tcol")
            S.dma("sp", tcol[:], A["hy_tcol_" + nm], writes=[tcol])
            S.ts(tcol[:], tcol[:], -1.0, None, ALU.mult, None, [tcol], [tcol])
            h2T = S.sb([64, L], F32, "h2T")
            S.push()
            zT = S.sb([33, L], F32, "zT")
            S.dma("sp", zT[:], A["hy_zT_" + nm], writes=[zT])
            h1T = S.sb([64, L], F32, "h1T")
            for b0 in range(0, L, 256):
                n = min(256, L - b0)
                ps = S.psum()
                S.mm(ps[0:64, 0:n], w1[:], zT[:, b0:b0 + n], True, True, [w1, zT], [ps])
                S._deps("dve", [ps], [])
                sinfn(h1T[:, b0:b0 + n], ps[0:64, 0:n], n, 0, 1)
                S._mark((S.csem["act"], S.ccnt["act"], "act"), [ps], [h1T])
            for b0 in range(0, L, 256):
                n = min(256, L - b0)
                ps = S.psum()
                S.mm(ps[0:64, 0:n], w2[:], h1T[:, b0:b0 + n], True, True, [w2, h1T], [ps])
                S._deps("dve", [ps], [])
                sinfn(h2T[:, b0:b0 + n], ps[0:64, 0:n], n, 2, 3)
                S._mark((S.csem["act"], S.ccnt["act"], "act"), [ps], [h2T])
            S.pop()
            win = S.sb([128, T_, 128], F32, "win")
            hp = [[S.sb([128, T_, 128], BF16, "hp") for o in range(2)] for _ in range(2)]
            hm = [[S.sb([128, T_, 128], BF16, "hm") for o in range(2)] for _ in range(2)]
            Hs = [[[S.sb([128, T_, 128], BF16, "Hs") for ri in range(2)] for o in range(2)] for _ in range(2)]
            Cf = [S.sb([128, T_, 128], BF16, "Cf") for _ in range(2)]
            Sf_ = [S.sb([128, T_, 128], BF16, "Sf") for _ in range(2)]
            for pair in range(4):
                for bi in range(2):
                    cb = pair * 2 + bi
                    ccs = slice(cb * 128, (cb + 1) * 128)
                    for tt in range(T_):
                        S.act(win[:, tt, :], dbc[:, ccs], AF.Exp, [dbc, tcol], [win], scale=tcol[:, tt:tt + 1])
                    S.ts(win[:], win[:], 0.05, None, ALU.add, None, [win], [win])
                    for o in range(2):
                        for tt in range(T_):
                            dsl = slice(tt * 128, (tt + 1) * 128)
                            psP = S.psum()
                            psM = S.psum()
                            S.mm(psP[:, 0:128], h2T[:, dsl], w3p[:, o, ccs], True, True, [h2T, w3p], [psP])
                            S.mm(psM[:, 0:128], h2T[:, dsl], w3m[:, o, ccs], True, True, [h2T, w3m], [psM])
                            S.tt(hp[bi][o][:, tt, :], psP[:, 0:128], win[:, tt, :], ALU.mult, [psP, win], [hp[bi][o]])
                            S.tt(hm[bi][o][:, tt, :], psM[:, 0:128], win[:, tt, :], ALU.mult, [psM, win], [hm[bi][o]])
                        S.tt(hp[bi][o][0:1, 0, :], hp[bi][o][0:1, 0, :], hm[bi][o][0:1, 0, :], ALU.subtract,
                             [hp[bi][o], hm[bi][o]], [hp[bi][o]])
                        S.ts(hp[bi][o][0:1, 0, :], hp[bi][o][0:1, 0, :], 0.5, None, ALU.mult, None, [hp[bi][o]], [hp[bi][o]])
                for ft in range(T_):
                    cf, sf = Cf[ft % 2], Sf_[ft % 2]
                    S.dma("sp", cf[:].rearrange("p a b -> p (a b)"), A["hy_Cf_" + nm][ft * 128:(ft + 1) * 128, :], writes=[cf])
                    S.dma("sp", sf[:].rearrange("p a b -> p (a b)"), A["hy_Sf_" + nm][ft * 128:(ft + 1) * 128, :], writes=[sf])
                    for bi in range(2):
                        for o in range(2):
                            for ri, (tab, src) in enumerate(((cf, hp[bi][o]), (sf, hm[bi][o]))):
                                ps = S.psum()
                                for tt in range(T_):
                                    S.mm(ps[:, 0:128], tab[:, tt, :], src[:, tt, :], tt == 0, tt == T_ - 1, [tab, src], [ps])
                                S.act(Hs[bi][o][ri][:, ft, :], ps[:, 0:128], AF.Identity, [ps], [Hs[bi][o][ri]], scale=2.0 / N)
                for bi in range(2):
                    cb = pair * 2 + bi
                    for o in range(2):
                        for ri in range(2):
                            S.dma("sp", HS[nm][o, cb, ri], Hs[bi][o][ri][:].rearrange("p a b -> p (a b)"),
                                  reads=[Hs[bi][o][ri]], writes=[self.hs_tok])
            S.pop()
        S.pop()

        S.push()
        H = self.hnorm()
        Win = A["hy_w_in"].rearrange("(c p) n -> p c n", p=128)
        pv = S.sb([128, 24, 6], F32, "hyp")
        S.dma("sp", pv[:, :, 0], A["hy_b_in"][0].rearrange("(c p) -> p c", p=128), writes=[pv])
        for k in range(3):
            S.dma("sp", pv[:, :, 1 + k], A["hy_short_w"][k].rearrange("(c p) -> p c", p=128), writes=[pv])
        S.dma("sp", pv[:, :, 4], A["hy_short_b"][0].rearrange("(c p) -> p c", p=128), writes=[pv])
        fb = S.sb([128, 2, NCH], F32, "hyfb")
        for o in range(2):
            S.dma("sp", fb[:, o, :], A["hy_filter_bias"][o].rearrange("(c p) -> p c", p=128), writes=[fb])
        bo = S.sb([128, NCH], F32, "hybo")
        S.dma("sp", bo[:], A["hy_b_out"][0].rearrange("(c p) -> p c", p=128), writes=[bo])
        W3b = S.sb([128, NCH, 3, 128], BF16, "W3b")
        wo = S.sb([128, D], BF16, "wo_h")
        upad = S.sb([128, 2312], BF16, "upad")
        S.memset(upad[:], 0.0, [upad])
        cv = S.sb([128, 2304], F32, "cv")
        ufm = [S.sb([128, 2304], BF16, "ufm") for _ in range(3)]
        z1 = S.sb([128, 2304], BF16, "z1")
        zT_ = S.sb([128, 2304], BF16, "zTh")
        utm = S.sb([128, 16, 128], BF16, "utm")
        Hr = S.sb([128, 16, 128], BF16, "Hr")
        Hi = S.sb([128, 16, 128], BF16, "Hi")
        Yr = S.sb([128, 16, 128], BF16, "Yr")
        nYi = S.sb([128, 16, 128], BF16, "nYi")
        Cf = [S.sb([128, 16, 128], BF16, "Cf") for _ in range(2)]
        Sf_ = [S.sb([128, 16, 128], BF16, "Sf") for _ in range(2)]
        ring = [S.sb([128, 512], BF16, "ring") for _ in range(8)]
        ur = S.sb([128, 128], F32, "ur")
        ui = S.sb([128, 128], F32, "ui")
        t1 = S.sb([128, 128], F32, "t1")
        t2 = S.sb([128, 128], F32, "t2")
        ytmp = S.sb([128, 512], F32, "ytmp")
        LO, CO = 1, 2052
        rk = 0

        for cb in range(8):
            for k in range(3):
                S.dma("pool", W3b[:, :, k, :], Win[:, :, k * D + cb * 128:k * D + (cb + 1) * 128], writes=[W3b])
            for k in range(3):
                ch = k * 8 + cb
                for (grp, o, n) in self.tok_groups(keep_ctx):
                    ps = S.psum()
                    for c in range(NCH):
                        S.mm(ps[:, 0:n], W3b[:, c, k, :], H[c][:, o:o + n], c == 0, c == NCH - 1, [W3b, H[c]], [ps])
                    po_ = (LO + o) if o < 2048 else (CO + o - 2048)
                    S.act(upad[:, po_:po_ + n], ps[:, 0:n], AF.Identity, [ps, pv], [upad], bias=pv[:, ch, 0:1])
                for (nm, L, o_) in seqs:
                    base = LO if nm == "l" else CO
                    S.ts(cv[:, o_:o_ + L], upad[:, base - 1:base - 1 + L], pv[:, ch, 1:2], pv[:, ch, 4:5], ALU.mult, ALU.add,
                         [upad, pv], [cv])
                    S.stt(cv[:, o_:o_ + L], upad[:, base:base + L], pv[:, ch, 2:3], cv[:, o_:o_ + L], ALU.mult, ALU.add,
                          [upad, pv, cv], [cv])
                    S.stt(ufm[k][:, o_:o_ + L], upad[:, base + 1:base + 1 + L], pv[:, ch, 3:4], cv[:, o_:o_ + L],
                          ALU.mult, ALU.add, [upad, pv, cv], [ufm[k]])
            for (nm, L, o_) in seqs:
                T_ = L // 128
                for o in range(2):
                    src = ufm[0] if o == 0 else z1
                    xg = ufm[1 + o]
                    dst = z1 if o == 0 else zT_
                    for tt in range(T_):
                        pT = S.psum()
                        pTb = pT[:].bitcast(BF16)
                        S.tr(pTb[:, 0:128], src[:, o_ + tt * 128:o_ + (tt + 1) * 128], self.ident_b[:], [src, self.ident_b], [pT])
                        S.cp(utm[:, tt, :], pTb[:, 0:128], [pT], [utm], e="act")
                    S.dma("sp", Hr[:, 0:T_, :].rearrange("p a b -> p (a b)"), HS[nm][o, cb, 0], reads=[self.hs_tok], writes=[Hr])
                    S.dma("sp", Hi[:, 0:T_, :].rearrange("p a b -> p (a b)"), HS[nm][o, cb, 1], reads=[self.hs_tok], writes=[Hi])
                    for ft in range(T_):
                        cf, sf = Cf[ft % 2], Sf_[ft % 2]
                        S.dma("sp", cf[:, 0:T_, :].rearrange("p a b -> p (a b)"), A["hy_Cf_" + nm][ft * 128:(ft + 1) * 128, :], writes=[cf])
                        S.dma("sp", sf[:, 0:T_, :].rearrange("p a b -> p (a b)"), A["hy_Sf_" + nm][ft * 128:(ft + 1) * 128, :], writes=[sf])
                        pr = S.psum()
                        for tt in range(T_):
                            S.mm(pr[:, 0:128], cf[:, tt, :], utm[:, tt, :], tt == 0, tt == T_ - 1, [cf, utm], [pr])
                        pi_ = S.psum()
                        for tt in range(T_):
                            S.mm(pi_[:, 0:128], sf[:, tt, :], utm[:, tt, :], tt == 0, tt == T_ - 1, [sf, utm], [pi_])
                        S.cp(ur[:], pr[:, 0:128], [pr], [ur], e="act")
                        S.cp(ui[:], pi_[:, 0:128], [pi_], [ui], e="act")
                        S.tt(t1[:], ur[:], Hr[:, ft, :], ALU.mult, [ur, Hr], [t1])
                        S.tt(t2[:], ui[:], Hi[:, ft, :], ALU.mult, [ui, Hi], [t2], e="pool")
                        S.tt(Yr[:, ft, :], t1[:], t2[:], ALU.add, [t1, t2], [Yr])
                        S.tt(t1[:], ui[:], Hr[:, ft, :], ALU.mult, [ui, Hr], [t1])
                        S.tt(t2[:], ur[:], Hi[:, ft, :], ALU.mult, [ur, Hi], [t2], e="pool")
                        S.tt(nYi[:, ft, :], t1[:], t2[:], ALU.subtract, [t1, t2], [nYi])
                    for tg in range(0, L, 512):
                        n = min(512, L - tg)
                        py = S.psum()
                        for ft in range(T_):
                            rc = ring[rk % 8]
                            rs = ring[(rk + 1) % 8]
                            rk += 2
                            S.dma("sp", rc[:, 0:n], A["hy_Ct_" + nm][ft * 128:(ft + 1) * 128, tg:tg + n], writes=[rc])
                            S.dma("sp", rs[:, 0:n], A["hy_St_" + nm][ft * 128:(ft + 1) * 128, tg:tg + n], writes=[rs])
                            S.mm(py[:, 0:n], Yr[:, ft, :], rc[:, 0:n], ft == 0, False, [Yr, rc], [py])
                            S.mm(py[:, 0:n], nYi[:, ft, :], rs[:, 0:n], False, ft == T_ - 1, [nYi, rs], [py])
                        osl = slice(o_ + tg, o_ + tg + n)
                        S.stt(ytmp[:, 0:n], src[:, osl], fb[:, o, cb:cb + 1], py[:, 0:n], ALU.mult, ALU.add, [src, fb, py], [ytmp])
                        S.tt(dst[:, osl], ytmp[:, 0:n], xg[:, osl], ALU.mult, [ytmp, xg], [dst], e="pool")
            self.out_proj_head(zT_, A["hy_w_out"], cb, wo, keep_ctx)
        gb = S.sb([128, 2, NCH], F32, "hygb")
        for si, sn in enumerate(("l", "c")):
            S.tt(gb[:, si, :], self.modT[:, 16:24, si], bo[:], ALU.mult, [self.modT, bo], [gb])
        for (grp, o, n) in self.tok_groups(keep_ctx):
            si = 0 if grp[0] == "l" else 1
            for c in range(NCH):
                rb = self.rbuf(grp, c)
                S.ts(rb[:, 0:n], rb[:, 0:n], gb[:, si, c:c + 1], None, ALU.add, None, [rb, gb], [rb])
        S.pop()

    def out_proj_head(self, zT, w_ap, h, wo, keep_ctx):
        S = self.S
        S.dma("pool", wo[:], w_ap[h * 128:(h + 1) * 128, :], writes=[wo])
        for (grp, o, n) in self.tok_groups(keep_ctx):
            for c in range(NCH):
                ps = S.psum()
                S.mm(ps[:, 0:n], wo[:, c * 128:(c + 1) * 128], zT[:, o:o + n], True, True, [wo, zT], [ps])
                rb = self.rbuf(grp, c)
                S.stt(rb[:, 0:n], ps[:, 0:n], self.mvec(grp[0], 2, c), rb[:, 0:n], ALU.mult, ALU.add,
                      [ps, self.modT, rb], [rb])

    def mixer_gdn(self, keep_ctx):
        S = self.S
        A = self.A
        W = A["gd_w_in"].rearrange("(c p) n -> p c n", p=128)
        ntile = 18
        S.push()
        H = self.hnorm()
        I_f = self.ident_f
        cw = S.sb([128, 3, 24], F32, "cw")
        for k in range(3):
            S.dma("sp", cw[:, k, :], A["gd_conv_w"][k].rearrange("(c p) -> p c", p=128), writes=[cw])
        nea = S.sb([1, 16], F32, "nea")
        dtb = S.sb([1, 16], F32, "dtb")
        S.dma("sp", nea[:], A["gd_a_log"], writes=[nea])
        S.dma("sp", dtb[:], A["gd_dt_bias"], writes=[dtb])
        S.act(nea[:], nea[:], AF.Exp, [nea], [nea])
        S.ts(nea[:], nea[:], -1.0, None, ALU.mult, None, [nea], [nea])
        onesr = S.sb([1, 128], F32, "onesr")
        S.memset(onesr[:], 1.0, [onesr])
        nwbc = S.sb([128, 128], F32, "nwbc")
        S.dma("sp", nwbc[:], A["gd_norm_w"][0].partition_broadcast(128), writes=[nwbc])
        cresr = S.sb([1, 128], F32, "cresr")
        S.dma("sp", cresr[:], A["chunk_reset"][0:1, :], writes=[cresr])
        tri = [S.sb([128, 128], F32, "tri") for _ in range(2)]
        tris = [S.sb([128, 128], F32, "tris") for _ in range(2)]
        S.dma("sp", tri[0][:], A["tri_fwd"], writes=[tri[0]])
        S.dma("sp", tri[1][:], A["tri_bwd"], writes=[tri[1]])
        S.dma("sp", tris[0][:], A["tri_fwd_s"], writes=[tris[0]])
        S.dma("sp", tris[1][:], A["tri_bwd_s"], writes=[tris[1]])
        eps1 = self.epsb
        wbab = S.sb([128, NCH, 32], BF16, "wbab")
        S.dma("pool", wbab[:], W[:, :, 4096:4128], writes=[wbab])
        wo = S.sb([128, D], BF16, "wo_h")
        cv = S.sb([128, 2304], F32, "cv")
        qT = S.sb([128, 2304], BF16, "qT")
        kT = S.sb([128, 2304], BF16, "kT")
        ktm = S.sb([128, ntile, 128], BF16, "ktm")
        vtm = S.sb([128, ntile, 128], BF16, "vtm")
        ztm = S.sb([128, ntile, 128], BF16, "ztm")
        of = S.sb([128, ntile, 128], F32, "of")
        zT = S.sb([128, 2304], BF16, "zT")
        vT = zT
        PV = []
        for _d in range(2):
            if _d == 1:
                mark = S.top
                W4 = S.sb([128, NCH, 4, 128], BF16, "W4")
                upad = S.sb([128, 2312], BF16, "upad")
                sqb = S.sb([128, 512], BF16, "sqb")
                rin = S.sb([128, 512], F32, "rin")
                top_after = S.top
                S.top = mark
            pvd = {}
            pvd["brow"] = S.sb([1, 128], F32, "brow")
            pvd["grow"] = S.sb([1, 128], F32, "grow")
            pvd["R"] = {nm: S.sb([1, 128], F32, nm) for nm in ("Gi", "G", "nG", "dl", "EGr")}
            pvd["Sf"] = S.sb([128, 128], F32, "Sf")
            pvd["Sb"] = S.sb([128, 128], BF16, "Sb")
            pvd["W2"] = S.sb([128, 2, 128], BF16, "W2")
            pvd["Q2"] = S.sb([128, 2, 128], BF16, "Q2")
            S.memset(pvd["W2"][:], 0.0, [pvd["W2"]])
            S.memset(pvd["Q2"][:], 0.0, [pvd["Q2"]])
            pvd["T"] = {nm: S.sb([128, 128], F32, nm) for nm in ("Em", "Ee", "Ei", "Es", "EB", "A", "B", "IA", "IB",
                                                                "P", "Pt", "u", "av", "o")}
            pvd["rhs"] = S.sb([128, 256], F32, "rhs")
            pvd["cols"] = S.sb([128, 8], F32, "cols")
            pvd["wsb"] = S.sb([128, 128], BF16, "wsb")
            pvd["kdtm"] = S.sb([128, 128], BF16, "kdtm")
            pvd["attnT"] = S.sb([128, 128], BF16, "attnT")
            pvd["vnew"] = S.sb([128, 128], BF16, "vnew")
            PV.append(pvd)
        S.top = max(S.top, top_after)
        TR = {nm: S.sb([128, 128], F32, nm) for nm in ("os", "nwg", "ysq")}
        yb = S.sb([128, 128], BF16, "yb")
        of1v = cv[:].rearrange("p (a b) -> p a b", b=128)
        st = S.sb([128, 4], F32, "gdst")
        LO, CO = 1, 2052

        def pad_view(o, n):
            return (LO + o) if o < 2048 else (CO + o - 2048)

        for h in range(8):
            S.barrier()
            S.memset(upad[:, 0:1], 0.0, [upad])
            S.memset(upad[:, 2049:2052], 0.0, [upad])
            S.memset(upad[:, 2308:2312], 0.0, [upad])
            for k in range(4):
                S.dma("pool", W4[:, :, k, :], W[:, :, k * D + h * 128:k * D + (h + 1) * 128], writes=[W4])
            for k, dst in ((0, qT), (1, kT), (2, vT)):
                for (grp, o, n) in self.tok_groups(True):
                    ps = S.psum()
                    for c in range(NCH):
                        S.mm(ps[:, 0:n], W4[:, c, k, :], H[c][:, o:o + n], c == 0, c == NCH - 1, [W4, H[c]], [ps])
                    po_ = pad_view(o, n)
                    S.cp(upad[:, po_:po_ + n], ps[:, 0:n], [ps], [upad], e="act")
                ch = k * 8 + h
                for (base, n_, o_) in ((LO, 2048, 0), (CO, 256, 2048)):
                    S.ts(cv[:, o_:o_ + n_], upad[:, base - 1:base - 1 + n_], cw[:, 0, ch:ch + 1], None, ALU.mult, None,
                         [upad, cw], [cv])
                    S.stt(cv[:, o_:o_ + n_], upad[:, base:base + n_], cw[:, 1, ch:ch + 1], cv[:, o_:o_ + n_],
                          ALU.mult, ALU.add, [upad, cw, cv], [cv])
                    S.stt(cv[:, o_:o_ + n_], upad[:, base + 1:base + 1 + n_], cw[:, 2, ch:ch + 1], cv[:, o_:o_ + n_],
                          ALU.mult, ALU.add, [upad, cw, cv], [cv])
                S.act(cv[:], cv[:], AF.Silu, [cv], [cv])
                if k == 2:
                    S.cp(dst[:], cv[:], [cv], [dst])
                    continue
                for (grp, o, n) in self.tok_groups(True):
                    S.act(sqb[:, 0:n], cv[:, o:o + n], AF.Square, [cv], [sqb])
                    ps = S.psum()
                    S.mm(ps[:, 0:n], self.ones_b[:], sqb[:, 0:n], True, True, [self.ones_b, sqb], [ps])
                    S.act(rin[:, 0:n], ps[:, 0:n], AF.Sqrt, [ps, eps1], [rin], bias=eps1[:, 0:1],
                          scale=(128.0 if k == 0 else 1.0))
                    S.recip(rin[:, 0:n], rin[:, 0:n], [rin], [rin])
                    S.tt(dst[:, o:o + n], cv[:, o:o + n], rin[:, 0:n], ALU.mult, [cv, rin], [dst])
            for t in range(ntile):
                tsl = slice(t * 128, (t + 1) * 128)
                for src, dstm in ((kT, ktm), (vT, vtm)):
                    pT = S.psum()
                    pTb = pT[:].bitcast(BF16)
                    S.tr(pTb[:, 0:128], src[:, tsl], self.ident_b[:], [src, self.ident_b], [pT])
                    S.cp(dstm[:, t, :], pTb[:, 0:128], [pT], [dstm], e="act")
                ps = S.psum()
                for c in range(NCH):
                    S.mm(ps[:, 0:128], H[c][:, tsl], W4[:, c, 3, :], c == 0, c == NCH - 1, [H[c], W4], [ps])
                S.act(ztm[:, t, :], ps[:, 0:128], AF.Silu, [ps], [ztm])
            def run_dir(d):
                pvd = PV[d]
                brow, grow, R, Sf, Sb, W2, Q2, T = (pvd[k] for k in ('brow', 'grow', 'R', 'Sf', 'Sb', 'W2', 'Q2', 'T'))
                rhs, cols, wsb, kdtm, attnT, vnew = (pvd[k] for k in ('rhs', 'cols', 'wsb', 'kdtm', 'attnT', 'vnew'))
                idx = d * 8 + h
                S.memset(Sf[:], 0.0, [Sf])
                S.memset(Sb[:], 0.0, [Sb])
                order = [16, 17] + list(range(16)) if d == 0 else [17, 16] + list(range(15, -1, -1))
                idx = d * 8 + h
                for tile in order:
                    o0 = tile * 128
                    tsl = slice(o0, o0 + 128)
                    for kind, dstr in ((0, brow), (1, grow)):
                        col = kind * 16 + idx
                        ps = S.psum()
                        for c in range(NCH):
                            S.mm(ps[0:1, 0:128], wbab[:, c, col:col + 1], H[c][:, tsl], c == 0, c == NCH - 1,
                                 [wbab, H[c]], [ps])
                        if kind == 0:
                            S.act(dstr[:], ps[0:1, 0:128], AF.Sigmoid, [ps], [dstr])
                        else:
                            S.act(dstr[:], ps[0:1, 0:128], AF.Exp, [ps, dtb], [dstr], bias=dtb[0:1, idx:idx + 1])
                            S.act(dstr[:], dstr[:], AF.Ln, [dstr, onesr], [dstr], bias=onesr[0:1, 0:1])
                            S.ts(dstr[:], dstr[:], nea[0:1, idx:idx + 1], None, ALU.mult, None, [dstr, nea], [dstr])
                    gr = grow[:]
                    S.scan(R["Gi"][:], cresr[:], gr, 0.0, ALU.mult, ALU.add, [cresr, grow], [R["Gi"]])
                    if d == 0:
                        G = R["Gi"]
                    else:
                        G = R["G"]
                        S.tt(G[:], gr, R["Gi"][:], ALU.subtract, [grow, R["Gi"]], [G])
                        for ci in range(2):
                            cs = slice(64 * ci, 64 * ci + 64)
                            S.ts(G[:, cs], G[:, cs], R["Gi"][:, 64 * ci + 63:64 * ci + 64], None, ALU.add, None,
                                 [G, R["Gi"]], [G])
                    S.ts(R["nG"][:], G[:], -1.0, None, ALU.mult, None, [G], [R["nG"]])
                    for ci in range(2):
                        cs = slice(64 * ci, 64 * ci + 64)
                        last = 64 * ci + (63 if d == 0 else 0)
                        S.ts(R["dl"][:, cs], G[:, cs], G[:, last:last + 1], None, ALU.subtract, None, [G], [R["dl"]])
                    S.act(R["EGr"][:], G[:], AF.Exp, [G], [R["EGr"]])
                    pD = S.psum()
                    S.mm(pD[:, 0:128], R["nG"][:], onesr[:], True, False, [R["nG"], onesr], [pD])
                    S.mm(pD[:, 0:128], onesr[:], G[:], False, True, [onesr, G], [pD])
                    pC = S.psum()
                    S.mm(pC[:, 0:1], G[:], onesr[:, 0:1], True, True, [G, onesr], [pC])
                    S.mm(pC[:, 1:2], R["dl"][:], onesr[:, 0:1], True, True, [R["dl"], onesr], [pC])
                    S.mm(pC[:, 2:3], brow[:], onesr[:, 0:1], True, True, [brow, onesr], [pC])
                    for ci in range(2):
                        last = 64 * ci + (63 if d == 0 else 0)
                        S.mm(pC[:, 4 + ci:5 + ci], onesr[:], R["EGr"][:, last:last + 1], True, True, [onesr, R["EGr"]], [pC])
                    pB = S.psum()
                    S.mm(pB[:, 0:128], onesr[:], brow[:], True, True, [onesr, brow], [pB])
                    S.ts(T["Em"][:], pD[:, 0:128], 0.0, None, ALU.min, None, [pD], [T["Em"]])
                    S.act(T["Ee"][:], T["Em"][:], AF.Exp, [T["Em"]], [T["Ee"]])
                    S.tt(T["Ei"][:], T["Ee"][:], tri[d][:], ALU.mult, [T["Ee"], tri[d]], [T["Ei"]], e="pool")
                    S.tt(T["Es"][:], T["Ee"][:], tris[d][:], ALU.mult, [T["Ee"], tris[d]], [T["Es"]], e="pool")
                    S.act(cols[:, 0:1], pC[:, 0:1], AF.Exp, [pC], [cols])
                    S.act(cols[:, 1:2], pC[:, 1:2], AF.Exp, [pC], [cols], scale=-1.0)
                    S.act(cols[:, 2:3], pC[:, 2:3], AF.Identity, [pC], [cols])
                    S.act(cols[:, 4:6], pC[:, 4:6], AF.Identity, [pC], [cols])
                    S.tt(cols[:, 3:4], cols[:, 2:3], cols[:, 0:1], ALU.mult, [cols], [cols])
                    S.tt(T["EB"][:], pB[:, 0:128], T["Es"][:], ALU.mult, [pB, T["Es"]], [T["EB"]])
                    pK = S.psum()
                    S.mm(pK[:, 0:128], kT[:, tsl], kT[:, tsl], True, True, [kT], [pK])
                    S.tt(T["B"][:], pK[:, 0:128], T["EB"][:], ALU.mult, [pK, T["EB"]], [T["B"]])
                    pA = S.psum()
                    S.trf(pA[:, 0:128], T["B"][:], I_f[:], [T["B"], I_f], [pA])
                    S.cp(T["A"][:], pA[:, 0:128], [pA], [T["A"]], e="act")
                    S.tt(T["P"][:], I_f[:], T["B"][:], ALU.subtract, [I_f, T["B"]], [T["P"]])
                    S.tt(T["Pt"][:], I_f[:], T["A"][:], ALU.subtract, [I_f, T["A"]], [T["Pt"]], e="pool")
                    for lvl in range(5):
                        pA2 = S.psum()
                        pB2 = S.psum()
                        S.mm(pA2[:, 0:128], T["B"][:], T["A"][:], True, True, [T["B"], T["A"]], [pA2])
                        S.mm(pB2[:, 0:128], T["A"][:], T["B"][:], True, True, [T["A"], T["B"]], [pB2])
                        S.tt(T["IA"][:], pA2[:, 0:128], I_f[:], ALU.add, [pA2, I_f], [T["IA"]])
                        S.tt(T["IB"][:], pB2[:, 0:128], I_f[:], ALU.add, [pB2, I_f], [T["IB"]])
                        if lvl < 4:
                            S.cp(T["A"][:], pA2[:, 0:128], [pA2], [T["A"]], e="act")
                            S.cp(T["B"][:], pB2[:, 0:128], [pB2], [T["B"]], e="act")
                        pP = S.psum()
                        pPt = S.psum()
                        S.mm(pP[:, 0:128], T["Pt"][:], T["IB"][:], True, True, [T["Pt"], T["IB"]], [pP])
                        S.mm(pPt[:, 0:128], T["P"][:], T["IA"][:], True, True, [T["P"], T["IA"]], [pPt])
                        S.cp(T["P"][:], pP[:, 0:128], [pP], [T["P"]])
                        S.cp(T["Pt"][:], pPt[:, 0:128], [pPt], [T["Pt"]], e="act")
                    S.ts(rhs[:, 0:128], vtm[:, tile, :], cols[:, 2:3], None, ALU.mult, None, [vtm, cols], [rhs])
                    S.ts(rhs[:, 128:256], ktm[:, tile, :], cols[:, 3:4], None, ALU.mult, None, [ktm, cols], [rhs])
                    pU = S.psum()
                    S.mm(pU[:, 0:256], T["P"][:], rhs[:], True, True, [T["P"], rhs], [pU])
                    S.cp(T["u"][:], pU[:, 0:128], [pU], [T["u"]], e="act")
                    S.cp(wsb[:], pU[:, 128:256], [pU], [wsb], e="act")
                    pW = S.psum()
                    pWb = pW[:].bitcast(BF16)
                    S.tr(pWb[:, 0:128], wsb[:], self.ident_b[:], [wsb, self.ident_b], [pW])
                    S.cp(W2[:, 0, 0:64], pWb[:, 0:64], [pW], [W2])
                    S.cp(W2[:, 1, 64:128], pWb[:, 64:128], [pW], [W2])
                    S.cp(Q2[:, 0, 0:64], qT[:, o0:o0 + 64], [qT], [Q2], e="pool")
                    S.cp(Q2[:, 1, 64:128], qT[:, o0 + 64:o0 + 128], [qT], [Q2], e="pool")
                    S.ts(kdtm[:], ktm[:, tile, :], cols[:, 1:2], None, ALU.mult, None, [ktm, cols], [kdtm])
                    pQK = S.psum()
                    S.mm(pQK[:, 0:128], kT[:, tsl], qT[:, tsl], True, True, [kT, qT], [pQK])
                    S.tt(attnT[:], pQK[:, 0:128], T["Ei"][:], ALU.mult, [pQK, T["Ei"]], [attnT])
                    pq = S.psum()
                    pv = S.psum()
                    psS = S.psum()
                    corder = (0, 1) if d == 0 else (1, 0)
                    for n_, ci in enumerate(corder):
                        pr = slice(64 * ci, 64 * ci + 64)
                        S.mm(pv[:, 0:128], W2[:, ci, :], Sb[:], True, True, [W2, Sb], [pv])
                        S.tt(vnew[pr, :], T["u"][pr, :], pv[pr, 0:128], ALU.subtract, [T["u"], pv], [vnew])
                        S.mm(pq[:, 0:128], Q2[:, ci, :], Sb[:], n_ == 0, n_ == 1, [Q2, Sb], [pq])
                        S.mm(psS[:, 0:128], kdtm[pr, :], vnew[pr, :], True, True, [kdtm, vnew], [psS])
                        S.stt(Sf[:], Sf[:], cols[:, 4 + ci:5 + ci], psS[:, 0:128], ALU.mult, ALU.add, [Sf, cols, psS], [Sf])
                        S.cp(Sb[:], Sf[:], [Sf], [Sb], e="act")
                    pav = S.psum()
                    S.mm(pav[:, 0:128], attnT[:], vnew[:], True, True, [attnT, vnew], [pav])
                    S.cp(T["av"][:], pav[:, 0:128], [pav], [T["av"]], e="act")
                    S.stt(T["o"][:], pq[:, 0:128], cols[:, 0:1], T["av"][:], ALU.mult, ALU.add, [pq, cols, T["av"]], [T["o"]])
                    if d == 0:
                        S.cp(of[:, tile, :], T["o"][:], [T["o"]], [of], e="pool")
                    else:
                        S.cp(of1v[:, tile, :], T["o"][:], [T["o"]], [cv], e="pool")

            S.barrier()
            for _d in range(2):
                S.memset(PV[_d]["W2"][:], 0.0, [PV[_d]["W2"]])
                S.memset(PV[_d]["Q2"][:], 0.0, [PV[_d]["Q2"]])
            S.run_interleaved([lambda: run_dir(0), lambda: run_dir(1)])
            for tile in range(18 if keep_ctx else 16):
                o0 = tile * 128
                tsl = slice(o0, o0 + 128)
                T = TR
                S.tt(T["os"][:], of1v[:, tile, :], of[:, tile, :], ALU.add, [cv, of], [T["os"]])
                S.act(T["ysq"][:], T["os"][:], AF.Square, [T["os"]], [T["ysq"], st], accum_out=st[:, 0:1])
                S.act(st[:, 1:2], st[:, 0:1], AF.Sqrt, [st, eps1], [st], bias=eps1[:, 0:1], scale=1.0 / 128)
                S.recip(st[:, 2:3], st[:, 1:2], [st], [st])
                S.tt(T["nwg"][:], nwbc[:], ztm[:, tile, :], ALU.mult, [nwbc, ztm], [T["nwg"]], e="pool")
                S.stt(yb[:], T["os"][:], st[:, 2:3], T["nwg"][:], ALU.mult, ALU.mult, [T["os"], st, T["nwg"]], [yb])
                pY = S.psum()
                pYb = pY[:].bitcast(BF16)
                S.tr(pYb[:, 0:128], yb[:], self.ident_b[:], [yb, self.ident_b], [pY])
                S.cp(zT[:, tsl], pYb[:, 0:128], [pY], [zT], e="act")
            self.out_proj_head(zT, A["gd_w_out"], h, wo, keep_ctx)
        S.pop()

    def mixer_hgrn2(self, layer, keep_ctx):
        assert not keep_ctx, "only the last layer uses HGRN2 here (no context outputs needed)"
        S = self.S
        A = self.A
        W = A["hg_w_in"].rearrange("(c p) n -> p c n", p=128)
        S.push()
        ZT = [S.sb([128, 2304], BF16, "ZT") for _ in range(8)]
        S.push()
        H = self.hnorm()
        lbr = S.sb([128, DEPTH, NCH], F32, "lbr")
        for i in range(DEPTH):
            S.dma("sp", lbr[:, i, :], A["hg_lb"][i].rearrange("(c p) -> p c", p=128), writes=[lbr])
        mx = S.sb([128, NCH], F32, "lbmx")
        S.tt(mx[:], lbr[:, 0, :], lbr[:, 1, :], ALU.max, [lbr], [mx])
        for i in range(2, DEPTH):
            S.tt(mx[:], mx[:], lbr[:, i, :], ALU.max, [mx, lbr], [mx])
        for i in range(DEPTH):
            S.tt(lbr[:, i, :], lbr[:, i, :], mx[:], ALU.subtract, [lbr, mx], [lbr])
        S.act(lbr[:], lbr[:], AF.Exp, [lbr], [lbr])
        den = S.sb([128, NCH], F32, "lbden")
        num = S.sb([128, NCH], F32, "lbnum")
        S.tt(den[:], lbr[:, 0, :], lbr[:, 1, :], ALU.add, [lbr], [den])
        for i in range(2, DEPTH):
            S.tt(den[:], den[:], lbr[:, i, :], ALU.add, [den, lbr], [den])
        S.cp(num[:], lbr[:, 1, :], [lbr], [num])
        for i in range(2, layer + 1):
            S.tt(num[:], num[:], lbr[:, i, :], ALU.add, [num, lbr], [num])
        S.recip(den[:], den[:], [den], [den])
        lb = S.sb([128, NCH], F32, "lb")
        oml = S.sb([128, NCH], F32, "oml")
        S.tt(lb[:], num[:], den[:], ALU.mult, [num, den], [lb])
        S.ts(oml[:], lb[:], -1.0, 1.0, ALU.mult, ALU.add, [lb], [oml])
        nwbc = S.sb([128, 128], F32, "nwbc")
        S.dma("sp", nwbc[:], A["hg_norm_w"][0].partition_broadcast(128), writes=[nwbc])
        creset = S.sb([128, 128], F32, "creset")
        S.dma("sp", creset[:], A["chunk_reset"], writes=[creset])
        tri = [S.sb([128, 128], BF16, "tri") for _ in range(2)]
        S.dma("pool", tri[0][:], A["tri_fwd"], writes=[tri[0]])
        S.dma("pool", tri[1][:], A["tri_bwd"], writes=[tri[1]])
        eps1 = self.epsb
        W5 = S.sb([128, NCH, 5, 128], BF16, "W5")
        qT = S.sb([128, TL], BF16, "qT")
        logf = S.sb([128, 2304], F32, "logf")
        kf = S.sb([128, 2304], BF16, "kf")
        vtm = S.sb([128, 18, 128], BF16, "vtm")
        gtm = S.sb([128, 16, 128], BF16, "gtm")
        of = S.sb([128, 16, 128], F32, "of")
        sgt = S.sb([128, 512], F32, "sgt")
        Sf = S.sb([128, 128], F32, "Sf")
        Sb = S.sb([128, 128], BF16, "Sb")
        K2 = S.sb([128, 2, 128], BF16, "K2")
        Q2 = S.sb([128, 2, 128], BF16, "Q2")
        S.memset(K2[:], 0.0, [K2])
        S.memset(Q2[:], 0.0, [Q2])
        T = {nm: S.sb([128, 128], F32, nm) for nm in ("Gi", "G", "df", "dl", "Eq", "Ek", "Ed", "EG", "at", "os", "nwg", "ysq")}
        qg = S.sb([128, 128], BF16, "qg")
        kdT = S.sb([128, 128], BF16, "kdT")
        kdtm = S.sb([128, 128], BF16, "kdtm")
        attnT = S.sb([128, 128], BF16, "attnT")
        yb = S.sb([128, 128], BF16, "yb")
        st = S.sb([128, 4], F32, "hgst")

        def strided_halves(buf):
            return [buf[:, 0, 0:64], buf[:, 1, 64:128]]

        for h in range(8):
            for k in range(5):
                S.dma("pool", W5[:, :, k, :], W[:, :, k * D + h * 128:k * D + (h + 1) * 128], writes=[W5])
            for (grp, o, n) in self.tok_groups(False):
                ps = S.psum()
                for c in range(NCH):
                    S.mm(ps[:, 0:n], W5[:, c, 0, :], H[c][:, o:o + n], c == 0, c == NCH - 1, [W5, H[c]], [ps])
                S.act(qT[:, o:o + n], ps[:, 0:n], AF.Silu, [ps], [qT])
            for t in range(18):
                ps = S.psum()
                for c in range(NCH):
                    S.mm(ps[:, 0:128], H[c][:, t * 128:(t + 1) * 128], W5[:, c, 3, :], c == 0, c == NCH - 1, [H[c], W5], [ps])
                S.cp(vtm[:, t, :], ps[:, 0:128], [ps], [vtm], e=("act" if t % 2 else "dve"))
                if t < 16:
                    ps2 = S.psum()
                    for c in range(NCH):
                        S.mm(ps2[:, 0:128], H[c][:, t * 128:(t + 1) * 128], W5[:, c, 4, :], c == 0, c == NCH - 1,
                             [H[c], W5], [ps2])
                    S.act(gtm[:, t, :], ps2[:, 0:128], AF.Silu, [ps2], [gtm])
            for d in range(2):
                for (grp, o, n) in self.tok_groups(True):
                    ps = S.psum()
                    for c in range(NCH):
                        S.mm(ps[:, 0:n], W5[:, c, 1 + d, :], H[c][:, o:o + n], c == 0, c == NCH - 1, [W5, H[c]], [ps])
                    S.act(sgt[:, 0:n], ps[:, 0:n], AF.Sigmoid, [ps], [sgt])
                    S.ts(sgt[:, 0:n], sgt[:, 0:n], oml[:, h:h + 1], lb[:, h:h + 1], ALU.mult, ALU.add, [sgt, oml, lb], [sgt])
                    S.act(logf[:, o:o + n], sgt[:, 0:n], AF.Ln, [sgt], [logf])
                    S.ts(kf[:, o:o + n], sgt[:, 0:n], -1.0, 1.0, ALU.mult, ALU.add, [sgt], [kf])
                S.memset(Sf[:], 0.0, [Sf])
                S.memset(Sb[:], 0.0, [Sb])
                order = [16, 17] + list(range(16)) if d == 0 else [17, 16] + list(range(15, -1, -1))
                for tile in order:
                    o0 = tile * 128
                    lf = logf[:, o0:o0 + 128]
                    kk = kf[:, o0:o0 + 128]
                    S.scan(T["Gi"][:], creset[:], lf, 0.0, ALU.mult, ALU.add, [creset, logf], [T["Gi"]])
                    if d == 0:
                        G = T["Gi"]
                    else:
                        G = T["G"]
                        S.tt(G[:], lf, T["Gi"][:], ALU.subtract, [logf, T["Gi"]], [G])
                        for ci in range(2):
                            cs = slice(64 * ci, 64 * ci + 64)
                            S.ts(G[:, cs], G[:, cs], T["Gi"][:, 64 * ci + 63:64 * ci + 64], None, ALU.add, None,
                                 [G, T["Gi"]], [G])
                    for ci in range(2):
                        cs = slice(64 * ci, 64 * ci + 64)
                        mid = 64 * ci + 32
                        last = 64 * ci + (63 if d == 0 else 0)
                        S.ts(T["df"][:, cs], G[:, cs], G[:, mid:mid + 1], None, ALU.subtract, None, [G], [T["df"]])
                        S.ts(T["dl"][:, cs], G[:, cs], G[:, last:last + 1], None, ALU.subtract, None, [G], [T["dl"]])
                    S.act(T["Eq"][:], T["df"][:], AF.Exp, [T["df"]], [T["Eq"]])
                    S.act(T["Ek"][:], T["df"][:], AF.Exp, [T["df"]], [T["Ek"]], scale=-1.0)
                    S.act(T["Ed"][:], T["dl"][:], AF.Exp, [T["dl"]], [T["Ed"]], scale=-1.0)
                    S.act(T["EG"][:], G[:], AF.Exp, [G], [T["EG"]])
                    islat = tile < 16
                    if islat:
                        qq = qT[:, o0:o0 + 128]
                        S.stt(qg[:], T["Eq"][:], 1e30, qq, ALU.min, ALU.mult, [T["Eq"], qT], [qg])
                        for ci, dst in enumerate(strided_halves(Q2)):
                            cs = slice(64 * ci, 64 * ci + 64)
                            S.tt(dst, T["EG"][:, cs], qT[:, o0 + 64 * ci:o0 + 64 * ci + 64], ALU.mult, [T["EG"], qT], [Q2])
                        for ci, dst in enumerate(strided_halves(K2)):
                            cs = slice(64 * ci, 64 * ci + 64)
                            S.stt(dst, T["Ek"][:, cs], 1e30, kk[:, cs], ALU.min, ALU.mult, [T["Ek"], kf], [K2])
                    S.tt(kdT[:], T["Ed"][:], kk, ALU.mult, [T["Ed"], kf], [kdT])
                    pT = S.psum()
                    pTb = pT[:].bitcast(BF16)
                    S.tr(pTb[:, 0:128], kdT[:], self.ident_b[:], [kdT, self.ident_b], [pT])
                    S.cp(kdtm[:], pTb[:, 0:128], [pT], [kdtm], e="act")
                    if islat:
                        psA = S.psum()
                        for ci in range(2):
                            cs = slice(64 * ci, 64 * ci + 64)
                            S.mm(psA[:, cs], K2[:, ci, :], qg[:, cs], True, True, [K2, qg], [psA])
                        S.ts(T["at"][:], psA[:, 0:128], 1e30, -1e30, ALU.min, ALU.max, [psA], [T["at"]])
                        S.tt(attnT[:], T["at"][:], tri[d][:], ALU.mult, [T["at"], tri[d]], [attnT])
                        po = S.psum()
                        S.mm(po[:, 0:128], attnT[:], vtm[:, tile, :], True, False, [attnT, vtm], [po])
                    corder = (0, 1) if d == 0 else (1, 0)
                    for n_, ci in enumerate(corder):
                        pr = slice(64 * ci, 64 * ci + 64)
                        last = 64 * ci + (63 if d == 0 else 0)
                        if islat:
                            S.mm(po[:, 0:128], Q2[:, ci, :], Sb[:], False, n_ == 1, [Q2, Sb], [po])
                        psS = S.psum()
                        S.mm(psS[:, 0:128], kdtm[pr, :], vtm[pr, tile, :], True, True, [kdtm, vtm], [psS])
                        S.stt(Sf[:], Sf[:], T["EG"][:, last:last + 1], psS[:, 0:128], ALU.mult, ALU.add,
                              [Sf, T["EG"], psS], [Sf])
                        S.cp(Sb[:], Sf[:], [Sf], [Sb], e="act")
                    if not islat:
                        continue
                    if d == 0:
                        S.cp(of[:, tile, :], po[:, 0:128], [po], [of])
                        continue
                    S.tt(T["os"][:], po[:, 0:128], of[:, tile, :], ALU.add, [po, of], [T["os"]])
                    S.act(T["ysq"][:], T["os"][:], AF.Square, [T["os"]], [T["ysq"], st], accum_out=st[:, 0:1])
                    S.act(st[:, 1:2], st[:, 0:1], AF.Sqrt, [st, eps1], [st], bias=eps1[:, 0:1], scale=1.0 / 128)
                    S.recip(st[:, 2:3], st[:, 1:2], [st], [st])
                    S.tt(T["nwg"][:], nwbc[:], gtm[:, tile, :], ALU.mult, [nwbc, gtm], [T["nwg"]], e="pool")
                    S.stt(yb[:], T["os"][:], st[:, 2:3], T["nwg"][:], ALU.mult, ALU.mult, [T["os"], st, T["nwg"]], [yb])
                    pY = S.psum()
                    pYb = pY[:].bitcast(BF16)
                    S.tr(pYb[:, 0:128], yb[:], self.ident_b[:], [yb, self.ident_b], [pY])
                    S.cp(ZT[h][:, o0:o0 + 128], pYb[:, 0:128], [pY], [ZT[h]], e="act")
        S.pop()
        self.out_proj(ZT, A["hg_w_out"], False)
        S.pop()

    def mixer_swa(self, keep_ctx):
        S = self.S
        A = self.A
        W = A["sw_w_in"].rearrange("(c p) n -> p c n", p=128)
        S.push()
        Q = [S.sb([128, 2304], BF16, "Q") for _ in range(8)]
        KD = [S.sb([128, 2304], BF16, "KD") for _ in range(4)]
        V1 = S.sb([128, 18, 4, 65], BF16, "V1")
        S.push()
        H = self.hnorm()
        cosb = S.sb([128, TL], BF16, "cosb")
        sinb = S.sb([128, TL], BF16, "sinb")
        S.dma("pool", cosb[:], A["rope_cos"], writes=[cosb])
        S.dma("pool", sinb[:], A["rope_sin"], writes=[sinb])
        wj = [S.sb([128, NCH, 128], BF16, "wj") for _ in range(2)]
        wjs = [S.sb([128, NCH, 128], BF16, "wjs") for _ in range(2)]
        t1 = S.sb([128, 512], F32, "t1")
        t2 = S.sb([128, 512], F32, "t2")
        S.memset(V1[:, :, :, 64:65], 1.0, [V1])

        def swapped(dst, src):
            d5 = dst[:].rearrange("p c (h two i) -> p c h two i", two=2, i=32)
            s5 = src[:].rearrange("p c (h two i) -> p c h two i", two=2, i=32)
            for c in range(NCH):
                S.cp(d5[:, c, :, 0, :], s5[:, c, :, 1, :], [src], [dst], e="pool")
                S.cp(d5[:, c, :, 1, :], s5[:, c, :, 0, :], [src], [dst], e="pool")

        def project_roped(dst, w, ws):
            for (grp, o, n) in self.tok_groups(True):
                psq = S.psum()
                for c in range(NCH):
                    S.mm(psq[:, 0:n], w[:, c, :], H[c][:, o:o + n], c == 0, c == NCH - 1, [w, H[c]], [psq])
                if grp[0] == "c":
                    S.cp(dst[:, o:o + n], psq[:, 0:n], [psq], [dst], e="act")
                    continue
                pss = S.psum()
                for c in range(NCH):
                    S.mm(pss[:, 0:n], ws[:, c, :], H[c][:, o:o + n], c == 0, c == NCH - 1, [ws, H[c]], [pss])
                S.tt(t1[:, 0:n], psq[:, 0:n], cosb[:, o:o + n], ALU.mult, [psq, cosb], [t1])
                S.tt(t2[:, 0:n], pss[:, 0:n], sinb[:, o:o + n], ALU.mult, [pss, sinb], [t2])
                S.tt(dst[:, o:o + n], t1[:, 0:n], t2[:, 0:n], ALU.add, [t1, t2], [dst], e="pool")

        k = 0
        stage = self.cfg.get("swa_stage", 9)
        for j in range(8 if stage >= 1 else 0):
            w, ws = wj[k % 2], wjs[k % 2]
            k += 1
            S.dma("pool", w[:], W[:, :, j * 128:(j + 1) * 128], writes=[w])
            swapped(ws, w)
            project_roped(Q[j], w, ws)
        for g in range(4 if stage >= 2 else 0):
            w, ws = wj[k % 2], wjs[k % 2]
            k += 1
            for half in range(2):
                S.dma("pool", w[:, :, half * 64:(half + 1) * 64], W[:, :, 1024 + g * 64:1024 + (g + 1) * 64], writes=[w])
            swapped(ws, w)
            project_roped(KD[g], w, ws)
        wv = S.sb([128, NCH, 256], BF16, "wv")
        S.dma("pool", wv[:], W[:, :, 1280:1536], writes=[wv])
        for t in range(18 if stage >= 3 else 0):
            ps = S.psum()
            for c in range(NCH):
                S.mm(ps[:, 0:256], H[c][:, t * 128:(t + 1) * 128], wv[:, c, :], c == 0, c == NCH - 1, [H[c], wv], [ps])
            S.cp(V1[:, t, :, 0:64], ps[:, 0:256].rearrange("p (g d) -> p g d", d=64), [ps], [V1],
                 e=("act" if t % 2 else "dve"))
        S.pop()

        S.push()
        OT = [S.sb([128, 2304], BF16, "OT") for _ in range(8)]
        sinkE = S.sb([128, 16], F32, "sinkE")
        S.dma("sp", sinkE[:], A["sw_sink"][0].partition_broadcast(128), writes=[sinkE])
        S.act(sinkE[:], sinkE[:], AF.Exp, [sinkE], [sinkE])
        mP = S.sb([128, 256], BF16, "mP")
        mN = S.sb([128, 256], BF16, "mN")
        S.dma("pool", mP[:], A["mask_prev"], writes=[mP])
        S.dma("pool", mN[:], A["mask_next"], writes=[mN])
        PT = [S.sb([128, 2, 8, 128], BF16, "PT") for _ in range(2)]
        ob = [S.sb([128, 128], BF16, "ob") for _ in range(2)]
        den = S.sb([128, 4], F32, "den")
        nblk = 18 if keep_ctx else 16
        it = 0
        for j in range(8 if stage >= 4 else 0):
            g = j // 2
            for blk in range(nblk):
                qo = blk * 128
                if blk < 16:
                    tiles = [(16, None), (17, None)]
                    if blk > 0:
                        tiles.append((blk - 1, mP))
                    tiles.append((blk, None))
                    if blk < 15:
                        tiles.append((blk + 1, mN))
                else:
                    tiles = [(16, None), (17, None)]
                nt = len(tiles)
                pt = PT[it % 2]
                o2 = ob[it % 2]
                it += 1
                nb = (nt + 3) // 4
                pss = [[S.psum() for _ in range(nb)] for hh in range(2)]
                for hh in range(2):
                    pr = slice(hh * 64, (hh + 1) * 64)
                    for k2, (kt, msk) in enumerate(tiles):
                        ps = pss[hh][k2 // 4]
                        co = (k2 % 4) * 128
                        S.mm(ps[:, co:co + 128], KD[g][pr, kt * 128:(kt + 1) * 128], Q[j][pr, qo:qo + 128], True, True,
                             [KD[g], Q[j]], [ps])
                for hh in range(2):
                    for b in range(nb):
                        w_ = min(4, nt - 4 * b) * 128
                        S.act(pt[:, hh, 4 * b:4 * b + 4, :].rearrange("p a b -> p (a b)")[:, 0:w_], pss[hh][b][:, 0:w_],
                              AF.Exp, [pss[hh][b]], [pt], scale=0.125)
                for k2, (kt, msk) in enumerate(tiles):
                    if msk is not None:
                        for hh in range(2):
                            S.tt(pt[:, hh, k2, :], pt[:, hh, k2, :], msk[:, 0:128], ALU.mult, [pt, msk], [pt], e="pool")
                po = S.psum()
                for hh in range(2):
                    for k2, (kt, msk) in enumerate(tiles):
                        S.mm(po[:, hh * 65:(hh + 1) * 65], pt[:, hh, k2, :], V1[:, kt, g, :],
                             k2 == 0, k2 == nt - 1, [pt, V1], [po])
                for hh in range(2):
                    h = 2 * j + hh
                    S.tt(den[:, hh:hh + 1], po[:, hh * 65 + 64:hh * 65 + 65], sinkE[:, h:h + 1], ALU.add,
                         [po, sinkE], [den])
                S.recip(den[:, 2:4], den[:, 0:2], [den], [den])
                for hh in range(2):
                    S.ts(o2[:, hh * 64:(hh + 1) * 64], po[:, hh * 65:hh * 65 + 64], den[:, 2 + hh:3 + hh], None,
                         ALU.mult, None, [po, den], [o2])
                pT = S.psum()
                pTb = pT[:].bitcast(BF16)
                S.tr(pTb[:, 0:128], o2[:], self.ident_b[:], [o2, self.ident_b], [pT])
                S.cp(OT[j][:, qo:qo + 128], pTb[:, 0:128], [pT], [OT[j]], e="act")
        if stage >= 5:
            self.out_proj(OT, A["sw_w_out"], keep_ctx)
        S.pop()
        S.pop()


def make_in_maps(inputs, consts, n_cores=8):
    f = lambda a: np.ascontiguousarray(np.asarray(a, dtype=np.float32))
    shared = {
        "ada_w": f(inputs["ada_w"]).reshape(DEPTH * D, 6 * D),
        "ada_b": f(inputs["ada_b"]),
        "norm1_w": f(inputs["norm1_w"]),
        "norm2_w": f(inputs["norm2_w"]),
        "final_norm_w": f(inputs["final_norm_w"]).reshape(1, D),
        "moe_router": f(inputs["moe_router"]).reshape(DEPTH * D, NE),
        "moe_w_gate": f(inputs["moe_w_gate"]).reshape(DEPTH * NE * D, FF),
        "moe_w_up": f(inputs["moe_w_up"]).reshape(DEPTH * NE * D, FF),
        "moe_w_down": f(inputs["moe_w_down"]).reshape(DEPTH * NE * FF, D),
        "hy_w_in": f(inputs["hy_w_in"]), "hy_b_in": f(inputs["hy_b_in"]).reshape(1, 3 * D),
        "hy_short_w": f(inputs["hy_short_w"]), "hy_short_b": f(inputs["hy_short_b"]).reshape(1, 3 * D),
        "hy_ffn_w1": f(inputs["hy_ffn_w1"]), "hy_ffn_b1": f(inputs["hy_ffn_b1"]).reshape(1, 64),
        "hy_ffn_w2": f(inputs["hy_ffn_w2"]), "hy_ffn_b2": f(inputs["hy_ffn_b2"]).reshape(1, 64),
        "hy_ffn_w3": f(inputs["hy_ffn_w3"]), "hy_sin_freq": f(inputs["hy_sin_freq"]),
        "hy_filter_bias": f(inputs["hy_filter_bias"]), "hy_w_out": f(inputs["hy_w_out"]),
        "hy_b_out": f(inputs["hy_b_out"]).reshape(1, D),
        "gd_w_in": f(inputs["gd_w_in"]),
        "gd_conv_w": f(inputs["gd_conv_w"]),
        "gd_a_log": f(inputs["gd_a_log"]).reshape(1, 16),
        "gd_dt_bias": f(inputs["gd_dt_bias"]).reshape(1, 16),
        "gd_norm_w": f(inputs["gd_norm_w"]).reshape(1, 128),
        "gd_w_out": f(inputs["gd_w_out"]),
        "hg_w_in": f(inputs["hg_w_in"]),
        "hg_lb": f(inputs["hg_lb"]),
        "hg_norm_w": f(inputs["hg_norm_w"]).reshape(1, 128),
        "hg_w_out": f(inputs["hg_w_out"]),
        "sw_w_in": f(inputs["sw_w_in"]),
        "sw_sink": f(inputs["sw_sink"]).reshape(1, 16),
        "sw_w_out": f(inputs["sw_w_out"]),
    }
    for k, v in consts.items():
        shared["k_" + k] = v
    maps = []
    for b in range(n_cores):
        mp = dict(shared)
        mp["x"] = f(inputs["x"][b])
        mp["ctx"] = f(inputs["ctx"][b])
        mp["cc"] = np.stack([f(inputs["c"][b]), f(inputs["c_ctx"])], 0)
        maps.append(mp)
    return maps


def kernel(**inputs):
    p = Prog({})
    nc = p.build()
    maps = make_in_maps(inputs, p.consts, 8)
    res = run_bass_kernel_spmd(nc, maps, core_ids=list(range(8)))
    return np.stack([r["out"] for r in res.results], 0).astype(np.float32)
```

```python
import numpy as np
from contextlib import ExitStack
import concourse.bass as bass
import concourse.mybir as mybir
from concourse.bass_utils import run_bass_kernel_spmd

F32 = mybir.dt.float32
BF16 = mybir.dt.bfloat16
I32 = mybir.dt.int32
AF = mybir.ActivationFunctionType
ALU = mybir.AluOpType
AX = mybir.AxisListType

D = 1024
NCH = 8
TL = 2048
TC = 256
DEPTH = 4
NE = 16
FF = 1024
EPS = 1e-6
NDS = 12


class Buf:
    __slots__ = ("t", "w", "r", "name", "psum")

    def __init__(self, t, name=""):
        self.t = t
        self.w = None
        self.r = {}
        self.name = name
        self.psum = False

    def __getitem__(self, idx):
        return self.t[idx]


class Sched:
    def __init__(self, nc):
        self.nc = nc
        self.eng = {"pe": nc.tensor, "act": nc.scalar, "dve": nc.vector, "pool": nc.gpsimd, "sp": nc.sync}
        self.semstack = ExitStack()
        self.csem = {e: self.semstack.enter_context(nc.semaphore("s_" + e)) for e in ("pe", "act", "dve", "pool")}
        self.prog = {e: [] for e in self.eng}
        self.ccnt = {e: 0 for e in self.csem}
        self.seen = {e: {} for e in self.eng}
        self.dq = {}
        for q in ("sp", "pool"):
            self.dq[q] = {"sems": [self.semstack.enter_context(nc.semaphore("d_%s%d" % (q, i))) for i in range(NDS)],
                          "vals": [0] * NDS, "rr": 0}
        self.nbuf = 0
        import threading
        self._coop = None
        self._tls = threading.local()
        self.stacks = []
        self.arena = None
        self.ps = [self.psum_raw("psb%d" % i) for i in range(8)]
        self.ps_rr = 0
        self.n_inst = 0

    def sb(self, shape, dt=F32, name=None):
        self.nbuf += 1
        nm = "%s_%d" % (name or "b", self.nbuf)
        if self.arena is None:
            self.arena_words = 212800 // 4
            self.arena = self.nc.alloc_sbuf_tensor("arena", [128, self.arena_words], F32)
            self.top = 0
        esz = 2 if dt == BF16 else 4
        n = 1
        for d_ in shape[1:]:
            n *= d_
        words = (n * esz + 3) // 4
        words = (words + 7) // 8 * 8
        assert self.top + words <= self.arena_words, "SBUF arena overflow allocating %s %s (top=%d)" % (nm, shape, self.top * 4)
        ap = self.arena[0:shape[0], self.top:self.top + words]
        self.top += words
        if dt != F32:
            ap = ap.bitcast(dt)
        ap = ap[:, 0:n]
        if len(shape) == 3:
            ap = ap.rearrange("p (a b) -> p a b", b=shape[2])
        elif len(shape) == 4:
            ap = ap.rearrange("p (a b c) -> p a b c", b=shape[2], c=shape[3])
        return Buf(ap, nm)

    def push(self):
        self.stacks.append(self.top)

    def pop(self):
        self.barrier()
        self.top = self.stacks.pop()

    def barrier(self):
        evs = [(self.csem[e], self.ccnt[e], e) for e in self.csem if self.ccnt[e] > 0]
        for q in self.dq.values():
            for sem, v in zip(q["sems"], q["vals"]):
                if v > 0:
                    evs.append((sem, v, "dma"))
        for e in self.eng:
            for ev in evs:
                if e == "pe" and ev[2] == "pe":
                    continue
                self._wait(e, ev)

    def psum_raw(self, name):
        b = Buf(self.nc.alloc_psum_tensor(name, [128, 512], F32), name)
        b.psum = True
        return b

    def psum(self):
        co = self._coop
        if co is not None:
            i = self._tls.idx
            b = self.ps[4 * i + co["rr"][i]]
            co["rr"][i] = (co["rr"][i] + 1) % 4
            return b
        b = self.ps[self.ps_rr]
        self.ps_rr = (self.ps_rr + 1) % 8
        return b

    def run_interleaved(self, fns):
        import threading
        assert len(fns) == 2
        cv = threading.Condition()
        co = {"turn": 0, "alive": [True, True], "rr": [0, 0], "cv": cv, "err": []}
        self._coop = co

        def body(i, fn):
            self._tls.idx = i
            with cv:
                while co["turn"] != i:
                    cv.wait()
            try:
                fn()
            except BaseException as ex:
                co["err"].append(ex)
            with cv:
                co["alive"][i] = False
                co["turn"] = 1 - i
                cv.notify_all()

        th = [threading.Thread(target=body, args=(i, f)) for i, f in enumerate(fns)]
        for t in th:
            t.start()
        for t in th:
            t.join()
        self._coop = None
        if co["err"]:
            raise co["err"][0]

    def _yield(self):
        co = self._coop
        if co is None:
            return
        i = self._tls.idx
        if not co["alive"][1 - i]:
            return
        cv = co["cv"]
        with cv:
            co["turn"] = 1 - i
            cv.notify_all()
            while co["turn"] != i and co["alive"][1 - i]:
                cv.wait()

    def _wait(self, e, ev):
        sem, val, src = ev
        k = id(sem)
        if self.seen[e].get(k, 0) < val:
            self.prog[e].append(("w", sem, val))
            self.seen[e][k] = val

    def _deps(self, e, reads, writes):
        for b in reads:
            if b.w is not None and not (e == "pe" and b.w[2] == "pe"):
                self._wait(e, b.w)
            if b.psum:
                for ev in b.r.values():
                    if ev[2] != e:
                        self._wait(e, ev)
        for b in writes:
            if b.w is not None and not (e == "pe" and b.w[2] == "pe"):
                self._wait(e, b.w)
            for ev in b.r.values():
                if not (e == "pe" and ev[2] == "pe"):
                    self._wait(e, ev)

    def _mark(self, ev, reads, writes):
        k = id(ev[0])
        for b in reads:
            b.r[k] = ev
        for b in writes:
            b.w = ev
            b.r = {}

    def op(self, e, fn, reads=(), writes=()):
        self._deps(e, reads, writes)
        self.ccnt[e] += 1
        self.prog[e].append(("i", fn, self.csem[e], 1))
        self._mark((self.csem[e], self.ccnt[e], e), reads, writes)
        self.n_inst += 1
        self._yield()

    def dma(self, q, out, in_, reads=(), writes=(), **kw):
        d = self.dq[q]
        i = d["rr"]
        d["rr"] = (i + 1) % NDS
        sem = d["sems"][i]
        if d["vals"][i] > 0:
            self._wait(q, (sem, d["vals"][i], "dma"))
        self._deps(q, reads, writes)
        self.prog[q].append(("i", lambda g: g.dma_start(out=out, in_=in_, allow_slow_non_contiguous=True, **kw), sem, 16))
        d["vals"][i] += 16
        ev = (sem, d["vals"][i], "dma")
        self._mark(ev, reads, writes)
        self.n_inst += 1
        return ev

    def emit(self):
        prog = self.prog

        def replay(name, eng):
            for it in prog[name]:
                if it[0] == "w":
                    eng.wait_ge(it[1], it[2])
                else:
                    it[1](eng).then_inc(it[2], it[3])

        with self.nc.Block() as block:
            @block.sync
            def _(eng):
                replay("sp", eng)

            @block.tensor
            def _(eng):
                replay("pe", eng)

            @block.scalar
            def _(eng):
                replay("act", eng)

            @block.vector
            def _(eng):
                replay("dve", eng)

            @block.gpsimd
            def _(eng):
                replay("pool", eng)

    def mm(self, out, lhsT, rhs, start, stop, reads, writes, **kw):
        self.op("pe", lambda e: e.matmul(out, lhsT, rhs, start=start, stop=stop, **kw), reads, writes)

    def tr(self, out, in_, ident, reads, writes):
        self.op("pe", lambda e: e.transpose(out, in_, ident), reads, writes)

    def trf(self, out, in_, ident, reads, writes):
        self.op("pe", lambda e: e.matmul(out, in_, ident, start=True, stop=True), reads, writes)

    def act(self, out, in_, func, reads, writes, bias=None, scale=None, accum_out=None):
        kw = {}
        if bias is not None:
            kw["bias"] = bias
        if scale is not None:
            kw["scale"] = scale
        if accum_out is not None:
            kw["accum_out"] = accum_out
        self.op("act", lambda e: e.activation(out, in_, func, **kw), reads, writes)

    def ts(self, out, in0, s1, s2, op0, op1, reads, writes, e="dve", accum_out=None):
        if op1 is None:
            self.op(e, lambda g: g.tensor_scalar(out, in0, s1, None, op0, accum_out=accum_out) if accum_out is not None
                    else g.tensor_scalar(out, in0, s1, None, op0), reads, writes)
        else:
            self.op(e, lambda g: g.tensor_scalar(out, in0, s1, s2, op0, op1, accum_out=accum_out) if accum_out is not None
                    else g.tensor_scalar(out, in0, s1, s2, op0, op1), reads, writes)

    def tt(self, out, in0, in1, op, reads, writes, e="dve"):
        self.op(e, lambda g: g.tensor_tensor(out, in0, in1, op), reads, writes)

    def stt(self, out, in0, scalar, in1, op0, op1, reads, writes):
        self.op("dve", lambda g: g.scalar_tensor_tensor(out, in0, scalar, in1, op0, op1), reads, writes)

    def recip(self, out, in_, reads, writes):
        self.op("dve", lambda g: g.reciprocal(out, in_), reads, writes)

    def red(self, out, in_, op, reads, writes, negate=False):
        self.op("dve", lambda g: g.tensor_reduce(out, in_, AX.X, op, negate=negate), reads, writes)

    def max8(self, out, in_, reads, writes):
        self.op("dve", lambda g: g.max(out, in_), reads, writes)

    def mrep(self, out, in_to_replace, in_values, imm, reads, writes):
        self.op("dve", lambda g: g.match_replace(out, in_to_replace, in_values, imm), reads, writes)

    def scan(self, out, d0, d1, init, op0, op1, reads, writes):
        self.op("dve", lambda g: g.tensor_tensor_scan(out, d0, d1, init, op0, op1), reads, writes)

    def memset(self, out, val, writes, e="dve"):
        self.op(e, lambda g: g.memset(out, val), [], writes)

    def cp(self, out, in_, reads, writes, e="dve"):
        if e == "act":
            self.op("act", lambda g: g.activation(out, in_, AF.Identity), reads, writes)
        else:
            self.op(e, lambda g: g.tensor_copy(out, in_), reads, writes)


def host_consts():
    c = {}
    c["ident_f"] = np.eye(128, dtype=np.float32)
    c["ones_f"] = np.ones((128, 128), np.float32)
    io = np.zeros((128, 288), np.float32)
    io[:] = np.arange(288, dtype=np.float32)[None, :]
    c["iota_row"] = io
    ip = np.zeros((128, 4), np.float32)
    for k in range(4):
        ip[:, k] = np.arange(128) + 128 * k
    c["iota_part"] = ip
    sel = np.zeros((16, 16, 128), np.float32)
    for e in range(16):
        sel[e, e, :] = 1.0
    c["sel"] = sel.reshape(16, 16 * 128)
    t = np.arange(TL)
    row = (t // 64).astype(np.float32)
    col = (t % 64).astype(np.float32)
    inv = (10000.0 ** (-np.arange(16, dtype=np.float32) / 16)).astype(np.float32)
    ang = np.concatenate([row[:, None] * inv, col[:, None] * inv], -1).astype(np.float32)
    cosf = np.zeros((128, TL), np.float32)
    sinf = np.zeros((128, TL), np.float32)
    for p in range(128):
        i = p % 64
        cosf[p] = np.cos(ang[:, i % 32])
        sinf[p] = np.sin(ang[:, i % 32]) * (-1.0 if i < 32 else 1.0)
    c["rope_cos"] = cosf
    c["rope_sin"] = sinf
    jj = np.arange(128)[:, None]
    ii = np.arange(128)[None, :]
    mp = (jj >= ii).astype(np.float32)
    mn = (jj <= ii).astype(np.float32)
    cm = np.ones((128, 128), np.float32)
    cm[:, 0] = 0.0
    cm[:, 64] = 0.0
    c["chunk_reset"] = cm
    blk = (jj // 64 == ii // 64)
    c["tri_fwd"] = (blk & (jj <= ii)).astype(np.float32)
    c["tri_bwd"] = (blk & (jj >= ii)).astype(np.float32)
    import ml_dtypes
    for nm, L in (("l", TL), ("c", TC)):
        N = 2 * L
        T_ = L // 128
        tt_ = np.linspace(0.0, 1.0, L, dtype=np.float32)
        w_ = (2.0 * np.pi * np.arange(L, dtype=np.float32) / L).astype(np.float32)
        f_ = np.linspace(1e-4, 15.0, 16, dtype=np.float32)[None, :]
        z_ = np.concatenate([tt_[:, None], np.cos(f_ * w_[:, None]), -np.sin(f_ * w_[:, None])], -1).astype(np.float32)
        c["hy_zT_" + nm] = np.ascontiguousarray(z_.T)
        c["hy_tcol_" + nm] = np.ascontiguousarray(tt_.reshape(T_, 128).T)
        tpos = np.arange(L, dtype=np.float64)
        fr = np.arange(L, dtype=np.float64) + 0.5
        th = 2.0 * np.pi * np.outer(tpos, fr) / N
        for tn, tab in (("C", np.cos(th)), ("S", np.sin(th))):
            fw = tab.reshape(T_, 128, T_, 128).transpose(2, 1, 0, 3)
            c["hy_%sf_%s" % (tn, nm)] = np.ascontiguousarray(fw).reshape(T_ * 128, T_ * 128).astype(ml_dtypes.bfloat16)
            c["hy_%st_%s" % (tn, nm)] = np.ascontiguousarray(tab.T).astype(ml_dtypes.bfloat16)
    maxd = np.log(1e-2) / 0.3
    mind = np.log(1e-2) / 1.5
    c["hy_delta"] = np.abs(np.linspace(mind, maxd, D, dtype=np.float32)).reshape(1, D).astype(np.float32)
    eye = np.eye(128, dtype=np.float32)
    c["tri_fwd_s"] = c["tri_fwd"] - eye
    c["tri_bwd_s"] = c["tri_bwd"] - eye
    c["mask_prev"] = np.concatenate([mp, mp], 1)
    c["mask_next"] = np.concatenate([mn, mn], 1)
    return c


class Prog:
    def __init__(self, cfg):
        self.cfg = cfg
        self.nc = bass.Bass("TRN2", target_bir_lowering=False)
        self.S = Sched(self.nc)
        self.din = {}
        self.consts = host_consts()

    def dram_in(self, name, shape, dt=F32):
        t = self.nc.dram_tensor(name, list(shape), dt, kind="ExternalInput")
        self.din[name] = t
        return t.ap()

    def declare(self):
        nc = self.nc
        A = {}
        A["x"] = self.dram_in("x", [TL, D])
        A["ctx"] = self.dram_in("ctx", [TC, D])
        A["cc"] = self.dram_in("cc", [2, D])
        A["ada_w"] = self.dram_in("ada_w", [DEPTH * D, 6 * D])
        A["ada_b"] = self.dram_in("ada_b", [DEPTH, 6 * D])
        A["norm1_w"] = self.dram_in("norm1_w", [DEPTH, D])
        A["norm2_w"] = self.dram_in("norm2_w", [DEPTH, D])
        A["final_norm_w"] = self.dram_in("final_norm_w", [1, D])
        A["moe_router"] = self.dram_in("moe_router", [DEPTH * D, NE])
        A["moe_w_gate"] = self.dram_in("moe_w_gate", [DEPTH * NE * D, FF])
        A["moe_w_up"] = self.dram_in("moe_w_up", [DEPTH * NE * D, FF])
        A["moe_w_down"] = self.dram_in("moe_w_down", [DEPTH * NE * FF, D])
        A["gd_w_in"] = self.dram_in("gd_w_in", [D, 4128])
        A["gd_conv_w"] = self.dram_in("gd_conv_w", [3, 3072])
        A["gd_a_log"] = self.dram_in("gd_a_log", [1, 16])
        A["gd_dt_bias"] = self.dram_in("gd_dt_bias", [1, 16])
        A["gd_norm_w"] = self.dram_in("gd_norm_w", [1, 128])
        A["gd_w_out"] = self.dram_in("gd_w_out", [D, D])
        A["hg_w_in"] = self.dram_in("hg_w_in", [D, 5 * D])
        A["hg_lb"] = self.dram_in("hg_lb", [DEPTH, D])
        A["hg_norm_w"] = self.dram_in("hg_norm_w", [1, 128])
        A["hg_w_out"] = self.dram_in("hg_w_out", [D, D])
        A["sw_w_in"] = self.dram_in("sw_w_in", [D, 1536])
        A["sw_sink"] = self.dram_in("sw_sink", [1, 16])
        A["sw_w_out"] = self.dram_in("sw_w_out", [D, D])
        for nm, shp in (("hy_w_in", [D, 3 * D]), ("hy_b_in", [1, 3 * D]), ("hy_short_w", [3, 3 * D]),
                        ("hy_short_b", [1, 3 * D]), ("hy_ffn_w1", [33, 64]), ("hy_ffn_b1", [1, 64]),
                        ("hy_ffn_w2", [64, 64]), ("hy_ffn_b2", [1, 64]), ("hy_ffn_w3", [64, 4 * D]),
                        ("hy_sin_freq", [2, 64]), ("hy_filter_bias", [2, D]), ("hy_w_out", [D, D]),
                        ("hy_b_out", [1, D])):
            A[nm] = self.dram_in(nm, shp)
        for k, v in self.consts.items():
            A[k] = self.dram_in("k_" + k, list(v.shape), BF16 if v.dtype != np.float32 else F32)
        self.out = nc.dram_tensor("out", [TL, D], F32, kind="ExternalOutput").ap()
        if self.cfg.get("dump_ctx"):
            self.out_c = nc.dram_tensor("out_c", [TC, D], F32, kind="ExternalOutput").ap()
        self.A = A

    def setup(self):
        S = self.S
        A = self.A
        self.ident_f = S.sb([128, 128], F32, "identf")
        self.ident_b = S.sb([128, 128], BF16, "identb")
        self.ones_b = S.sb([128, 128], BF16, "onesb")
        self.iota_row = S.sb([128, 256], F32, "iotar")
        self.iota_part = S.sb([128, 4], F32, "iotap")
        self.sel = S.sb([16, 16 * 128], BF16, "sel")
        self.epsb = S.sb([128, 1], F32, "eps")
        self.hs_tok = S.sb([1, 8], F32, "hstok")
        self.RL = [[S.sb([128, 512], F32, "RL") for g in range(4)] for c in range(NCH)]
        self.RC = [S.sb([128, 256], F32, "RC") for c in range(NCH)]
        self.sc = S.sb([128, NCH, 2], F32, "sc")
        self.modT = S.sb([128, 48, 2], F32, "modT")
        self.a1 = [S.sb([128, NCH], F32, "a1") for _ in range(2)]
        self.a2 = [S.sb([128, NCH], F32, "a2") for _ in range(2)]
        S.dma("sp", self.ident_f[:], A["ident_f"], writes=[self.ident_f])
        S.dma("sp", self.iota_row[:], A["iota_row"][:, 0:256], writes=[self.iota_row])
        S.dma("sp", self.iota_part[:], A["iota_part"], writes=[self.iota_part])
        S.push()
        tmp = S.sb([128, 128], F32, "onesf")
        tsel = S.sb([16, 16 * 128], F32, "tsel")
        craw = S.sb([128, NCH, 2], F32, "craw")
        S.dma("sp", tsel[:], A["sel"], writes=[tsel])
        S.dma("sp", tmp[:], A["ones_f"], writes=[tmp])
        S.cp(self.sel[:], tsel[:], [tsel], [self.sel])
        S.cp(self.ones_b[:], tmp[:], [tmp], [self.ones_b])
        S.cp(self.ident_b[:], self.ident_f[:], [self.ident_f], [self.ident_b])
        S.memset(self.epsb[:], EPS, [self.epsb])
        with self.nc.allow_non_contiguous_dma("tiny"):
            for j in range(2):
                S.dma("sp", craw[:, :, j], A["cc"][j].rearrange("(c p) -> p c", p=128), writes=[craw])
        S.act(self.sc[:], craw[:], AF.Silu, [craw], [self.sc])
        S.pop()
        self.groups = [("l", g, g * 512, 512) for g in range(4)] + [("c", 0, 0, 256)]

    def dbg(self, name, ap, buf, shape):
        if not self.cfg.get("debug"):
            return
        d = self.nc.dram_tensor("dbg_" + name, list(shape), ap.dtype, kind="ExternalOutput").ap()
        ev = self.S.dma("sp", d, ap, reads=[buf])
        self.S._wait("sp", ev)

    def rbuf(self, grp, c):
        return self.RL[c][grp[1]] if grp[0] == "l" else self.RC[c]

    def load_stream(self):
        S = self.S
        A = self.A
        S.push()
        tin = [S.sb([128, D], F32, "tin") for _ in range(2)]
        k = 0
        for grp in self.groups:
            src = A["x"] if grp[0] == "l" else A["ctx"]
            for tt in range(grp[3] // 128):
                t0 = grp[2] + tt * 128
                tb = tin[k % 2]
                if k >= self.cfg.get("ls_tiles", 99):
                    continue
                k += 1
                S.dma("sp", tb[:], src[t0:t0 + 128, :], writes=[tb])
                for half in range(2):
                    ps = S.psum()
                    for j in range(4):
                        c = half * 4 + j
                        S.trf(ps[:, j * 128:(j + 1) * 128], tb[:, c * 128:(c + 1) * 128], self.ident_f[:],
                             [tb, self.ident_f], [ps])
                    for j in range(4):
                        c = half * 4 + j
                        rb = self.rbuf(grp, c)
                        S.cp(rb[:, tt * 128:(tt + 1) * 128], ps[:, j * 128:(j + 1) * 128], [ps], [rb],
                             e=("act" if j % 2 else "dve"))
        S.pop()

    def adaln(self, layer):
        S = self.S
        A = self.A
        S.push()
        modT = self.modT
        adab = S.sb([128, 48], F32, "adab")
        with self.nc.allow_non_contiguous_dma("tiny"):
            S.dma("sp", adab[:], A["ada_b"][layer].rearrange("(j p) -> p j", p=128), writes=[adab])
        wv = A["ada_w"][layer * D:(layer + 1) * D, :].rearrange("(c p) n -> p c n", p=128)
        wb = [S.sb([128, NCH, 512], F32, "adaw") for _ in range(2)]
        ps = S.psum()
        for piece in range(12):
            w = wb[piece % 2]
            S.dma("sp", w[:], wv[:, :, piece * 512:(piece + 1) * 512], writes=[w])
            for jj in range(4):
                j = piece * 4 + jj
                for c in range(NCH):
                    S.mm(ps[:, 2 * j:2 * j + 2], w[:, c, jj * 128:(jj + 1) * 128], self.sc[:, c, :],
                         c == 0, c == NCH - 1, [w, self.sc], [ps])
        for s in range(2):
            S.tt(modT[:, :, s], ps[:, 0:96].rearrange("p (j s) -> p j s", s=2)[:, :, s], adab[:], ALU.add,
                 [ps, adab], [modT])
        n1 = S.sb([128, NCH], F32, "n1w")
        n2 = S.sb([128, NCH], F32, "n2w")
        with self.nc.allow_non_contiguous_dma("tiny"):
            S.dma("sp", n1[:], A["norm1_w"][layer].rearrange("(c p) -> p c", p=128), writes=[n1])
            S.dma("sp", n2[:], A["norm2_w"][layer].rearrange("(c p) -> p c", p=128), writes=[n2])
        M = {}
        for s, sn in enumerate(("l", "c")):
            S.stt(self.a1[s][:], modT[:, 8:16, s], 1.0, n1[:], ALU.add, ALU.mult, [modT, n1], [self.a1[s]])
            S.stt(self.a2[s][:], modT[:, 32:40, s], 1.0, n2[:], ALU.add, ALU.mult, [modT, n2], [self.a2[s]])
            M[sn] = {"a1": self.a1[s], "a2": self.a2[s], "modT": modT, "s": s}
        self.M = M
        S.pop()
        return M

    def mvec(self, sn, which, c):
        m = self.M[sn]
        return m["modT"][:, which * 8 + c, m["s"]:m["s"] + 1]

    def norm_scratch(self):
        S = self.S
        self._sq = S.sb([128, NCH, 512], BF16, "sq")
        self._rstd = S.sb([128, 512], F32, "rstd")
        self._ntmp = S.sb([128, 512], F32, "ntmp")

    def rstd_group(self, grp):
        S = self.S
        n = grp[3]
        sq, rstd = self._sq, self._rstd
        rbs = [self.rbuf(grp, c) for c in range(NCH)]
        for c in range(NCH):
            S.act(sq[:, c, 0:n], rbs[c][:, 0:n], AF.Square, [rbs[c]], [sq])
        ps = S.psum()
        for c in range(NCH):
            S.mm(ps[:, 0:n], self.ones_b[:], sq[:, c, 0:n], c == 0, c == NCH - 1, [self.ones_b, sq], [ps])
        S.act(rstd[:, 0:n], ps[:, 0:n], AF.Sqrt, [ps, self.epsb], [rstd], bias=self.epsb[:, 0:1], scale=1.0 / D)
        S.recip(rstd[:, 0:n], rstd[:, 0:n], [rstd], [rstd])
        return rbs, rstd

    def norm_group(self, grp, a_buf, a_which_shift, sn, out_bf=None, out_f=None):
        S = self.S
        n = grp[3]
        tmp = self._ntmp
        rbs, rstd = self.rstd_group(grp)
        for c in range(NCH):
            S.stt(tmp[:, 0:n], rbs[c][:, 0:n], a_buf[:, c:c + 1], rstd[:, 0:n], ALU.mult, ALU.mult,
                  [rbs[c], a_buf, rstd], [tmp])
            sh = self.mvec(sn, a_which_shift, c)
            if out_f is not None:
                S.act(out_f[c][0], tmp[:, 0:n], AF.Identity, [tmp, self.modT], [out_f[c][1]], bias=sh)
            if out_bf is not None:
                S.act(out_bf[c][0], tmp[:, 0:n], AF.Identity, [tmp, self.modT], [out_bf[c][1]], bias=sh)

    def moe_layer(self, layer, keep_ctx):
        S = self.S
        A = self.A
        S.push()
        m = {}
        m["h2tm"] = [S.sb([128, D], BF16, "h2tm") for _ in range(18)]
        m["slot_tm"] = S.sb([128, 18, NE], F32, "slottm")
        m["affTb"] = S.sb([16, 2304], BF16, "affTb")
        m["slotTb"] = S.sb([16, 2304], BF16, "slotTb")
        m["rw"] = S.sb([128, NCH, NE], F32, "rw")
        m["sm"] = S.sb([128, 4], F32, "sm")
        m["m8"] = S.sb([16, 8], F32, "m8")
        groups = self.groups if keep_ctx else self.groups[:4]
        ntiles = 18 if keep_ctx else 16
        ncap = 288 if keep_ctx else 256
        with self.nc.allow_non_contiguous_dma("tiny"):
            S.dma("sp", m["rw"][:], A["moe_router"][layer * D:(layer + 1) * D, :].rearrange("(c p) e -> p c e", p=128),
                  writes=[m["rw"]])
        self._wk = 0

        def wsrc(kind, e):
            nm = {"g": "moe_w_gate", "u": "moe_w_up", "d": "moe_w_down"}[kind]
            base = (layer * NE + e) * D
            return A[nm][base:base + D, :].rearrange("(c p) n -> p c n", p=128)

        def wload(kind, e):
            b = m["w"][self._wk % 2]
            self._wk += 1
            if self.cfg.get("moe_noload") and self._wk > 2:
                return b
            S.dma("pool", b[:], wsrc(kind, e), writes=[b])
            return b

        S.push()
        m["affT"] = S.sb([16, 2304], F32, "affT")
        m["aff_tm"] = S.sb([128, 18, NE], F32, "afftm")
        S.push()
        self.norm_scratch()
        m["hf"] = [S.sb([128, 512], F32, "hf") for _ in range(NCH)]
        m["hb"] = [S.sb([128, 512], BF16, "hb") for _ in range(NCH)]
        sm = m["sm"]
        tile = 0
        for grp in groups:
            sn = grp[0]
            n = grp[3]
            self.norm_group(grp, self.M[sn]["a2"], 3, sn,
                            out_bf=[(m["hb"][c][:, 0:n], m["hb"][c]) for c in range(NCH)],
                            out_f=[(m["hf"][c][:, 0:n], m["hf"][c]) for c in range(NCH)])
            for tt in range(n // 128):
                tsl = slice(tt * 128, (tt + 1) * 128)
                ps = S.psum()
                for c in range(NCH):
                    S.mm(ps[:, 0:NE], m["hf"][c][:, tsl], m["rw"][:, c, :], c == 0, c == NCH - 1,
                         [m["hf"][c], m["rw"]], [ps])
                S.red(sm[:, 0:1], ps[:, 0:NE], ALU.max, [ps], [sm], negate=True)
                S.act(m["aff_tm"][:, tile, :], ps[:, 0:NE], AF.Exp, [ps, sm], [m["aff_tm"], sm], bias=sm[:, 0:1],
                      accum_out=sm[:, 1:2])
                S.recip(sm[:, 2:3], sm[:, 1:2], [sm], [sm])
                S.ts(m["aff_tm"][:, tile, :], m["aff_tm"][:, tile, :], sm[:, 2:3], None, ALU.mult, None,
                     [m["aff_tm"], sm], [m["aff_tm"]])
                ps2 = S.psum()
                S.trf(ps2[0:NE, 0:128], m["aff_tm"][:, tile, :], self.ident_f[:], [m["aff_tm"], self.ident_f], [ps2])
                S.cp(m["affT"][:, tile * 128:(tile + 1) * 128], ps2[0:NE, 0:128], [ps2], [m["affT"]], e="act")
                ps3 = S.psum()
                psb = ps3[:].bitcast(BF16)
                for c in range(NCH):
                    S.tr(psb[:, c * 128:(c + 1) * 128], m["hb"][c][:, tsl], self.ident_b[:],
                         [m["hb"][c], self.ident_b], [ps3])
                S.cp(m["h2tm"][tile][:], psb[:, 0:D], [ps3], [m["h2tm"][tile]], e=("act" if tile % 2 else "dve"))
                tile += 1
        for c_ in range(NCH):
            self.dbg("hf%d" % c_, m["hf"][c_][:], m["hf"][c_], [128, 512])
        self.dbg("sm", m["sm"][:, 0:3], m["sm"], [128, 3])
        self.dbg("rc0", self.RC[0][:], self.RC[0], [128, 256])
        self.dbg("modT", self.modT[:].rearrange("p a b -> p (a b)"), self.modT, [128, 96])
        self.dbg("aff", m["aff_tm"][:].rearrange("p a b -> p (a b)"), m["aff_tm"], [128, 18 * NE])
        self.dbg("h2tm0", m["h2tm"][0][:], m["h2tm"][0], [128, D])
        S.pop()

        self.dbg("affT", m["affT"][:], m["affT"], [16, 2304])
        S.push()
        m["wk"] = S.sb([16, 2304], F32, "wk")
        m["rankT"] = S.sb([16, 2304], F32, "rankT")
        regions = [(0, 2048, 256)] + ([(2048, 256, 32)] if keep_ctx else [])
        for (r0, rn, k) in regions:
            rs = slice(r0, r0 + rn)
            S.cp(m["wk"][:, rs], m["affT"][:, rs], [m["affT"]], [m["wk"]])
            for rnd in range(k // 8):
                S.max8(m["m8"][:], m["wk"][:, rs], [m["wk"]], [m["m8"]])
                if rnd < k // 8 - 1:
                    S.mrep(m["wk"][:, rs], m["m8"][:], m["wk"][:, rs], -1.0, [m["wk"], m["m8"]], [m["wk"]])
            S.ts(m["wk"][:, rs], m["affT"][:, rs], m["m8"][:, 7:8], None, ALU.is_ge, None,
                 [m["affT"], m["m8"]], [m["wk"]])
            S.scan(m["rankT"][:, rs], m["wk"][:, rs], m["wk"][:, rs], 0.0, ALU.add, ALU.max, [m["wk"]], [m["rankT"]])
            S.tt(m["rankT"][:, rs], m["rankT"][:, rs], m["wk"][:, rs], ALU.mult, [m["rankT"], m["wk"]], [m["rankT"]])
            S.ts(m["rankT"][:, rs], m["rankT"][:, rs], -1.0, None, ALU.add, None, [m["rankT"]], [m["rankT"]])
        nT = ntiles * 128
        S.cp(m["slotTb"][:, 0:nT], m["rankT"][:, 0:nT], [m["rankT"]], [m["slotTb"]])
        S.cp(m["affTb"][:, 0:nT], m["affT"][:, 0:nT], [m["affT"]], [m["affTb"]], e="act")
        for t in range(ntiles):
            ps = S.psum()
            S.trf(ps[:, 0:NE], m["rankT"][:, t * 128:(t + 1) * 128], self.ident_f[0:16, 0:16],
                 [m["rankT"], self.ident_f], [ps])
            S.cp(m["slot_tm"][:, t, :], ps[:, 0:NE], [ps], [m["slot_tm"]], e=("act" if t % 2 else "dve"))
        self.dbg("slot", m["slot_tm"][:].rearrange("p a b -> p (a b)"), m["slot_tm"], [128, 18 * NE])
        self.dbg("m8", m["m8"][:], m["m8"], [16, 8])
        S.pop()
        S.pop()

        S.push()
        m["w"] = [S.sb([128, NCH, 1024], BF16, "wexp") for _ in range(2)]
        wq = [wload("g", 0), wload("u", 0)]
        m["P"] = [S.sb([128, 16 * 256 + 2 * 32], BF16, "P") for _ in range(2)]
        m["PT"] = [S.sb([128, 2048], BF16, "PT") for _ in range(2)] + [S.sb([32, 256], BF16, "PTc")]
        m["affbc"] = S.sb([128, 512], BF16, "affbc")
        m["xeT"] = S.sb([128, NCH, 288], BF16, "xeT")
        m["actT"] = S.sb([128, NCH, 288], BF16, "actT")
        m["sil"] = S.sb([128, NCH, 288], BF16, "sil")
        m["ye"] = [S.sb([128, D], BF16, "ye") for _ in range(3)]
        g2 = {sn: [self.mvec(sn, 5, c) for c in range(NCH)] for sn in ("l", "c")}
        modT = self.modT
        PT = m["PT"]
        def build_P(e_):
            P_ = m["P"][e_ % 2]
            for t in range(ntiles):
                if t < 16:
                    S.ts(P_[:, t * 256:(t + 1) * 256], self.iota_row[:, 0:256], m["slot_tm"][:, t, e_:e_ + 1], None,
                         ALU.is_equal, None, [self.iota_row, m["slot_tm"]], [P_])
                else:
                    o = 4096 + (t - 16) * 32
                    S.ts(P_[:, o:o + 32], self.iota_row[:, 0:32], m["slot_tm"][:, t, e_:e_ + 1], None,
                         ALU.is_equal, None, [self.iota_row, m["slot_tm"]], [P_])

        build_P(0)
        for e in range(NE):
            P = m["P"][e % 2]
            wg, wu = wq
            for c in range(NCH):
                ps = S.psum()
                for t in range(16):
                    S.mm(ps[:, 0:256], m["h2tm"][t][:, c * 128:(c + 1) * 128], P[:, t * 256:(t + 1) * 256],
                         t == 0, t == 15, [m["h2tm"][t], P], [ps])
                if keep_ctx:
                    for t in range(16, 18):
                        o = 4096 + (t - 16) * 32
                        S.mm(ps[:, 256:288], m["h2tm"][t][:, c * 128:(c + 1) * 128], P[:, o:o + 32],
                             t == 16, t == 17, [m["h2tm"][t], P], [ps])
                S.cp(m["xeT"][:, c, 0:ncap], ps[:, 0:ncap], [ps], [m["xeT"]], e="act")
            selE = self.sel[:, e * 128:(e + 1) * 128]
            ab = m["affbc"]
            for gi, grp in enumerate(groups):
                n = grp[3]
                r0 = grp[2] if grp[0] == "l" else 2048
                psB = S.psum()
                S.mm(psB[:, 0:n], selE, m["affTb"][:, r0:r0 + n], True, True, [self.sel, m["affTb"]], [psB])
                S.cp(ab[:, 0:n], psB[:, 0:n], [psB], [ab], e="act")
                psA = S.psum()
                if grp[0] == "l":
                    S.mm(psA[:, 0:n], selE, m["slotTb"][:, r0:r0 + n], True, True, [self.sel, m["slotTb"]], [psA])
                    for cc in range(2):
                        S.stt(PT[cc][:, r0:r0 + n], psA[:, 0:n], self.iota_part[:, cc:cc + 1], ab[:, 0:n],
                              ALU.is_equal, ALU.mult, [psA, self.iota_part, ab], [PT[cc]])
                else:
                    S.mm(psA[0:32, 0:n], selE[:, 0:32], m["slotTb"][:, r0:r0 + n], True, True,
                         [self.sel, m["slotTb"]], [psA])
                    S.stt(PT[2][:, 0:n], psA[0:32, 0:n], self.iota_part[0:32, 0:1], ab[0:32, 0:n],
                          ALU.is_equal, ALU.mult, [psA, self.iota_part, ab], [PT[2]])
            for f in range(NCH):
                psA = S.psum()
                for c in range(NCH):
                    S.mm(psA[:, 0:ncap], wg[:, c, f * 128:(f + 1) * 128], m["xeT"][:, c, 0:ncap], c == 0, c == NCH - 1,
                         [wg, m["xeT"]], [psA])
                S.act(m["sil"][:, f, 0:ncap], psA[:, 0:ncap], AF.Silu, [psA], [m["sil"]])
            wd = wload("d", e)
            for f in range(NCH):
                psU = S.psum()
                for c in range(NCH):
                    S.mm(psU[:, 0:ncap], wu[:, c, f * 128:(f + 1) * 128], m["xeT"][:, c, 0:ncap], c == 0, c == NCH - 1,
                         [wu, m["xeT"]], [psU])
                S.tt(m["actT"][:, f, 0:ncap], m["sil"][:, f, 0:ncap], psU[:, 0:ncap], ALU.mult, [m["sil"], psU], [m["actT"]])
            if e + 1 < NE:
                wg_n = wload("g", e + 1)
                build_P(e + 1)
            for cc, rows in enumerate((128, 128, 32)[:3 if keep_ctx else 2]):
                for dh in range(2):
                    ps = S.psum()
                    for f in range(NCH):
                        S.mm(ps[0:rows, :], m["actT"][:, f, cc * 128:cc * 128 + rows], wd[:, f, dh * 512:(dh + 1) * 512],
                             f == 0, f == NCH - 1, [m["actT"], wd], [ps])
                    S.cp(m["ye"][cc][0:rows, dh * 512:(dh + 1) * 512], ps[0:rows, :], [ps], [m["ye"][cc]], e="act")
            if e + 1 < NE:
                wu_n = wload("u", e + 1)
                wq = [wg_n, wu_n]
            for grp in groups:
                n = grp[3]
                r0 = grp[2]
                for c in range(NCH):
                    ps = S.psum()
                    rb = self.rbuf(grp, c)
                    if grp[0] == "l":
                        for cc in range(2):
                            S.mm(ps[:, 0:n], m["ye"][cc][:, c * 128:(c + 1) * 128], PT[cc][:, r0:r0 + n], cc == 0, cc == 1,
                                 [m["ye"][cc], PT[cc]], [ps])
                    else:
                        S.mm(ps[:, 0:n], m["ye"][2][0:32, c * 128:(c + 1) * 128], PT[2][:, 0:n], True, True,
                             [m["ye"][2], PT[2]], [ps])
                    S.stt(rb[:, 0:n], ps[:, 0:n], g2[grp[0]][c], rb[:, 0:n], ALU.mult, ALU.add, [ps, modT, rb], [rb])
        S.pop()
        S.pop()

    def final_store(self, do_norm=True):
        S = self.S
        A = self.A
        S.push()
        self.norm_scratch()
        fw = S.sb([128, NCH], F32, "fnw")
        with self.nc.allow_non_contiguous_dma("tiny"):
            S.dma("sp", fw[:], A["final_norm_w"][0].rearrange("(c p) -> p c", p=128), writes=[fw])
        ob = [S.sb([128, D], F32, "ob") for _ in range(2)]
        hf = [S.sb([128, 512], F32, "hf") for _ in range(NCH)]
        k = 0
        out_evs = []
        glist = [(g, self.out) for g in self.groups[:4]]
        if self.cfg.get("dump_ctx"):
            glist.append((self.groups[4], self.out_c))
        for grp, dst in glist:
            n = grp[3]
            norm = do_norm and grp[0] == "l"
            if norm:
                rbs, rstd = self.rstd_group(grp)
                for c in range(NCH):
                    S.stt(hf[c][:, 0:n], rbs[c][:, 0:n], fw[:, c:c + 1], rstd[:, 0:n], ALU.mult, ALU.mult,
                          [rbs[c], fw, rstd], [hf[c]])
            for tt in range(n // 128):
                o = ob[k % 2]
                k += 1
                for half in range(2):
                    ps = S.psum()
                    for j in range(4):
                        c = half * 4 + j
                        src = hf[c] if norm else self.rbuf(grp, c)
                        S.trf(ps[:, j * 128:(j + 1) * 128], src[:, tt * 128:(tt + 1) * 128], self.ident_f[:],
                             [src, self.ident_f], [ps])
                    S.cp(o[:, half * 512:(half + 1) * 512], ps[:, :], [ps], [o], e=("act" if half else "dve"))
                t0 = grp[2] + tt * 128
                out_evs.append(S.dma("sp", dst[t0:t0 + 128, :], o[:], reads=[o]))
        for ev in out_evs:
            S._wait("sp", ev)
        S.pop()

    def build(self):
        cfg = self.cfg
        self.declare()
        self.setup()
        self.load_stream()
        for layer in cfg.get("layers", range(DEPTH)):
            keep_ctx = layer < DEPTH - 1
            self.adaln(layer)
            if cfg.get("mixer", True):
                self.mixer(layer, keep_ctx)
            if cfg.get("moe", True):
                self.moe_layer(layer, keep_ctx)
        self.final_store(cfg.get("final_norm", True))
        self.S.emit()
        return self.nc

    def hnorm(self, keep_ctx=True):
        S = self.S
        H = [S.sb([128, 2304], BF16, "H") for _ in range(NCH)]
        S.push()
        self.norm_scratch()
        for grp in self.groups:
            sn = grp[0]
            n = grp[3]
            o = grp[2] if sn == "l" else 2048
            self.norm_group(grp, self.M[sn]["a1"], 0, sn, out_bf=[(H[c][:, o:o + n], H[c]) for c in range(NCH)])
        S.pop()
        return H

    def tok_groups(self, keep_ctx=True):
        gs = [(g, g[2], 512) for g in self.groups[:4]]
        if keep_ctx:
            gs.append((self.groups[4], 2048, 256))
        return gs

    def out_proj(self, Z, w_ap, keep_ctx):
        S = self.S
        wo = S.sb([128, NCH, D], BF16, "wo")
        S.dma("pool", wo[:], w_ap.rearrange("(c p) n -> p c n", p=128), writes=[wo])
        for (grp, o, n) in self.tok_groups(keep_ctx):
            for c in range(NCH):
                ps = S.psum()
                for k in range(NCH):
                    S.mm(ps[:, 0:n], wo[:, k, c * 128:(c + 1) * 128], Z[k][:, o:o + n], k == 0, k == NCH - 1,
                         [wo, Z[k]], [ps])
                rb = self.rbuf(grp, c)
                S.stt(rb[:, 0:n], ps[:, 0:n], self.mvec(grp[0], 2, c), rb[:, 0:n], ALU.mult, ALU.add,
                      [ps, self.modT, rb], [rb])

    def mixer(self, layer, keep_ctx):
        kind = layer % 4
        if kind == 0:
            self.mixer_hyena(keep_ctx)
        if kind == 1:
            self.mixer_swa(keep_ctx)
        if kind == 2:
            self.mixer_gdn(keep_ctx)
        if kind == 3:
            self.mixer_hgrn2(layer, keep_ctx)

    def mixer_hyena(self, keep_ctx):
        S = self.S
        A = self.A
        nc = self.nc
        seqs = [("l", TL, 0)] + ([("c", TC, 2048)] if keep_ctx else [])
        TWO_PI = 2.0 * np.pi
        HS = {}
        for nm, L, _ in seqs:
            HS[nm] = nc.dram_tensor("hy_spec_" + nm, [2, 8, 2, 128, L], BF16, kind="Internal").ap()

        S.push()
        w1 = S.sb([33, 64], F32, "w1")
        w2 = S.sb([64, 64], F32, "w2")
        S.dma("sp", w1[:], A["hy_ffn_w1"], writes=[w1])
        S.dma("sp", w2[:], A["hy_ffn_w2"], writes=[w2])
        pcol = S.sb([64, 4], F32, "pcol")
        S.dma("sp", pcol[:, 0:1], A["hy_ffn_b1"].rearrange("o p -> p o"), writes=[pcol])
        S.dma("sp", pcol[:, 1:2], A["hy_sin_freq"][0:1, :].rearrange("o p -> p o"), writes=[pcol])
        S.dma("sp", pcol[:, 2:3], A["hy_ffn_b2"].rearrange("o p -> p o"), writes=[pcol])
        S.dma("sp", pcol[:, 3:4], A["hy_sin_freq"][1:2, :].rearrange("o p -> p o"), writes=[pcol])
        w3p = S.sb([64, 2, D], F32, "w3p")
        w3m = S.sb([64, 2, D], F32, "w3m")
        dbc = S.sb([128, D], F32, "dbc")
        sa = {nm: S.sb([64, 256], F32, "sa_" + nm) for nm in ("arg", "t", "kf", "m")}
        ki = S.sb([64, 256], I32, "ki")
        S.push()
        w3 = S.sb([64, 4 * D], F32, "w3")
        S.dma("sp", w3[:], A["hy_ffn_w3"], writes=[w3])
        for o in range(2):
            fw_ = w3[:, o * 2048:o * 2048 + D]
            bw_ = w3[:, o * 2048 + D:o * 2048 + 2 * D]
            S.tt(w3p[:, o, :], fw_, bw_, ALU.add, [w3], [w3p])
            S.tt(w3m[:, o, :], bw_, fw_, ALU.subtract, [w3], [w3m], e="pool")
        S.pop()
        S.dma("sp", dbc[:], A["hy_delta"][0].partition_broadcast(128), writes=[dbc])

        def sinfn(dst, ps, n, bcol, fcol):
            S.ts(sa["arg"][:, 0:n], ps, pcol[:, bcol:bcol + 1], pcol[:, fcol:fcol + 1], ALU.add, ALU.mult,
                 [pcol] + ([] if isinstance(ps, int) else []), [sa["arg"]])
            S.ts(sa["t"][:, 0:n], sa["arg"][:, 0:n], 1.0 / TWO_PI, 8.5, ALU.mult, ALU.add, [sa["arg"]], [sa["t"]])
            S.cp(ki[:, 0:n], sa["t"][:, 0:n], [sa["t"]], [ki])
            S.cp(sa["kf"][:, 0:n], ki[:, 0:n], [ki], [sa["kf"]])
            S.tt(sa["t"][:, 0:n], sa["t"][:, 0:n], sa["kf"][:, 0:n], ALU.subtract, [sa["t"], sa["kf"]], [sa["t"]])
            S.ts(sa["m"][:, 0:n], sa["t"][:, 0:n], 0.5, None, ALU.is_gt, None, [sa["t"]], [sa["m"]])
            S.tt(sa["t"][:, 0:n], sa["t"][:, 0:n], sa["m"][:, 0:n], ALU.subtract, [sa["t"], sa["m"]], [sa["t"]])
            S.act(dst, sa["t"][:, 0:n], AF.Sin, [sa["t"]], [], scale=-TWO_PI)

        for nm, L, _ in seqs:
            T_ = L // 128
            N = 2 * L
            S.push()
            tcol = S.sb([128, T_], F32, "tcol")
            S.dma("sp", tcol[:], A["hy_tcol_" + nm], writes=[tcol])
            S.ts(tcol[:], tcol[:], -1.0, None, ALU.mult, None, [tcol], [tcol])
            h2T = S.sb([64, L], F32, "h2T")
            S.push()
            zT = S.sb([33, L], F32, "zT")
            S.dma("sp", zT[:], A["hy_zT_" + nm], writes=[zT])
            h1T = S.sb([64, L], F32, "h1T")
            for b0 in range(0, L, 256):
                n = min(256, L - b0)
                ps = S.psum()
                S.mm(ps[0:64, 0:n], w1[:], zT[:, b0:b0 + n], True, True, [w1, zT], [ps])
                S._deps("dve", [ps], [])
                sinfn(h1T[:, b0:b0 + n], ps[0:64, 0:n], n, 0, 1)
                S._mark((S.csem["act"], S.ccnt["act"], "act"), [ps], [h1T])
            for b0 in range(0, L, 256):
                n = min(256, L - b0)
                ps = S.psum()
                S.mm(ps[0:64, 0:n], w2[:], h1T[:, b0:b0 + n], True, True, [w2, h1T], [ps])
                S._deps("dve", [ps], [])
                sinfn(h2T[:, b0:b0 + n], ps[0:64, 0:n], n, 2, 3)
                S._mark((S.csem["act"], S.ccnt["act"], "act"), [ps], [h2T])
            S.pop()
            win = S.sb([128, T_, 128], F32, "win")
            hp = [[S.sb([128, T_, 128], BF16, "hp") for o in range(2)] for _ in range(2)]
            hm = [[S.sb([128, T_, 128], BF16, "hm") for o in range(2)] for _ in range(2)]
            Hs = [[[S.sb([128, T_, 128], BF16, "Hs") for ri in range(2)] for o in range(2)] for _ in range(2)]
            Cf = [S.sb([128, T_, 128], BF16, "Cf") for _ in range(2)]
            Sf_ = [S.sb([128, T_, 128], BF16, "Sf") for _ in range(2)]
            for pair in range(4):
                for bi in range(2):
                    cb = pair * 2 + bi
                    ccs = slice(cb * 128, (cb + 1) * 128)
                    for tt in range(T_):
                        S.act(win[:, tt, :], dbc[:, ccs], AF.Exp, [dbc, tcol], [win], scale=tcol[:, tt:tt + 1])
                    S.ts(win[:], win[:], 0.05, None, ALU.add, None, [win], [win])
                    for o in range(2):
                        for tt in range(T_):
                            dsl = slice(tt * 128, (tt + 1) * 128)
                            psP = S.psum()
                            psM = S.psum()
                            S.mm(psP[:, 0:128], h2T[:, dsl], w3p[:, o, ccs], True, True, [h2T, w3p], [psP])
                            S.mm(psM[:, 0:128], h2T[:, dsl], w3m[:, o, ccs], True, True, [h2T, w3m], [psM])
                            S.tt(hp[bi][o][:, tt, :], psP[:, 0:128], win[:, tt, :], ALU.mult, [psP, win], [hp[bi][o]])
                            S.tt(hm[bi][o][:, tt, :], psM[:, 0:128], win[:, tt, :], ALU.mult, [psM, win], [hm[bi][o]])
                        S.tt(hp[bi][o][0:1, 0, :], hp[bi][o][0:1, 0, :], hm[bi][o][0:1, 0, :], ALU.subtract,
                             [hp[bi][o], hm[bi][o]], [hp[bi][o]])
                        S.ts(hp[bi][o][0:1, 0, :], hp[bi][o][0:1, 0, :], 0.5, None, ALU.mult, None, [hp[bi][o]], [hp[bi][o]])
                for ft in range(T_):
                    cf, sf = Cf[ft % 2], Sf_[ft % 2]
                    S.dma("sp", cf[:].rearrange("p a b -> p (a b)"), A["hy_Cf_" + nm][ft * 128:(ft + 1) * 128, :], writes=[cf])
                    S.dma("sp", sf[:].rearrange("p a b -> p (a b)"), A["hy_Sf_" + nm][ft * 128:(ft + 1) * 128, :], writes=[sf])
                    for bi in range(2):
                        for o in range(2):
                            for ri, (tab, src) in enumerate(((cf, hp[bi][o]), (sf, hm[bi][o]))):
                                ps = S.psum()
                                for tt in range(T_):
                                    S.mm(ps[:, 0:128], tab[:, tt, :], src[:, tt, :], tt == 0, tt == T_ - 1, [tab, src], [ps])
                                S.act(Hs[bi][o][ri][:, ft, :], ps[:, 0:128], AF.Identity, [ps], [Hs[bi][o][ri]], scale=2.0 / N)
                for bi in range(2):
                    cb = pair * 2 + bi
                    for o in range(2):
                        for ri in range(2):
                            S.dma("sp", HS[nm][o, cb, ri], Hs[bi][o][ri][:].rearrange("p a b -> p (a b)"),
                                  reads=[Hs[bi][o][ri]], writes=[self.hs_tok])
            S.pop()
        S.pop()

        S.push()
        H = self.hnorm()
        Win = A["hy_w_in"].rearrange("(c p) n -> p c n", p=128)
        pv = S.sb([128, 24, 6], F32, "hyp")
        S.dma("sp", pv[:, :, 0], A["hy_b_in"][0].rearrange("(c p) -> p c", p=128), writes=[pv])
        for k in range(3):
            S.dma("sp", pv[:, :, 1 + k], A["hy_short_w"][k].rearrange("(c p) -> p c", p=128), writes=[pv])
        S.dma("sp", pv[:, :, 4], A["hy_short_b"][0].rearrange("(c p) -> p c", p=128), writes=[pv])
        fb = S.sb([128, 2, NCH], F32, "hyfb")
        for o in range(2):
            S.dma("sp", fb[:, o, :], A["hy_filter_bias"][o].rearrange("(c p) -> p c", p=128), writes=[fb])
        bo = S.sb([128, NCH], F32, "hybo")
        S.dma("sp", bo[:], A["hy_b_out"][0].rearrange("(c p) -> p c", p=128), writes=[bo])
        W3b = S.sb([128, NCH, 3, 128], BF16, "W3b")
        wo = S.sb([128, D], BF16, "wo_h")
        upad = S.sb([128, 2312], BF16, "upad")
        S.memset(upad[:], 0.0, [upad])
        cv = S.sb([128, 2304], F32, "cv")
        ufm = [S.sb([128, 2304], BF16, "ufm") for _ in range(3)]
        z1 = S.sb([128, 2304], BF16, "z1")
        zT_ = S.sb([128, 2304], BF16, "zTh")
        utm = S.sb([128, 16, 128], BF16, "utm")
        Hr = S.sb([128, 16, 128], BF16, "Hr")
        Hi = S.sb([128, 16, 128], BF16, "Hi")
        Yr = S.sb([128, 16, 128], BF16, "Yr")
        nYi = S.sb([128, 16, 128], BF16, "nYi")
        Cf = [S.sb([128, 16, 128], BF16, "Cf") for _ in range(2)]
        Sf_ = [S.sb([128, 16, 128], BF16, "Sf") for _ in range(2)]
        ring = [S.sb([128, 512], BF16, "ring") for _ in range(8)]
        ur = S.sb([128, 128], F32, "ur")
        ui = S.sb([128, 128], F32, "ui")
        t1 = S.sb([128, 128], F32, "t1")
        t2 = S.sb([128, 128], F32, "t2")
        ytmp = S.sb([128, 512], F32, "ytmp")
        LO, CO = 1, 2052
        rk = 0

        for cb in range(8):
            for k in range(3):
                S.dma("pool", W3b[:, :, k, :], Win[:, :, k * D + cb * 128:k * D + (cb + 1) * 128], writes=[W3b])
            for k in range(3):
                ch = k * 8 + cb
                for (grp, o, n) in self.tok_groups(keep_ctx):
                    ps = S.psum()
                    for c in range(NCH):
                        S.mm(ps[:, 0:n], W3b[:, c, k, :], H[c][:, o:o + n], c == 0, c == NCH - 1, [W3b, H[c]], [ps])
                    po_ = (LO + o) if o < 2048 else (CO + o - 2048)
                    S.act(upad[:, po_:po_ + n], ps[:, 0:n], AF.Identity, [ps, pv], [upad], bias=pv[:, ch, 0:1])
                for (nm, L, o_) in seqs:
                    base = LO if nm == "l" else CO
                    S.ts(cv[:, o_:o_ + L], upad[:, base - 1:base - 1 + L], pv[:, ch, 1:2], pv[:, ch, 4:5], ALU.mult, ALU.add,
                         [upad, pv], [cv])
                    S.stt(cv[:, o_:o_ + L], upad[:, base:base + L], pv[:, ch, 2:3], cv[:, o_:o_ + L], ALU.mult, ALU.add,
                          [upad, pv, cv], [cv])
                    S.stt(ufm[k][:, o_:o_ + L], upad[:, base + 1:base + 1 + L], pv[:, ch, 3:4], cv[:, o_:o_ + L],
                          ALU.mult, ALU.add, [upad, pv, cv], [ufm[k]])
            for (nm, L, o_) in seqs:
                T_ = L // 128
                for o in range(2):
                    src = ufm[0] if o == 0 else z1
                    xg = ufm[1 + o]
                    dst = z1 if o == 0 else zT_
                    for tt in range(T_):
                        pT = S.psum()
                        pTb = pT[:].bitcast(BF16)
                        S.tr(pTb[:, 0:128], src[:, o_ + tt * 128:o_ + (tt + 1) * 128], self.ident_b[:], [src, self.ident_b], [pT])
                        S.cp(utm[:, tt, :], pTb[:, 0:128], [pT], [utm], e="act")
                    S.dma("sp", Hr[:, 0:T_, :].rearrange("p a b -> p (a b)"), HS[nm][o, cb, 0], reads=[self.hs_tok], writes=[Hr])
                    S.dma("sp", Hi[:, 0:T_, :].rearrange("p a b -> p (a b)"), HS[nm][o, cb, 1], reads=[self.hs_tok], writes=[Hi])
                    for ft in range(T_):
                        cf, sf = Cf[ft % 2], Sf_[ft % 2]
                        if not (self.cfg.get("hy_noload") and (cb > 0 or ft > 1)):
                            S.dma("sp", cf[:, 0:T_, :].rearrange("p a b -> p (a b)"), A["hy_Cf_" + nm][ft * 128:(ft + 1) * 128, :], writes=[cf])
                            S.dma("sp", sf[:, 0:T_, :].rearrange("p a b -> p (a b)"), A["hy_Sf_" + nm][ft * 128:(ft + 1) * 128, :], writes=[sf])
                        pr = S.psum()
                        for tt in range(T_):
                            S.mm(pr[:, 0:128], cf[:, tt, :], utm[:, tt, :], tt == 0, tt == T_ - 1, [cf, utm], [pr])
                        pi_ = S.psum()
                        for tt in range(T_):
                            S.mm(pi_[:, 0:128], sf[:, tt, :], utm[:, tt, :], tt == 0, tt == T_ - 1, [sf, utm], [pi_])
                        S.cp(ur[:], pr[:, 0:128], [pr], [ur], e="act")
                        S.cp(ui[:], pi_[:, 0:128], [pi_], [ui], e="act")
                        S.tt(t1[:], ur[:], Hr[:, ft, :], ALU.mult, [ur, Hr], [t1])
                        S.tt(t2[:], ui[:], Hi[:, ft, :], ALU.mult, [ui, Hi], [t2], e="pool")
                        S.tt(Yr[:, ft, :], t1[:], t2[:], ALU.add, [t1, t2], [Yr])
                        S.tt(t1[:], ui[:], Hr[:, ft, :], ALU.mult, [ui, Hr], [t1])
                        S.tt(t2[:], ur[:], Hi[:, ft, :], ALU.mult, [ur, Hi], [t2], e="pool")
                        S.tt(nYi[:, ft, :], t1[:], t2[:], ALU.subtract, [t1, t2], [nYi])
                    for tg in range(0, L, 512):
                        n = min(512, L - tg)
                        py = S.psum()
                        for ft in range(T_):
                            rc = ring[rk % 8]
                            rs = ring[(rk + 1) % 8]
                            rk += 2
                            if not (self.cfg.get("hy_noload") and rk > 16):
                                S.dma("sp", rc[:, 0:n], A["hy_Ct_" + nm][ft * 128:(ft + 1) * 128, tg:tg + n], writes=[rc])
                                S.dma("sp", rs[:, 0:n], A["hy_St_" + nm][ft * 128:(ft + 1) * 128, tg:tg + n], writes=[rs])
                            S.mm(py[:, 0:n], Yr[:, ft, :], rc[:, 0:n], ft == 0, False, [Yr, rc], [py])
                            S.mm(py[:, 0:n], nYi[:, ft, :], rs[:, 0:n], False, ft == T_ - 1, [nYi, rs], [py])
                        osl = slice(o_ + tg, o_ + tg + n)
                        S.stt(ytmp[:, 0:n], src[:, osl], fb[:, o, cb:cb + 1], py[:, 0:n], ALU.mult, ALU.add, [src, fb, py], [ytmp])
                        S.tt(dst[:, osl], ytmp[:, 0:n], xg[:, osl], ALU.mult, [ytmp, xg], [dst], e="pool")
            self.out_proj_head(zT_, A["hy_w_out"], cb, wo, keep_ctx)
        gb = S.sb([128, 2, NCH], F32, "hygb")
        for si, sn in enumerate(("l", "c")):
            S.tt(gb[:, si, :], self.modT[:, 16:24, si], bo[:], ALU.mult, [self.modT, bo], [gb])
        for (grp, o, n) in self.tok_groups(keep_ctx):
            si = 0 if grp[0] == "l" else 1
            for c in range(NCH):
                rb = self.rbuf(grp, c)
                S.ts(rb[:, 0:n], rb[:, 0:n], gb[:, si, c:c + 1], None, ALU.add, None, [rb, gb], [rb])
        S.pop()

    def out_proj_head(self, zT, w_ap, h, wo, keep_ctx):
        S = self.S
        S.dma("pool", wo[:], w_ap[h * 128:(h + 1) * 128, :], writes=[wo])
        for (grp, o, n) in self.tok_groups(keep_ctx):
            for c in range(NCH):
                ps = S.psum()
                S.mm(ps[:, 0:n], wo[:, c * 128:(c + 1) * 128], zT[:, o:o + n], True, True, [wo, zT], [ps])
                rb = self.rbuf(grp, c)
                S.stt(rb[:, 0:n], ps[:, 0:n], self.mvec(grp[0], 2, c), rb[:, 0:n], ALU.mult, ALU.add,
                      [ps, self.modT, rb], [rb])

    def mixer_gdn(self, keep_ctx):
        S = self.S
        A = self.A
        W = A["gd_w_in"].rearrange("(c p) n -> p c n", p=128)
        ntile = 18
        S.push()
        H = self.hnorm()
        I_f = self.ident_f
        cw = S.sb([128, 3, 24], F32, "cw")
        for k in range(3):
            S.dma("sp", cw[:, k, :], A["gd_conv_w"][k].rearrange("(c p) -> p c", p=128), writes=[cw])
        nea = S.sb([1, 16], F32, "nea")
        dtb = S.sb([1, 16], F32, "dtb")
        S.dma("sp", nea[:], A["gd_a_log"], writes=[nea])
        S.dma("sp", dtb[:], A["gd_dt_bias"], writes=[dtb])
        S.act(nea[:], nea[:], AF.Exp, [nea], [nea])
        S.ts(nea[:], nea[:], -1.0, None, ALU.mult, None, [nea], [nea])
        onesr = S.sb([1, 128], F32, "onesr")
        S.memset(onesr[:], 1.0, [onesr])
        nwbc = S.sb([128, 128], F32, "nwbc")
        S.dma("sp", nwbc[:], A["gd_norm_w"][0].partition_broadcast(128), writes=[nwbc])
        cresr = S.sb([1, 128], F32, "cresr")
        S.dma("sp", cresr[:], A["chunk_reset"][0:1, :], writes=[cresr])
        tri = [S.sb([128, 128], F32, "tri") for _ in range(2)]
        tris = [S.sb([128, 128], F32, "tris") for _ in range(2)]
        S.dma("sp", tri[0][:], A["tri_fwd"], writes=[tri[0]])
        S.dma("sp", tri[1][:], A["tri_bwd"], writes=[tri[1]])
        S.dma("sp", tris[0][:], A["tri_fwd_s"], writes=[tris[0]])
        S.dma("sp", tris[1][:], A["tri_bwd_s"], writes=[tris[1]])
        eps1 = self.epsb
        wbab = S.sb([128, NCH, 32], BF16, "wbab")
        S.dma("pool", wbab[:], W[:, :, 4096:4128], writes=[wbab])
        wo = S.sb([128, D], BF16, "wo_h")
        cv = S.sb([128, 2304], F32, "cv")
        qT = S.sb([128, 2304], BF16, "qT")
        kT = S.sb([128, 2304], BF16, "kT")
        ktm = S.sb([128, ntile, 128], BF16, "ktm")
        vtm = S.sb([128, ntile, 128], BF16, "vtm")
        ztm = S.sb([128, ntile, 128], BF16, "ztm")
        of = S.sb([128, ntile, 128], F32, "of")
        zT = S.sb([128, 2304], BF16, "zT")
        vT = zT
        PV = []
        for _d in range(2):
            if _d == 1:
                mark = S.top
                W4 = S.sb([128, NCH, 4, 128], BF16, "W4")
                upad = S.sb([128, 2312], BF16, "upad")
                sqb = S.sb([128, 512], BF16, "sqb")
                rin = S.sb([128, 512], F32, "rin")
                top_after = S.top
                S.top = mark
            pvd = {}
            pvd["brow"] = S.sb([1, 128], F32, "brow")
            pvd["grow"] = S.sb([1, 128], F32, "grow")
            pvd["R"] = {nm: S.sb([1, 128], F32, nm) for nm in ("Gi", "G", "nG", "dl", "EGr")}
            pvd["Sf"] = S.sb([128, 128], F32, "Sf")
            pvd["Sb"] = S.sb([128, 128], BF16, "Sb")
            pvd["W2"] = S.sb([128, 2, 128], BF16, "W2")
            pvd["Q2"] = S.sb([128, 2, 128], BF16, "Q2")
            S.memset(pvd["W2"][:], 0.0, [pvd["W2"]])
            S.memset(pvd["Q2"][:], 0.0, [pvd["Q2"]])
            pvd["T"] = {nm: S.sb([128, 128], F32, nm) for nm in ("Em", "Ee", "Ei", "Es", "EB", "A", "B", "IA", "IB",
                                                                "P", "Pt", "u", "av", "o")}
            pvd["rhs"] = S.sb([128, 256], F32, "rhs")
            pvd["cols"] = S.sb([128, 8], F32, "cols")
            pvd["wsb"] = S.sb([128, 128], BF16, "wsb")
            pvd["kdtm"] = S.sb([128, 128], BF16, "kdtm")
            pvd["attnT"] = S.sb([128, 128], BF16, "attnT")
            pvd["vnew"] = S.sb([128, 128], BF16, "vnew")
            PV.append(pvd)
        S.top = max(S.top, top_after)
        TR = {nm: S.sb([128, 128], F32, nm) for nm in ("os", "nwg", "ysq")}
        yb = S.sb([128, 128], BF16, "yb")
        of1v = cv[:].rearrange("p (a b) -> p a b", b=128)
        st = S.sb([128, 4], F32, "gdst")
        LO, CO = 1, 2052

        def pad_view(o, n):
            return (LO + o) if o < 2048 else (CO + o - 2048)

        for h in range(8):
            S.barrier()
            S.memset(upad[:, 0:1], 0.0, [upad])
            S.memset(upad[:, 2049:2052], 0.0, [upad])
            S.memset(upad[:, 2308:2312], 0.0, [upad])
            for k in range(4):
                S.dma("pool", W4[:, :, k, :], W[:, :, k * D + h * 128:k * D + (h + 1) * 128], writes=[W4])
            for k, dst in ((0, qT), (1, kT), (2, vT)):
                for (grp, o, n) in self.tok_groups(True):
                    ps = S.psum()
                    for c in range(NCH):
                        S.mm(ps[:, 0:n], W4[:, c, k, :], H[c][:, o:o + n], c == 0, c == NCH - 1, [W4, H[c]], [ps])
                    po_ = pad_view(o, n)
                    S.cp(upad[:, po_:po_ + n], ps[:, 0:n], [ps], [upad], e="act")
                ch = k * 8 + h
                for (base, n_, o_) in ((LO, 2048, 0), (CO, 256, 2048)):
                    S.ts(cv[:, o_:o_ + n_], upad[:, base - 1:base - 1 + n_], cw[:, 0, ch:ch + 1], None, ALU.mult, None,
                         [upad, cw], [cv])
                    S.stt(cv[:, o_:o_ + n_], upad[:, base:base + n_], cw[:, 1, ch:ch + 1], cv[:, o_:o_ + n_],
                          ALU.mult, ALU.add, [upad, cw, cv], [cv])
                    S.stt(cv[:, o_:o_ + n_], upad[:, base + 1:base + 1 + n_], cw[:, 2, ch:ch + 1], cv[:, o_:o_ + n_],
                          ALU.mult, ALU.add, [upad, cw, cv], [cv])
                S.act(cv[:], cv[:], AF.Silu, [cv], [cv])
                if k == 2:
                    S.cp(dst[:], cv[:], [cv], [dst])
                    continue
                for (grp, o, n) in self.tok_groups(True):
                    S.act(sqb[:, 0:n], cv[:, o:o + n], AF.Square, [cv], [sqb])
                    ps = S.psum()
                    S.mm(ps[:, 0:n], self.ones_b[:], sqb[:, 0:n], True, True, [self.ones_b, sqb], [ps])
                    S.act(rin[:, 0:n], ps[:, 0:n], AF.Sqrt, [ps, eps1], [rin], bias=eps1[:, 0:1],
                          scale=(128.0 if k == 0 else 1.0))
                    S.recip(rin[:, 0:n], rin[:, 0:n], [rin], [rin])
                    S.tt(dst[:, o:o + n], cv[:, o:o + n], rin[:, 0:n], ALU.mult, [cv, rin], [dst])
            for t in range(ntile):
                tsl = slice(t * 128, (t + 1) * 128)
                for src, dstm in ((kT, ktm), (vT, vtm)):
                    pT = S.psum()
                    pTb = pT[:].bitcast(BF16)
                    S.tr(pTb[:, 0:128], src[:, tsl], self.ident_b[:], [src, self.ident_b], [pT])
                    S.cp(dstm[:, t, :], pTb[:, 0:128], [pT], [dstm], e="act")
                ps = S.psum()
                for c in range(NCH):
                    S.mm(ps[:, 0:128], H[c][:, tsl], W4[:, c, 3, :], c == 0, c == NCH - 1, [H[c], W4], [ps])
                S.act(ztm[:, t, :], ps[:, 0:128], AF.Silu, [ps], [ztm])
            def run_dir(d):
                pvd = PV[d]
                brow, grow, R, Sf, Sb, W2, Q2, T = (pvd[k] for k in ('brow', 'grow', 'R', 'Sf', 'Sb', 'W2', 'Q2', 'T'))
                rhs, cols, wsb, kdtm, attnT, vnew = (pvd[k] for k in ('rhs', 'cols', 'wsb', 'kdtm', 'attnT', 'vnew'))
                idx = d * 8 + h
                S.memset(Sf[:], 0.0, [Sf])
                S.memset(Sb[:], 0.0, [Sb])
                order = [16, 17] + list(range(16)) if d == 0 else [17, 16] + list(range(15, -1, -1))
                idx = d * 8 + h
                for tile in order:
                    o0 = tile * 128
                    tsl = slice(o0, o0 + 128)
                    for kind, dstr in ((0, brow), (1, grow)):
                        col = kind * 16 + idx
                        ps = S.psum()
                        for c in range(NCH):
                            S.mm(ps[0:1, 0:128], wbab[:, c, col:col + 1], H[c][:, tsl], c == 0, c == NCH - 1,
                                 [wbab, H[c]], [ps])
                        if kind == 0:
                            S.act(dstr[:], ps[0:1, 0:128], AF.Sigmoid, [ps], [dstr])
                        else:
                            S.act(dstr[:], ps[0:1, 0:128], AF.Exp, [ps, dtb], [dstr], bias=dtb[0:1, idx:idx + 1])
                            S.act(dstr[:], dstr[:], AF.Ln, [dstr, onesr], [dstr], bias=onesr[0:1, 0:1])
                            S.ts(dstr[:], dstr[:], nea[0:1, idx:idx + 1], None, ALU.mult, None, [dstr, nea], [dstr])
                    gr = grow[:]
                    S.scan(R["Gi"][:], cresr[:], gr, 0.0, ALU.mult, ALU.add, [cresr, grow], [R["Gi"]])
                    if d == 0:
                        G = R["Gi"]
                    else:
                        G = R["G"]
                        S.tt(G[:], gr, R["Gi"][:], ALU.subtract, [grow, R["Gi"]], [G])
                        for ci in range(2):
                            cs = slice(64 * ci, 64 * ci + 64)
                            S.ts(G[:, cs], G[:, cs], R["Gi"][:, 64 * ci + 63:64 * ci + 64], None, ALU.add, None,
                                 [G, R["Gi"]], [G])
                    S.ts(R["nG"][:], G[:], -1.0, None, ALU.mult, None, [G], [R["nG"]])
                    for ci in range(2):
                        cs = slice(64 * ci, 64 * ci + 64)
                        last = 64 * ci + (63 if d == 0 else 0)
                        S.ts(R["dl"][:, cs], G[:, cs], G[:, last:last + 1], None, ALU.subtract, None, [G], [R["dl"]])
                    S.act(R["EGr"][:], G[:], AF.Exp, [G], [R["EGr"]])
                    pD = S.psum()
                    S.mm(pD[:, 0:128], R["nG"][:], onesr[:], True, False, [R["nG"], onesr], [pD])
                    S.mm(pD[:, 0:128], onesr[:], G[:], False, True, [onesr, G], [pD])
                    pC = S.psum()
                    S.mm(pC[:, 0:1], G[:], onesr[:, 0:1], True, True, [G, onesr], [pC])
                    S.mm(pC[:, 1:2], R["dl"][:], onesr[:, 0:1], True, True, [R["dl"], onesr], [pC])
                    S.mm(pC[:, 2:3], brow[:], onesr[:, 0:1], True, True, [brow, onesr], [pC])
                    for ci in range(2):
                        last = 64 * ci + (63 if d == 0 else 0)
                        S.mm(pC[:, 4 + ci:5 + ci], onesr[:], R["EGr"][:, last:last + 1], True, True, [onesr, R["EGr"]], [pC])
                    pB = S.psum()
                    S.mm(pB[:, 0:128], onesr[:], brow[:], True, True, [onesr, brow], [pB])
                    S.ts(T["Em"][:], pD[:, 0:128], 0.0, None, ALU.min, None, [pD], [T["Em"]])
                    S.act(T["Ee"][:], T["Em"][:], AF.Exp, [T["Em"]], [T["Ee"]])
                    S.tt(T["Ei"][:], T["Ee"][:], tri[d][:], ALU.mult, [T["Ee"], tri[d]], [T["Ei"]], e="pool")
                    S.tt(T["Es"][:], T["Ee"][:], tris[d][:], ALU.mult, [T["Ee"], tris[d]], [T["Es"]], e="pool")
                    S.act(cols[:, 0:1], pC[:, 0:1], AF.Exp, [pC], [cols])
                    S.act(cols[:, 1:2], pC[:, 1:2], AF.Exp, [pC], [cols], scale=-1.0)
                    S.act(cols[:, 2:3], pC[:, 2:3], AF.Identity, [pC], [cols])
                    S.act(cols[:, 4:6], pC[:, 4:6], AF.Identity, [pC], [cols])
                    S.tt(cols[:, 3:4], cols[:, 2:3], cols[:, 0:1], ALU.mult, [cols], [cols])
                    S.tt(T["EB"][:], pB[:, 0:128], T["Es"][:], ALU.mult, [pB, T["Es"]], [T["EB"]])
                    pK = S.psum()
                    S.mm(pK[:, 0:128], kT[:, tsl], kT[:, tsl], True, True, [kT], [pK])
                    S.tt(T["B"][:], pK[:, 0:128], T["EB"][:], ALU.mult, [pK, T["EB"]], [T["B"]])
                    pA = S.psum()
                    S.trf(pA[:, 0:128], T["B"][:], I_f[:], [T["B"], I_f], [pA])
                    S.cp(T["A"][:], pA[:, 0:128], [pA], [T["A"]], e="act")
                    S.tt(T["P"][:], I_f[:], T["B"][:], ALU.subtract, [I_f, T["B"]], [T["P"]])
                    S.tt(T["Pt"][:], I_f[:], T["A"][:], ALU.subtract, [I_f, T["A"]], [T["Pt"]], e="pool")
                    for lvl in range(5):
                        pA2 = S.psum()
                        pB2 = S.psum()
                        S.mm(pA2[:, 0:128], T["B"][:], T["A"][:], True, True, [T["B"], T["A"]], [pA2])
                        S.mm(pB2[:, 0:128], T["A"][:], T["B"][:], True, True, [T["A"], T["B"]], [pB2])
                        S.tt(T["IA"][:], pA2[:, 0:128], I_f[:], ALU.add, [pA2, I_f], [T["IA"]])
                        S.tt(T["IB"][:], pB2[:, 0:128], I_f[:], ALU.add, [pB2, I_f], [T["IB"]])
                        if lvl < 4:
                            S.cp(T["A"][:], pA2[:, 0:128], [pA2], [T["A"]], e="act")
                            S.cp(T["B"][:], pB2[:, 0:128], [pB2], [T["B"]], e="act")
                        pP = S.psum()
                        pPt = S.psum()
                        S.mm(pP[:, 0:128], T["Pt"][:], T["IB"][:], True, True, [T["Pt"], T["IB"]], [pP])
                        S.mm(pPt[:, 0:128], T["P"][:], T["IA"][:], True, True, [T["P"], T["IA"]], [pPt])
                        S.cp(T["P"][:], pP[:, 0:128], [pP], [T["P"]])
                        S.cp(T["Pt"][:], pPt[:, 0:128], [pPt], [T["Pt"]], e="act")
                    S.ts(rhs[:, 0:128], vtm[:, tile, :], cols[:, 2:3], None, ALU.mult, None, [vtm, cols], [rhs])
                    S.ts(rhs[:, 128:256], ktm[:, tile, :], cols[:, 3:4], None, ALU.mult, None, [ktm, cols], [rhs])
                    pU = S.psum()
                    S.mm(pU[:, 0:256], T["P"][:], rhs[:], True, True, [T["P"], rhs], [pU])
                    S.cp(T["u"][:], pU[:, 0:128], [pU], [T["u"]], e="act")
                    S.cp(wsb[:], pU[:, 128:256], [pU], [wsb], e="act")
                    pW = S.psum()
                    pWb = pW[:].bitcast(BF16)
                    S.tr(pWb[:, 0:128], wsb[:], self.ident_b[:], [wsb, self.ident_b], [pW])
                    S.cp(W2[:, 0, 0:64], pWb[:, 0:64], [pW], [W2])
                    S.cp(W2[:, 1, 64:128], pWb[:, 64:128], [pW], [W2])
                    S.cp(Q2[:, 0, 0:64], qT[:, o0:o0 + 64], [qT], [Q2], e="pool")
                    S.cp(Q2[:, 1, 64:128], qT[:, o0 + 64:o0 + 128], [qT], [Q2], e="pool")
                    S.ts(kdtm[:], ktm[:, tile, :], cols[:, 1:2], None, ALU.mult, None, [ktm, cols], [kdtm])
                    pQK = S.psum()
                    S.mm(pQK[:, 0:128], kT[:, tsl], qT[:, tsl], True, True, [kT, qT], [pQK])
                    S.tt(attnT[:], pQK[:, 0:128], T["Ei"][:], ALU.mult, [pQK, T["Ei"]], [attnT])
                    pq = S.psum()
                    pv = S.psum()
                    psS = S.psum()
                    corder = (0, 1) if d == 0 else (1, 0)
                    for n_, ci in enumerate(corder):
                        pr = slice(64 * ci, 64 * ci + 64)
                        S.mm(pv[:, 0:128], W2[:, ci, :], Sb[:], True, True, [W2, Sb], [pv])
                        S.tt(vnew[pr, :], T["u"][pr, :], pv[pr, 0:128], ALU.subtract, [T["u"], pv], [vnew])
                        S.mm(pq[:, 0:128], Q2[:, ci, :], Sb[:], n_ == 0, n_ == 1, [Q2, Sb], [pq])
                        S.mm(psS[:, 0:128], kdtm[pr, :], vnew[pr, :], True, True, [kdtm, vnew], [psS])
                        S.stt(Sf[:], Sf[:], cols[:, 4 + ci:5 + ci], psS[:, 0:128], ALU.mult, ALU.add, [Sf, cols, psS], [Sf])
                        S.cp(Sb[:], Sf[:], [Sf], [Sb], e="act")
                    pav = S.psum()
                    S.mm(pav[:, 0:128], attnT[:], vnew[:], True, True, [attnT, vnew], [pav])
                    S.cp(T["av"][:], pav[:, 0:128], [pav], [T["av"]], e="act")
                    S.stt(T["o"][:], pq[:, 0:128], cols[:, 0:1], T["av"][:], ALU.mult, ALU.add, [pq, cols, T["av"]], [T["o"]])
                    if d == 0:
                        S.cp(of[:, tile, :], T["o"][:], [T["o"]], [of], e="pool")
                    else:
                        S.cp(of1v[:, tile, :], T["o"][:], [T["o"]], [cv], e="pool")

            S.barrier()
            for _d in range(2):
                S.memset(PV[_d]["W2"][:], 0.0, [PV[_d]["W2"]])
                S.memset(PV[_d]["Q2"][:], 0.0, [PV[_d]["Q2"]])
            S.run_interleaved([lambda: run_dir(0), lambda: run_dir(1)])
            for tile in range(18 if keep_ctx else 16):
                o0 = tile * 128
                tsl = slice(o0, o0 + 128)
                T = TR
                S.tt(T["os"][:], of1v[:, tile, :], of[:, tile, :], ALU.add, [cv, of], [T["os"]])
                S.act(T["ysq"][:], T["os"][:], AF.Square, [T["os"]], [T["ysq"], st], accum_out=st[:, 0:1])
                S.act(st[:, 1:2], st[:, 0:1], AF.Sqrt, [st, eps1], [st], bias=eps1[:, 0:1], scale=1.0 / 128)
                S.recip(st[:, 2:3], st[:, 1:2], [st], [st])
                S.tt(T["nwg"][:], nwbc[:], ztm[:, tile, :], ALU.mult, [nwbc, ztm], [T["nwg"]], e="pool")
                S.stt(yb[:], T["os"][:], st[:, 2:3], T["nwg"][:], ALU.mult, ALU.mult, [T["os"], st, T["nwg"]], [yb])
                pY = S.psum()
                pYb = pY[:].bitcast(BF16)
                S.tr(pYb[:, 0:128], yb[:], self.ident_b[:], [yb, self.ident_b], [pY])
                S.cp(zT[:, tsl], pYb[:, 0:128], [pY], [zT], e="act")
            self.out_proj_head(zT, A["gd_w_out"], h, wo, keep_ctx)
        S.pop()

    def mixer_hgrn2(self, layer, keep_ctx):
        assert not keep_ctx, "only the last layer uses HGRN2 here (no context outputs needed)"
        S = self.S
        A = self.A
        W = A["hg_w_in"].rearrange("(c p) n -> p c n", p=128)
        S.push()
        S.push()
        H = self.hnorm()
        zT = S.sb([128, TL], BF16, "zT")
        wo = S.sb([128, D], BF16, "wo_h")
        lbr = S.sb([128, DEPTH, NCH], F32, "lbr")
        for i in range(DEPTH):
            S.dma("sp", lbr[:, i, :], A["hg_lb"][i].rearrange("(c p) -> p c", p=128), writes=[lbr])
        mx = S.sb([128, NCH], F32, "lbmx")
        S.tt(mx[:], lbr[:, 0, :], lbr[:, 1, :], ALU.max, [lbr], [mx])
        for i in range(2, DEPTH):
            S.tt(mx[:], mx[:], lbr[:, i, :], ALU.max, [mx, lbr], [mx])
        for i in range(DEPTH):
            S.tt(lbr[:, i, :], lbr[:, i, :], mx[:], ALU.subtract, [lbr, mx], [lbr])
        S.act(lbr[:], lbr[:], AF.Exp, [lbr], [lbr])
        den = S.sb([128, NCH], F32, "lbden")
        num = S.sb([128, NCH], F32, "lbnum")
        S.tt(den[:], lbr[:, 0, :], lbr[:, 1, :], ALU.add, [lbr], [den])
        for i in range(2, DEPTH):
            S.tt(den[:], den[:], lbr[:, i, :], ALU.add, [den, lbr], [den])
        S.cp(num[:], lbr[:, 1, :], [lbr], [num])
        for i in range(2, layer + 1):
            S.tt(num[:], num[:], lbr[:, i, :], ALU.add, [num, lbr], [num])
        S.recip(den[:], den[:], [den], [den])
        lb = S.sb([128, NCH], F32, "lb")
        oml = S.sb([128, NCH], F32, "oml")
        S.tt(lb[:], num[:], den[:], ALU.mult, [num, den], [lb])
        S.ts(oml[:], lb[:], -1.0, 1.0, ALU.mult, ALU.add, [lb], [oml])
        nwbc = S.sb([128, 128], F32, "nwbc")
        S.dma("sp", nwbc[:], A["hg_norm_w"][0].partition_broadcast(128), writes=[nwbc])
        creset = S.sb([128, 128], F32, "creset")
        S.dma("sp", creset[:], A["chunk_reset"], writes=[creset])
        tri = [S.sb([128, 128], BF16, "tri") for _ in range(2)]
        S.dma("pool", tri[0][:], A["tri_fwd"], writes=[tri[0]])
        S.dma("pool", tri[1][:], A["tri_bwd"], writes=[tri[1]])
        eps1 = self.epsb
        qT = S.sb([128, TL], BF16, "qT")
        vtm = S.sb([128, 18, 128], BF16, "vtm")
        gtm = S.sb([128, 16, 128], BF16, "gtm")
        PV = []
        for _d in range(2):
            pvd = {}
            pvd["logf"] = S.sb([128, 2304], F32, "logf")
            pvd["kf"] = S.sb([128, 2304], BF16, "kf")
            pvd["of"] = S.sb([128, 16, 128], F32, "of")
            if _d == 1:
                mark = S.top
                W5 = S.sb([128, NCH, 5, 128], BF16, "W5")
                sgt = S.sb([128, 512], F32, "sgt")
                top_after = S.top
                S.top = mark
            pvd["Sf"] = S.sb([128, 128], F32, "Sf")
            pvd["Sb"] = S.sb([128, 128], BF16, "Sb")
            pvd["K2"] = S.sb([128, 2, 128], BF16, "K2")
            pvd["Q2"] = S.sb([128, 2, 128], BF16, "Q2")
            pvd["T"] = {nm: S.sb([128, 128], F32, nm) for nm in ("Gi", "G", "df", "dl", "Eq", "Ek", "Ed", "EG", "at")}
            for nm in ("qg", "kdT", "kdtm", "attnT"):
                pvd[nm] = S.sb([128, 128], BF16, nm)
            PV.append(pvd)
        S.top = max(S.top, top_after)
        TR = {nm: S.sb([128, 128], F32, nm) for nm in ("os", "nwg", "ysq")}
        yb = S.sb([128, 128], BF16, "yb")
        st = S.sb([128, 4], F32, "hgst")

        def strided_halves(buf):
            return [buf[:, 0, 0:64], buf[:, 1, 64:128]]

        for h in range(8):
            S.barrier()
            for k in range(5):
                S.dma("pool", W5[:, :, k, :], W[:, :, k * D + h * 128:k * D + (h + 1) * 128], writes=[W5])
            for (grp, o, n) in self.tok_groups(False):
                ps = S.psum()
                for c in range(NCH):
                    S.mm(ps[:, 0:n], W5[:, c, 0, :], H[c][:, o:o + n], c == 0, c == NCH - 1, [W5, H[c]], [ps])
                S.act(qT[:, o:o + n], ps[:, 0:n], AF.Silu, [ps], [qT])
            for t in range(18):
                ps = S.psum()
                for c in range(NCH):
                    S.mm(ps[:, 0:128], H[c][:, t * 128:(t + 1) * 128], W5[:, c, 3, :], c == 0, c == NCH - 1, [H[c], W5], [ps])
                S.cp(vtm[:, t, :], ps[:, 0:128], [ps], [vtm], e=("act" if t % 2 else "dve"))
                if t < 16:
                    ps2 = S.psum()
                    for c in range(NCH):
                        S.mm(ps2[:, 0:128], H[c][:, t * 128:(t + 1) * 128], W5[:, c, 4, :], c == 0, c == NCH - 1,
                             [H[c], W5], [ps2])
                    S.act(gtm[:, t, :], ps2[:, 0:128], AF.Silu, [ps2], [gtm])
            for d in range(2):
                logf, kf = PV[d]["logf"], PV[d]["kf"]
                for (grp, o, n) in self.tok_groups(True):
                    ps = S.psum()
                    for c in range(NCH):
                        S.mm(ps[:, 0:n], W5[:, c, 1 + d, :], H[c][:, o:o + n], c == 0, c == NCH - 1, [W5, H[c]], [ps])
                    S.act(sgt[:, 0:n], ps[:, 0:n], AF.Sigmoid, [ps], [sgt])
                    S.ts(sgt[:, 0:n], sgt[:, 0:n], oml[:, h:h + 1], lb[:, h:h + 1], ALU.mult, ALU.add, [sgt, oml, lb], [sgt])
                    S.act(logf[:, o:o + n], sgt[:, 0:n], AF.Ln, [sgt], [logf])
                    S.ts(kf[:, o:o + n], sgt[:, 0:n], -1.0, 1.0, ALU.mult, ALU.add, [sgt], [kf])
            S.barrier()
            for _d in range(2):
                S.memset(PV[_d]["K2"][:], 0.0, [PV[_d]["K2"]])
                S.memset(PV[_d]["Q2"][:], 0.0, [PV[_d]["Q2"]])

            def run_dir(d):
                pvd = PV[d]
                logf, kf, of, Sf, Sb, K2, Q2, T = (pvd[k] for k in ("logf", "kf", "of", "Sf", "Sb", "K2", "Q2", "T"))
                qg, kdT, kdtm, attnT = (pvd[k] for k in ("qg", "kdT", "kdtm", "attnT"))
                S.memset(Sf[:], 0.0, [Sf])
                S.memset(Sb[:], 0.0, [Sb])
                order = [16, 17] + list(range(16)) if d == 0 else [17, 16] + list(range(15, -1, -1))
                for tile in order:
                    o0 = tile * 128
                    lf = logf[:, o0:o0 + 128]
                    kk = kf[:, o0:o0 + 128]
                    S.scan(T["Gi"][:], creset[:], lf, 0.0, ALU.mult, ALU.add, [creset, logf], [T["Gi"]])
                    if d == 0:
                        G = T["Gi"]
                    else:
                        G = T["G"]
                        S.tt(G[:], lf, T["Gi"][:], ALU.subtract, [logf, T["Gi"]], [G])
                        for ci in range(2):
                            cs = slice(64 * ci, 64 * ci + 64)
                            S.ts(G[:, cs], G[:, cs], T["Gi"][:, 64 * ci + 63:64 * ci + 64], None, ALU.add, None,
                                 [G, T["Gi"]], [G])
                    for ci in range(2):
                        cs = slice(64 * ci, 64 * ci + 64)
                        mid = 64 * ci + 32
                        last = 64 * ci + (63 if d == 0 else 0)
                        S.ts(T["df"][:, cs], G[:, cs], G[:, mid:mid + 1], None, ALU.subtract, None, [G], [T["df"]])
                        S.ts(T["dl"][:, cs], G[:, cs], G[:, last:last + 1], None, ALU.subtract, None, [G], [T["dl"]])
                    S.act(T["Eq"][:], T["df"][:], AF.Exp, [T["df"]], [T["Eq"]])
                    S.act(T["Ek"][:], T["df"][:], AF.Exp, [T["df"]], [T["Ek"]], scale=-1.0)
                    S.act(T["Ed"][:], T["dl"][:], AF.Exp, [T["dl"]], [T["Ed"]], scale=-1.0)
                    S.act(T["EG"][:], G[:], AF.Exp, [G], [T["EG"]])
                    islat = tile < 16
                    if islat:
                        qq = qT[:, o0:o0 + 128]
                        S.stt(qg[:], T["Eq"][:], 1e30, qq, ALU.min, ALU.mult, [T["Eq"], qT], [qg])
                        for ci, dst in enumerate(strided_halves(Q2)):
                            cs = slice(64 * ci, 64 * ci + 64)
                            S.tt(dst, T["EG"][:, cs], qT[:, o0 + 64 * ci:o0 + 64 * ci + 64], ALU.mult, [T["EG"], qT], [Q2])
                        for ci, dst in enumerate(strided_halves(K2)):
                            cs = slice(64 * ci, 64 * ci + 64)
                            S.stt(dst, T["Ek"][:, cs], 1e30, kk[:, cs], ALU.min, ALU.mult, [T["Ek"], kf], [K2])
                    S.tt(kdT[:], T["Ed"][:], kk, ALU.mult, [T["Ed"], kf], [kdT])
                    pT = S.psum()
                    pTb = pT[:].bitcast(BF16)
                    S.tr(pTb[:, 0:128], kdT[:], self.ident_b[:], [kdT, self.ident_b], [pT])
                    S.cp(kdtm[:], pTb[:, 0:128], [pT], [kdtm], e="act")
                    if islat:
                        psA = S.psum()
                        for ci in range(2):
                            cs = slice(64 * ci, 64 * ci + 64)
                            S.mm(psA[:, cs], K2[:, ci, :], qg[:, cs], True, True, [K2, qg], [psA])
                        S.ts(T["at"][:], psA[:, 0:128], 1e30, -1e30, ALU.min, ALU.max, [psA], [T["at"]])
                        S.tt(attnT[:], T["at"][:], tri[d][:], ALU.mult, [T["at"], tri[d]], [attnT])
                        po = S.psum()
                        S.mm(po[:, 0:128], attnT[:], vtm[:, tile, :], True, False, [attnT, vtm], [po])
                    corder = (0, 1) if d == 0 else (1, 0)
                    psS = S.psum()
                    for n_, ci in enumerate(corder):
                        pr = slice(64 * ci, 64 * ci + 64)
                        last = 64 * ci + (63 if d == 0 else 0)
                        if islat:
                            S.mm(po[:, 0:128], Q2[:, ci, :], Sb[:], False, n_ == 1, [Q2, Sb], [po])
                        S.mm(psS[:, 0:128], kdtm[pr, :], vtm[pr, tile, :], True, True, [kdtm, vtm], [psS])
                        S.stt(Sf[:], Sf[:], T["EG"][:, last:last + 1], psS[:, 0:128], ALU.mult, ALU.add,
                              [Sf, T["EG"], psS], [Sf])
                        S.cp(Sb[:], Sf[:], [Sf], [Sb], e="act")
                    if not islat:
                        continue
                    S.cp(of[:, tile, :], po[:, 0:128], [po], [of])

            S.run_interleaved([lambda: run_dir(0), lambda: run_dir(1)])
            T = TR
            for tile in range(16):
                o0 = tile * 128
                S.tt(T["os"][:], PV[1]["of"][:, tile, :], PV[0]["of"][:, tile, :], ALU.add, [PV[1]["of"], PV[0]["of"]], [T["os"]])
                S.act(T["ysq"][:], T["os"][:], AF.Square, [T["os"]], [T["ysq"], st], accum_out=st[:, 0:1])
                S.act(st[:, 1:2], st[:, 0:1], AF.Sqrt, [st, eps1], [st], bias=eps1[:, 0:1], scale=1.0 / 128)
                S.recip(st[:, 2:3], st[:, 1:2], [st], [st])
                S.tt(T["nwg"][:], nwbc[:], gtm[:, tile, :], ALU.mult, [nwbc, gtm], [T["nwg"]], e="pool")
                S.stt(yb[:], T["os"][:], st[:, 2:3], T["nwg"][:], ALU.mult, ALU.mult, [T["os"], st, T["nwg"]], [yb])
                pY = S.psum()
                pYb = pY[:].bitcast(BF16)
                S.tr(pYb[:, 0:128], yb[:], self.ident_b[:], [yb, self.ident_b], [pY])
                S.cp(zT[:, o0:o0 + 128], pYb[:, 0:128], [pY], [zT], e="act")
            self.out_proj_head(zT, A["hg_w_out"], h, wo, False)
        S.pop()
        S.pop()

    def mixer_swa(self, keep_ctx):
        S = self.S
        A = self.A
        W = A["sw_w_in"].rearrange("(c p) n -> p c n", p=128)
        S.push()
        Q = [S.sb([128, 2304], BF16, "Q") for _ in range(8)]
        KD = [S.sb([128, 2304], BF16, "KD") for _ in range(4)]
        V1 = S.sb([128, 18, 4, 65], BF16, "V1")
        S.push()
        H = self.hnorm()
        cosb = S.sb([128, TL], BF16, "cosb")
        sinb = S.sb([128, TL], BF16, "sinb")
        S.dma("pool", cosb[:], A["rope_cos"], writes=[cosb])
        S.dma("pool", sinb[:], A["rope_sin"], writes=[sinb])
        wj = [S.sb([128, NCH, 128], BF16, "wj") for _ in range(2)]
        wjs = [S.sb([128, NCH, 128], BF16, "wjs") for _ in range(2)]
        t1 = S.sb([128, 512], F32, "t1")
        t2 = S.sb([128, 512], F32, "t2")
        S.memset(V1[:, :, :, 64:65], 1.0, [V1])

        def swapped(dst, src):
            d5 = dst[:].rearrange("p c (h two i) -> p c h two i", two=2, i=32)
            s5 = src[:].rearrange("p c (h two i) -> p c h two i", two=2, i=32)
            for c in range(NCH):
                S.cp(d5[:, c, :, 0, :], s5[:, c, :, 1, :], [src], [dst], e="pool")
                S.cp(d5[:, c, :, 1, :], s5[:, c, :, 0, :], [src], [dst], e="pool")

        def project_roped(dst, w, ws):
            for (grp, o, n) in self.tok_groups(True):
                psq = S.psum()
                for c in range(NCH):
                    S.mm(psq[:, 0:n], w[:, c, :], H[c][:, o:o + n], c == 0, c == NCH - 1, [w, H[c]], [psq])
                if grp[0] == "c":
                    S.cp(dst[:, o:o + n], psq[:, 0:n], [psq], [dst], e="act")
                    continue
                pss = S.psum()
                for c in range(NCH):
                    S.mm(pss[:, 0:n], ws[:, c, :], H[c][:, o:o + n], c == 0, c == NCH - 1, [ws, H[c]], [pss])
                S.tt(t1[:, 0:n], psq[:, 0:n], cosb[:, o:o + n], ALU.mult, [psq, cosb], [t1])
                S.tt(t2[:, 0:n], pss[:, 0:n], sinb[:, o:o + n], ALU.mult, [pss, sinb], [t2])
                S.tt(dst[:, o:o + n], t1[:, 0:n], t2[:, 0:n], ALU.add, [t1, t2], [dst], e="pool")

        k = 0
        stage = self.cfg.get("swa_stage", 9)
        for j in range(8 if stage >= 1 else 0):
            w, ws = wj[k % 2], wjs[k % 2]
            k += 1
            S.dma("pool", w[:], W[:, :, j * 128:(j + 1) * 128], writes=[w])
            swapped(ws, w)
            project_roped(Q[j], w, ws)
        for g in range(4 if stage >= 2 else 0):
            w, ws = wj[k % 2], wjs[k % 2]
            k += 1
            for half in range(2):
                S.dma("pool", w[:, :, half * 64:(half + 1) * 64], W[:, :, 1024 + g * 64:1024 + (g + 1) * 64], writes=[w])
            swapped(ws, w)
            project_roped(KD[g], w, ws)
        wv = S.sb([128, NCH, 256], BF16, "wv")
        S.dma("pool", wv[:], W[:, :, 1280:1536], writes=[wv])
        for t in range(18 if stage >= 3 else 0):
            ps = S.psum()
            for c in range(NCH):
                S.mm(ps[:, 0:256], H[c][:, t * 128:(t + 1) * 128], wv[:, c, :], c == 0, c == NCH - 1, [H[c], wv], [ps])
            S.cp(V1[:, t, :, 0:64], ps[:, 0:256].rearrange("p (g d) -> p g d", d=64), [ps], [V1],
                 e=("act" if t % 2 else "dve"))
        S.pop()

        S.push()
        OT = [S.sb([128, 2304], BF16, "OT") for _ in range(8)]
        sinkE = S.sb([128, 16], F32, "sinkE")
        S.dma("sp", sinkE[:], A["sw_sink"][0].partition_broadcast(128), writes=[sinkE])
        S.act(sinkE[:], sinkE[:], AF.Exp, [sinkE], [sinkE])
        mP = S.sb([128, 256], BF16, "mP")
        mN = S.sb([128, 256], BF16, "mN")
        S.dma("pool", mP[:], A["mask_prev"], writes=[mP])
        S.dma("pool", mN[:], A["mask_next"], writes=[mN])
        PT = [S.sb([128, 2, 8, 128], BF16, "PT") for _ in range(2)]
        ob = [S.sb([128, 128], BF16, "ob") for _ in range(2)]
        den = S.sb([128, 4], F32, "den")
        nblk = 18 if keep_ctx else 16
        it = 0
        for j in range(8 if stage >= 4 else 0):
            g = j // 2
            for blk in range(nblk):
                qo = blk * 128
                if blk < 16:
                    tiles = [(16, None), (17, None)]
                    if blk > 0:
                        tiles.append((blk - 1, mP))
                    tiles.append((blk, None))
                    if blk < 15:
                        tiles.append((blk + 1, mN))
                else:
                    tiles = [(16, None), (17, None)]
                nt = len(tiles)
                pt = PT[it % 2]
                o2 = ob[it % 2]
                it += 1
                nb = (nt + 3) // 4
                pss = [[S.psum() for _ in range(nb)] for hh in range(2)]
                for hh in range(2):
                    pr = slice(hh * 64, (hh + 1) * 64)
                    for k2, (kt, msk) in enumerate(tiles):
                        ps = pss[hh][k2 // 4]
                        co = (k2 % 4) * 128
                        S.mm(ps[:, co:co + 128], KD[g][pr, kt * 128:(kt + 1) * 128], Q[j][pr, qo:qo + 128], True, True,
                             [KD[g], Q[j]], [ps])
                for hh in range(2):
                    for b in range(nb):
                        w_ = min(4, nt - 4 * b) * 128
                        S.act(pt[:, hh, 4 * b:4 * b + 4, :].rearrange("p a b -> p (a b)")[:, 0:w_], pss[hh][b][:, 0:w_],
                              AF.Exp, [pss[hh][b]], [pt], scale=0.125)
                for k2, (kt, msk) in enumerate(tiles):
                    if msk is not None:
                        for hh in range(2):
                            S.tt(pt[:, hh, k2, :], pt[:, hh, k2, :], msk[:, 0:128], ALU.mult, [pt, msk], [pt], e="pool")
                po = S.psum()
                for hh in range(2):
                    for k2, (kt, msk) in enumerate(tiles):
                        S.mm(po[:, hh * 65:(hh + 1) * 65], pt[:, hh, k2, :], V1[:, kt, g, :],
                             k2 == 0, k2 == nt - 1, [pt, V1], [po])
                for hh in range(2):
                    h = 2 * j + hh
                    S.tt(den[:, hh:hh + 1], po[:, hh * 65 + 64:hh * 65 + 65], sinkE[:, h:h + 1], ALU.add,
                         [po, sinkE], [den])
                S.recip(den[:, 2:4], den[:, 0:2], [den], [den])
                for hh in range(2):
                    S.ts(o2[:, hh * 64:(hh + 1) * 64], po[:, hh * 65:hh * 65 + 64], den[:, 2 + hh:3 + hh], None,
                         ALU.mult, None, [po, den], [o2])
                pT = S.psum()
                pTb = pT[:].bitcast(BF16)
                S.tr(pTb[:, 0:128], o2[:], self.ident_b[:], [o2, self.ident_b], [pT])
                S.cp(OT[j][:, qo:qo + 128], pTb[:, 0:128], [pT], [OT[j]], e="act")
        if stage >= 5:
            self.out_proj(OT, A["sw_w_out"], keep_ctx)
        S.pop()
        S.pop()


def make_in_maps(inputs, consts, n_cores=8):
    f = lambda a: np.ascontiguousarray(np.asarray(a, dtype=np.float32))
    shared = {
        "ada_w": f(inputs["ada_w"]).reshape(DEPTH * D, 6 * D),
        "ada_b": f(inputs["ada_b"]),
        "norm1_w": f(inputs["norm1_w"]),
        "norm2_w": f(inputs["norm2_w"]),
        "final_norm_w": f(inputs["final_norm_w"]).reshape(1, D),
        "moe_router": f(inputs["moe_router"]).reshape(DEPTH * D, NE),
        "moe_w_gate": f(inputs["moe_w_gate"]).reshape(DEPTH * NE * D, FF),
        "moe_w_up": f(inputs["moe_w_up"]).reshape(DEPTH * NE * D, FF),
        "moe_w_down": f(inputs["moe_w_down"]).reshape(DEPTH * NE * FF, D),
        "hy_w_in": f(inputs["hy_w_in"]), "hy_b_in": f(inputs["hy_b_in"]).reshape(1, 3 * D),
        "hy_short_w": f(inputs["hy_short_w"]), "hy_short_b": f(inputs["hy_short_b"]).reshape(1, 3 * D),
        "hy_ffn_w1": f(inputs["hy_ffn_w1"]), "hy_ffn_b1": f(inputs["hy_ffn_b1"]).reshape(1, 64),
        "hy_ffn_w2": f(inputs["hy_ffn_w2"]), "hy_ffn_b2": f(inputs["hy_ffn_b2"]).reshape(1, 64),
        "hy_ffn_w3": f(inputs["hy_ffn_w3"]), "hy_sin_freq": f(inputs["hy_sin_freq"]),
        "hy_filter_bias": f(inputs["hy_filter_bias"]), "hy_w_out": f(inputs["hy_w_out"]),
        "hy_b_out": f(inputs["hy_b_out"]).reshape(1, D),
        "gd_w_in": f(inputs["gd_w_in"]),
        "gd_conv_w": f(inputs["gd_conv_w"]),
        "gd_a_log": f(inputs["gd_a_log"]).reshape(1, 16),
        "gd_dt_bias": f(inputs["gd_dt_bias"]).reshape(1, 16),
        "gd_norm_w": f(inputs["gd_norm_w"]).reshape(1, 128),
        "gd_w_out": f(inputs["gd_w_out"]),
        "hg_w_in": f(inputs["hg_w_in"]),
        "hg_lb": f(inputs["hg_lb"]),
        "hg_norm_w": f(inputs["hg_norm_w"]).reshape(1, 128),
        "hg_w_out": f(inputs["hg_w_out"]),
        "sw_w_in": f(inputs["sw_w_in"]),
        "sw_sink": f(inputs["sw_sink"]).reshape(1, 16),
        "sw_w_out": f(inputs["sw_w_out"]),
    }
    for k, v in consts.items():
        shared["k_" + k] = v
    maps = []
    for b in range(n_cores):
        mp = dict(shared)
        mp["x"] = f(inputs["x"][b])
        mp["ctx"] = f(inputs["ctx"][b])
        mp["cc"] = np.stack([f(inputs["c"][b]), f(inputs["c_ctx"])], 0)
        maps.append(mp)
    return maps


def kernel(**inputs):
    p = Prog({})
    nc = p.build()
    maps = make_in_maps(inputs, p.consts, 8)
    res = run_bass_kernel_spmd(nc, maps, core_ids=list(range(8)))
    return np.stack([r["out"] for r in res.results], 0).astype(np.float32)
```

```python
import numpy as np
from contextlib import ExitStack
import concourse.bass as bass
import concourse.mybir as mybir
from concourse.bass_utils import run_bass_kernel_spmd

F32 = mybir.dt.float32
BF16 = mybir.dt.bfloat16
I32 = mybir.dt.int32
AF = mybir.ActivationFunctionType
ALU = mybir.AluOpType
AX = mybir.AxisListType

D = 1024
NCH = 8
TL = 2048
TC = 256
DEPTH = 4
NE = 16
FF = 1024
EPS = 1e-6
NDS = 12


class Buf:
    __slots__ = ("t", "w", "r", "name", "psum")

    def __init__(self, t, name=""):
        self.t = t
        self.w = None
        self.r = {}
        self.name = name
        self.psum = False

    def __getitem__(self, idx):
        return self.t[idx]


class Sched:
    def __init__(self, nc):
        self.nc = nc
        self.eng = {"pe": nc.tensor, "act": nc.scalar, "dve": nc.vector, "pool": nc.gpsimd, "sp": nc.sync}
        self.semstack = ExitStack()
        self.csem = {e: self.semstack.enter_context(nc.semaphore("s_" + e)) for e in ("pe", "act", "dve", "pool")}
        self.prog = {e: [] for e in self.eng}
        self.ccnt = {e: 0 for e in self.csem}
        self.seen = {e: {} for e in self.eng}
        self.dq = {}
        for q in ("sp", "pool"):
            self.dq[q] = {"sems": [self.semstack.enter_context(nc.semaphore("d_%s%d" % (q, i))) for i in range(NDS)],
                          "vals": [0] * NDS, "rr": 0}
        self.nbuf = 0
        import threading
        self._coop = None
        self._tls = threading.local()
        self.stacks = []
        self.arena = None
        self.ps = [self.psum_raw("psb%d" % i) for i in range(8)]
        self.ps_rr = 0
        self.n_inst = 0

    def sb(self, shape, dt=F32, name=None):
        self.nbuf += 1
        nm = "%s_%d" % (name or "b", self.nbuf)
        if self.arena is None:
            self.arena_words = 212800 // 4
            self.arena = self.nc.alloc_sbuf_tensor("arena", [128, self.arena_words], F32)
            self.top = 0
        esz = 2 if dt == BF16 else 4
        n = 1
        for d_ in shape[1:]:
            n *= d_
        words = (n * esz + 3) // 4
        words = (words + 7) // 8 * 8
        assert self.top + words <= self.arena_words, "SBUF arena overflow allocating %s %s (top=%d)" % (nm, shape, self.top * 4)
        ap = self.arena[0:shape[0], self.top:self.top + words]
        self.top += words
        if dt != F32:
            ap = ap.bitcast(dt)
        ap = ap[:, 0:n]
        if len(shape) == 3:
            ap = ap.rearrange("p (a b) -> p a b", b=shape[2])
        elif len(shape) == 4:
            ap = ap.rearrange("p (a b c) -> p a b c", b=shape[2], c=shape[3])
        return Buf(ap, nm)

    def push(self):
        self.stacks.append(self.top)

    def pop(self):
        self.barrier()
        self.top = self.stacks.pop()

    def barrier(self):
        evs = [(self.csem[e], self.ccnt[e], e) for e in self.csem if self.ccnt[e] > 0]
        for q in self.dq.values():
            for sem, v in zip(q["sems"], q["vals"]):
                if v > 0:
                    evs.append((sem, v, "dma"))
        for e in self.eng:
            for ev in evs:
                if e == "pe" and ev[2] == "pe":
                    continue
                self._wait(e, ev)

    def psum_raw(self, name):
        b = Buf(self.nc.alloc_psum_tensor(name, [128, 512], F32), name)
        b.psum = True
        return b

    def psum(self):
        co = self._coop
        if co is not None:
            i = self._tls.idx
            b = self.ps[4 * i + co["rr"][i]]
            co["rr"][i] = (co["rr"][i] + 1) % 4
            return b
        b = self.ps[self.ps_rr]
        self.ps_rr = (self.ps_rr + 1) % 8
        return b

    def run_interleaved(self, fns):
        import threading
        assert len(fns) == 2
        cv = threading.Condition()
        co = {"turn": 0, "alive": [True, True], "rr": [0, 0], "cv": cv, "err": []}
        self._coop = co

        def body(i, fn):
            self._tls.idx = i
            with cv:
                while co["turn"] != i:
                    cv.wait()
            try:
                fn()
            except BaseException as ex:
                co["err"].append(ex)
            with cv:
                co["alive"][i] = False
                co["turn"] = 1 - i
                cv.notify_all()

        th = [threading.Thread(target=body, args=(i, f)) for i, f in enumerate(fns)]
        for t in th:
            t.start()
        for t in th:
            t.join()
        self._coop = None
        if co["err"]:
            raise co["err"][0]

    def _yield(self):
        co = self._coop
        if co is None:
            return
        i = self._tls.idx
        if not co["alive"][1 - i]:
            return
        cv = co["cv"]
        with cv:
            co["turn"] = 1 - i
            cv.notify_all()
            while co["turn"] != i and co["alive"][1 - i]:
                cv.wait()

    def _wait(self, e, ev):
        sem, val, src = ev
        k = id(sem)
        if self.seen[e].get(k, 0) < val:
            self.prog[e].append(("w", sem, val))
            self.seen[e][k] = val

    def _deps(self, e, reads, writes):
        for b in reads:
            if b.w is not None and not (e == "pe" and b.w[2] == "pe"):
                self._wait(e, b.w)
            if b.psum:
                for ev in b.r.values():
                    if ev[2] != e:
                        self._wait(e, ev)
        for b in writes:
            if b.w is not None and not (e == "pe" and b.w[2] == "pe"):
                self._wait(e, b.w)
            for ev in b.r.values():
                if not (e == "pe" and ev[2] == "pe"):
                    self._wait(e, ev)

    def _mark(self, ev, reads, writes):
        k = id(ev[0])
        for b in reads:
            b.r[k] = ev
        for b in writes:
            b.w = ev
            b.r = {}

    def op(self, e, fn, reads=(), writes=()):
        self._deps(e, reads, writes)
        self.ccnt[e] += 1
        self.prog[e].append(("i", fn, self.csem[e], 1))
        self._mark((self.csem[e], self.ccnt[e], e), reads, writes)
        self.n_inst += 1
        self._yield()

    def dma(self, q, out, in_, reads=(), writes=(), **kw):
        d = self.dq[q]
        i = d["rr"]
        d["rr"] = (i + 1) % NDS
        sem = d["sems"][i]
        if d["vals"][i] > 0:
            self._wait(q, (sem, d["vals"][i], "dma"))
        self._deps(q, reads, writes)
        self.prog[q].append(("i", lambda g: g.dma_start(out=out, in_=in_, allow_slow_non_contiguous=True, **kw), sem, 16))
        d["vals"][i] += 16
        ev = (sem, d["vals"][i], "dma")
        self._mark(ev, reads, writes)
        self.n_inst += 1
        return ev

    def emit(self):
        prog = self.prog

        def replay(name, eng):
            for it in prog[name]:
                if it[0] == "w":
                    eng.wait_ge(it[1], it[2])
                else:
                    it[1](eng).then_inc(it[2], it[3])

        with self.nc.Block() as block:
            @block.sync
            def _(eng):
                replay("sp", eng)

            @block.tensor
            def _(eng):
                replay("pe", eng)

            @block.scalar
            def _(eng):
                replay("act", eng)

            @block.vector
            def _(eng):
                replay("dve", eng)

            @block.gpsimd
            def _(eng):
                replay("pool", eng)

    def mm(self, out, lhsT, rhs, start, stop, reads, writes, **kw):
        self.op("pe", lambda e: e.matmul(out, lhsT, rhs, start=start, stop=stop, **kw), reads, writes)

    def tr(self, out, in_, ident, reads, writes):
        self.op("pe", lambda e: e.transpose(out, in_, ident), reads, writes)

    def trf(self, out, in_, ident, reads, writes):
        self.op("pe", lambda e: e.matmul(out, in_, ident, start=True, stop=True), reads, writes)

    def act(self, out, in_, func, reads, writes, bias=None, scale=None, accum_out=None):
        kw = {}
        if bias is not None:
            kw["bias"] = bias
        if scale is not None:
            kw["scale"] = scale
        if accum_out is not None:
            kw["accum_out"] = accum_out
        self.op("act", lambda e: e.activation(out, in_, func, **kw), reads, writes)

    def ts(self, out, in0, s1, s2, op0, op1, reads, writes, e="dve", accum_out=None):
        if op1 is None:
            self.op(e, lambda g: g.tensor_scalar(out, in0, s1, None, op0, accum_out=accum_out) if accum_out is not None
                    else g.tensor_scalar(out, in0, s1, None, op0), reads, writes)
        else:
            self.op(e, lambda g: g.tensor_scalar(out, in0, s1, s2, op0, op1, accum_out=accum_out) if accum_out is not None
                    else g.tensor_scalar(out, in0, s1, s2, op0, op1), reads, writes)

    def tt(self, out, in0, in1, op, reads, writes, e="dve"):
        self.op(e, lambda g: g.tensor_tensor(out, in0, in1, op), reads, writes)

    def stt(self, out, in0, scalar, in1, op0, op1, reads, writes):
        self.op("dve", lambda g: g.scalar_tensor_tensor(out, in0, scalar, in1, op0, op1), reads, writes)

    def recip(self, out, in_, reads, writes):
        self.op("dve", lambda g: g.reciprocal(out, in_), reads, writes)

    def red(self, out, in_, op, reads, writes, negate=False):
        self.op("dve", lambda g: g.tensor_reduce(out, in_, AX.X, op, negate=negate), reads, writes)

    def max8(self, out, in_, reads, writes):
        self.op("dve", lambda g: g.max(out, in_), reads, writes)

    def mrep(self, out, in_to_replace, in_values, imm, reads, writes):
        self.op("dve", lambda g: g.match_replace(out, in_to_replace, in_values, imm), reads, writes)

    def scan(self, out, d0, d1, init, op0, op1, reads, writes):
        self.op("dve", lambda g: g.tensor_tensor_scan(out, d0, d1, init, op0, op1), reads, writes)

    def memset(self, out, val, writes, e="dve"):
        self.op(e, lambda g: g.memset(out, val), [], writes)

    def cp(self, out, in_, reads, writes, e="dve"):
        if e == "act":
            self.op("act", lambda g: g.activation(out, in_, AF.Identity), reads, writes)
        else:
            self.op(e, lambda g: g.tensor_copy(out, in_), reads, writes)


def host_consts():
    c = {}
    c["ident_f"] = np.eye(128, dtype=np.float32)
    c["ones_f"] = np.ones((128, 128), np.float32)
    io = np.zeros((128, 288), np.float32)
    io[:] = np.arange(288, dtype=np.float32)[None, :]
    c["iota_row"] = io
    ip = np.zeros((128, 4), np.float32)
    for k in range(4):
        ip[:, k] = np.arange(128) + 128 * k
    c["iota_part"] = ip
    sel = np.zeros((16, 16, 128), np.float32)
    for e in range(16):
        sel[e, e, :] = 1.0
    c["sel"] = sel.reshape(16, 16 * 128)
    t = np.arange(TL)
    row = (t // 64).astype(np.float32)
    col = (t % 64).astype(np.float32)
    inv = (10000.0 ** (-np.arange(16, dtype=np.float32) / 16)).astype(np.float32)
    ang = np.concatenate([row[:, None] * inv, col[:, None] * inv], -1).astype(np.float32)
    cosf = np.zeros((128, TL), np.float32)
    sinf = np.zeros((128, TL), np.float32)
    for p in range(128):
        i = p % 64
        cosf[p] = np.cos(ang[:, i % 32])
        sinf[p] = np.sin(ang[:, i % 32]) * (-1.0 if i < 32 else 1.0)
    c["rope_cos"] = cosf
    c["rope_sin"] = sinf
    jj = np.arange(128)[:, None]
    ii = np.arange(128)[None, :]
    mp = (jj >= ii).astype(np.float32)
    mn = (jj <= ii).astype(np.float32)
    cm = np.ones((128, 128), np.float32)
    cm[:, 0] = 0.0
    cm[:, 64] = 0.0
    c["chunk_reset"] = cm
    blk = (jj // 64 == ii // 64)
    c["tri_fwd"] = (blk & (jj <= ii)).astype(np.float32)
    c["tri_bwd"] = (blk & (jj >= ii)).astype(np.float32)
    import ml_dtypes
    for nm, L in (("l", TL), ("c", TC)):
        N = 2 * L
        T_ = L // 128
        tt_ = np.linspace(0.0, 1.0, L, dtype=np.float32)
        w_ = (2.0 * np.pi * np.arange(L, dtype=np.float32) / L).astype(np.float32)
        f_ = np.linspace(1e-4, 15.0, 16, dtype=np.float32)[None, :]
        z_ = np.concatenate([tt_[:, None], np.cos(f_ * w_[:, None]), -np.sin(f_ * w_[:, None])], -1).astype(np.float32)
        c["hy_zT_" + nm] = np.ascontiguousarray(z_.T)
        c["hy_tcol_" + nm] = np.ascontiguousarray(tt_.reshape(T_, 128).T)
        tpos = np.arange(L, dtype=np.float64)
        fr = np.arange(L, dtype=np.float64) + 0.5
        th = 2.0 * np.pi * np.outer(tpos, fr) / N
        for tn, tab in (("C", np.cos(th)), ("S", np.sin(th))):
            fw = tab.reshape(T_, 128, T_, 128).transpose(2, 1, 0, 3)
            c["hy_%sf_%s" % (tn, nm)] = np.ascontiguousarray(fw).reshape(T_ * 128, T_ * 128).astype(ml_dtypes.bfloat16)
            c["hy_%st_%s" % (tn, nm)] = np.ascontiguousarray(tab.T).astype(ml_dtypes.bfloat16)
    maxd = np.log(1e-2) / 0.3
    mind = np.log(1e-2) / 1.5
    c["hy_delta"] = np.abs(np.linspace(mind, maxd, D, dtype=np.float32)).reshape(1, D).astype(np.float32)
    eye = np.eye(128, dtype=np.float32)
    c["tri_fwd_s"] = c["tri_fwd"] - eye
    c["tri_bwd_s"] = c["tri_bwd"] - eye
    c["mask_prev"] = np.concatenate([mp, mp], 1)
    c["mask_next"] = np.concatenate([mn, mn], 1)
    return c


class Prog:
    def __init__(self, cfg):
        self.cfg = cfg
        self.nc = bass.Bass("TRN2", target_bir_lowering=False)
        self.S = Sched(self.nc)
        self.din = {}
        self.consts = host_consts()

    def dram_in(self, name, shape, dt=F32):
        t = self.nc.dram_tensor(name, list(shape), dt, kind="ExternalInput")
        self.din[name] = t
        return t.ap()

    def declare(self):
        nc = self.nc
        A = {}
        A["x"] = self.dram_in("x", [TL, D])
        A["ctx"] = self.dram_in("ctx", [TC, D])
        A["cc"] = self.dram_in("cc", [2, D])
        A["ada_w"] = self.dram_in("ada_w", [DEPTH * D, 6 * D])
        A["ada_b"] = self.dram_in("ada_b", [DEPTH, 6 * D])
        A["norm1_w"] = self.dram_in("norm1_w", [DEPTH, D])
        A["norm2_w"] = self.dram_in("norm2_w", [DEPTH, D])
        A["final_norm_w"] = self.dram_in("final_norm_w", [1, D])
        A["moe_router"] = self.dram_in("moe_router", [DEPTH * D, NE])
        A["moe_w_gate"] = self.dram_in("moe_w_gate", [DEPTH * NE * D, FF])
        A["moe_w_up"] = self.dram_in("moe_w_up", [DEPTH * NE * D, FF])
        A["moe_w_down"] = self.dram_in("moe_w_down", [DEPTH * NE * FF, D])
        A["gd_w_in"] = self.dram_in("gd_w_in", [D, 4128])
        A["gd_conv_w"] = self.dram_in("gd_conv_w", [3, 3072])
        A["gd_a_log"] = self.dram_in("gd_a_log", [1, 16])
        A["gd_dt_bias"] = self.dram_in("gd_dt_bias", [1, 16])
        A["gd_norm_w"] = self.dram_in("gd_norm_w", [1, 128])
        A["gd_w_out"] = self.dram_in("gd_w_out", [D, D])
        A["hg_w_in"] = self.dram_in("hg_w_in", [D, 5 * D])
        A["hg_lb"] = self.dram_in("hg_lb", [DEPTH, D])
        A["hg_norm_w"] = self.dram_in("hg_norm_w", [1, 128])
        A["hg_w_out"] = self.dram_in("hg_w_out", [D, D])
        A["sw_w_in"] = self.dram_in("sw_w_in", [D, 1536])
        A["sw_sink"] = self.dram_in("sw_sink", [1, 16])
        A["sw_w_out"] = self.dram_in("sw_w_out", [D, D])
        for nm, shp in (("hy_w_in", [D, 3 * D]), ("hy_b_in", [1, 3 * D]), ("hy_short_w", [3, 3 * D]),
                        ("hy_short_b", [1, 3 * D]), ("hy_ffn_w1", [33, 64]), ("hy_ffn_b1", [1, 64]),
                        ("hy_ffn_w2", [64, 64]), ("hy_ffn_b2", [1, 64]), ("hy_ffn_w3", [64, 4 * D]),
                        ("hy_sin_freq", [2, 64]), ("hy_filter_bias", [2, D]), ("hy_w_out", [D, D]),
                        ("hy_b_out", [1, D])):
            A[nm] = self.dram_in(nm, shp)
        for k, v in self.consts.items():
            A[k] = self.dram_in("k_" + k, list(v.shape), BF16 if v.dtype != np.float32 else F32)
        self.out = nc.dram_tensor("out", [TL, D], F32, kind="ExternalOutput").ap()
        if self.cfg.get("dump_ctx"):
            self.out_c = nc.dram_tensor("out_c", [TC, D], F32, kind="ExternalOutput").ap()
        self.A = A

    def setup(self):
        S = self.S
        A = self.A
        self.ident_f = S.sb([128, 128], F32, "identf")
        self.ident_b = S.sb([128, 128], BF16, "identb")
        self.ones_b = S.sb([128, 128], BF16, "onesb")
        self.iota_row = S.sb([128, 256], F32, "iotar")
        self.iota_part = S.sb([128, 4], F32, "iotap")
        self.sel = S.sb([16, 16 * 128], BF16, "sel")
        self.epsb = S.sb([128, 1], F32, "eps")
        self.hs_tok = S.sb([1, 8], F32, "hstok")
        self.RL = [[S.sb([128, 512], F32, "RL") for g in range(4)] for c in range(NCH)]
        self.RC = [S.sb([128, 256], F32, "RC") for c in range(NCH)]
        self.sc = S.sb([128, NCH, 2], F32, "sc")
        self.modT = S.sb([128, 48, 2], F32, "modT")
        self.a1 = [S.sb([128, NCH], F32, "a1") for _ in range(2)]
        self.a2 = [S.sb([128, NCH], F32, "a2") for _ in range(2)]
        S.dma("sp", self.ident_f[:], A["ident_f"], writes=[self.ident_f])
        S.dma("sp", self.iota_row[:], A["iota_row"][:, 0:256], writes=[self.iota_row])
        S.dma("sp", self.iota_part[:], A["iota_part"], writes=[self.iota_part])
        S.push()
        tmp = S.sb([128, 128], F32, "onesf")
        tsel = S.sb([16, 16 * 128], F32, "tsel")
        craw = S.sb([128, NCH, 2], F32, "craw")
        S.dma("sp", tsel[:], A["sel"], writes=[tsel])
        S.dma("sp", tmp[:], A["ones_f"], writes=[tmp])
        S.cp(self.sel[:], tsel[:], [tsel], [self.sel])
        S.cp(self.ones_b[:], tmp[:], [tmp], [self.ones_b])
        S.cp(self.ident_b[:], self.ident_f[:], [self.ident_f], [self.ident_b])
        S.memset(self.epsb[:], EPS, [self.epsb])
        with self.nc.allow_non_contiguous_dma("tiny"):
            for j in range(2):
                S.dma("sp", craw[:, :, j], A["cc"][j].rearrange("(c p) -> p c", p=128), writes=[craw])
        S.act(self.sc[:], craw[:], AF.Silu, [craw], [self.sc])
        S.pop()
        self.groups = [("l", g, g * 512, 512) for g in range(4)] + [("c", 0, 0, 256)]

    def dbg(self, name, ap, buf, shape):
        if not self.cfg.get("debug"):
            return
        d = self.nc.dram_tensor("dbg_" + name, list(shape), ap.dtype, kind="ExternalOutput").ap()
        ev = self.S.dma("sp", d, ap, reads=[buf])
        self.S._wait("sp", ev)

    def rbuf(self, grp, c):
        return self.RL[c][grp[1]] if grp[0] == "l" else self.RC[c]

    def load_stream(self):
        S = self.S
        A = self.A
        S.push()
        tin = [S.sb([128, D], F32, "tin") for _ in range(2)]
        k = 0
        for grp in self.groups:
            src = A["x"] if grp[0] == "l" else A["ctx"]
            for tt in range(grp[3] // 128):
                t0 = grp[2] + tt * 128
                tb = tin[k % 2]
                if k >= self.cfg.get("ls_tiles", 99):
                    continue
                k += 1
                S.dma("sp", tb[:], src[t0:t0 + 128, :], writes=[tb])
                for half in range(2):
                    ps = S.psum()
                    for j in range(4):
                        c = half * 4 + j
                        S.trf(ps[:, j * 128:(j + 1) * 128], tb[:, c * 128:(c + 1) * 128], self.ident_f[:],
                             [tb, self.ident_f], [ps])
                    for j in range(4):
                        c = half * 4 + j
                        rb = self.rbuf(grp, c)
                        S.cp(rb[:, tt * 128:(tt + 1) * 128], ps[:, j * 128:(j + 1) * 128], [ps], [rb],
                             e=("act" if j % 2 else "dve"))
        S.pop()

    def adaln(self, layer):
        S = self.S
        A = self.A
        S.push()
        modT = self.modT
        adab = S.sb([128, 48], F32, "adab")
        with self.nc.allow_non_contiguous_dma("tiny"):
            S.dma("sp", adab[:], A["ada_b"][layer].rearrange("(j p) -> p j", p=128), writes=[adab])
        wv = A["ada_w"][layer * D:(layer + 1) * D, :].rearrange("(c p) n -> p c n", p=128)
        wb = [S.sb([128, NCH, 512], F32, "adaw") for _ in range(2)]
        ps = S.psum()
        for piece in range(12):
            w = wb[piece % 2]
            S.dma("sp", w[:], wv[:, :, piece * 512:(piece + 1) * 512], writes=[w])
            for jj in range(4):
                j = piece * 4 + jj
                for c in range(NCH):
                    S.mm(ps[:, 2 * j:2 * j + 2], w[:, c, jj * 128:(jj + 1) * 128], self.sc[:, c, :],
                         c == 0, c == NCH - 1, [w, self.sc], [ps])
        for s in range(2):
            S.tt(modT[:, :, s], ps[:, 0:96].rearrange("p (j s) -> p j s", s=2)[:, :, s], adab[:], ALU.add,
                 [ps, adab], [modT])
        n1 = S.sb([128, NCH], F32, "n1w")
        n2 = S.sb([128, NCH], F32, "n2w")
        with self.nc.allow_non_contiguous_dma("tiny"):
            S.dma("sp", n1[:], A["norm1_w"][layer].rearrange("(c p) -> p c", p=128), writes=[n1])
            S.dma("sp", n2[:], A["norm2_w"][layer].rearrange("(c p) -> p c", p=128), writes=[n2])
        M = {}
        for s, sn in enumerate(("l", "c")):
            S.stt(self.a1[s][:], modT[:, 8:16, s], 1.0, n1[:], ALU.add, ALU.mult, [modT, n1], [self.a1[s]])
            S.stt(self.a2[s][:], modT[:, 32:40, s], 1.0, n2[:], ALU.add, ALU.mult, [modT, n2], [self.a2[s]])
            M[sn] = {"a1": self.a1[s], "a2": self.a2[s], "modT": modT, "s": s}
        self.M = M
        S.pop()
        return M

    def mvec(self, sn, which, c):
        m = self.M[sn]
        return m["modT"][:, which * 8 + c, m["s"]:m["s"] + 1]

    def norm_scratch(self):
        S = self.S
        self._sq = S.sb([128, NCH, 512], BF16, "sq")
        self._rstd = S.sb([128, 512], F32, "rstd")
        self._ntmp = S.sb([128, 512], F32, "ntmp")

    def rstd_group(self, grp):
        S = self.S
        n = grp[3]
        sq, rstd = self._sq, self._rstd
        rbs = [self.rbuf(grp, c) for c in range(NCH)]
        for c in range(NCH):
            S.act(sq[:, c, 0:n], rbs[c][:, 0:n], AF.Square, [rbs[c]], [sq])
        ps = S.psum()
        for c in range(NCH):
            S.mm(ps[:, 0:n], self.ones_b[:], sq[:, c, 0:n], c == 0, c == NCH - 1, [self.ones_b, sq], [ps])
        S.act(rstd[:, 0:n], ps[:, 0:n], AF.Sqrt, [ps, self.epsb], [rstd], bias=self.epsb[:, 0:1], scale=1.0 / D)
        S.recip(rstd[:, 0:n], rstd[:, 0:n], [rstd], [rstd])
        return rbs, rstd

    def norm_group(self, grp, a_buf, a_which_shift, sn, out_bf=None, out_f=None):
        S = self.S
        n = grp[3]
        tmp = self._ntmp
        rbs, rstd = self.rstd_group(grp)
        for c in range(NCH):
            S.stt(tmp[:, 0:n], rbs[c][:, 0:n], a_buf[:, c:c + 1], rstd[:, 0:n], ALU.mult, ALU.mult,
                  [rbs[c], a_buf, rstd], [tmp])
            sh = self.mvec(sn, a_which_shift, c)
            if out_f is not None:
                S.act(out_f[c][0], tmp[:, 0:n], AF.Identity, [tmp, self.modT], [out_f[c][1]], bias=sh)
            if out_bf is not None:
                S.act(out_bf[c][0], tmp[:, 0:n], AF.Identity, [tmp, self.modT], [out_bf[c][1]], bias=sh)

    def moe_layer(self, layer, keep_ctx):
        S = self.S
        A = self.A
        S.push()
        m = {}
        m["h2tm"] = [S.sb([128, D], BF16, "h2tm") for _ in range(18)]
        m["slot_tm"] = S.sb([128, 18, NE], F32, "slottm")
        m["affTb"] = S.sb([16, 2304], BF16, "affTb")
        m["slotTb"] = S.sb([16, 2304], BF16, "slotTb")
        m["rw"] = S.sb([128, NCH, NE], F32, "rw")
        m["sm"] = S.sb([128, 4], F32, "sm")
        m["m8"] = S.sb([16, 8], F32, "m8")
        groups = self.groups if keep_ctx else self.groups[:4]
        ntiles = 18 if keep_ctx else 16
        ncap = 288 if keep_ctx else 256
        with self.nc.allow_non_contiguous_dma("tiny"):
            S.dma("sp", m["rw"][:], A["moe_router"][layer * D:(layer + 1) * D, :].rearrange("(c p) e -> p c e", p=128),
                  writes=[m["rw"]])
        self._wk = 0

        def wsrc(kind, e):
            nm = {"g": "moe_w_gate", "u": "moe_w_up", "d": "moe_w_down"}[kind]
            base = (layer * NE + e) * D
            return A[nm][base:base + D, :].rearrange("(c p) n -> p c n", p=128)

        def wload(kind, e):
            b = m["w"][self._wk % 2]
            self._wk += 1
            if self.cfg.get("moe_noload") and self._wk > 2:
                return b
            S.dma("pool", b[:], wsrc(kind, e), writes=[b])
            return b

        S.push()
        m["affT"] = S.sb([16, 2304], F32, "affT")
        m["aff_tm"] = S.sb([128, 18, NE], F32, "afftm")
        S.push()
        self.norm_scratch()
        m["hf"] = [S.sb([128, 512], F32, "hf") for _ in range(NCH)]
        m["hb"] = [S.sb([128, 512], BF16, "hb") for _ in range(NCH)]
        sm = m["sm"]
        tile = 0
        for grp in groups:
            sn = grp[0]
            n = grp[3]
            self.norm_group(grp, self.M[sn]["a2"], 3, sn,
                            out_bf=[(m["hb"][c][:, 0:n], m["hb"][c]) for c in range(NCH)],
                            out_f=[(m["hf"][c][:, 0:n], m["hf"][c]) for c in range(NCH)])
            for tt in range(n // 128):
                tsl = slice(tt * 128, (tt + 1) * 128)
                ps = S.psum()
                for c in range(NCH):
                    S.mm(ps[:, 0:NE], m["hf"][c][:, tsl], m["rw"][:, c, :], c == 0, c == NCH - 1,
                         [m["hf"][c], m["rw"]], [ps])
                S.red(sm[:, 0:1], ps[:, 0:NE], ALU.max, [ps], [sm], negate=True)
                S.act(m["aff_tm"][:, tile, :], ps[:, 0:NE], AF.Exp, [ps, sm], [m["aff_tm"], sm], bias=sm[:, 0:1],
                      accum_out=sm[:, 1:2])
                S.recip(sm[:, 2:3], sm[:, 1:2], [sm], [sm])
                S.ts(m["aff_tm"][:, tile, :], m["aff_tm"][:, tile, :], sm[:, 2:3], None, ALU.mult, None,
                     [m["aff_tm"], sm], [m["aff_tm"]])
                ps2 = S.psum()
                S.trf(ps2[0:NE, 0:128], m["aff_tm"][:, tile, :], self.ident_f[:], [m["aff_tm"], self.ident_f], [ps2])
                S.cp(m["affT"][:, tile * 128:(tile + 1) * 128], ps2[0:NE, 0:128], [ps2], [m["affT"]], e="act")
                ps3 = S.psum()
                psb = ps3[:].bitcast(BF16)
                for c in range(NCH):
                    S.tr(psb[:, c * 128:(c + 1) * 128], m["hb"][c][:, tsl], self.ident_b[:],
                         [m["hb"][c], self.ident_b], [ps3])
                S.cp(m["h2tm"][tile][:], psb[:, 0:D], [ps3], [m["h2tm"][tile]], e=("act" if tile % 2 else "dve"))
                tile += 1
        for c_ in range(NCH):
            self.dbg("hf%d" % c_, m["hf"][c_][:], m["hf"][c_], [128, 512])
        self.dbg("sm", m["sm"][:, 0:3], m["sm"], [128, 3])
        self.dbg("rc0", self.RC[0][:], self.RC[0], [128, 256])
        self.dbg("modT", self.modT[:].rearrange("p a b -> p (a b)"), self.modT, [128, 96])
        self.dbg("aff", m["aff_tm"][:].rearrange("p a b -> p (a b)"), m["aff_tm"], [128, 18 * NE])
        self.dbg("h2tm0", m["h2tm"][0][:], m["h2tm"][0], [128, D])
        S.pop()

        self.dbg("affT", m["affT"][:], m["affT"], [16, 2304])
        S.push()
        m["wk"] = S.sb([16, 2304], F32, "wk")
        m["rankT"] = S.sb([16, 2304], F32, "rankT")
        regions = [(0, 2048, 256)] + ([(2048, 256, 32)] if keep_ctx else [])
        for (r0, rn, k) in regions:
            rs = slice(r0, r0 + rn)
            S.cp(m["wk"][:, rs], m["affT"][:, rs], [m["affT"]], [m["wk"]])
            for rnd in range(k // 8):
                S.max8(m["m8"][:], m["wk"][:, rs], [m["wk"]], [m["m8"]])
                if rnd < k // 8 - 1:
                    S.mrep(m["wk"][:, rs], m["m8"][:], m["wk"][:, rs], -1.0, [m["wk"], m["m8"]], [m["wk"]])
            S.ts(m["wk"][:, rs], m["affT"][:, rs], m["m8"][:, 7:8], None, ALU.is_ge, None,
                 [m["affT"], m["m8"]], [m["wk"]])
            S.scan(m["rankT"][:, rs], m["wk"][:, rs], m["wk"][:, rs], 0.0, ALU.add, ALU.max, [m["wk"]], [m["rankT"]])
            S.tt(m["rankT"][:, rs], m["rankT"][:, rs], m["wk"][:, rs], ALU.mult, [m["rankT"], m["wk"]], [m["rankT"]])
            S.ts(m["rankT"][:, rs], m["rankT"][:, rs], -1.0, None, ALU.add, None, [m["rankT"]], [m["rankT"]])
        nT = ntiles * 128
        S.cp(m["slotTb"][:, 0:nT], m["rankT"][:, 0:nT], [m["rankT"]], [m["slotTb"]])
        S.cp(m["affTb"][:, 0:nT], m["affT"][:, 0:nT], [m["affT"]], [m["affTb"]], e="act")
        for t in range(ntiles):
            ps = S.psum()
            S.trf(ps[:, 0:NE], m["rankT"][:, t * 128:(t + 1) * 128], self.ident_f[0:16, 0:16],
                 [m["rankT"], self.ident_f], [ps])
            S.cp(m["slot_tm"][:, t, :], ps[:, 0:NE], [ps], [m["slot_tm"]], e=("act" if t % 2 else "dve"))
        self.dbg("slot", m["slot_tm"][:].rearrange("p a b -> p (a b)"), m["slot_tm"], [128, 18 * NE])
        self.dbg("m8", m["m8"][:], m["m8"], [16, 8])
        S.pop()
        S.pop()

        S.push()
        m["w"] = [S.sb([128, NCH, 1024], BF16, "wexp") for _ in range(2)]
        wq = [wload("g", 0), wload("u", 0)]
        m["P"] = [S.sb([128, 16 * 256 + 2 * 32], BF16, "P") for _ in range(2)]
        m["PT"] = [S.sb([128, 2048], BF16, "PT") for _ in range(2)] + [S.sb([32, 256], BF16, "PTc")]
        m["affbc"] = S.sb([128, 512], BF16, "affbc")
        m["xeT"] = S.sb([128, NCH, 288], BF16, "xeT")
        m["actT"] = S.sb([128, NCH, 288], BF16, "actT")
        m["sil"] = S.sb([128, NCH, 288], BF16, "sil")
        m["ye"] = [S.sb([128, D], BF16, "ye") for _ in range(3)]
        g2 = {sn: [self.mvec(sn, 5, c) for c in range(NCH)] for sn in ("l", "c")}
        modT = self.modT
        PT = m["PT"]
        def build_P(e_):
            P_ = m["P"][e_ % 2]
            for t in range(ntiles):
                if t < 16:
                    S.ts(P_[:, t * 256:(t + 1) * 256], self.iota_row[:, 0:256], m["slot_tm"][:, t, e_:e_ + 1], None,
                         ALU.is_equal, None, [self.iota_row, m["slot_tm"]], [P_])
                else:
                    o = 4096 + (t - 16) * 32
                    S.ts(P_[:, o:o + 32], self.iota_row[:, 0:32], m["slot_tm"][:, t, e_:e_ + 1], None,
                         ALU.is_equal, None, [self.iota_row, m["slot_tm"]], [P_])

        build_P(0)
        for e in range(NE):
            P = m["P"][e % 2]
            wg, wu = wq
            for c in range(NCH):
                ps = S.psum()
                for t in range(16):
                    S.mm(ps[:, 0:256], m["h2tm"][t][:, c * 128:(c + 1) * 128], P[:, t * 256:(t + 1) * 256],
                         t == 0, t == 15, [m["h2tm"][t], P], [ps])
                if keep_ctx:
                    for t in range(16, 18):
                        o = 4096 + (t - 16) * 32
                        S.mm(ps[:, 256:288], m["h2tm"][t][:, c * 128:(c + 1) * 128], P[:, o:o + 32],
                             t == 16, t == 17, [m["h2tm"][t], P], [ps])
                S.cp(m["xeT"][:, c, 0:ncap], ps[:, 0:ncap], [ps], [m["xeT"]], e="act")
            selE = self.sel[:, e * 128:(e + 1) * 128]
            ab = m["affbc"]
            for gi, grp in enumerate(groups):
                n = grp[3]
                r0 = grp[2] if grp[0] == "l" else 2048
                psB = S.psum()
                S.mm(psB[:, 0:n], selE, m["affTb"][:, r0:r0 + n], True, True, [self.sel, m["affTb"]], [psB])
                S.cp(ab[:, 0:n], psB[:, 0:n], [psB], [ab], e="act")
                psA = S.psum()
                if grp[0] == "l":
                    S.mm(psA[:, 0:n], selE, m["slotTb"][:, r0:r0 + n], True, True, [self.sel, m["slotTb"]], [psA])
                    for cc in range(2):
                        S.stt(PT[cc][:, r0:r0 + n], psA[:, 0:n], self.iota_part[:, cc:cc + 1], ab[:, 0:n],
                              ALU.is_equal, ALU.mult, [psA, self.iota_part, ab], [PT[cc]])
                else:
                    S.mm(psA[0:32, 0:n], selE[:, 0:32], m["slotTb"][:, r0:r0 + n], True, True,
                         [self.sel, m["slotTb"]], [psA])
                    S.stt(PT[2][:, 0:n], psA[0:32, 0:n], self.iota_part[0:32, 0:1], ab[0:32, 0:n],
                          ALU.is_equal, ALU.mult, [psA, self.iota_part, ab], [PT[2]])
            for f in range(NCH):
                psA = S.psum()
                for c in range(NCH):
                    S.mm(psA[:, 0:ncap], wg[:, c, f * 128:(f + 1) * 128], m["xeT"][:, c, 0:ncap], c == 0, c == NCH - 1,
                         [wg, m["xeT"]], [psA])
                S.act(m["sil"][:, f, 0:ncap], psA[:, 0:ncap], AF.Silu, [psA], [m["sil"]])
            wd = wload("d", e)
            for f in range(NCH):
                psU = S.psum()
                for c in range(NCH):
                    S.mm(psU[:, 0:ncap], wu[:, c, f * 128:(f + 1) * 128], m["xeT"][:, c, 0:ncap], c == 0, c == NCH - 1,
                         [wu, m["xeT"]], [psU])
                S.tt(m["actT"][:, f, 0:ncap], m["sil"][:, f, 0:ncap], psU[:, 0:ncap], ALU.mult, [m["sil"], psU], [m["actT"]])
            if e + 1 < NE:
                wg_n = wload("g", e + 1)
                build_P(e + 1)
            for cc, rows in enumerate((128, 128, 32)[:3 if keep_ctx else 2]):
                for dh in range(2):
                    ps = S.psum()
                    for f in range(NCH):
                        S.mm(ps[0:rows, :], m["actT"][:, f, cc * 128:cc * 128 + rows], wd[:, f, dh * 512:(dh + 1) * 512],
                             f == 0, f == NCH - 1, [m["actT"], wd], [ps])
                    S.cp(m["ye"][cc][0:rows, dh * 512:(dh + 1) * 512], ps[0:rows, :], [ps], [m["ye"][cc]], e="act")
            if e + 1 < NE:
                wu_n = wload("u", e + 1)
                wq = [wg_n, wu_n]
            for grp in groups:
                n = grp[3]
                r0 = grp[2]
                for c in range(NCH):
                    ps = S.psum()
                    rb = self.rbuf(grp, c)
                    if grp[0] == "l":
                        for cc in range(2):
                            S.mm(ps[:, 0:n], m["ye"][cc][:, c * 128:(c + 1) * 128], PT[cc][:, r0:r0 + n], cc == 0, cc == 1,
                                 [m["ye"][cc], PT[cc]], [ps])
                    else:
                        S.mm(ps[:, 0:n], m["ye"][2][0:32, c * 128:(c + 1) * 128], PT[2][:, 0:n], True, True,
                             [m["ye"][2], PT[2]], [ps])
                    S.stt(rb[:, 0:n], ps[:, 0:n], g2[grp[0]][c], rb[:, 0:n], ALU.mult, ALU.add, [ps, modT, rb], [rb])
        S.pop()
        S.pop()

    def final_store(self, do_norm=True):
        S = self.S
        A = self.A
        S.push()
        self.norm_scratch()
        fw = S.sb([128, NCH], F32, "fnw")
        with self.nc.allow_non_contiguous_dma("tiny"):
            S.dma("sp", fw[:], A["final_norm_w"][0].rearrange("(c p) -> p c", p=128), writes=[fw])
        ob = [S.sb([128, D], F32, "ob") for _ in range(2)]
        hf = [S.sb([128, 512], F32, "hf") for _ in range(NCH)]
        k = 0
        out_evs = []
        glist = [(g, self.out) for g in self.groups[:4]]
        if self.cfg.get("dump_ctx"):
            glist.append((self.groups[4], self.out_c))
        for grp, dst in glist:
            n = grp[3]
            norm = do_norm and grp[0] == "l"
            if norm:
                rbs, rstd = self.rstd_group(grp)
                for c in range(NCH):
                    S.stt(hf[c][:, 0:n], rbs[c][:, 0:n], fw[:, c:c + 1], rstd[:, 0:n], ALU.mult, ALU.mult,
                          [rbs[c], fw, rstd], [hf[c]])
            for tt in range(n // 128):
                o = ob[k % 2]
                k += 1
                for half in range(2):
                    ps = S.psum()
                    for j in range(4):
                        c = half * 4 + j
                        src = hf[c] if norm else self.rbuf(grp, c)
                        S.trf(ps[:, j * 128:(j + 1) * 128], src[:, tt * 128:(tt + 1) * 128], self.ident_f[:],
                             [src, self.ident_f], [ps])
                    S.cp(o[:, half * 512:(half + 1) * 512], ps[:, :], [ps], [o], e=("act" if half else "dve"))
                t0 = grp[2] + tt * 128
                out_evs.append(S.dma("sp", dst[t0:t0 + 128, :], o[:], reads=[o]))
        for ev in out_evs:
            S._wait("sp", ev)
        S.pop()

    def build(self):
        cfg = self.cfg
        self.declare()
        self.setup()
        self.load_stream()
        for layer in cfg.get("layers", range(DEPTH)):
            keep_ctx = layer < DEPTH - 1
            self.adaln(layer)
            if cfg.get("mixer", True):
                self.mixer(layer, keep_ctx)
            if cfg.get("moe", True):
                self.moe_layer(layer, keep_ctx)
        self.final_store(cfg.get("final_norm", True))
        self.S.emit()
        return self.nc

    def hnorm(self, keep_ctx=True):
        S = self.S
        H = [S.sb([128, 2304], BF16, "H") for _ in range(NCH)]
        S.push()
        self.norm_scratch()
        for grp in self.groups:
            sn = grp[0]
            n = grp[3]
            o = grp[2] if sn == "l" else 2048
            self.norm_group(grp, self.M[sn]["a1"], 0, sn, out_bf=[(H[c][:, o:o + n], H[c]) for c in range(NCH)])
        S.pop()
        return H

    def tok_groups(self, keep_ctx=True):
        gs = [(g, g[2], 512) for g in self.groups[:4]]
        if keep_ctx:
            gs.append((self.groups[4], 2048, 256))
        return gs

    def out_proj(self, Z, w_ap, keep_ctx):
        S = self.S
        wo = S.sb([128, NCH, D], BF16, "wo")
        S.dma("pool", wo[:], w_ap.rearrange("(c p) n -> p c n", p=128), writes=[wo])
        for (grp, o, n) in self.tok_groups(keep_ctx):
            for c in range(NCH):
                ps = S.psum()
                for k in range(NCH):
                    S.mm(ps[:, 0:n], wo[:, k, c * 128:(c + 1) * 128], Z[k][:, o:o + n], k == 0, k == NCH - 1,
                         [wo, Z[k]], [ps])
                rb = self.rbuf(grp, c)
                S.stt(rb[:, 0:n], ps[:, 0:n], self.mvec(grp[0], 2, c), rb[:, 0:n], ALU.mult, ALU.add,
                      [ps, self.modT, rb], [rb])

    def mixer(self, layer, keep_ctx):
        kind = layer % 4
        if kind == 0:
            self.mixer_hyena(keep_ctx)
        if kind == 1:
            self.mixer_swa(keep_ctx)
        if kind == 2:
            self.mixer_gdn(keep_ctx)
        if kind == 3:
            self.mixer_hgrn2(layer, keep_ctx)

    def mixer_hyena(self, keep_ctx):
        S = self.S
        A = self.A
        nc = self.nc
        seqs = [("l", TL, 0)] + ([("c", TC, 2048)] if keep_ctx else [])
        TWO_PI = 2.0 * np.pi
        HS = {}
        for nm, L, _ in seqs:
            HS[nm] = nc.dram_tensor("hy_spec_" + nm, [2, 8, 2, 128, L], BF16, kind="Internal").ap()

        S.push()
        w1 = S.sb([33, 64], F32, "w1")
        w2 = S.sb([64, 64], F32, "w2")
        S.dma("sp", w1[:], A["hy_ffn_w1"], writes=[w1])
        S.dma("sp", w2[:], A["hy_ffn_w2"], writes=[w2])
        pcol = S.sb([64, 4], F32, "pcol")
        S.dma("sp", pcol[:, 0:1], A["hy_ffn_b1"].rearrange("o p -> p o"), writes=[pcol])
        S.dma("sp", pcol[:, 1:2], A["hy_sin_freq"][0:1, :].rearrange("o p -> p o"), writes=[pcol])
        S.dma("sp", pcol[:, 2:3], A["hy_ffn_b2"].rearrange("o p -> p o"), writes=[pcol])
        S.dma("sp", pcol[:, 3:4], A["hy_sin_freq"][1:2, :].rearrange("o p -> p o"), writes=[pcol])
        w3p = S.sb([64, 2, D], F32, "w3p")
        w3m = S.sb([64, 2, D], F32, "w3m")
        dbc = S.sb([128, D], F32, "dbc")
        sa = {nm: S.sb([64, 256], F32, "sa_" + nm) for nm in ("arg", "t", "kf", "m")}
        ki = S.sb([64, 256], I32, "ki")
        S.push()
        w3 = S.sb([64, 4 * D], F32, "w3")
        S.dma("sp", w3[:], A["hy_ffn_w3"], writes=[w3])
        for o in range(2):
            fw_ = w3[:, o * 2048:o * 2048 + D]
            bw_ = w3[:, o * 2048 + D:o * 2048 + 2 * D]
            S.tt(w3p[:, o, :], fw_, bw_, ALU.add, [w3], [w3p])
            S.tt(w3m[:, o, :], bw_, fw_, ALU.subtract, [w3], [w3m], e="pool")
        S.pop()
        S.dma("sp", dbc[:], A["hy_delta"][0].partition_broadcast(128), writes=[dbc])

        def sinfn(dst, ps, n, bcol, fcol):
            S.ts(sa["arg"][:, 0:n], ps, pcol[:, bcol:bcol + 1], pcol[:, fcol:fcol + 1], ALU.add, ALU.mult,
                 [pcol] + ([] if isinstance(ps, int) else []), [sa["arg"]])
            S.ts(sa["t"][:, 0:n], sa["arg"][:, 0:n], 1.0 / TWO_PI, 8.5, ALU.mult, ALU.add, [sa["arg"]], [sa["t"]])
            S.cp(ki[:, 0:n], sa["t"][:, 0:n], [sa["t"]], [ki])
            S.cp(sa["kf"][:, 0:n], ki[:, 0:n], [ki], [sa["kf"]])
            S.tt(sa["t"][:, 0:n], sa["t"][:, 0:n], sa["kf"][:, 0:n], ALU.subtract, [sa["t"], sa["kf"]], [sa["t"]])
            S.ts(sa["m"][:, 0:n], sa["t"][:, 0:n], 0.5, None, ALU.is_gt, None, [sa["t"]], [sa["m"]])
            S.tt(sa["t"][:, 0:n], sa["t"][:, 0:n], sa["m"][:, 0:n], ALU.subtract, [sa["t"], sa["m"]], [sa["t"]])
            S.act(dst, sa["t"][:, 0:n], AF.Sin, [sa["t"]], [], scale=-TWO_PI)

        for nm, L, _ in seqs:
            T_ = L // 128
            N = 2 * L
            S.push()
            tcol = S.sb([128, T_], F32, "tcol")
            S.dma("sp", tcol[:], A["hy_tcol_" + nm], writes=[tcol])
            S.ts(tcol[:], tcol[:], -1.0, None, ALU.mult, None, [tcol], [tcol])
            h2T = S.sb([64, L], F32, "h2T")
            S.push()
            zT = S.sb([33, L], F32, "zT")
            S.dma("sp", zT[:], A["hy_zT_" + nm], writes=[zT])
            h1T = S.sb([64, L], F32, "h1T")
            for b0 in range(0, L, 256):
                n = min(256, L - b0)
                ps = S.psum()
                S.mm(ps[0:64, 0:n], w1[:], zT[:, b0:b0 + n], True, True, [w1, zT], [ps])
                S._deps("dve", [ps], [])
                sinfn(h1T[:, b0:b0 + n], ps[0:64, 0:n], n, 0, 1)
                S._mark((S.csem["act"], S.ccnt["act"], "act"), [ps], [h1T])
            for b0 in range(0, L, 256):
                n = min(256, L - b0)
                ps = S.psum()
                S.mm(ps[0:64, 0:n], w2[:], h1T[:, b0:b0 + n], True, True, [w2, h1T], [ps])
                S._deps("dve", [ps], [])
                sinfn(h2T[:, b0:b0 + n], ps[0:64, 0:n], n, 2, 3)
                S._mark((S.csem["act"], S.ccnt["act"], "act"), [ps], [h2T])
            S.pop()
            win = S.sb([128, T_, 128], F32, "win")
            hp = [[S.sb([128, T_, 128], BF16, "hp") for o in range(2)] for _ in range(2)]
            hm = [[S.sb([128, T_, 128], BF16, "hm") for o in range(2)] for _ in range(2)]
            Hs = [[[S.sb([128, T_, 128], BF16, "Hs") for ri in range(2)] for o in range(2)] for _ in range(2)]
            Cf = [S.sb([128, T_, 128], BF16, "Cf") for _ in range(2)]
            Sf_ = [S.sb([128, T_, 128], BF16, "Sf") for _ in range(2)]
            for pair in range(4):
                for bi in range(2):
                    cb = pair * 2 + bi
                    ccs = slice(cb * 128, (cb + 1) * 128)
                    for tt in range(T_):
                        S.act(win[:, tt, :], dbc[:, ccs], AF.Exp, [dbc, tcol], [win], scale=tcol[:, tt:tt + 1])
                    S.ts(win[:], win[:], 0.05, None, ALU.add, None, [win], [win])
                    for o in range(2):
                        for tt in range(T_):
                            dsl = slice(tt * 128, (tt + 1) * 128)
                            psP = S.psum()
                            psM = S.psum()
                            S.mm(psP[:, 0:128], h2T[:, dsl], w3p[:, o, ccs], True, True, [h2T, w3p], [psP])
                            S.mm(psM[:, 0:128], h2T[:, dsl], w3m[:, o, ccs], True, True, [h2T, w3m], [psM])
                            S.tt(hp[bi][o][:, tt, :], psP[:, 0:128], win[:, tt, :], ALU.mult, [psP, win], [hp[bi][o]])
                            S.tt(hm[bi][o][:, tt, :], psM[:, 0:128], win[:, tt, :], ALU.mult, [psM, win], [hm[bi][o]])
                        S.tt(hp[bi][o][0:1, 0, :], hp[bi][o][0:1, 0, :], hm[bi][o][0:1, 0, :], ALU.subtract,
                             [hp[bi][o], hm[bi][o]], [hp[bi][o]])
                        S.ts(hp[bi][o][0:1, 0, :], hp[bi][o][0:1, 0, :], 0.5, None, ALU.mult, None, [hp[bi][o]], [hp[bi][o]])
                for ft in range(T_):
                    cf, sf = Cf[ft % 2], Sf_[ft % 2]
                    S.dma("sp", cf[:].rearrange("p a b -> p (a b)"), A["hy_Cf_" + nm][ft * 128:(ft + 1) * 128, :], writes=[cf])
                    S.dma("sp", sf[:].rearrange("p a b -> p (a b)"), A["hy_Sf_" + nm][ft * 128:(ft + 1) * 128, :], writes=[sf])
                    for bi in range(2):
                        for o in range(2):
                            for ri, (tab, src) in enumerate(((cf, hp[bi][o]), (sf, hm[bi][o]))):
                                ps = S.psum()
                                for tt in range(T_):
                                    S.mm(ps[:, 0:128], tab[:, tt, :], src[:, tt, :], tt == 0, tt == T_ - 1, [tab, src], [ps])
                                S.act(Hs[bi][o][ri][:, ft, :], ps[:, 0:128], AF.Identity, [ps], [Hs[bi][o][ri]], scale=2.0 / N)
                for bi in range(2):
                    cb = pair * 2 + bi
                    for o in range(2):
                        for ri in range(2):
                            S.dma("sp", HS[nm][o, cb, ri], Hs[bi][o][ri][:].rearrange("p a b -> p (a b)"),
                                  reads=[Hs[bi][o][ri]], writes=[self.hs_tok])
            S.pop()
        S.pop()

        S.push()
        H = self.hnorm()
        Win = A["hy_w_in"].rearrange("(c p) n -> p c n", p=128)
        pv = S.sb([128, 24, 6], F32, "hyp")
        S.dma("sp", pv[:, :, 0], A["hy_b_in"][0].rearrange("(c p) -> p c", p=128), writes=[pv])
        for k in range(3):
            S.dma("sp", pv[:, :, 1 + k], A["hy_short_w"][k].rearrange("(c p) -> p c", p=128), writes=[pv])
        S.dma("sp", pv[:, :, 4], A["hy_short_b"][0].rearrange("(c p) -> p c", p=128), writes=[pv])
        fb = S.sb([128, 2, NCH], F32, "hyfb")
        for o in range(2):
            S.dma("sp", fb[:, o, :], A["hy_filter_bias"][o].rearrange("(c p) -> p c", p=128), writes=[fb])
        bo = S.sb([128, NCH], F32, "hybo")
        S.dma("sp", bo[:], A["hy_b_out"][0].rearrange("(c p) -> p c", p=128), writes=[bo])
        W3b = S.sb([128, NCH, 3, 128], BF16, "W3b")
        wo = S.sb([128, D], BF16, "wo_h")
        upad = S.sb([128, 2312], BF16, "upad")
        S.memset(upad[:], 0.0, [upad])
        cv = S.sb([128, 2304], F32, "cv")
        ufm = [S.sb([128, 2304], BF16, "ufm") for _ in range(3)]
        z1 = S.sb([128, 2304], BF16, "z1")
        zT_ = S.sb([128, 2304], BF16, "zTh")
        utm = S.sb([128, 16, 128], BF16, "utm")
        Hr = S.sb([128, 16, 128], BF16, "Hr")
        Hi = S.sb([128, 16, 128], BF16, "Hi")
        Yr = S.sb([128, 16, 128], BF16, "Yr")
        nYi = S.sb([128, 16, 128], BF16, "nYi")
        Cf = [S.sb([128, 16, 128], BF16, "Cf") for _ in range(2)]
        Sf_ = [S.sb([128, 16, 128], BF16, "Sf") for _ in range(2)]
        ring = [S.sb([128, 512], BF16, "ring") for _ in range(8)]
        ur = S.sb([128, 128], F32, "ur")
        ui = S.sb([128, 128], F32, "ui")
        t1 = S.sb([128, 128], F32, "t1")
        t2 = S.sb([128, 128], F32, "t2")
        ytmp = S.sb([128, 512], F32, "ytmp")
        LO, CO = 1, 2052
        rk = 0

        for cb in range(8):
            for k in range(3):
                S.dma("pool", W3b[:, :, k, :], Win[:, :, k * D + cb * 128:k * D + (cb + 1) * 128], writes=[W3b])
            for k in range(3):
                ch = k * 8 + cb
                for (grp, o, n) in self.tok_groups(keep_ctx):
                    ps = S.psum()
                    for c in range(NCH):
                        S.mm(ps[:, 0:n], W3b[:, c, k, :], H[c][:, o:o + n], c == 0, c == NCH - 1, [W3b, H[c]], [ps])
                    po_ = (LO + o) if o < 2048 else (CO + o - 2048)
                    S.act(upad[:, po_:po_ + n], ps[:, 0:n], AF.Identity, [ps, pv], [upad], bias=pv[:, ch, 0:1])
                for (nm, L, o_) in seqs:
                    base = LO if nm == "l" else CO
                    S.ts(cv[:, o_:o_ + L], upad[:, base - 1:base - 1 + L], pv[:, ch, 1:2], pv[:, ch, 4:5], ALU.mult, ALU.add,
                         [upad, pv], [cv])
                    S.stt(cv[:, o_:o_ + L], upad[:, base:base + L], pv[:, ch, 2:3], cv[:, o_:o_ + L], ALU.mult, ALU.add,
                          [upad, pv, cv], [cv])
                    S.stt(ufm[k][:, o_:o_ + L], upad[:, base + 1:base + 1 + L], pv[:, ch, 3:4], cv[:, o_:o_ + L],
                          ALU.mult, ALU.add, [upad, pv, cv], [ufm[k]])
            for (nm, L, o_) in seqs:
                T_ = L // 128
                for o in range(2):
                    src = ufm[0] if o == 0 else z1
                    xg = ufm[1 + o]
                    dst = z1 if o == 0 else zT_
                    for tt in range(T_):
                        pT = S.psum()
                        pTb = pT[:].bitcast(BF16)
                        S.tr(pTb[:, 0:128], src[:, o_ + tt * 128:o_ + (tt + 1) * 128], self.ident_b[:], [src, self.ident_b], [pT])
                        S.cp(utm[:, tt, :], pTb[:, 0:128], [pT], [utm], e="act")
                    S.dma("sp", Hr[:, 0:T_, :].rearrange("p a b -> p (a b)"), HS[nm][o, cb, 0], reads=[self.hs_tok], writes=[Hr])
                    S.dma("sp", Hi[:, 0:T_, :].rearrange("p a b -> p (a b)"), HS[nm][o, cb, 1], reads=[self.hs_tok], writes=[Hi])
                    for ft in range(T_):
                        cf, sf = Cf[ft % 2], Sf_[ft % 2]
                        if not (self.cfg.get("hy_noload") and (cb > 0 or ft > 1)):
                            S.dma("sp", cf[:, 0:T_, :].rearrange("p a b -> p (a b)"), A["hy_Cf_" + nm][ft * 128:(ft + 1) * 128, :], writes=[cf])
                            S.dma("sp", sf[:, 0:T_, :].rearrange("p a b -> p (a b)"), A["hy_Sf_" + nm][ft * 128:(ft + 1) * 128, :], writes=[sf])
                        pr = S.psum()
                        for tt in range(T_):
                            S.mm(pr[:, 0:128], cf[:, tt, :], utm[:, tt, :], tt == 0, tt == T_ - 1, [cf, utm], [pr])
                        pi_ = S.psum()
                        for tt in range(T_):
                            S.mm(pi_[:, 0:128], sf[:, tt, :], utm[:, tt, :], tt == 0, tt == T_ - 1, [sf, utm], [pi_])
                        S.cp(ur[:], pr[:, 0:128], [pr], [ur], e="act")
                        S.cp(ui[:], pi_[:, 0:128], [pi_], [ui], e="act")
                        S.tt(t1[:], ur[:], Hr[:, ft, :], ALU.mult, [ur, Hr], [t1])
                        S.tt(t2[:], ui[:], Hi[:, ft, :], ALU.mult, [ui, Hi], [t2], e="pool")
                        S.tt(Yr[:, ft, :], t1[:], t2[:], ALU.add, [t1, t2], [Yr])
                        S.tt(t1[:], ui[:], Hr[:, ft, :], ALU.mult, [ui, Hr], [t1])
                        S.tt(t2[:], ur[:], Hi[:, ft, :], ALU.mult, [ur, Hi], [t2], e="pool")
                        S.tt(nYi[:, ft, :], t1[:], t2[:], ALU.subtract, [t1, t2], [nYi])
                    for tg in range(0, L, 512):
                        n = min(512, L - tg)
                        py = S.psum()
                        for ft in range(T_):
                            rc = ring[rk % 8]
                            rs = ring[(rk + 1) % 8]
                            rk += 2
                            if not (self.cfg.get("hy_noload") and rk > 16):
                                S.dma("sp", rc[:, 0:n], A["hy_Ct_" + nm][ft * 128:(ft + 1) * 128, tg:tg + n], writes=[rc])
                                S.dma("sp", rs[:, 0:n], A["hy_St_" + nm][ft * 128:(ft + 1) * 128, tg:tg + n], writes=[rs])
                            S.mm(py[:, 0:n], Yr[:, ft, :], rc[:, 0:n], ft == 0, False, [Yr, rc], [py])
                            S.mm(py[:, 0:n], nYi[:, ft, :], rs[:, 0:n], False, ft == T_ - 1, [nYi, rs], [py])
                        osl = slice(o_ + tg, o_ + tg + n)
                        S.stt(ytmp[:, 0:n], src[:, osl], fb[:, o, cb:cb + 1], py[:, 0:n], ALU.mult, ALU.add, [src, fb, py], [ytmp])
                        S.tt(dst[:, osl], ytmp[:, 0:n], xg[:, osl], ALU.mult, [ytmp, xg], [dst], e="pool")
            self.out_proj_head(zT_, A["hy_w_out"], cb, wo, keep_ctx)
        gb = S.sb([128, 2, NCH], F32, "hygb")
        for si, sn in enumerate(("l", "c")):
            S.tt(gb[:, si, :], self.modT[:, 16:24, si], bo[:], ALU.mult, [self.modT, bo], [gb])
        for (grp, o, n) in self.tok_groups(keep_ctx):
            si = 0 if grp[0] == "l" else 1
            for c in range(NCH):
                rb = self.rbuf(grp, c)
                S.ts(rb[:, 0:n], rb[:, 0:n], gb[:, si, c:c + 1], None, ALU.add, None, [rb, gb], [rb])
        S.pop()

    def out_proj_head(self, zT, w_ap, h, wo, keep_ctx):
        S = self.S
        S.dma("pool", wo[:], w_ap[h * 128:(h + 1) * 128, :], writes=[wo])
        for (grp, o, n) in self.tok_groups(keep_ctx):
            for c in range(NCH):
                ps = S.psum()
                S.mm(ps[:, 0:n], wo[:, c * 128:(c + 1) * 128], zT[:, o:o + n], True, True, [wo, zT], [ps])
                rb = self.rbuf(grp, c)
                S.stt(rb[:, 0:n], ps[:, 0:n], self.mvec(grp[0], 2, c), rb[:, 0:n], ALU.mult, ALU.add,
                      [ps, self.modT, rb], [rb])

    def mixer_gdn(self, keep_ctx):
        S = self.S
        A = self.A
        W = A["gd_w_in"].rearrange("(c p) n -> p c n", p=128)
        ntile = 18
        S.push()
        H = self.hnorm()
        I_f = self.ident_f
        cw = S.sb([128, 3, 24], F32, "cw")
        for k in range(3):
            S.dma("sp", cw[:, k, :], A["gd_conv_w"][k].rearrange("(c p) -> p c", p=128), writes=[cw])
        nea = S.sb([1, 16], F32, "nea")
        dtb = S.sb([1, 16], F32, "dtb")
        S.dma("sp", nea[:], A["gd_a_log"], writes=[nea])
        S.dma("sp", dtb[:], A["gd_dt_bias"], writes=[dtb])
        S.act(nea[:], nea[:], AF.Exp, [nea], [nea])
        S.ts(nea[:], nea[:], -1.0, None, ALU.mult, None, [nea], [nea])
        onesr = S.sb([1, 128], F32, "onesr")
        S.memset(onesr[:], 1.0, [onesr])
        nwbc = S.sb([128, 128], F32, "nwbc")
        S.dma("sp", nwbc[:], A["gd_norm_w"][0].partition_broadcast(128), writes=[nwbc])
        cresr = S.sb([1, 128], F32, "cresr")
        S.dma("sp", cresr[:], A["chunk_reset"][0:1, :], writes=[cresr])
        tri = [S.sb([128, 128], F32, "tri") for _ in range(2)]
        tris = [S.sb([128, 128], F32, "tris") for _ in range(2)]
        S.dma("sp", tri[0][:], A["tri_fwd"], writes=[tri[0]])
        S.dma("sp", tri[1][:], A["tri_bwd"], writes=[tri[1]])
        S.dma("sp", tris[0][:], A["tri_fwd_s"], writes=[tris[0]])
        S.dma("sp", tris[1][:], A["tri_bwd_s"], writes=[tris[1]])
        eps1 = self.epsb
        wbab = S.sb([128, NCH, 32], BF16, "wbab")
        S.dma("pool", wbab[:], W[:, :, 4096:4128], writes=[wbab])
        wo = S.sb([128, D], BF16, "wo_h")
        cv = S.sb([128, 2304], F32, "cv")
        qT = S.sb([128, 2304], BF16, "qT")
        kT = S.sb([128, 2304], BF16, "kT")
        ktm = S.sb([128, ntile, 128], BF16, "ktm")
        vtm = S.sb([128, ntile, 128], BF16, "vtm")
        ztm = S.sb([128, ntile, 128], BF16, "ztm")
        of = S.sb([128, ntile, 128], F32, "of")
        zT = S.sb([128, 2304], BF16, "zT")
        vT = zT
        PV = []
        for _d in range(2):
            if _d == 1:
                mark = S.top
                W4 = S.sb([128, NCH, 4, 128], BF16, "W4")
                upad = S.sb([128, 2312], BF16, "upad")
                sqb = S.sb([128, 512], BF16, "sqb")
                rin = S.sb([128, 512], F32, "rin")
                top_after = S.top
                S.top = mark
            pvd = {}
            pvd["brow"] = S.sb([1, 128], F32, "brow")
            pvd["grow"] = S.sb([1, 128], F32, "grow")
            pvd["R"] = {nm: S.sb([1, 128], F32, nm) for nm in ("Gi", "G", "nG", "dl", "EGr")}
            pvd["Sf"] = S.sb([128, 128], F32, "Sf")
            pvd["Sb"] = S.sb([128, 128], BF16, "Sb")
            pvd["W2"] = S.sb([128, 2, 128], BF16, "W2")
            pvd["Q2"] = S.sb([128, 2, 128], BF16, "Q2")
            S.memset(pvd["W2"][:], 0.0, [pvd["W2"]])
            S.memset(pvd["Q2"][:], 0.0, [pvd["Q2"]])
            pvd["T"] = {nm: S.sb([128, 128], F32, nm) for nm in ("Em", "Ee", "Ei", "Es", "EB", "A", "B", "IA", "IB",
                                                                "P", "Pt", "u", "av", "o")}
            pvd["rhs"] = S.sb([128, 256], F32, "rhs")
            pvd["cols"] = S.sb([128, 8], F32, "cols")
            pvd["wsb"] = S.sb([128, 128], BF16, "wsb")
            pvd["kdtm"] = S.sb([128, 128], BF16, "kdtm")
            pvd["attnT"] = S.sb([128, 128], BF16, "attnT")
            pvd["vnew"] = S.sb([128, 128], BF16, "vnew")
            PV.append(pvd)
        S.top = max(S.top, top_after)
        TR = {nm: S.sb([128, 128], F32, nm) for nm in ("os", "nwg", "ysq")}
        yb = S.sb([128, 128], BF16, "yb")
        of1v = cv[:].rearrange("p (a b) -> p a b", b=128)
        st = S.sb([128, 4], F32, "gdst")
        LO, CO = 1, 2052

        def pad_view(o, n):
            return (LO + o) if o < 2048 else (CO + o - 2048)

        for h in range(8):
            S.barrier()
            S.memset(upad[:, 0:1], 0.0, [upad])
            S.memset(upad[:, 2049:2052], 0.0, [upad])
            S.memset(upad[:, 2308:2312], 0.0, [upad])
            for k in range(4):
                S.dma("pool", W4[:, :, k, :], W[:, :, k * D + h * 128:k * D + (h + 1) * 128], writes=[W4])
            for k, dst in ((0, qT), (1, kT), (2, vT)):
                for (grp, o, n) in self.tok_groups(True):
                    ps = S.psum()
                    for c in range(NCH):
                        S.mm(ps[:, 0:n], W4[:, c, k, :], H[c][:, o:o + n], c == 0, c == NCH - 1, [W4, H[c]], [ps])
                    po_ = pad_view(o, n)
                    S.cp(upad[:, po_:po_ + n], ps[:, 0:n], [ps], [upad], e="act")
                ch = k * 8 + h
                for (base, n_, o_) in ((LO, 2048, 0), (CO, 256, 2048)):
                    S.ts(cv[:, o_:o_ + n_], upad[:, base - 1:base - 1 + n_], cw[:, 0, ch:ch + 1], None, ALU.mult, None,
                         [upad, cw], [cv])
                    S.stt(cv[:, o_:o_ + n_], upad[:, base:base + n_], cw[:, 1, ch:ch + 1], cv[:, o_:o_ + n_],
                          ALU.mult, ALU.add, [upad, cw, cv], [cv])
                    S.stt(cv[:, o_:o_ + n_], upad[:, base + 1:base + 1 + n_], cw[:, 2, ch:ch + 1], cv[:, o_:o_ + n_],
                          ALU.mult, ALU.add, [upad, cw, cv], [cv])
                S.act(cv[:], cv[:], AF.Silu, [cv], [cv])
                if k == 2:
                    S.cp(dst[:], cv[:], [cv], [dst])
                    continue
                for (grp, o, n) in self.tok_groups(True):
                    S.act(sqb[:, 0:n], cv[:, o:o + n], AF.Square, [cv], [sqb])
                    ps = S.psum()
                    S.mm(ps[:, 0:n], self.ones_b[:], sqb[:, 0:n], True, True, [self.ones_b, sqb], [ps])
                    S.act(rin[:, 0:n], ps[:, 0:n], AF.Sqrt, [ps, eps1], [rin], bias=eps1[:, 0:1],
                          scale=(128.0 if k == 0 else 1.0))
                    S.recip(rin[:, 0:n], rin[:, 0:n], [rin], [rin])
                    S.tt(dst[:, o:o + n], cv[:, o:o + n], rin[:, 0:n], ALU.mult, [cv, rin], [dst])
            for t in range(ntile):
                tsl = slice(t * 128, (t + 1) * 128)
                for src, dstm in ((kT, ktm), (vT, vtm)):
                    pT = S.psum()
                    pTb = pT[:].bitcast(BF16)
                    S.tr(pTb[:, 0:128], src[:, tsl], self.ident_b[:], [src, self.ident_b], [pT])
                    S.cp(dstm[:, t, :], pTb[:, 0:128], [pT], [dstm], e="act")
                ps = S.psum()
                for c in range(NCH):
                    S.mm(ps[:, 0:128], H[c][:, tsl], W4[:, c, 3, :], c == 0, c == NCH - 1, [H[c], W4], [ps])
                S.act(ztm[:, t, :], ps[:, 0:128], AF.Silu, [ps], [ztm])
            def run_dir(d):
                pvd = PV[d]
                brow, grow, R, Sf, Sb, W2, Q2, T = (pvd[k] for k in ('brow', 'grow', 'R', 'Sf', 'Sb', 'W2', 'Q2', 'T'))
                rhs, cols, wsb, kdtm, attnT, vnew = (pvd[k] for k in ('rhs', 'cols', 'wsb', 'kdtm', 'attnT', 'vnew'))
                idx = d * 8 + h
                S.memset(Sf[:], 0.0, [Sf])
                S.memset(Sb[:], 0.0, [Sb])
                order = [16, 17] + list(range(16)) if d == 0 else [17, 16] + list(range(15, -1, -1))
                idx = d * 8 + h
                for tile in order:
                    o0 = tile * 128
                    tsl = slice(o0, o0 + 128)
                    for kind, dstr in ((0, brow), (1, grow)):
                        col = kind * 16 + idx
                        ps = S.psum()
                        for c in range(NCH):
                            S.mm(ps[0:1, 0:128], wbab[:, c, col:col + 1], H[c][:, tsl], c == 0, c == NCH - 1,
                                 [wbab, H[c]], [ps])
                        if kind == 0:
                            S.act(dstr[:], ps[0:1, 0:128], AF.Sigmoid, [ps], [dstr])
                        else:
                            S.act(dstr[:], ps[0:1, 0:128], AF.Exp, [ps, dtb], [dstr], bias=dtb[0:1, idx:idx + 1])
                            S.act(dstr[:], dstr[:], AF.Ln, [dstr, onesr], [dstr], bias=onesr[0:1, 0:1])
                            S.ts(dstr[:], dstr[:], nea[0:1, idx:idx + 1], None, ALU.mult, None, [dstr, nea], [dstr])
                    gr = grow[:]
                    S.scan(R["Gi"][:], cresr[:], gr, 0.0, ALU.mult, ALU.add, [cresr, grow], [R["Gi"]])
                    if d == 0:
                        G = R["Gi"]
                    else:
                        G = R["G"]
                        S.tt(G[:], gr, R["Gi"][:], ALU.subtract, [grow, R["Gi"]], [G])
                        for ci in range(2):
                            cs = slice(64 * ci, 64 * ci + 64)
                            S.ts(G[:, cs], G[:, cs], R["Gi"][:, 64 * ci + 63:64 * ci + 64], None, ALU.add, None,
                                 [G, R["Gi"]], [G])
                    S.ts(R["nG"][:], G[:], -1.0, None, ALU.mult, None, [G], [R["nG"]])
                    for ci in range(2):
                        cs = slice(64 * ci, 64 * ci + 64)
                        last = 64 * ci + (63 if d == 0 else 0)
                        S.ts(R["dl"][:, cs], G[:, cs], G[:, last:last + 1], None, ALU.subtract, None, [G], [R["dl"]])
                    S.act(R["EGr"][:], G[:], AF.Exp, [G], [R["EGr"]])
                    pD = S.psum()
                    S.mm(pD[:, 0:128], R["nG"][:], onesr[:], True, False, [R["nG"], onesr], [pD])
                    S.mm(pD[:, 0:128], onesr[:], G[:], False, True, [onesr, G], [pD])
                    pC = S.psum()
                    S.mm(pC[:, 0:1], G[:], onesr[:, 0:1], True, True, [G, onesr], [pC])
                    S.mm(pC[:, 1:2], R["dl"][:], onesr[:, 0:1], True, True, [R["dl"], onesr], [pC])
                    S.mm(pC[:, 2:3], brow[:], onesr[:, 0:1], True, True, [brow, onesr], [pC])
                    for ci in range(2):
                        last = 64 * ci + (63 if d == 0 else 0)
                        S.mm(pC[:, 4 + ci:5 + ci], onesr[:], R["EGr"][:, last:last + 1], True, True, [onesr, R["EGr"]], [pC])
                    pB = S.psum()
                    S.mm(pB[:, 0:128], onesr[:], brow[:], True, True, [onesr, brow], [pB])
                    S.ts(T["Em"][:], pD[:, 0:128], 0.0, None, ALU.min, None, [pD], [T["Em"]])
                    S.act(T["Ee"][:], T["Em"][:], AF.Exp, [T["Em"]], [T["Ee"]])
                    S.tt(T["Ei"][:], T["Ee"][:], tri[d][:], ALU.mult, [T["Ee"], tri[d]], [T["Ei"]], e="pool")
                    S.tt(T["Es"][:], T["Ee"][:], tris[d][:], ALU.mult, [T["Ee"], tris[d]], [T["Es"]], e="pool")
                    S.act(cols[:, 0:1], pC[:, 0:1], AF.Exp, [pC], [cols])
                    S.act(cols[:, 1:2], pC[:, 1:2], AF.Exp, [pC], [cols], scale=-1.0)
                    S.act(cols[:, 2:3], pC[:, 2:3], AF.Identity, [pC], [cols])
                    S.act(cols[:, 4:6], pC[:, 4:6], AF.Identity, [pC], [cols])
                    S.tt(cols[:, 3:4], cols[:, 2:3], cols[:, 0:1], ALU.mult, [cols], [cols])
                    S.tt(T["EB"][:], pB[:, 0:128], T["Es"][:], ALU.mult, [pB, T["Es"]], [T["EB"]])
                    pK = S.psum()
                    S.mm(pK[:, 0:128], kT[:, tsl], kT[:, tsl], True, True, [kT], [pK])
                    S.tt(T["B"][:], pK[:, 0:128], T["EB"][:], ALU.mult, [pK, T["EB"]], [T["B"]])
                    pA = S.psum()
                    S.trf(pA[:, 0:128], T["B"][:], I_f[:], [T["B"], I_f], [pA])
                    S.cp(T["A"][:], pA[:, 0:128], [pA], [T["A"]], e="act")
                    S.tt(T["P"][:], I_f[:], T["B"][:], ALU.subtract, [I_f, T["B"]], [T["P"]])
                    for lvl in range(5):
                        pA2 = S.psum()
                        S.mm(pA2[:, 0:128], T["B"][:], T["A"][:], True, True, [T["B"], T["A"]], [pA2])
                        if lvl < 4:
                            pB2 = S.psum()
                            S.mm(pB2[:, 0:128], T["A"][:], T["B"][:], True, True, [T["A"], T["B"]], [pB2])
                        S.tt(T["IA"][:], pA2[:, 0:128], I_f[:], ALU.add, [pA2, I_f], [T["IA"]])
                        if lvl < 4:
                            S.cp(T["A"][:], pA2[:, 0:128], [pA2], [T["A"]], e="act")
                            S.cp(T["B"][:], pB2[:, 0:128], [pB2], [T["B"]], e="act")
                        pP = S.psum()
                        S.mm(pP[:, 0:128], T["IA"][:], T["P"][:], True, True, [T["IA"], T["P"]], [pP])
                        S.cp(T["P"][:], pP[:, 0:128], [pP], [T["P"]])
                    S.ts(rhs[:, 0:128], vtm[:, tile, :], cols[:, 2:3], None, ALU.mult, None, [vtm, cols], [rhs])
                    S.ts(rhs[:, 128:256], ktm[:, tile, :], cols[:, 3:4], None, ALU.mult, None, [ktm, cols], [rhs])
                    pU = S.psum()
                    S.mm(pU[:, 0:256], T["P"][:], rhs[:], True, True, [T["P"], rhs], [pU])
                    S.cp(T["u"][:], pU[:, 0:128], [pU], [T["u"]], e="act")
                    S.cp(wsb[:], pU[:, 128:256], [pU], [wsb], e="act")
                    pW = S.psum()
                    pWb = pW[:].bitcast(BF16)
                    S.tr(pWb[:, 0:128], wsb[:], self.ident_b[:], [wsb, self.ident_b], [pW])
                    S.cp(W2[:, 0, 0:64], pWb[:, 0:64], [pW], [W2])
                    S.cp(W2[:, 1, 64:128], pWb[:, 64:128], [pW], [W2])
                    S.cp(Q2[:, 0, 0:64], qT[:, o0:o0 + 64], [qT], [Q2], e="pool")
                    S.cp(Q2[:, 1, 64:128], qT[:, o0 + 64:o0 + 128], [qT], [Q2], e="pool")
                    S.ts(kdtm[:], ktm[:, tile, :], cols[:, 1:2], None, ALU.mult, None, [ktm, cols], [kdtm])
                    pQK = S.psum()
                    S.mm(pQK[:, 0:128], kT[:, tsl], qT[:, tsl], True, True, [kT, qT], [pQK])
                    S.tt(attnT[:], pQK[:, 0:128], T["Ei"][:], ALU.mult, [pQK, T["Ei"]], [attnT])
                    pq = S.psum()
                    pv = S.psum()
                    psS = S.psum()
                    corder = (0, 1) if d == 0 else (1, 0)
                    for n_, ci in enumerate(corder):
                        pr = slice(64 * ci, 64 * ci + 64)
                        S.mm(pv[:, 0:128], W2[:, ci, :], Sb[:], True, True, [W2, Sb], [pv])
                        S.tt(vnew[pr, :], T["u"][pr, :], pv[pr, 0:128], ALU.subtract, [T["u"], pv], [vnew])
                        S.mm(pq[:, 0:128], Q2[:, ci, :], Sb[:], n_ == 0, n_ == 1, [Q2, Sb], [pq])
                        S.mm(psS[:, 0:128], kdtm[pr, :], vnew[pr, :], True, True, [kdtm, vnew], [psS])
                        S.stt(Sf[:], Sf[:], cols[:, 4 + ci:5 + ci], psS[:, 0:128], ALU.mult, ALU.add, [Sf, cols, psS], [Sf])
                        S.cp(Sb[:], Sf[:], [Sf], [Sb], e="act")
                    pav = S.psum()
                    S.mm(pav[:, 0:128], attnT[:], vnew[:], True, True, [attnT, vnew], [pav])
                    S.cp(T["av"][:], pav[:, 0:128], [pav], [T["av"]], e="act")
                    S.stt(T["o"][:], pq[:, 0:128], cols[:, 0:1], T["av"][:], ALU.mult, ALU.add, [pq, cols, T["av"]], [T["o"]])
                    if d == 0:
                        S.cp(of[:, tile, :], T["o"][:], [T["o"]], [of], e="pool")
                    else:
                        S.cp(of1v[:, tile, :], T["o"][:], [T["o"]], [cv], e="pool")

            S.barrier()
            for _d in range(2):
                S.memset(PV[_d]["W2"][:], 0.0, [PV[_d]["W2"]])
                S.memset(PV[_d]["Q2"][:], 0.0, [PV[_d]["Q2"]])
            S.run_interleaved([lambda: run_dir(0), lambda: run_dir(1)])
            for tile in range(18 if keep_ctx else 16):
                o0 = tile * 128
                tsl = slice(o0, o0 + 128)
                T = TR
                S.tt(T["os"][:], of1v[:, tile, :], of[:, tile, :], ALU.add, [cv, of], [T["os"]])
                S.act(T["ysq"][:], T["os"][:], AF.Square, [T["os"]], [T["ysq"], st], accum_out=st[:, 0:1])
                S.act(st[:, 1:2], st[:, 0:1], AF.Sqrt, [st, eps1], [st], bias=eps1[:, 0:1], scale=1.0 / 128)
                S.recip(st[:, 2:3], st[:, 1:2], [st], [st])
                S.tt(T["nwg"][:], nwbc[:], ztm[:, tile, :], ALU.mult, [nwbc, ztm], [T["nwg"]], e="pool")
                S.stt(yb[:], T["os"][:], st[:, 2:3], T["nwg"][:], ALU.mult, ALU.mult, [T["os"], st, T["nwg"]], [yb])
                pY = S.psum()
                pYb = pY[:].bitcast(BF16)
                S.tr(pYb[:, 0:128], yb[:], self.ident_b[:], [yb, self.ident_b], [pY])
                S.cp(zT[:, tsl], pYb[:, 0:128], [pY], [zT], e="act")
            self.out_proj_head(zT, A["gd_w_out"], h, wo, keep_ctx)
        S.pop()

    def mixer_hgrn2(self, layer, keep_ctx):
        assert not keep_ctx, "only the last layer uses HGRN2 here (no context outputs needed)"
        S = self.S
        A = self.A
        W = A["hg_w_in"].rearrange("(c p) n -> p c n", p=128)
        S.push()
        S.push()
        H = self.hnorm()
        zT = S.sb([128, TL], BF16, "zT")
        wo = S.sb([128, D], BF16, "wo_h")
        lbr = S.sb([128, DEPTH, NCH], F32, "lbr")
        for i in range(DEPTH):
            S.dma("sp", lbr[:, i, :], A["hg_lb"][i].rearrange("(c p) -> p c", p=128), writes=[lbr])
        mx = S.sb([128, NCH], F32, "lbmx")
        S.tt(mx[:], lbr[:, 0, :], lbr[:, 1, :], ALU.max, [lbr], [mx])
        for i in range(2, DEPTH):
            S.tt(mx[:], mx[:], lbr[:, i, :], ALU.max, [mx, lbr], [mx])
        for i in range(DEPTH):
            S.tt(lbr[:, i, :], lbr[:, i, :], mx[:], ALU.subtract, [lbr, mx], [lbr])
        S.act(lbr[:], lbr[:], AF.Exp, [lbr], [lbr])
        den = S.sb([128, NCH], F32, "lbden")
        num = S.sb([128, NCH], F32, "lbnum")
        S.tt(den[:], lbr[:, 0, :], lbr[:, 1, :], ALU.add, [lbr], [den])
        for i in range(2, DEPTH):
            S.tt(den[:], den[:], lbr[:, i, :], ALU.add, [den, lbr], [den])
        S.cp(num[:], lbr[:, 1, :], [lbr], [num])
        for i in range(2, layer + 1):
            S.tt(num[:], num[:], lbr[:, i, :], ALU.add, [num, lbr], [num])
        S.recip(den[:], den[:], [den], [den])
        lb = S.sb([128, NCH], F32, "lb")
        oml = S.sb([128, NCH], F32, "oml")
        S.tt(lb[:], num[:], den[:], ALU.mult, [num, den], [lb])
        S.ts(oml[:], lb[:], -1.0, 1.0, ALU.mult, ALU.add, [lb], [oml])
        nwbc = S.sb([128, 128], F32, "nwbc")
        S.dma("sp", nwbc[:], A["hg_norm_w"][0].partition_broadcast(128), writes=[nwbc])
        creset = S.sb([128, 128], F32, "creset")
        S.dma("sp", creset[:], A["chunk_reset"], writes=[creset])
        tri = [S.sb([128, 128], BF16, "tri") for _ in range(2)]
        S.dma("pool", tri[0][:], A["tri_fwd"], writes=[tri[0]])
        S.dma("pool", tri[1][:], A["tri_bwd"], writes=[tri[1]])
        eps1 = self.epsb
        qT = S.sb([128, TL], BF16, "qT")
        vtm = S.sb([128, 18, 128], BF16, "vtm")
        gtm = S.sb([128, 16, 128], BF16, "gtm")
        PV = []
        for _d in range(2):
            pvd = {}
            pvd["logf"] = S.sb([128, 2304], F32, "logf")
            pvd["kf"] = S.sb([128, 2304], BF16, "kf")
            pvd["of"] = S.sb([128, 16, 128], F32, "of")
            if _d == 1:
                mark = S.top
                W5 = S.sb([128, NCH, 5, 128], BF16, "W5")
                sgt = S.sb([128, 512], F32, "sgt")
                top_after = S.top
                S.top = mark
            pvd["Sf"] = S.sb([128, 128], F32, "Sf")
            pvd["Sb"] = S.sb([128, 128], BF16, "Sb")
            pvd["K2"] = S.sb([128, 2, 128], BF16, "K2")
            pvd["Q2"] = S.sb([128, 2, 128], BF16, "Q2")
            pvd["T"] = {nm: S.sb([128, 128], F32, nm) for nm in ("Gi", "G", "df", "dl", "Eq", "Ek", "Ed", "EG", "at")}
            for nm in ("qg", "kdT", "kdtm", "attnT"):
                pvd[nm] = S.sb([128, 128], BF16, nm)
            PV.append(pvd)
        S.top = max(S.top, top_after)
        TR = {nm: S.sb([128, 128], F32, nm) for nm in ("os", "nwg", "ysq")}
        yb = S.sb([128, 128], BF16, "yb")
        st = S.sb([128, 4], F32, "hgst")

        def strided_halves(buf):
            return [buf[:, 0, 0:64], buf[:, 1, 64:128]]

        for h in range(8):
            S.barrier()
            for k in range(5):
                S.dma("pool", W5[:, :, k, :], W[:, :, k * D + h * 128:k * D + (h + 1) * 128], writes=[W5])
            for (grp, o, n) in self.tok_groups(False):
                ps = S.psum()
                for c in range(NCH):
                    S.mm(ps[:, 0:n], W5[:, c, 0, :], H[c][:, o:o + n], c == 0, c == NCH - 1, [W5, H[c]], [ps])
                S.act(qT[:, o:o + n], ps[:, 0:n], AF.Silu, [ps], [qT])
            for t in range(18):
                ps = S.psum()
                for c in range(NCH):
                    S.mm(ps[:, 0:128], H[c][:, t * 128:(t + 1) * 128], W5[:, c, 3, :], c == 0, c == NCH - 1, [H[c], W5], [ps])
                S.cp(vtm[:, t, :], ps[:, 0:128], [ps], [vtm], e=("act" if t % 2 else "dve"))
                if t < 16:
                    ps2 = S.psum()
                    for c in range(NCH):
                        S.mm(ps2[:, 0:128], H[c][:, t * 128:(t + 1) * 128], W5[:, c, 4, :], c == 0, c == NCH - 1,
                             [H[c], W5], [ps2])
                    S.act(gtm[:, t, :], ps2[:, 0:128], AF.Silu, [ps2], [gtm])
            for d in range(2):
                logf, kf = PV[d]["logf"], PV[d]["kf"]
                for (grp, o, n) in self.tok_groups(True):
                    ps = S.psum()
                    for c in range(NCH):
                        S.mm(ps[:, 0:n], W5[:, c, 1 + d, :], H[c][:, o:o + n], c == 0, c == NCH - 1, [W5, H[c]], [ps])
                    S.act(sgt[:, 0:n], ps[:, 0:n], AF.Sigmoid, [ps], [sgt])
                    S.ts(sgt[:, 0:n], sgt[:, 0:n], oml[:, h:h + 1], lb[:, h:h + 1], ALU.mult, ALU.add, [sgt, oml, lb], [sgt])
                    S.act(logf[:, o:o + n], sgt[:, 0:n], AF.Ln, [sgt], [logf])
                    S.ts(kf[:, o:o + n], sgt[:, 0:n], -1.0, 1.0, ALU.mult, ALU.add, [sgt], [kf])
            S.barrier()
            for _d in range(2):
                S.memset(PV[_d]["K2"][:], 0.0, [PV[_d]["K2"]])
                S.memset(PV[_d]["Q2"][:], 0.0, [PV[_d]["Q2"]])

            def run_dir(d):
                pvd = PV[d]
                logf, kf, of, Sf, Sb, K2, Q2, T = (pvd[k] for k in ("logf", "kf", "of", "Sf", "Sb", "K2", "Q2", "T"))
                qg, kdT, kdtm, attnT = (pvd[k] for k in ("qg", "kdT", "kdtm", "attnT"))
                S.memset(Sf[:], 0.0, [Sf])
                S.memset(Sb[:], 0.0, [Sb])
                order = [16, 17] + list(range(16)) if d == 0 else [17, 16] + list(range(15, -1, -1))
                for tile in order:
                    o0 = tile * 128
                    lf = logf[:, o0:o0 + 128]
                    kk = kf[:, o0:o0 + 128]
                    S.scan(T["Gi"][:], creset[:], lf, 0.0, ALU.mult, ALU.add, [creset, logf], [T["Gi"]])
                    if d == 0:
                        G = T["Gi"]
                    else:
                        G = T["G"]
                        S.tt(G[:], lf, T["Gi"][:], ALU.subtract, [logf, T["Gi"]], [G])
                        for ci in range(2):
                            cs = slice(64 * ci, 64 * ci + 64)
                            S.ts(G[:, cs], G[:, cs], T["Gi"][:, 64 * ci + 63:64 * ci + 64], None, ALU.add, None,
                                 [G, T["Gi"]], [G])
                    for ci in range(2):
                        cs = slice(64 * ci, 64 * ci + 64)
                        mid = 64 * ci + 32
                        last = 64 * ci + (63 if d == 0 else 0)
                        S.ts(T["df"][:, cs], G[:, cs], G[:, mid:mid + 1], None, ALU.subtract, None, [G], [T["df"]])
                        S.ts(T["dl"][:, cs], G[:, cs], G[:, last:last + 1], None, ALU.subtract, None, [G], [T["dl"]])
                    S.act(T["Eq"][:], T["df"][:], AF.Exp, [T["df"]], [T["Eq"]])
                    S.act(T["Ek"][:], T["df"][:], AF.Exp, [T["df"]], [T["Ek"]], scale=-1.0)
                    S.act(T["Ed"][:], T["dl"][:], AF.Exp, [T["dl"]], [T["Ed"]], scale=-1.0)
                    S.act(T["EG"][:], G[:], AF.Exp, [G], [T["EG"]])
                    islat = tile < 16
                    if islat:
                        qq = qT[:, o0:o0 + 128]
                        S.stt(qg[:], T["Eq"][:], 1e30, qq, ALU.min, ALU.mult, [T["Eq"], qT], [qg])
                        for ci, dst in enumerate(strided_halves(Q2)):
                            cs = slice(64 * ci, 64 * ci + 64)
                            S.tt(dst, T["EG"][:, cs], qT[:, o0 + 64 * ci:o0 + 64 * ci + 64], ALU.mult, [T["EG"], qT], [Q2])
                        for ci, dst in enumerate(strided_halves(K2)):
                            cs = slice(64 * ci, 64 * ci + 64)
                            S.stt(dst, T["Ek"][:, cs], 1e30, kk[:, cs], ALU.min, ALU.mult, [T["Ek"], kf], [K2])
                    S.tt(kdT[:], T["Ed"][:], kk, ALU.mult, [T["Ed"], kf], [kdT])
                    pT = S.psum()
                    pTb = pT[:].bitcast(BF16)
                    S.tr(pTb[:, 0:128], kdT[:], self.ident_b[:], [kdT, self.ident_b], [pT])
                    S.cp(kdtm[:], pTb[:, 0:128], [pT], [kdtm], e="act")
                    if islat:
                        psA = S.psum()
                        for ci in range(2):
                            cs = slice(64 * ci, 64 * ci + 64)
                            S.mm(psA[:, cs], K2[:, ci, :], qg[:, cs], True, True, [K2, qg], [psA])
                        S.ts(T["at"][:], psA[:, 0:128], 1e30, -1e30, ALU.min, ALU.max, [psA], [T["at"]])
                        S.tt(attnT[:], T["at"][:], tri[d][:], ALU.mult, [T["at"], tri[d]], [attnT])
                        po = S.psum()
                        S.mm(po[:, 0:128], attnT[:], vtm[:, tile, :], True, False, [attnT, vtm], [po])
                    corder = (0, 1) if d == 0 else (1, 0)
                    psS = S.psum()
                    for n_, ci in enumerate(corder):
                        pr = slice(64 * ci, 64 * ci + 64)
                        last = 64 * ci + (63 if d == 0 else 0)
                        if islat:
                            S.mm(po[:, 0:128], Q2[:, ci, :], Sb[:], False, n_ == 1, [Q2, Sb], [po])
                        S.mm(psS[:, 0:128], kdtm[pr, :], vtm[pr, tile, :], True, True, [kdtm, vtm], [psS])
                        S.stt(Sf[:], Sf[:], T["EG"][:, last:last + 1], psS[:, 0:128], ALU.mult, ALU.add,
                              [Sf, T["EG"], psS], [Sf])
                        S.cp(Sb[:], Sf[:], [Sf], [Sb], e="act")
                    if not islat:
                        continue
                    S.cp(of[:, tile, :], po[:, 0:128], [po], [of])

            S.run_interleaved([lambda: run_dir(0), lambda: run_dir(1)])
            T = TR
            for tile in range(16):
                o0 = tile * 128
                S.tt(T["os"][:], PV[1]["of"][:, tile, :], PV[0]["of"][:, tile, :], ALU.add, [PV[1]["of"], PV[0]["of"]], [T["os"]])
                S.act(T["ysq"][:], T["os"][:], AF.Square, [T["os"]], [T["ysq"], st], accum_out=st[:, 0:1])
                S.act(st[:, 1:2], st[:, 0:1], AF.Sqrt, [st, eps1], [st], bias=eps1[:, 0:1], scale=1.0 / 128)
                S.recip(st[:, 2:3], st[:, 1:2], [st], [st])
                S.tt(T["nwg"][:], nwbc[:], gtm[:, tile, :], ALU.mult, [nwbc, gtm], [T["nwg"]], e="pool")
                S.stt(yb[:], T["os"][:], st[:, 2:3], T["nwg"][:], ALU.mult, ALU.mult, [T["os"], st, T["nwg"]], [yb])
                pY = S.psum()
                pYb = pY[:].bitcast(BF16)
                S.tr(pYb[:, 0:128], yb[:], self.ident_b[:], [yb, self.ident_b], [pY])
                S.cp(zT[:, o0:o0 + 128], pYb[:, 0:128], [pY], [zT], e="act")
            self.out_proj_head(zT, A["hg_w_out"], h, wo, False)
        S.pop()
        S.pop()

    def mixer_swa(self, keep_ctx):
        S = self.S
        A = self.A
        W = A["sw_w_in"].rearrange("(c p) n -> p c n", p=128)
        S.push()
        Q = [S.sb([128, 2304], BF16, "Q") for _ in range(8)]
        KD = [S.sb([128, 2304], BF16, "KD") for _ in range(4)]
        V1 = S.sb([128, 18, 4, 65], BF16, "V1")
        S.push()
        H = self.hnorm()
        cosb = S.sb([128, TL], BF16, "cosb")
        sinb = S.sb([128, TL], BF16, "sinb")
        S.dma("pool", cosb[:], A["rope_cos"], writes=[cosb])
        S.dma("pool", sinb[:], A["rope_sin"], writes=[sinb])
        wj = [S.sb([128, NCH, 128], BF16, "wj") for _ in range(2)]
        wjs = [S.sb([128, NCH, 128], BF16, "wjs") for _ in range(2)]
        t1 = S.sb([128, 512], F32, "t1")
        t2 = S.sb([128, 512], F32, "t2")
        S.memset(V1[:, :, :, 64:65], 1.0, [V1])

        def swapped(dst, src):
            d5 = dst[:].rearrange("p c (h two i) -> p c h two i", two=2, i=32)
            s5 = src[:].rearrange("p c (h two i) -> p c h two i", two=2, i=32)
            for c in range(NCH):
                S.cp(d5[:, c, :, 0, :], s5[:, c, :, 1, :], [src], [dst], e="pool")
                S.cp(d5[:, c, :, 1, :], s5[:, c, :, 0, :], [src], [dst], e="pool")

        def project_roped(dst, w, ws):
            for (grp, o, n) in self.tok_groups(True):
                psq = S.psum()
                for c in range(NCH):
                    S.mm(psq[:, 0:n], w[:, c, :], H[c][:, o:o + n], c == 0, c == NCH - 1, [w, H[c]], [psq])
                if grp[0] == "c":
                    S.cp(dst[:, o:o + n], psq[:, 0:n], [psq], [dst], e="act")
                    continue
                pss = S.psum()
                for c in range(NCH):
                    S.mm(pss[:, 0:n], ws[:, c, :], H[c][:, o:o + n], c == 0, c == NCH - 1, [ws, H[c]], [pss])
                S.tt(t1[:, 0:n], psq[:, 0:n], cosb[:, o:o + n], ALU.mult, [psq, cosb], [t1])
                S.tt(t2[:, 0:n], pss[:, 0:n], sinb[:, o:o + n], ALU.mult, [pss, sinb], [t2])
                S.tt(dst[:, o:o + n], t1[:, 0:n], t2[:, 0:n], ALU.add, [t1, t2], [dst], e="pool")

        k = 0
        stage = self.cfg.get("swa_stage", 9)
        for j in range(8 if stage >= 1 else 0):
            w, ws = wj[k % 2], wjs[k % 2]
            k += 1
            S.dma("pool", w[:], W[:, :, j * 128:(j + 1) * 128], writes=[w])
            swapped(ws, w)
            project_roped(Q[j], w, ws)
        for g in range(4 if stage >= 2 else 0):
            w, ws = wj[k % 2], wjs[k % 2]
            k += 1
            for half in range(2):
                S.dma("pool", w[:, :, half * 64:(half + 1) * 64], W[:, :, 1024 + g * 64:1024 + (g + 1) * 64], writes=[w])
            swapped(ws, w)
            project_roped(KD[g], w, ws)
        wv = S.sb([128, NCH, 256], BF16, "wv")
        S.dma("pool", wv[:], W[:, :, 1280:1536], writes=[wv])
        for t in range(18 if stage >= 3 else 0):
            ps = S.psum()
            for c in range(NCH):
                S.mm(ps[:, 0:256], H[c][:, t * 128:(t + 1) * 128], wv[:, c, :], c == 0, c == NCH - 1, [H[c], wv], [ps])
            S.cp(V1[:, t, :, 0:64], ps[:, 0:256].rearrange("p (g d) -> p g d", d=64), [ps], [V1],
                 e=("act" if t % 2 else "dve"))
        S.pop()

        S.push()
        OT = [S.sb([128, 2304], BF16, "OT") for _ in range(8)]
        sinkE = S.sb([128, 16], F32, "sinkE")
        S.dma("sp", sinkE[:], A["sw_sink"][0].partition_broadcast(128), writes=[sinkE])
        S.act(sinkE[:], sinkE[:], AF.Exp, [sinkE], [sinkE])
        mP = S.sb([128, 256], BF16, "mP")
        mN = S.sb([128, 256], BF16, "mN")
        S.dma("pool", mP[:], A["mask_prev"], writes=[mP])
        S.dma("pool", mN[:], A["mask_next"], writes=[mN])
        PT = [S.sb([128, 2, 8, 128], BF16, "PT") for _ in range(2)]
        ob = [S.sb([128, 128], BF16, "ob") for _ in range(2)]
        den = S.sb([128, 4], F32, "den")
        nblk = 18 if keep_ctx else 16
        it = 0
        for j in range(8 if stage >= 4 else 0):
            g = j // 2
            for blk in range(nblk):
                qo = blk * 128
                if blk < 16:
                    tiles = [(16, None), (17, None)]
                    if blk > 0:
                        tiles.append((blk - 1, mP))
                    tiles.append((blk, None))
                    if blk < 15:
                        tiles.append((blk + 1, mN))
                else:
                    tiles = [(16, None), (17, None)]
                nt = len(tiles)
                pt = PT[it % 2]
                o2 = ob[it % 2]
                it += 1
                nb = (nt + 3) // 4
                pss = [[S.psum() for _ in range(nb)] for hh in range(2)]
                for hh in range(2):
                    pr = slice(hh * 64, (hh + 1) * 64)
                    for k2, (kt, msk) in enumerate(tiles):
                        ps = pss[hh][k2 // 4]
                        co = (k2 % 4) * 128
                        S.mm(ps[:, co:co + 128], KD[g][pr, kt * 128:(kt + 1) * 128], Q[j][pr, qo:qo + 128], True, True,
                             [KD[g], Q[j]], [ps])
                for hh in range(2):
                    for b in range(nb):
                        w_ = min(4, nt - 4 * b) * 128
                        S.act(pt[:, hh, 4 * b:4 * b + 4, :].rearrange("p a b -> p (a b)")[:, 0:w_], pss[hh][b][:, 0:w_],
                              AF.Exp, [pss[hh][b]], [pt], scale=0.125)
                for k2, (kt, msk) in enumerate(tiles):
                    if msk is not None:
                        for hh in range(2):
                            S.tt(pt[:, hh, k2, :], pt[:, hh, k2, :], msk[:, 0:128], ALU.mult, [pt, msk], [pt], e="pool")
                po = S.psum()
                for hh in range(2):
                    for k2, (kt, msk) in enumerate(tiles):
                        S.mm(po[:, hh * 65:(hh + 1) * 65], pt[:, hh, k2, :], V1[:, kt, g, :],
                             k2 == 0, k2 == nt - 1, [pt, V1], [po])
                for hh in range(2):
                    h = 2 * j + hh
                    S.tt(den[:, hh:hh + 1], po[:, hh * 65 + 64:hh * 65 + 65], sinkE[:, h:h + 1], ALU.add,
                         [po, sinkE], [den])
                S.recip(den[:, 2:4], den[:, 0:2], [den], [den])
                for hh in range(2):
                    S.ts(o2[:, hh * 64:(hh + 1) * 64], po[:, hh * 65:hh * 65 + 64], den[:, 2 + hh:3 + hh], None,
                         ALU.mult, None, [po, den], [o2])
                pT = S.psum()
                pTb = pT[:].bitcast(BF16)
                S.tr(pTb[:, 0:128], o2[:], self.ident_b[:], [o2, self.ident_b], [pT])
                S.cp(OT[j][:, qo:qo + 128], pTb[:, 0:128], [pT], [OT[j]], e="act")
        if stage >= 5:
            self.out_proj(OT, A["sw_w_out"], keep_ctx)
        S.pop()
        S.pop()


def make_in_maps(inputs, consts, n_cores=8):
    f = lambda a: np.ascontiguousarray(np.asarray(a, dtype=np.float32))
    shared = {
        "ada_w": f(inputs["ada_w"]).reshape(DEPTH * D, 6 * D),
        "ada_b": f(inputs["ada_b"]),
        "norm1_w": f(inputs["norm1_w"]),
        "norm2_w": f(inputs["norm2_w"]),
        "final_norm_w": f(inputs["final_norm_w"]).reshape(1, D),
        "moe_router": f(inputs["moe_router"]).reshape(DEPTH * D, NE),
        "moe_w_gate": f(inputs["moe_w_gate"]).reshape(DEPTH * NE * D, FF),
        "moe_w_up": f(inputs["moe_w_up"]).reshape(DEPTH * NE * D, FF),
        "moe_w_down": f(inputs["moe_w_down"]).reshape(DEPTH * NE * FF, D),
        "hy_w_in": f(inputs["hy_w_in"]), "hy_b_in": f(inputs["hy_b_in"]).reshape(1, 3 * D),
        "hy_short_w": f(inputs["hy_short_w"]), "hy_short_b": f(inputs["hy_short_b"]).reshape(1, 3 * D),
        "hy_ffn_w1": f(inputs["hy_ffn_w1"]), "hy_ffn_b1": f(inputs["hy_ffn_b1"]).reshape(1, 64),
        "hy_ffn_w2": f(inputs["hy_ffn_w2"]), "hy_ffn_b2": f(inputs["hy_ffn_b2"]).reshape(1, 64),
        "hy_ffn_w3": f(inputs["hy_ffn_w3"]), "hy_sin_freq": f(inputs["hy_sin_freq"]),
        "hy_filter_bias": f(inputs["hy_filter_bias"]), "hy_w_out": f(inputs["hy_w_out"]),
        "hy_b_out": f(inputs["hy_b_out"]).reshape(1, D),
        "gd_w_in": f(inputs["gd_w_in"]),
        "gd_conv_w": f(inputs["gd_conv_w"]),
        "gd_a_log": f(inputs["gd_a_log"]).reshape(1, 16),
        "gd_dt_bias": f(inputs["gd_dt_bias"]).reshape(1, 16),
        "gd_norm_w": f(inputs["gd_norm_w"]).reshape(1, 128),
        "gd_w_out": f(inputs["gd_w_out"]),
        "hg_w_in": f(inputs["hg_w_in"]),
        "hg_lb": f(inputs["hg_lb"]),
        "hg_norm_w": f(inputs["hg_norm_w"]).reshape(1, 128),
        "hg_w_out": f(inputs["hg_w_out"]),
        "sw_w_in": f(inputs["sw_w_in"]),
        "sw_sink": f(inputs["sw_sink"]).reshape(1, 16),
        "sw_w_out": f(inputs["sw_w_out"]),
    }
    for k, v in consts.items():
        shared["k_" + k] = v
    maps = []
    for b in range(n_cores):
        mp = dict(shared)
        mp["x"] = f(inputs["x"][b])
        mp["ctx"] = f(inputs["ctx"][b])
        mp["cc"] = np.stack([f(inputs["c"][b]), f(inputs["c_ctx"])], 0)
        maps.append(mp)
    return maps


def kernel(**inputs):
    p = Prog({})
    nc = p.build()
    maps = make_in_maps(inputs, p.consts, 8)
    res = run_bass_kernel_spmd(nc, maps, core_ids=list(range(8)))
    return np.stack([r["out"] for r in res.results], 0).astype(np.float32)
```
